# Optimizing a Trainium2 kernel written in Bass

```python
import jax, jax.numpy as jnp
from jax import lax
import numpy as np

D_MODEL = 2048
BATCH = 8
SEQ = 2048
DEPTH = 1

CHUNK = 64
Q_BLOCK = 128
ROPE_THETA = 500000.0
EPS = 1e-6
NEG_INF = -1e30
N_BRANCHES = 2

MLA_HEADS = 8
MLA_NOPE = 128
MLA_ROPE = 64
MLA_V = 128
MLA_Q_RANK = 512
MLA_KV_RANK = 256

DSA_HEADS = 8
DSA_KV_HEADS = 2
DSA_GROUP = DSA_HEADS // DSA_KV_HEADS
DSA_HEAD_DIM = 128
DSA_ROT = DSA_HEAD_DIM // 4
IDX_HEADS = 16
IDX_DIM = 64
IDX_ROT = IDX_DIM // 4
DSA_TOPK_MAX = 256

PEER_HEADS = 8
PEER_N_KEYS = 128
PEER_N_EXPERTS = PEER_N_KEYS * PEER_N_KEYS
PEER_KEY_DIM = 128
PEER_TOPK = 16
PEER_TOKEN_BLOCK = 128

IN_SIZES = (
    MLA_Q_RANK,
    MLA_KV_RANK,
    MLA_ROPE,
    DSA_HEADS * DSA_HEAD_DIM,
    DSA_KV_HEADS * DSA_HEAD_DIM,
    DSA_KV_HEADS * DSA_HEAD_DIM,
    IDX_HEADS * IDX_DIM,
    IDX_DIM,
    IDX_HEADS,
    N_BRANCHES * D_MODEL,
)
IN_WIDTH = sum(IN_SIZES)

kernel_name = "hybrid_mla_dsa_peer_chunk_causal"


def rmsnorm(t, g):
    tf = t.astype(jnp.float32)
    y = tf * lax.rsqrt(jnp.mean(tf * tf, axis=-1, keepdims=True) + EPS)
    return (y * g.astype(jnp.float32)).astype(t.dtype)


def rope(t, positions, rot_dim):
    half = rot_dim // 2
    inv_freq = ROPE_THETA ** (-(jnp.arange(half, dtype=jnp.float32) * 2.0) / rot_dim)
    ang = positions.astype(jnp.float32)[..., None] * inv_freq
    cos = jnp.cos(ang)[:, :, None, :]
    sin = jnp.sin(ang)[:, :, None, :]
    t1 = t[..., :half].astype(jnp.float32)
    t2 = t[..., half:rot_dim].astype(jnp.float32)
    rest = t[..., rot_dim:]
    return jnp.concatenate([(t1 * cos - t2 * sin).astype(t.dtype),
                            (t2 * cos + t1 * sin).astype(t.dtype), rest], axis=-1)


def to_blocks(t):
    B, S = t.shape[:2]
    return t.reshape((B, S // Q_BLOCK, Q_BLOCK) + t.shape[2:]).swapaxes(0, 1)


def from_blocks(t):
    nb, B, qb, w = t.shape
    return t.swapaxes(0, 1).reshape(B, nb * qb, w)


def chunk_causal_attention(q, k, v, scale):
    S = q.shape[1]
    n_blocks = S // Q_BLOCK
    starts = jnp.arange(n_blocks, dtype=jnp.int32) * Q_BLOCK
    key_chunk = jnp.arange(S, dtype=jnp.int32) // CHUNK

    def one_block(args):
        q_blk, q0 = args
        q_chunk = (q0 + jnp.arange(Q_BLOCK, dtype=jnp.int32)) // CHUNK
        allowed = key_chunk[None, :] <= q_chunk[:, None]
        s = jnp.einsum('bqhd,bkhd->bhqk', q_blk, k).astype(jnp.float32) * scale
        s = jnp.where(allowed[None, None], s, NEG_INF)
        p = jax.nn.softmax(s, axis=-1).astype(v.dtype)
        o = jnp.einsum('bhqk,bkhd->bqhd', p, v)
        return o.reshape(o.shape[0], Q_BLOCK, -1)

    return from_blocks(lax.map(one_block, (to_blocks(q), starts)))


def mla_branch(c_q, c_kv, k_rope, positions, q_norm_g, kv_norm_g, w_uq, w_uk, w_uv):
    B, S, _ = c_q.shape
    q = jnp.einsum('bsr,rhd->bshd', rmsnorm(c_q, q_norm_g), w_uq)
    q = jnp.concatenate([q[..., :MLA_NOPE], rope(q[..., MLA_NOPE:], positions, MLA_ROPE)], axis=-1)
    ckv = rmsnorm(c_kv, kv_norm_g)
    k_nope = jnp.einsum('bsr,rhd->bshd', ckv, w_uk)
    v = jnp.einsum('bsr,rhd->bshd', ckv, w_uv)
    k_pe = rope(k_rope[:, :, None, :], positions, MLA_ROPE)
    k = jnp.concatenate([k_nope, jnp.broadcast_to(k_pe, (B, S, MLA_HEADS, MLA_ROPE))], axis=-1)
    return chunk_causal_attention(q, k, v, (MLA_NOPE + MLA_ROPE) ** -0.5)


def dsa_branch(q, k, v, q_idx, k_idx, w_idx, positions):
    B, S, _ = q.shape
    q = rope(q.reshape(B, S, DSA_HEADS, DSA_HEAD_DIM), positions, DSA_ROT)
    k = rope(k.reshape(B, S, DSA_KV_HEADS, DSA_HEAD_DIM), positions, DSA_ROT)
    v = v.reshape(B, S, DSA_KV_HEADS, DSA_HEAD_DIM)
    q_idx = rope(q_idx.reshape(B, S, IDX_HEADS, IDX_DIM), positions, IDX_ROT)
    k_idx = rope(k_idx[:, :, None, :], positions, IDX_ROT)[:, :, 0, :]
    top_k = min(DSA_TOPK_MAX, S // 4)
    n_blocks = S // Q_BLOCK
    starts = jnp.arange(n_blocks, dtype=jnp.int32) * Q_BLOCK
    key_chunk = jnp.arange(S, dtype=jnp.int32) // CHUNK
    scale = DSA_HEAD_DIM ** -0.5
    gather_rows = jax.vmap(lambda table, idx: table[idx])

    def one_block(args):
        q_blk, qi_blk, w_blk, q0 = args
        q_chunk = (q0 + jnp.arange(Q_BLOCK, dtype=jnp.int32)) // CHUNK
        allowed = key_chunk[None, :] <= q_chunk[:, None]
        dots = jnp.einsum('bqhd,bsd->bqhs', qi_blk, k_idx).astype(jnp.float32) * IDX_DIM ** -0.5
        w = w_blk.astype(jnp.float32) * IDX_HEADS ** -0.5
        index_score = jnp.einsum('bqh,bqhs->bqs', w, jax.nn.relu(dots))
        index_score = jnp.where(allowed[None], index_score, NEG_INF)
        _, sel = lax.top_k(index_score, top_k)
        valid = (sel // CHUNK) <= q_chunk[None, :, None]
        k_sel = gather_rows(k, sel)
        v_sel = gather_rows(v, sel)
        qg = q_blk.reshape(B, Q_BLOCK, DSA_KV_HEADS, DSA_GROUP, DSA_HEAD_DIM)
        s = jnp.einsum('bqgnd,bqkgd->bqgnk', qg, k_sel).astype(jnp.float32) * scale
        s = jnp.where(valid[:, :, None, None, :], s, NEG_INF)
        p = jax.nn.softmax(s, axis=-1).astype(v.dtype)
        o = jnp.einsum('bqgnk,bqkgd->bqgnd', p, v_sel)
        return o.reshape(B, Q_BLOCK, DSA_HEADS * DSA_HEAD_DIM)

    out = lax.map(one_block, (to_blocks(q), to_blocks(q_idx), to_blocks(w_idx), starts))
    return from_blocks(out)


def peer_ffn(h, w_q, sub_keys, u_emb, v_emb):
    B, S, D = h.shape
    n_blocks = (B * S) // PEER_TOKEN_BLOCK
    half = PEER_KEY_DIM // 2

    def one_block(xb):
        q = jnp.einsum('td,dhk->thk', xb, w_q).reshape(PEER_TOKEN_BLOCK, PEER_HEADS, 2, half)
        s = jnp.einsum('thpc,hpnc->thpn', q, sub_keys).astype(jnp.float32)
        s1, i1 = lax.top_k(s[:, :, 0], PEER_TOPK)
        s2, i2 = lax.top_k(s[:, :, 1], PEER_TOPK)
        cand_s = (s1[..., :, None] + s2[..., None, :]).reshape(PEER_TOKEN_BLOCK, PEER_HEADS, -1)
        cand_i = (i1[..., :, None] * PEER_N_KEYS + i2[..., None, :]).reshape(PEER_TOKEN_BLOCK, PEER_HEADS, -1)
        top_s, pos = lax.top_k(cand_s, PEER_TOPK)
        expert = jnp.take_along_axis(cand_i, pos, axis=-1)
        g = jax.nn.softmax(top_s, axis=-1)
        u = u_emb[expert]
        a = jax.nn.gelu(jnp.einsum('thkd,td->thk', u, xb).astype(jnp.float32), approximate=False)
        coef = (g * a).astype(xb.dtype)
        return jnp.einsum('thk,thkd->td', coef, v_emb[expert])

    out = lax.map(one_block, h.reshape(n_blocks, PEER_TOKEN_BLOCK, D))
    return out.reshape(B, S, D)


def setup_inputs(seed: int = 0) -> dict:
    key = jax.random.key(seed)
    ks = jax.random.split(key, 20)
    f32 = jnp.float32
    L = DEPTH

    def nrm(k, shape, fan_in):
        return jax.random.normal(k, shape, f32) * (fan_in ** -0.5)

    def gain(k, shape):
        return 1.0 + 0.01 * jax.random.normal(k, shape, f32)

    x = jax.random.normal(ks[0], (BATCH, SEQ, D_MODEL), f32)
    start = jax.random.randint(ks[1], (BATCH, 1), 0, 64, dtype=jnp.int32) * CHUNK
    positions = start + jnp.arange(SEQ, dtype=jnp.int32)[None, :]
    return {
        'x': x,
        'positions': positions,
        'norm_mix_g': gain(ks[2], (L, D_MODEL)),
        'w_in': nrm(ks[3], (L, D_MODEL, IN_WIDTH), D_MODEL),
        'mla_q_norm_g': gain(ks[4], (L, MLA_Q_RANK)),
        'mla_kv_norm_g': gain(ks[5], (L, MLA_KV_RANK)),
        'mla_w_uq': nrm(ks[6], (L, MLA_Q_RANK, MLA_HEADS, MLA_NOPE + MLA_ROPE), MLA_Q_RANK),
        'mla_w_uk': nrm(ks[7], (L, MLA_KV_RANK, MLA_HEADS, MLA_NOPE), MLA_KV_RANK),
        'mla_w_uv': nrm(ks[8], (L, MLA_KV_RANK, MLA_HEADS, MLA_V), MLA_KV_RANK),
        'w_branch_a': nrm(ks[9], (L, MLA_HEADS * MLA_V, D_MODEL), MLA_HEADS * MLA_V),
        'w_branch_b': nrm(ks[10], (L, DSA_HEADS * DSA_HEAD_DIM, D_MODEL), DSA_HEADS * DSA_HEAD_DIM),
        'w_out': nrm(ks[11], (L, D_MODEL, D_MODEL), D_MODEL),
        'norm_ffn_g': gain(ks[12], (L, D_MODEL)),
        'peer_w_q': nrm(ks[13], (L, D_MODEL, PEER_HEADS, PEER_KEY_DIM), D_MODEL),
        'peer_sub_keys': nrm(ks[14], (L, PEER_HEADS, 2, PEER_N_KEYS, PEER_KEY_DIM // 2), PEER_KEY_DIM // 2),
        'peer_u': nrm(ks[15], (L, PEER_N_EXPERTS, D_MODEL), D_MODEL),
        'peer_v': nrm(ks[16], (L, PEER_N_EXPERTS, D_MODEL), PEER_HEADS),
        'norm_final_g': gain(ks[17], (D_MODEL,)),
    }


def reference(x, positions, norm_mix_g, w_in, mla_q_norm_g, mla_kv_norm_g, mla_w_uq, mla_w_uk,
              mla_w_uv, w_branch_a, w_branch_b, w_out, norm_ffn_g, peer_w_q, peer_sub_keys,
              peer_u, peer_v, norm_final_g):
    B, S, _ = x.shape
    splits = [int(c) for c in np.cumsum(IN_SIZES)[:-1]]
    for l in range(DEPTH):
        h = rmsnorm(x, norm_mix_g[l])
        proj = jnp.einsum('bsd,de->bse', h, w_in[l])
        c_q, c_kv, k_rope, dq, dk, dv, iq, ik, iw, gates = jnp.split(proj, splits, axis=-1)
        o_a = mla_branch(c_q, c_kv, k_rope, positions, mla_q_norm_g[l], mla_kv_norm_g[l],
                         mla_w_uq[l], mla_w_uk[l], mla_w_uv[l])
        o_b = dsa_branch(dq, dk, dv, iq, ik, iw, positions)
        gate = jax.nn.sigmoid(gates.astype(jnp.float32)).astype(x.dtype).reshape(B, S, N_BRANCHES, D_MODEL)
        merged = gate[:, :, 0, :] * (o_a @ w_branch_a[l]) + gate[:, :, 1, :] * (o_b @ w_branch_b[l])
        x = x + merged @ w_out[l]
        x = x + peer_ffn(rmsnorm(x, norm_ffn_g[l]), peer_w_q[l], peer_sub_keys[l], peer_u[l], peer_v[l])
    return rmsnorm(x, norm_final_g)
```

```python
import numpy as np
from contextlib import ExitStack
import concourse.bass as bass
import concourse.mybir as mybir
from concourse.bass_utils import run_bass_kernel_spmd

F32 = mybir.dt.float32
BF16 = mybir.dt.bfloat16
I32 = mybir.dt.int32
U32 = mybir.dt.uint32
AF = mybir.ActivationFunctionType
ALU = mybir.AluOpType
AX = mybir.AxisListType

D = 2048
S = 2048
NT = 16
EPS = 1e-6
PI = float(np.pi)
MAGIC = 12582912.0
NEG = -1e30
ROPE_THETA = 500000.0

ENGS = ["pe", "act", "dve", "pool", "sp"]
N_DMA_SEMS = {"sp": 40, "pool": 24, "act": 8}


class Prog:
    def __init__(self, nc, semstate):
        self.nc = nc
        self.semstate = semstate
        self.ins = {e: [] for e in ENGS}
        self.last_w = {}
        self.readers = {}
        self.dma_rr = {e: 0 for e in N_DMA_SEMS}
        self.dma_cnt = {}

    def add(self, eng, fn, reads=(), writes=(), dma=False):
        idx = len(self.ins[eng])
        deps = set()
        for r in reads:
            w = self.last_w.get(r)
            if w is not None:
                deps.add(w)
        for r in writes:
            w = self.last_w.get(r)
            if w is not None:
                deps.add(w)
            for rd in self.readers.get(r, ()):
                deps.add(rd)
        deps.discard((eng, idx))
        rec = dict(fn=fn, deps=deps, dma=dma)
        if dma:
            j = self.dma_rr[eng]
            self.dma_rr[eng] = (j + 1) % N_DMA_SEMS[eng]
            n = self.dma_cnt.get((eng, j), 0) + 1
            self.dma_cnt[(eng, j)] = n
            rec["dsem"] = (eng, j, n)
        self.ins[eng].append(rec)
        for r in reads:
            self.readers.setdefault(r, []).append((eng, idx))
        for r in writes:
            self.last_w[r] = (eng, idx)
            self.readers[r] = []
        return (eng, idx)

    def op(self, eng, method, reads, writes, **kw):
        dma = method in ("dma_start", "indirect_dma_start")
        return self.add(eng, lambda e: getattr(e, method)(**kw), reads, writes, dma=dma)

    def emit(self, stack):
        nc = self.nc
        need = {e: set() for e in ENGS}
        for e in ENGS:
            for ins in self.ins[e]:
                for (de, di) in ins["deps"]:
                    if self.ins[de][di]["dma"]:
                        continue
                    if de == e and e == "pe":
                        continue
                    need[de].add(di)
        sigcount = {}
        for e in ENGS:
            c = 0
            for i, ins in enumerate(self.ins[e]):
                if ins["dma"]:
                    continue
                if i in need[e]:
                    c += 1
                    sigcount[(e, i)] = c
        ss = self.semstate
        gstack = ss["stack"]
        for e in ["pe", "act", "dve", "pool"]:
            if e not in ss["esem"]:
                ss["esem"][e] = gstack.enter_context(nc.semaphore("se_%s" % e))
                ss["ebase"][e] = 0
        for e, n in N_DMA_SEMS.items():
            for j in range(n):
                if self.dma_cnt.get((e, j), 0) > 0 and (e, j) not in ss["dsem"]:
                    ss["dsem"][(e, j)] = gstack.enter_context(nc.semaphore("sd_%s_%d" % (e, j)))
                    ss["dbase"][(e, j)] = 0
        esem = ss["esem"]
        dsem = ss["dsem"]
        ebase = dict(ss["ebase"])
        dbase = dict(ss["dbase"])
        for (e, i) in list(sigcount.keys()):
            sigcount[(e, i)] += ebase[e]
        for e in ["pe", "act", "dve", "pool"]:
            ss["ebase"][e] += sum(1 for (ee, i) in sigcount if ee == e)
        for (e, j), n in self.dma_cnt.items():
            ss["dbase"][(e, j)] += n
        block = stack.enter_context(nc.Block())
        prog = self

        def run(ename, eng):
            waited = {}

            def w(key, sem, val):
                if waited.get(key, 0) >= val:
                    return
                eng.wait_ge(sem, val)
                waited[key] = val

            for i, ins in enumerate(prog.ins[ename]):
                for (de, di) in sorted(ins["deps"]):
                    dins = prog.ins[de][di]
                    if dins["dma"]:
                        (qe, j, n) = dins["dsem"]
                        w(("d", qe, j), dsem[(qe, j)], 16 * (n + dbase[(qe, j)]))
                    else:
                        if de == ename and ename == "pe":
                            continue
                        w(("e", de), esem[de], sigcount[(de, di)])
                if ins["dma"]:
                    (qe, j, n) = ins["dsem"]
                    if n > 1:
                        w(("d", qe, j), dsem[(qe, j)], 16 * (n - 1 + dbase[(qe, j)]))
                    inst = ins["fn"](eng)
                    inst.then_inc(dsem[(qe, j)], 16)
                else:
                    inst = ins["fn"](eng)
                    if (ename, i) in sigcount:
                        inst.then_inc(esem[ename], 1)
            for (qe, j), n in prog.dma_cnt.items():
                if qe == ename:
                    w(("d", qe, j), dsem[(qe, j)], 16 * (n + dbase[(qe, j)]))

        block.tensor(lambda eng: run("pe", eng))
        block.scalar(lambda eng: run("act", eng))
        block.vector(lambda eng: run("dve", eng))
        block.gpsimd(lambda eng: run("pool", eng))
        block.sync(lambda eng: run("sp", eng))


O_CQ, O_CKV, O_KR, O_DQ, O_DK, O_DV, O_IQ, O_IK, O_IW, O_G = 0, 512, 768, 832, 1856, 2112, 2368, 3392, 3456, 3472


def _rot_perm(n, half, rot, blocks=1, bw=None):
    bw = bw or n
    idx = np.arange(n)
    out = idx.copy()
    for b in range(n // bw):
        o = b * bw
        out[o:o + half] = idx[o + half:o + rot]
        out[o + half:o + rot] = idx[o:o + half]
    return out


def _pk(w, ncol):
    K = w.shape[0]
    return np.ascontiguousarray(w.reshape(K // 128, 128, ncol).transpose(1, 0, 2))


def host_layout(inp):
    f = np.float32
    w_in = np.asarray(inp["w_in"], f)[0]
    out = {}
    tiles = []
    for h in range(8):
        tiles.append(np.arange(O_DQ + h * 128, O_DQ + (h + 1) * 128))
    for g in range(2):
        tiles.append(np.arange(O_DK + g * 128, O_DK + (g + 1) * 128))
    for hp in range(8):
        tiles.append(np.arange(O_IQ + hp * 128, O_IQ + (hp + 1) * 128))
    tiles.append(np.concatenate([np.arange(O_IK, O_IK + 64)] * 2))
    tiles.append(np.concatenate([np.arange(O_KR, O_KR + 64)] * 2))
    perms = [_rot_perm(128, 16, 32)] * 10 + [_rot_perm(128, 8, 16, bw=64)] * 9 + [_rot_perm(128, 32, 64, bw=64)]
    w1f = np.empty((20, 128, 16, 256), f)
    for i, (cols, pm) in enumerate(zip(tiles, perms)):
        w1f[i, :, :, 0:128] = _pk(w_in[:, cols], 128)
        w1f[i, :, :, 128:256] = _pk(w_in[:, cols[pm]], 128)
    out["w1f"] = w1f
    tcols = [np.arange(O_CQ, O_CQ + 512), np.concatenate([np.arange(O_CKV, O_CKV + 256), np.arange(O_DV, O_DV + 256)])]
    for j in range(8):
        tcols.append(np.arange(O_G + j * 512, O_G + (j + 1) * 512))
    out["w1t"] = np.stack([_pk(w_in[:, c], 512) for c in tcols])
    out["w1iw"] = _pk(w_in[:, O_IW:O_IW + 16], 16)
    out["gmixT"] = np.ascontiguousarray(np.asarray(inp["norm_mix_g"], f)[0].reshape(16, 128).T)
    rc = np.zeros((128, 9), f)
    for ty, (half, rot, bw) in enumerate([(16, 32, 128), (8, 16, 64), (32, 64, 64)]):
        invf = (ROPE_THETA ** (-(np.arange(half, dtype=np.float32) * 2.0) / rot)).astype(f)
        for r in range(128):
            j = r % bw
            rc[r, ty * 3 + 1] = PI / 2
            if j < rot:
                rc[r, ty * 3 + 0] = invf[j % half]
                rc[r, ty * 3 + 2] = PI if j < half else 0.0
    out["ropec"] = rc
    out["ident"] = np.eye(128, dtype=f)
    wuq = np.asarray(inp["mla_w_uq"], f)[0]
    pm = _rot_perm(64, 32, 64)
    wq = np.empty((8, 128, 4, 256), f)
    for h in range(8):
        wq[h, :, :, 0:128] = _pk(wuq[:, h, 0:128], 128)
        wq[h, :, :, 128:192] = _pk(wuq[:, h, 128:192], 64)
        wq[h, :, :, 192:256] = _pk(wuq[:, h, 128:192][:, pm], 64)
    out["wq"] = wq
    out["gq"] = np.ascontiguousarray(np.asarray(inp["mla_q_norm_g"], f)[0].reshape(4, 128).T)
    out["gkv"] = np.ascontiguousarray(np.asarray(inp["mla_kv_norm_g"], f)[0].reshape(2, 128).T)
    out["wuk"] = _pk(np.asarray(inp["mla_w_uk"], f)[0].reshape(256, 1024), 1024)
    out["wuv"] = _pk(np.asarray(inp["mla_w_uv"], f)[0].reshape(256, 1024), 1024)
    out["wa"] = _pk(np.asarray(inp["w_branch_a"], f)[0], 2048)
    out["wb"] = _pk(np.asarray(inp["w_branch_b"], f)[0], 2048)
    out["wo"] = _pk(np.asarray(inp["w_out"], f)[0], 2048)
    out["wpq"] = _pk(np.asarray(inp["peer_w_q"], f)[0].reshape(2048, 1024), 1024)
    sk = np.asarray(inp["peer_sub_keys"], f)[0]
    kb = np.zeros((128, 8, 256), f)
    for h in range(8):
        for p in range(2):
            kb[p * 64:(p + 1) * 64, h, p * 128:(p + 1) * 128] = sk[h, p].T
    out["kb"] = kb
    out["gffn"] = np.asarray(inp["norm_ffn_g"], f)[0]
    out["gfin"] = np.asarray(inp["norm_final_g"], f)
    out["put"] = np.ascontiguousarray(np.asarray(inp["peer_u"], f)[0].reshape(128, 128, 16, 128).transpose(0, 3, 2, 1)).reshape(16384, 2048)
    out["gffnT"] = np.ascontiguousarray(np.asarray(inp["norm_ffn_g"], f)[0].reshape(16, 128).T)
    out["iota128"] = np.tile(np.arange(128, dtype=f)[None, :], (128, 1))
    out["pv"] = np.asarray(inp["peer_v"], f)[0]
    out["iota16"] = np.tile(np.arange(16, dtype=f)[None, :], (128, 1))
    return out


SHAPES = {
    "x": ([S, D], F32), "pos": ([S], I32),
    "w1f": ([20, 128, 16, 256], F32), "w1t": ([10, 128, 16, 512], F32), "w1iw": ([128, 16, 16], F32),
    "gmixT": ([128, 16], F32), "ropec": ([128, 9], F32), "ident": ([128, 128], F32),
    "wq": ([8, 128, 4, 256], F32), "gq": ([128, 4], F32), "gkv": ([128, 2], F32),
    "wuk": ([128, 2, 1024], F32), "wuv": ([128, 2, 1024], F32),
    "wa": ([128, 8, 2048], F32), "wb": ([128, 8, 2048], F32), "wo": ([128, 16, 2048], F32),
    "wpq": ([128, 16, 1024], F32), "kb": ([128, 8, 256], F32), "gffn": ([D], F32), "gfin": ([D], F32),
    "put": ([16384, D], F32), "pv": ([16384, D], F32), "iota16": ([128, 16], F32),
    "gffnT": ([128, 16], F32), "iota128": ([128, 128], F32),
}


def build_nc(phases=("p0", "p1", "p2", "p3", "p4", "p5"), debug=()):
    nc = bass.Bass("TRN2", target_bir_lowering=False)
    IN = {k: nc.dram_tensor(k, sh, dt, kind="ExternalInput").ap() for k, (sh, dt) in SHAPES.items()}
    y = nc.dram_tensor("y", [S, D], F32, kind="ExternalOutput").ap()

    def scratch(name, shape, dt):
        kind = "ExternalOutput" if name in debug else "Internal"
        return nc.dram_tensor(name, shape, dt, kind=kind).ap()

    SC = dict(
        dqT=scratch("dqT", [8, 128, S], BF16), dkT=scratch("dkT", [2, 128, S], BF16),
        iqT=scratch("iqT", [8, 128, S], BF16), ikT=scratch("ikT", [128, S], BF16),
        kpeT=scratch("kpeT", [128, S], BF16), cqnT=scratch("cqnT", [128, 4, S], BF16),
        ckvnT=scratch("ckvnT", [128, 2, S], BF16), dv=scratch("dv", [S, 256], BF16),
        iw=scratch("iw", [S, 16], F32), gs=scratch("gs", [S, 4096], BF16),
        oaT=scratch("oaT", [8, 128, S], BF16), obT=scratch("obT", [8, 128, S], BF16),
        x2=scratch("x2", [S, D], F32),
        pub=scratch("pub", [16384, D], BF16), pvb=scratch("pvb", [16384, D], BF16),
    )

    with ExitStack() as gst:
        def gsb(name, shape, dt):
            return gst.enter_context(nc.sbuf_tensor(name, shape, dt))

        SEMS = dict(stack=gst, esem={}, dsem={}, ebase={}, dbase={})
        identb = gsb("identb", [128, 128], BF16)
        identf = gsb("identf", [128, 128], F32)
        onesb = gsb("onesb", [128, 128], BF16)
        trig_cm = nc.sbuf_tensor("trig", [128, 6, S], BF16)
        trig = trig_cm.__enter__()

        if "p0" in phases:
            with ExitStack() as st:
                sb = lambda n, s, d, _p="q1_": st.enter_context(nc.sbuf_tensor(_p + n, s, d))
                P = Prog(nc, SEMS)
                posi = sb("posi", [128, S], I32)
                posf = sb("posf", [128, S], F32)
                ang = sb("ang", [128, S], F32)
                kk = sb("kk", [128, S], F32)
                rc = sb("rc", [128, 9], F32)
                P.op("sp", "dma_start", [], ["identf"], out=identf[:], in_=IN["ident"])
                P.op("sp", "dma_start", [], ["rc"], out=rc[:], in_=IN["ropec"])
                P.op("sp", "dma_start", [], ["posi"], out=posi[:], in_=IN["pos"].partition_broadcast(128))
                P.op("dve", "tensor_copy", ["identf"], ["identb"], out=identb[:], in_=identf[:])
                P.op("pool", "memset", [], ["onesb"], ap=onesb[:], constant=1.0)
                P.op("dve", "tensor_copy", ["posi"], ["posf"], out=posf[:], in_=posi[:])
                for ty in range(3):
                    for cs in range(2):
                        P.op("dve", "tensor_scalar", ["posf", "rc"], ["ang"], out=ang[:], in0=posf[:],
                             scalar1=rc[:, ty * 3:ty * 3 + 1], scalar2=rc[:, ty * 3 + 1 + cs:ty * 3 + 2 + cs],
                             op0=ALU.mult, op1=ALU.add)
                        P.op("dve", "tensor_scalar", ["ang"], ["kk"], out=kk[:], in0=ang[:], scalar1=1.0 / (2 * PI),
                             scalar2=MAGIC, op0=ALU.mult, op1=ALU.add)
                        P.op("dve", "tensor_scalar", ["kk"], ["kk"], out=kk[:], in0=kk[:], scalar1=-MAGIC, scalar2=None,
                             op0=ALU.add)
                        P.op("dve", "scalar_tensor_tensor", ["kk", "ang"], ["kk"], out=kk[:], in0=kk[:], scalar=-2 * PI,
                             in1=ang[:], op0=ALU.mult, op1=ALU.add)
                        P.op("dve", "tensor_scalar", ["kk"], ["kk"], out=kk[:], in0=kk[:], scalar1=-PI, scalar2=PI,
                             op0=ALU.max, op1=ALU.min)
                        P.op("act", "activation", ["kk"], ["trig%d" % (ty * 2 + cs)], out=trig[:, ty * 2 + cs, :],
                             in_=kk[:], func=AF.Sin)
                P.emit(st)

        if "p1" in phases:
            with ExitStack() as st:
                sb = lambda n, s, d, _p="q2_": st.enter_context(nc.sbuf_tensor(_p + n, s, d))
                psb = lambda n, s, d, _p="q4_": st.enter_context(nc.psum_tensor(_p + n, s, d))
                P = Prog(nc, SEMS)
                hT = sb("hT", [128, 16, S], BF16)
                xt = [sb("xt%d" % i, [128, D], F32) for i in range(2)]
                xs = [sb("xs%d" % i, [128, D], BF16) for i in range(2)]
                junk = sb("junk", [128, D], BF16)
                ss = sb("ss", [128, 16], F32)
                sq = sb("sq", [128, 16], F32)
                rstd = sb("rstd", [128, 16], F32)
                gmix = sb("gmix", [128, 16], F32)
                wbuf = [sb("wbuf%d" % i, [128, 16, 512], BF16) for i in range(2)]
                ost = [sb("ost%d" % i, [128, 16, 512], BF16) for i in range(2)]
                tmp = [sb("tmp%d" % i, [128, 512], F32) for i in range(4)]
                cqs = sb("cqs", [128, 16], F32)
                cqn = [sb("cqn%d" % i, [128, 512], BF16) for i in range(2)]
                iwst = sb("iwst", [128, 16, 16], F32)
                wiw = sb("wiw", [128, 16, 16], BF16)
                ptr = [psb("ptr%d" % i, [128, 8, 128], BF16) for i in range(2)]
                pA = [psb("pA%d" % i, [128, 512], F32) for i in range(3)]
                pB = [psb("pB%d" % i, [128, 512], F32) for i in range(3)]
                P.op("sp", "dma_start", [], ["gmix"], out=gmix[:], in_=IN["gmixT"])
                for T in range(NT):
                    s = T % 2
                    tsl = slice(T * 128, (T + 1) * 128)
                    P.op("sp", "dma_start", [], ["xt%d" % s], out=xt[s][:], in_=IN["x"][tsl, :])
                    P.op("act", "activation", ["xt%d" % s], ["junk", "ss%d" % T], out=junk[:], in_=xt[s][:], func=AF.Square,
                         accum_out=ss[:, T:T + 1])
                    P.op("act", "activation", ["ss%d" % T], ["sq%d" % T], out=sq[:, T:T + 1], in_=ss[:, T:T + 1], func=AF.Sqrt,
                         scale=1.0 / D, bias=EPS)
                    P.op("dve", "reciprocal", ["sq%d" % T], ["rstd%d" % T], out=rstd[:, T:T + 1], in_=sq[:, T:T + 1])
                    P.op("act", "activation", ["xt%d" % s, "rstd%d" % T], ["xs%d" % s], out=xs[s][:], in_=xt[s][:], func=AF.Copy,
                         scale=rstd[:, T:T + 1])
                    for dk in range(16):
                        b = dk // 8
                        P.op("pe", "transpose", ["xs%d" % s, "identb"], ["ptr%d" % b], out=ptr[b][:, dk % 8, :],
                             in_=xs[s][:, dk * 128:(dk + 1) * 128], identity=identb[:])
                    for dk in range(16):
                        b = dk // 8
                        if dk % 2 == 0:
                            P.op("act", "activation", ["ptr%d" % b, "gmix"], ["hT%d" % T], out=hT[:, dk, tsl], in_=ptr[b][:, dk % 8, :],
                                 func=AF.Copy, scale=gmix[:, dk:dk + 1])
                        else:
                            P.op("dve", "tensor_scalar", ["ptr%d" % b, "gmix"], ["hT%d" % T], out=hT[:, dk, tsl], in0=ptr[b][:, dk % 8, :],
                                 scalar1=gmix[:, dk:dk + 1], scalar2=None, op0=ALU.mult)
                hT_all = ["hT%d" % T for T in range(NT)]
                dests = [SC["dqT"][h] for h in range(8)] + [SC["dkT"][g] for g in range(2)] + [SC["iqT"][h] for h in range(8)] + [SC["ikT"], SC["kpeT"]]
                types = [0] * 10 + [1] * 9 + [2]
                nblk = 0
                for bi in range(20):
                    ws = nblk % 2
                    osl = nblk % 2
                    nblk += 1
                    ty = types[bi]
                    P.op("pool", "dma_start", [], ["wbuf%d" % ws], out=wbuf[ws][:, :, 0:256], in_=IN["w1f"][bi])
                    for tg in range(4):
                        csl = slice(tg * 512, (tg + 1) * 512)
                        pi = (bi * 4 + tg) % 3
                        for dk in range(16):
                            P.op("pe", "matmul", ["wbuf%d" % ws] + hT_all[tg * 4:tg * 4 + 4], ["pA%d" % pi], out=pA[pi][:],
                                 lhsT=wbuf[ws][:, dk, 0:128], rhs=hT[:, dk, csl], start=(dk == 0), stop=(dk == 15))
                        for dk in range(16):
                            P.op("pe", "matmul", ["wbuf%d" % ws] + hT_all[tg * 4:tg * 4 + 4], ["pB%d" % pi], out=pB[pi][:],
                                 lhsT=wbuf[ws][:, dk, 128:256], rhs=hT[:, dk, csl], start=(dk == 0), stop=(dk == 15))
                        ti = (bi * 4 + tg) % 2
                        P.op("dve", "tensor_tensor", ["pA%d" % pi, "trig"], ["tmp%d" % (2 * ti)], out=tmp[2 * ti][:], in0=pA[pi][:],
                             in1=trig[:, ty * 2, csl], op=ALU.mult)
                        P.op("dve", "tensor_tensor", ["pB%d" % pi, "trig"], ["tmp%d" % (2 * ti + 1)], out=tmp[2 * ti + 1][:], in0=pB[pi][:],
                             in1=trig[:, ty * 2 + 1, csl], op=ALU.mult)
                        P.op("pool", "tensor_tensor", ["tmp%d" % (2 * ti), "tmp%d" % (2 * ti + 1)], ["ost%d" % osl],
                             out=ost[osl][:].rearrange("p a b -> p (a b)")[:, csl], in0=tmp[2 * ti][:], in1=tmp[2 * ti + 1][:], op=ALU.add)
                    P.op("sp", "dma_start", ["ost%d" % osl], [], out=dests[bi], in_=ost[osl][:].rearrange("p a b -> p (a b)")[:, 0:S])
                for bi in range(10):
                    ws = nblk % 2
                    osl = nblk % 2
                    nblk += 1
                    P.op("pool", "dma_start", [], ["wbuf%d" % ws], out=wbuf[ws][:], in_=IN["w1t"][bi])
                    for T in range(NT):
                        tsl = slice(T * 128, (T + 1) * 128)
                        pi = T % 3
                        for dk in range(16):
                            P.op("pe", "matmul", ["wbuf%d" % ws, "hT%d" % T], ["pA%d" % pi], out=pA[pi][:], lhsT=hT[:, dk, tsl],
                                 rhs=wbuf[ws][:, dk, :], start=(dk == 0), stop=(dk == 15))
                        if bi >= 2:
                            P.op("act", "activation", ["pA%d" % pi], ["ost%d" % osl], out=ost[osl][:, T, :], in_=pA[pi][:], func=AF.Sigmoid)
                        else:
                            ncq = 512 if bi == 0 else 256
                            c = T % 2
                            P.op("act", "activation", ["pA%d" % pi], ["tmp0", "cqs"], out=tmp[0][:, 0:ncq], in_=pA[pi][:, 0:ncq], func=AF.Square,
                                 accum_out=cqs[:, 0:1])
                            P.op("act", "activation", ["cqs"], ["cqs1"], out=cqs[:, 1:2], in_=cqs[:, 0:1], func=AF.Sqrt, scale=1.0 / ncq, bias=EPS)
                            P.op("dve", "reciprocal", ["cqs1"], ["cqs2"], out=cqs[:, 2:3], in_=cqs[:, 1:2])
                            P.op("dve", "tensor_scalar", ["pA%d" % pi, "cqs2"], ["cqn%d" % c], out=cqn[c][:, 0:ncq], in0=pA[pi][:, 0:ncq],
                                 scalar1=cqs[:, 2:3], scalar2=None, op0=ALU.mult)
                            if bi == 1:
                                P.op("act", "activation", ["pA%d" % pi], ["ost%d" % osl], out=ost[osl][:, T, 0:256], in_=pA[pi][:, 256:512], func=AF.Copy)
                            nk = ncq // 128
                            for kc in range(nk):
                                P.op("pe", "transpose", ["cqn%d" % c, "identb"], ["ptr%d" % c], out=ptr[c][:, kc, :],
                                     in_=cqn[c][:, kc * 128:(kc + 1) * 128], identity=identb[:])
                            lo = 0 if bi == 0 else 256
                            P.op("act", "activation", ["ptr%d" % c], ["ost%d" % osl], out=ost[osl][:, T, lo:lo + ncq].rearrange("p (k q) -> p k q", q=128),
                                 in_=ptr[c][:, 0:nk, :], func=AF.Copy)
                    if bi >= 2:
                        P.op("sp", "dma_start", ["ost%d" % osl], [], out=SC["gs"][:, (bi - 2) * 512:(bi - 1) * 512].rearrange("(t p) c -> p t c", p=128),
                             in_=ost[osl][:])
                    else:
                        nk = 4 if bi == 0 else 2
                        lo = 0 if bi == 0 else 256
                        dst = SC["cqnT"] if bi == 0 else SC["ckvnT"]
                        for kc in range(nk):
                            P.op("sp", "dma_start", ["ost%d" % osl], [], out=dst[:, kc, :].rearrange("p (t q) -> p t q", q=128),
                                 in_=ost[osl][:, :, lo + kc * 128:lo + (kc + 1) * 128])
                        if bi == 1:
                            P.op("sp", "dma_start", ["ost%d" % osl], [], out=SC["dv"].rearrange("(t p) c -> p t c", p=128), in_=ost[osl][:, :, 0:256])
                P.op("pool", "dma_start", [], ["wiw"], out=wiw[:], in_=IN["w1iw"])
                for T in range(NT):
                    tsl = slice(T * 128, (T + 1) * 128)
                    pi = T % 3
                    for dk in range(16):
                        P.op("pe", "matmul", ["wiw", "hT%d" % T], ["pB%d" % pi], out=pB[pi][:, 0:16], lhsT=hT[:, dk, tsl], rhs=wiw[:, dk, :],
                             start=(dk == 0), stop=(dk == 15))
                    P.op("act", "activation", ["pB%d" % pi], ["iwst"], out=iwst[:, T, :], in_=pB[pi][:, 0:16], func=AF.Copy)
                P.op("sp", "dma_start", ["iwst"], [], out=SC["iw"].rearrange("(t p) c -> p t c", p=128), in_=iwst[:])
                P.emit(st)

        if "p2" in phases:
            with ExitStack() as st:
                sb = lambda n, s, d, _p="q3_": st.enter_context(nc.sbuf_tensor(_p + n, s, d))
                psb = lambda n, s, d, _p="q5_": st.enter_context(nc.psum_tensor(_p + n, s, d))
                P = Prog(nc, SEMS)
                cq = sb("cq_s", [128, 4, S], BF16)
                ckv = sb("ckv_s", [128, 2, S], BF16)
                kpe = sb("kpe_s", [128, S], BF16)
                wq = [sb("wq%d" % i, [128, 4, 256], BF16) for i in range(2)]
                gq = sb("gq", [128, 4], F32)
                gkv = sb("gkv", [128, 2], F32)
                wuk = sb("wuk", [128, 2, 1024], BF16)
                wuv = sb("wuv", [128, 2, 1024], BF16)
                vall = sb("vall", [128, 16, 1024], BF16)
                qn = [sb("qn%d" % i, [128, S], BF16) for i in range(2)]
                qr = [sb("qr%d" % i, [128, S], BF16) for i in range(2)]
                kn = [sb("kn%d" % i, [128, S], BF16) for i in range(2)]
                pT = [sb("pT%d" % i, [128, 512], BF16) for i in range(3)]
                rden = sb("rden", [128, 512], F32)
                ost = [sb("oast%d" % i, [128, S], BF16) for i in range(2)]
                t1 = sb("t1", [128, 512], F32)
                t2 = sb("t2", [128, 512], F32)
                pp = [psb("pp%d" % i, [128, 512], F32) for i in range(4)]
                pS = [psb("pS%d" % i, [128, 512], F32) for i in range(2)]
                pO = psb("pO", [128, 512], F32)
                pD = psb("pD", [128, 512], F32)
                P.op("sp", "dma_start", [], ["cq"], out=cq[:], in_=SC["cqnT"])
                P.op("sp", "dma_start", [], ["ckv"], out=ckv[:], in_=SC["ckvnT"])
                P.op("sp", "dma_start", [], ["kpe"], out=kpe[:], in_=SC["kpeT"])
                P.op("sp", "dma_start", [], ["gq"], out=gq[:], in_=IN["gq"])
                P.op("sp", "dma_start", [], ["gkv"], out=gkv[:], in_=IN["gkv"])
                for kc in range(2):
                    P.op("pool", "dma_start", [], ["wuk"], out=wuk[:, kc, :], in_=IN["wuk"][:, kc, :])
                    P.op("pool", "dma_start", [], ["wuv"], out=wuv[:, kc, :], in_=IN["wuv"][:, kc, :])
                for kc in range(2):
                    P.op("dve", "tensor_scalar", ["wuk", "gkv"], ["wuk"], out=wuk[:, kc, :], in0=wuk[:, kc, :], scalar1=gkv[:, kc:kc + 1], scalar2=None, op0=ALU.mult)
                    P.op("dve", "tensor_scalar", ["wuv", "gkv"], ["wuv"], out=wuv[:, kc, :], in0=wuv[:, kc, :], scalar1=gkv[:, kc:kc + 1], scalar2=None, op0=ALU.mult)
                n = 0
                for kt in range(16):
                    ksl = slice(kt * 128, (kt + 1) * 128)
                    for hf in range(2):
                        pi = n % 4
                        n += 1
                        for kc in range(2):
                            P.op("pe", "matmul", ["ckv", "wuv"], ["pp%d" % pi], out=pp[pi][:], lhsT=ckv[:, kc, ksl], rhs=wuv[:, kc, hf * 512:(hf + 1) * 512],
                                 start=(kc == 0), stop=(kc == 1))
                        P.op("act", "activation", ["pp%d" % pi], ["vall"], out=vall[:, kt, hf * 512:(hf + 1) * 512], in_=pp[pi][:], func=AF.Copy)
                sc_mla = float(192 ** -0.5)
                for h in range(8):
                    s = h % 2
                    P.op("pool", "dma_start", [], ["wq%d" % s], out=wq[s][:], in_=IN["wq"][h])
                    for kc in range(4):
                        P.op("dve", "tensor_scalar", ["wq%d" % s, "gq"], ["wq%d" % s], out=wq[s][:, kc, :], in0=wq[s][:, kc, :], scalar1=gq[:, kc:kc + 1], scalar2=None, op0=ALU.mult)
                    for tg in range(4):
                        csl = slice(tg * 512, (tg + 1) * 512)
                        for kc in range(4):
                            P.op("pe", "matmul", ["wq%d" % s, "cq"], ["pp0"], out=pp[0][:], lhsT=wq[s][:, kc, 0:128], rhs=cq[:, kc, csl], start=(kc == 0), stop=(kc == 3))
                        P.op("act", "activation", ["pp0"], ["qn%d" % s], out=qn[s][:, csl], in_=pp[0][:], func=AF.Copy)
                        for kc in range(4):
                            P.op("pe", "matmul", ["wq%d" % s, "cq"], ["pp1"], out=pp[1][0:64, :], lhsT=wq[s][:, kc, 128:192], rhs=cq[:, kc, csl], start=(kc == 0), stop=(kc == 3))
                        for kc in range(4):
                            P.op("pe", "matmul", ["wq%d" % s, "cq"], ["pp2"], out=pp[2][0:64, :], lhsT=wq[s][:, kc, 192:256], rhs=cq[:, kc, csl], start=(kc == 0), stop=(kc == 3))
                        P.op("dve", "tensor_tensor", ["pp1", "trig"], ["t1"], out=t1[0:64, :], in0=pp[1][0:64, :], in1=trig[0:64, 4, csl], op=ALU.mult)
                        P.op("dve", "tensor_tensor", ["pp2", "trig"], ["t2"], out=t2[0:64, :], in0=pp[2][0:64, :], in1=trig[0:64, 5, csl], op=ALU.mult)
                        P.op("pool", "tensor_tensor", ["t1", "t2"], ["qr%d" % s], out=qr[s][0:64, csl], in0=t1[0:64, :], in1=t2[0:64, :], op=ALU.add)
                        for kc in range(2):
                            P.op("pe", "matmul", ["wuk", "ckv"], ["pp3"], out=pp[3][:], lhsT=wuk[:, kc, h * 128:(h + 1) * 128], rhs=ckv[:, kc, csl], start=(kc == 0), stop=(kc == 1))
                        P.op("act", "activation", ["pp3"], ["kn%d" % s], out=kn[s][:, csl], in_=pp[3][:], func=AF.Copy)
                    cnt = 0
                    for qg in range(4):
                        nkt = 4 * (qg + 1)
                        for kt in range(nkt):
                            j = kt - 4 * qg
                            c0 = 128 * j if j > 0 else 0
                            cols = slice(qg * 512 + c0, (qg + 1) * 512)
                            ksl = slice(kt * 128, (kt + 1) * 128)
                            a = cnt % 2
                            k = cnt % 3
                            cnt += 1
                            P.op("pe", "matmul", ["kn%d" % s, "qn%d" % s], ["pS%d" % a], out=pS[a][:, c0:512], lhsT=kn[s][:, ksl], rhs=qn[s][:, cols], start=True, stop=False)
                            P.op("pe", "matmul", ["kpe", "qr%d" % s], ["pS%d" % a], out=pS[a][:, c0:512], lhsT=kpe[0:64, ksl], rhs=qr[s][0:64, cols], start=False, stop=True)
                            P.op("act", "activation", ["pS%d" % a], ["pT%d" % k], out=pT[k][:, c0:512], in_=pS[a][:, c0:512], func=AF.Exp, scale=sc_mla)
                            if j >= 0:
                                P.op("pool", "memset", [], ["pT%d" % k], ap=pT[k][64:128, c0:c0 + 64], constant=0.0)
                            P.op("pe", "matmul", ["vall", "pT%d" % k], ["pO"], out=pO[:, c0:512], lhsT=vall[:, kt, h * 128:(h + 1) * 128], rhs=pT[k][:, c0:512],
                                 start=(kt == 0), stop=(kt == nkt - 1))
                            P.op("pe", "matmul", ["onesb", "pT%d" % k], ["pD"], out=pD[:, c0:512], lhsT=onesb[:], rhs=pT[k][:, c0:512],
                                 start=(kt == 0), stop=(kt == nkt - 1))
                        P.op("dve", "reciprocal", ["pD"], ["rden"], out=rden[:], in_=pD[:])
                        P.op("dve", "tensor_tensor", ["pO", "rden"], ["oast%d" % s], out=ost[s][:, qg * 512:(qg + 1) * 512], in0=pO[:], in1=rden[:], op=ALU.mult)
                    P.op("sp", "dma_start", ["oast%d" % s], [], out=SC["oaT"][h], in_=ost[s][:])
                P.emit(st)

        trig_cm.__exit__(None, None, None)

        if "p3" in phases:
            with ExitStack() as st:
                sb = lambda n, s, d, _p="p3_": st.enter_context(nc.sbuf_tensor(_p + n, s, d))
                psb = lambda n, s, d, _p="p3_": st.enter_context(nc.psum_tensor(_p + n, s, d))
                P = Prog(nc, SEMS)
                iqT = sb("iqT", [128, 8, S], BF16)
                ikT = sb("ikT", [128, S], BF16)
                iw = sb("iw", [128, 16, 16], F32)
                dqT = sb("dqT", [128, 8, S], BF16)
                dkT = sb("dkT", [128, 2, S], BF16)
                dvs = sb("dvs", [128, 16, 256], BF16)
                origs = [sb("orig%d" % i, [128, S], F32) for i in range(4)]
                works = [sb("work%d" % i, [128, S], F32) for i in range(2)]
                masks = [sb("mask%d" % i, [128, S], BF16) for i in range(2)]
                mxs = [sb("mx%d" % i, [128, 8], F32) for i in range(2)]
                MTs = [sb("MT%d" % i, [128, 16, 512], BF16) for i in range(2)]
                diag = [sb("diag%d" % i, [128, 16, 128], BF16) for i in range(2)]
                Rb = [sb("R%d" % i, [128, 512], BF16) for i in range(4)]
                pT = [sb("pT%d" % i, [128, 512], BF16) for i in range(3)]
                rden = sb("rden", [128, 512], F32)
                obst = [sb("obst%d" % i, [128, 8, 512], BF16) for i in range(2)]
                pDs = [psb("pDs%d" % i, [128, 512], F32) for i in range(2)]
                pIS = psb("pIS", [128, 512], F32)
                ptr = psb("ptr", [128, 8, 128], BF16)
                pS = [psb("pS%d" % i, [128, 512], F32) for i in range(2)]
                pO = psb("pO", [128, 512], F32)
                pD = psb("pD", [128, 512], F32)
                for h in range(8):
                    P.op("sp", "dma_start", [], ["iqT"], out=iqT[:, h, :], in_=SC["iqT"][h])
                    P.op("sp", "dma_start", [], ["dqT"], out=dqT[:, h, :], in_=SC["dqT"][h])
                for g in range(2):
                    P.op("sp", "dma_start", [], ["dkT"], out=dkT[:, g, :], in_=SC["dkT"][g])
                P.op("sp", "dma_start", [], ["ikT"], out=ikT[:], in_=SC["ikT"])
                P.op("sp", "dma_start", [], ["iw"], out=iw[:], in_=SC["iw"].rearrange("(t p) c -> p t c", p=128))
                P.op("sp", "dma_start", [], ["dvs"], out=dvs[:], in_=SC["dv"].rearrange("(t p) c -> p t c", p=128))
                for (src_, dst_) in ((IN["put"], SC["pub"]), (IN["pv"], SC["pvb"])):
                    for r0 in range(0, 16384, 1024):
                        P.op("pool", "dma_start", ["iqT", "dqT", "dkT", "ikT", "iw", "dvs"], [], out=dst_[r0:r0 + 1024, :].rearrange("r (a b) -> (r a) b", b=1024),
                             in_=src_[r0:r0 + 1024, :].rearrange("r (a b) -> (r a) b", b=1024))
                sc_idx = float(64 ** -0.5 * 16 ** -0.5)
                sc_dsa = float(128 ** -0.5)
                nR = 0
                nD = 0
                cnt = 0
                def stage_IS(g):
                    qg = g // 2
                    st_ = dict(nD=0)
                    for T in (2 * g, 2 * g + 1):
                        i = T % 4
                        tsl = slice(T * 128, (T + 1) * 128)
                        nk = 128 * (T + 1)
                        dd = T % 2
                        orig = origs[i]
                        P.op("dve", "tensor_tensor", ["identf", "iw"], ["diag%d" % dd], out=diag[dd][:],
                             in0=identf[:].unsqueeze(1).to_broadcast([128, 16, 128]), in1=iw[:, T, :].unsqueeze(2).to_broadcast([128, 16, 128]), op=ALU.mult)
                        for kg in range((nk + 511) // 512):
                            ncol = min(512, nk - kg * 512)
                            ksl = slice(kg * 512, kg * 512 + ncol)
                            for h in range(16):
                                rows = slice((h % 2) * 64, (h % 2) * 64 + 64)
                                a = CN["nD"] % 2
                                CN["nD"] += 1
                                r = CN["nR"] % 4
                                CN["nR"] += 1
                                P.op("pe", "matmul", ["iqT", "ikT"], ["pDs%d" % a], out=pDs[a][:, 0:ncol], lhsT=iqT[rows, h // 2, tsl], rhs=ikT[rows, ksl], start=True, stop=True)
                                P.op("act", "activation", ["pDs%d" % a], ["R%d" % r], out=Rb[r][:, 0:ncol], in_=pDs[a][:, 0:ncol], func=AF.Relu, scale=sc_idx)
                                P.op("pe", "matmul", ["diag%d" % dd, "R%d" % r], ["pIS"], out=pIS[:, 0:ncol], lhsT=diag[dd][:, h, :], rhs=Rb[r][:, 0:ncol],
                                     start=(h == 0), stop=(h == 15))
                            P.op("act", "activation", ["pIS"], ["orig%d" % i], out=orig[:, ksl], in_=pIS[:, 0:ncol], func=AF.Copy)
                        P.op("pool", "memset", [], ["orig%d" % i], ap=orig[0:64, nk - 64:nk], constant=NEG)

                def stage_topk(g, att_qg=None):
                    tiles = (2 * g, 2 * g + 1)
                    heads_done = 0
                    if tiles[0] >= 2:
                        for rnd in range(32):
                            for T in tiles:
                                i, dd, nk = T % 4, T % 2, 128 * (T + 1)
                                src = origs[i] if rnd == 0 else works[dd]
                                sname = ("orig%d" % i) if rnd == 0 else ("work%d" % dd)
                                P.op("dve", "max", [sname], ["mx%d" % dd], out=mxs[dd][:], in_=src[:, 0:nk])
                            for T in tiles:
                                i, dd, nk = T % 4, T % 2, 128 * (T + 1)
                                src = origs[i] if rnd == 0 else works[dd]
                                sname = ("orig%d" % i) if rnd == 0 else ("work%d" % dd)
                                P.op("dve", "match_replace", ["mx%d" % dd, sname], ["work%d" % dd], out=works[dd][:, 0:nk], in_to_replace=mxs[dd][:],
                                     in_values=src[:, 0:nk], imm_value=NEG)
                            if att_qg is not None and rnd % 4 == 3:
                                stage_att_head(att_qg, heads_done)
                                heads_done += 1
                    if att_qg is not None:
                        while heads_done < 8:
                            stage_att_head(att_qg, heads_done)
                            heads_done += 1
                    for T in tiles:
                        i, dd, nk = T % 4, T % 2, 128 * (T + 1)
                        mask = masks[dd]
                        if T >= 2:
                            P.op("dve", "tensor_tensor", ["work%d" % dd, "orig%d" % i], ["mask%d" % dd], out=mask[:, 0:nk], in0=works[dd][:, 0:nk], in1=origs[i][:, 0:nk], op=ALU.not_equal)
                        else:
                            P.op("dve", "tensor_scalar", ["orig%d" % i], ["mask%d" % dd], out=mask[:, 0:nk], in0=origs[i][:, 0:nk], scalar1=NEG / 2, scalar2=None, op0=ALU.is_gt)

                def stage_maskT(g):
                    mp = (g // 2) % 2
                    MT = MTs[mp]
                    if g % 2 == 0:
                        P.op("pool", "memset", [], ["MT%d" % mp], ap=MT[:], constant=0.0)
                    for T in (2 * g, 2 * g + 1):
                        i, dd = T % 4, T % 2
                        mask = masks[dd]
                        for k0 in range(0, T + 1, 8):
                            n8 = min(8, T + 1 - k0)
                            for kt in range(k0, k0 + n8):
                                P.op("pe", "transpose", ["mask%d" % dd, "identb"], ["ptr"], out=ptr[:, kt - k0, :], in_=mask[:, kt * 128:(kt + 1) * 128], identity=identb[:])
                            P.op("act", "activation", ["ptr"], ["MT%d" % mp], out=MT[:, k0:k0 + n8, i * 128:(i + 1) * 128], in_=ptr[:, 0:n8, :], func=AF.Copy)

                def stage_att_head(qg, h):
                    os_ = qg % 2
                    g = h // 4
                    nkt = 4 * (qg + 1)
                    for kt in range(nkt):
                        ksl = slice(kt * 128, (kt + 1) * 128)
                        a = CN["cnt"] % 2
                        k = CN["cnt"] % 3
                        CN["cnt"] += 1
                        P.op("pe", "matmul", ["dkT", "dqT"], ["pS%d" % a], out=pS[a][:], lhsT=dkT[:, g, ksl], rhs=dqT[:, h, qg * 512:(qg + 1) * 512], start=True, stop=True)
                        P.op("act", "activation", ["pS%d" % a], ["pT%d" % k], out=pT[k][:], in_=pS[a][:], func=AF.Exp, scale=sc_dsa)
                        P.op("pool", "tensor_tensor", ["pT%d" % k, "MT%d" % os_], ["pT%d" % k], out=pT[k][:], in0=pT[k][:], in1=MTs[os_][:, kt, :], op=ALU.mult)
                        P.op("pe", "matmul", ["dvs", "pT%d" % k], ["pO"], out=pO[:], lhsT=dvs[:, kt, g * 128:(g + 1) * 128], rhs=pT[k][:], start=(kt == 0), stop=(kt == nkt - 1))
                        P.op("pe", "matmul", ["onesb", "pT%d" % k], ["pD"], out=pD[:], lhsT=onesb[:], rhs=pT[k][:], start=(kt == 0), stop=(kt == nkt - 1))
                    P.op("dve", "reciprocal", ["pD"], ["rden"], out=rden[:], in_=pD[:])
                    P.op("dve", "tensor_tensor", ["pO", "rden"], ["obst%d" % os_], out=obst[os_][:, h, :], in0=pO[:], in1=rden[:], op=ALU.mult)
                    if h == 7:
                        P.op("sp", "dma_start", ["obst%d" % os_], [], out=SC["obT"][:, :, qg * 512:(qg + 1) * 512].rearrange("h p s -> p h s"), in_=obst[os_][:])

                CN = dict(nD=0, nR=0, cnt=0)
                stage_IS(0)
                pending = None
                for g in range(8):
                    if g + 1 < 8:
                        stage_IS(g + 1)
                    stage_topk(g, att_qg=pending)
                    pending = None
                    stage_maskT(g)
                    if g % 2 == 1:
                        pending = g // 2
                for h in range(8):
                    stage_att_head(pending, h)
                P.emit(st)

        if "p4" in phases:
            with ExitStack() as st:
                sb = lambda n, s, d, _p="p4_": st.enter_context(nc.sbuf_tensor(_p + n, s, d))
                psb = lambda n, s, d, _p="p4_": st.enter_context(nc.psum_tensor(_p + n, s, d))
                P = Prog(nc, SEMS)
                wa = sb("wa", [128, 8, D], BF16)
                wb = sb("wb", [128, 8, D], BF16)
                wo = sb("wo", [128, 16, D], BF16)
                oat = [sb("oat%d" % i, [128, 8, 128], BF16) for i in range(2)]
                obt = [sb("obt%d" % i, [128, 8, 128], BF16) for i in range(2)]
                gst_ = [sb("gs%d" % i, [128, 4096], BF16) for i in range(1)] * 2
                xt = [sb("xt%d" % i, [128, D], F32) for i in range(2)]
                mg = sb("mg", [128, D], BF16)
                mT = sb("mT", [128, 16, 128], BF16)
                t1 = [sb("t1_%d" % i, [128, 512], F32) for i in range(2)]
                t2 = [sb("t2_%d" % i, [128, 512], F32) for i in range(2)]
                pY = [psb("pY%d" % i, [128, 512], F32) for i in range(4)]
                ptr = [psb("ptr%d" % i, [128, 8, 128], BF16) for i in range(2)]
                pZ = [psb("pZ%d" % i, [128, 512], F32) for i in range(2)]
                for h in range(8):
                    for hf in range(2):
                        P.op("pool", "dma_start", [], ["wa"], out=wa[:, h, hf * 1024:(hf + 1) * 1024], in_=IN["wa"][:, h, hf * 1024:(hf + 1) * 1024])
                        P.op("pool", "dma_start", [], ["wb"], out=wb[:, h, hf * 1024:(hf + 1) * 1024], in_=IN["wb"][:, h, hf * 1024:(hf + 1) * 1024])
                for dk in range(16):
                    for hf in range(2):
                        P.op("pool", "dma_start", [], ["wo"], out=wo[:, dk, hf * 1024:(hf + 1) * 1024], in_=IN["wo"][:, dk, hf * 1024:(hf + 1) * 1024])
                ny = 0
                for T in range(NT):
                    s = T % 2
                    tsl = slice(T * 128, (T + 1) * 128)
                    P.op("sp", "dma_start", [], ["oat%d" % s], out=oat[s][:], in_=SC["oaT"][:, :, tsl].rearrange("h p s -> p h s"))
                    P.op("sp", "dma_start", [], ["obt%d" % s], out=obt[s][:], in_=SC["obT"][:, :, tsl].rearrange("h p s -> p h s"))
                    P.op("sp", "dma_start", [], ["gs0"], out=gst_[s][:], in_=SC["gs"][tsl, :])
                    P.op("sp", "dma_start", [], ["xt%d" % s], out=xt[s][:], in_=IN["x"][tsl, :])
                    for cg in range(4):
                        csl = slice(cg * 512, (cg + 1) * 512)
                        ya = ny % 4
                        yb = (ny + 1) % 4
                        ny += 2
                        u = cg % 2
                        for h in range(8):
                            P.op("pe", "matmul", ["oat%d" % s, "wa"], ["pY%d" % ya], out=pY[ya][:], lhsT=oat[s][:, h, :], rhs=wa[:, h, csl], start=(h == 0), stop=(h == 7))
                        for h in range(8):
                            P.op("pe", "matmul", ["obt%d" % s, "wb"], ["pY%d" % yb], out=pY[yb][:], lhsT=obt[s][:, h, :], rhs=wb[:, h, csl], start=(h == 0), stop=(h == 7))
                        P.op("dve", "tensor_tensor", ["pY%d" % ya, "gs0"], ["t1_%d" % u], out=t1[u][:], in0=pY[ya][:], in1=gst_[s][:, csl], op=ALU.mult)
                        P.op("dve", "tensor_tensor", ["pY%d" % yb, "gs0"], ["t2_%d" % u], out=t2[u][:], in0=pY[yb][:], in1=gst_[s][:, 2048 + cg * 512:2048 + (cg + 1) * 512], op=ALU.mult)
                        P.op("pool", "tensor_tensor", ["t1_%d" % u, "t2_%d" % u], ["mg"], out=mg[:, csl], in0=t1[u][:], in1=t2[u][:], op=ALU.add)
                    for dk in range(16):
                        b = dk // 8
                        P.op("pe", "transpose", ["mg", "identb"], ["ptr%d" % b], out=ptr[b][:, dk % 8, :], in_=mg[:, dk * 128:(dk + 1) * 128], identity=identb[:])
                    for b in range(2):
                        P.op("act", "activation", ["ptr%d" % b], ["mT"], out=mT[:, b * 8:(b + 1) * 8, :], in_=ptr[b][:], func=AF.Copy)
                    for og in range(4):
                        csl = slice(og * 512, (og + 1) * 512)
                        z = og % 2
                        for dk in range(16):
                            P.op("pe", "matmul", ["mT", "wo"], ["pZ%d" % z], out=pZ[z][:], lhsT=mT[:, dk, :], rhs=wo[:, dk, csl], start=(dk == 0), stop=(dk == 15))
                        P.op("dve", "tensor_tensor", ["pZ%d" % z, "xt%d" % s], ["xt%d" % s], out=xt[s][:, csl], in0=pZ[z][:], in1=xt[s][:, csl], op=ALU.add)
                    P.op("sp", "dma_start", ["xt%d" % s], [], out=SC["x2"][tsl, :], in_=xt[s][:])
                P.emit(st)

        if "p5" in phases:
            with ExitStack() as st:
                sb = lambda n, s, d, _p="p5_": st.enter_context(nc.sbuf_tensor(_p + n, s, d))
                psb = lambda n, s, d, _p="p5_": st.enter_context(nc.psum_tensor(_p + n, s, d))
                P = Prog(nc, SEMS)
                wpq = sb("wpq", [128, 16, 1024], BF16)
                kb = sb("kb", [128, 8, 256], BF16)
                gffnT = sb("gffnT", [128, 16], F32)
                gfin = sb("gfin", [128, D], F32)
                iota = sb("iota", [128, 16], F32)
                iota128 = sb("iota128", [128, 128], F32)
                GT = sb("GT", [128, 256, 128], BF16)
                NSB = 16
                OH2 = sb("OH2", [128, NSB, 128], BF16)
                OH1 = sb("OH1", [128, NSB, 128], BF16)
                hnTGs = [sb("hnTG%d" % i, [128, 16, 256], BF16) for i in range(2)]
                NUB = 4
                ub = [sb("ub%d" % i, [128, D], BF16) for i in range(NUB)]
                vb = [sb("vb%d" % i, [128, 1024], BF16) for i in range(3)]
                x2t = [sb("x2t%d" % i, [128, D], F32) for i in range(2)]
                hnf = sb("hnf", [128, D], F32)
                qTs = sb("qTs", [128, 8, 128], BF16)
                ssb = sb("ssb", [128, 8, 256], F32)
                wk = sb("wk", [128, 256], F32)
                vals = sb("vals", [128, 16, 16], F32)
                idxs = sb("idxs", [128, 16, 16], U32)
                idxf = sb("idxf", [128, 16, 16], F32)
                tv = sb("tv", [128, 8, 16], F32)
                tp = sb("tp", [128, 8, 16], U32)
                ti = sb("ti", [128, 8, 16], U32)
                tj = sb("tj", [128, 8, 16], U32)
                tif = sb("tif", [128, 8, 16], F32)
                tjf = sb("tjf", [128, 8, 16], F32)
                e1 = [sb("e1_%d" % i, [128, 8, 16], F32) for i in range(4)]
                e2 = [sb("e2_%d" % i, [128, 8, 16], F32) for i in range(4)]
                gw = [sb("gw%d" % i, [128, 128], F32) for i in range(4)]
                selT = sb("selT", [128, 3, 128], F32)
                ex = sb("ex", [128, 8, 16], F32)
                zs = sb("zs", [128, 8], F32)
                ga = [sb("ga%d" % i, [128, 256], BF16) for i in range(2)]
                stA = sb("stA", [128, 4], F32)
                stC = sb("stC", [128, 4], F32)
                B = [psb("B%d" % i, [128, 512], F32) for i in range(8)]
                for dk in range(16):
                    P.op("pool", "dma_start", [], ["wpq"], out=wpq[:, dk, :], in_=IN["wpq"][:, dk, :])
                for h in range(8):
                    P.op("pool", "dma_start", [], ["kb"], out=kb[:, h, :], in_=IN["kb"][:, h, :])
                P.op("sp", "dma_start", [], ["gffnT"], out=gffnT[:], in_=IN["gffnT"])
                P.op("sp", "dma_start", [], ["gfin"], out=gfin[:], in_=IN["gfin"].partition_broadcast(128))
                P.op("sp", "dma_start", [], ["iota"], out=iota[:], in_=IN["iota16"])
                P.op("sp", "dma_start", [], ["iota128"], out=iota128[:], in_=IN["iota128"])
                CN = dict(u=0, v=0, g=0, a=0)

                def stage_A(T):
                    p = T % 4
                    gp = (T // 2) % 2
                    hnTG = hnTGs[gp]
                    HT = "hnTG%d" % gp
                    tcol = slice((T % 2) * 128, (T % 2) * 128 + 128)
                    tsl = slice(T * 128, (T + 1) * 128)
                    yield P.op("sp", "dma_start", [], ["hnf"], out=hnf[:], in_=SC["x2"][tsl, :])
                    yield P.op("act", "activation", ["hnf"], ["ssb", "stA0"], out=ssb[:].rearrange("p a b -> p (a b)"), in_=hnf[:], func=AF.Square, accum_out=stA[:, 0:1])
                    yield P.op("act", "activation", ["stA0"], ["stA1"], out=stA[:, 1:2], in_=stA[:, 0:1], func=AF.Sqrt, scale=1.0 / D, bias=EPS)
                    yield P.op("dve", "reciprocal", ["stA1"], ["stA2"], out=stA[:, 2:3], in_=stA[:, 1:2])
                    yield P.op("act", "activation", ["hnf", "stA2"], ["hnf"], out=hnf[:], in_=hnf[:], func=AF.Copy, scale=stA[:, 2:3])
                    for r in range(4):
                        z = 6 + r % 2
                        for q4 in range(4):
                            dk = r * 4 + q4
                            yield P.op("pe", "transpose", ["hnf", "identf"], ["B%d" % z], out=B[z][:, q4 * 128:(q4 + 1) * 128], in_=hnf[:, dk * 128:(dk + 1) * 128], identity=identf[:])
                        for q4 in range(4):
                            dk = r * 4 + q4
                            if dk % 2 == 0:
                                yield P.op("act", "activation", ["B%d" % z, "gffnT"], [HT], out=hnTG[:, dk, tcol], in_=B[z][:, q4 * 128:(q4 + 1) * 128],
                                           func=AF.Copy, scale=gffnT[:, dk:dk + 1])
                            else:
                                yield P.op("dve", "tensor_scalar", ["B%d" % z, "gffnT"], [HT], out=hnTG[:, dk, tcol], in0=B[z][:, q4 * 128:(q4 + 1) * 128],
                                           scalar1=gffnT[:, dk:dk + 1], scalar2=None, op0=ALU.mult)
                    for hh in range(2):
                        z = 6 + hh
                        for h4 in range(4):
                            h = hh * 4 + h4
                            for dk in range(16):
                                yield P.op("pe", "matmul", ["wpq", HT], ["B%d" % z], out=B[z][:, h4 * 128:(h4 + 1) * 128], lhsT=wpq[:, dk, h * 128:(h + 1) * 128],
                                           rhs=hnTG[:, dk, tcol], start=(dk == 0), stop=(dk == 15))
                        yield P.op("act", "activation", ["B%d" % z], ["qTs"], out=qTs[:, hh * 4:(hh + 1) * 4, :].rearrange("p a b -> p (a b)"), in_=B[z][:], func=AF.Copy)
                    for h2 in range(4):
                        z = 6 + h2 % 2
                        for hi in range(2):
                            h = h2 * 2 + hi
                            yield P.op("pe", "matmul", ["qTs", "kb"], ["B%d" % z], out=B[z][:, hi * 256:(hi + 1) * 256], lhsT=qTs[:, h, :], rhs=kb[:, h, :], start=True, stop=True)
                        yield P.op("act", "activation", ["B%d" % z], ["ssb"], out=ssb[:, h2 * 2:h2 * 2 + 2, :].rearrange("p a b -> p (a b)"), in_=B[z][:], func=AF.Copy)
                    for hp in range(16):
                        src = ssb[:, hp // 2, (hp % 2) * 128:(hp % 2) * 128 + 128]
                        yield P.op("dve", "max", ["ssb"], ["vals"], out=vals[:, hp, 0:8], in_=src)
                        yield P.op("dve", "max_index", ["ssb", "vals"], ["idxs"], out=idxs[:, hp, 0:8], in_max=vals[:, hp, 0:8], in_values=src)
                        yield P.op("dve", "match_replace", ["ssb", "vals"], ["wk"], out=wk[:, 0:128], in_to_replace=vals[:, hp, 0:8], in_values=src, imm_value=NEG)
                        yield P.op("dve", "max", ["wk"], ["vals"], out=vals[:, hp, 8:16], in_=wk[:, 0:128])
                        yield P.op("dve", "max_index", ["wk", "vals"], ["idxs"], out=idxs[:, hp, 8:16], in_max=vals[:, hp, 8:16], in_values=wk[:, 0:128])
                    yield P.op("dve", "tensor_copy", ["idxs"], ["idxf"], out=idxf[:], in_=idxs[:])
                    cand = ssb[:].rearrange("p h (a b) -> p h a b", b=16)
                    v4 = vals[:].rearrange("p (h two) k -> p h two k", two=2)
                    i4 = idxf[:].rearrange("p (h two) k -> p h two k", two=2)
                    yield P.op("dve", "tensor_tensor", ["vals"], ["ssb"], out=cand, in0=v4[:, :, 0, :].unsqueeze(3).to_broadcast([128, 8, 16, 16]),
                         in1=v4[:, :, 1, :].unsqueeze(2).to_broadcast([128, 8, 16, 16]), op=ALU.add)
                    for h in range(8):
                        src = ssb[:, h, :]
                        yield P.op("dve", "max", ["ssb"], ["tv"], out=tv[:, h, 0:8], in_=src)
                        yield P.op("dve", "max_index", ["ssb", "tv"], ["tp"], out=tp[:, h, 0:8], in_max=tv[:, h, 0:8], in_values=src)
                        yield P.op("dve", "match_replace", ["ssb", "tv"], ["wk"], out=wk[:], in_to_replace=tv[:, h, 0:8], in_values=src, imm_value=NEG)
                        yield P.op("dve", "max", ["wk"], ["tv"], out=tv[:, h, 8:16], in_=wk[:])
                        yield P.op("dve", "max_index", ["wk", "tv"], ["tp"], out=tp[:, h, 8:16], in_max=tv[:, h, 8:16], in_values=wk[:])
                    yield P.op("dve", "tensor_single_scalar", ["tp"], ["ti"], out=ti[:], in_=tp[:], scalar=4, op=ALU.logical_shift_right)
                    yield P.op("dve", "tensor_single_scalar", ["tp"], ["tj"], out=tj[:], in_=tp[:], scalar=15, op=ALU.bitwise_and)
                    yield P.op("dve", "tensor_copy", ["ti"], ["tif"], out=tif[:], in_=ti[:])
                    yield P.op("dve", "tensor_copy", ["tj"], ["tjf"], out=tjf[:], in_=tj[:])
                    oh = ssb[:].rearrange("p h (a b) -> p h a b", b=16)
                    iob = iota[:].unsqueeze(1).unsqueeze(1).to_broadcast([128, 8, 16, 16])
                    for (sel, side, dst, dn) in ((tif, 0, e1[p], "e1_%d" % p), (tjf, 1, e2[p], "e2_%d" % p)):
                        yield P.op("dve", "tensor_tensor", ["tif", "tjf", "iota"], ["ssb"], out=oh, in0=sel[:].unsqueeze(3).to_broadcast([128, 8, 16, 16]), in1=iob, op=ALU.is_equal)
                        yield P.op("dve", "tensor_tensor", ["ssb", "idxf"], ["ssb"], out=oh, in0=oh, in1=i4[:, :, side, :].unsqueeze(2).to_broadcast([128, 8, 16, 16]), op=ALU.mult)
                        yield P.op("dve", "tensor_reduce", ["ssb"], [dn], out=dst[:], in_=oh, axis=AX.X, op=ALU.add)
                    yield P.op("dve", "tensor_tensor", ["tv"], ["ex"], out=ex[:], in0=tv[:], in1=tv[:, :, 0:1].to_broadcast([128, 8, 16]), op=ALU.subtract)
                    yield P.op("act", "activation", ["ex"], ["ex"], out=ex[:], in_=ex[:], func=AF.Exp)
                    yield P.op("dve", "tensor_reduce", ["ex"], ["zs"], out=zs[:], in_=ex[:], axis=AX.X, op=ALU.add)
                    yield P.op("dve", "reciprocal", ["zs"], ["zs"], out=zs[:], in_=zs[:])
                    yield P.op("dve", "tensor_tensor", ["ex", "zs"], ["gw%d" % p], out=gw[p][:].rearrange("p (a b) -> p a b", b=16), in0=ex[:], in1=zs[:].unsqueeze(2).to_broadcast([128, 8, 16]), op=ALU.mult)

                def stage_GT(T):
                    p = T % 4
                    tp_ = T % 2
                    srcs = [(e1[p][:].rearrange("p a b -> p (a b)"), "e1_%d" % p), (e2[p][:].rearrange("p a b -> p (a b)"), "e2_%d" % p), (gw[p][:], "gw%d" % p)]
                    for j, (ap_, nm) in enumerate(srcs):
                        P.op("pe", "transpose", [nm, "identf"], ["B7"], out=B[7][:, j * 128:(j + 1) * 128], in_=ap_, identity=identf[:])
                    P.op("act", "activation", ["B7"], ["selT"], out=selT[:].rearrange("p a b -> p (a b)"), in_=B[7][:, 0:384], func=AF.Copy)
                    for sub in range(128 // NSB):
                        tsub = slice(sub * NSB, (sub + 1) * NSB)
                        iob = iota128[:].unsqueeze(1).to_broadcast([128, NSB, 128])
                        P.op("dve", "tensor_tensor", ["iota128", "selT"], ["OH2"], out=OH2[:], in0=iob, in1=selT[:, 1, tsub].unsqueeze(2).to_broadcast([128, NSB, 128]), op=ALU.is_equal)
                        P.op("dve", "tensor_tensor", ["iota128", "selT"], ["OH1"], out=OH1[:], in0=iob, in1=selT[:, 0, tsub].unsqueeze(2).to_broadcast([128, NSB, 128]), op=ALU.is_equal)
                        P.op("pool", "tensor_tensor", ["OH1", "selT"], ["OH1"], out=OH1[:], in0=OH1[:], in1=selT[:, 2, tsub].unsqueeze(2).to_broadcast([128, NSB, 128]), op=ALU.mult)
                        for t4 in range(NSB // 4):
                            z = 6 + CN["g"] % 2
                            CN["g"] += 1
                            for tt in range(4):
                                t = t4 * 4 + tt
                                P.op("pe", "matmul", ["OH2", "OH1"], ["B%d" % z], out=B[z][:, tt * 128:(tt + 1) * 128], lhsT=OH2[:, t, :], rhs=OH1[:, t, :], start=True, stop=True)
                            tg0 = tp_ * 128 + sub * NSB + t4 * 4
                            P.op("act", "activation", ["B%d" % z], ["GT"], out=GT[:, tg0:tg0 + 4, :].rearrange("p t c -> p (t c)"), in_=B[z][:], func=AF.Copy)

                def stage_U(G, step):
                    gp = G % 2
                    hnTG = hnTGs[gp]
                    for c in range(128):
                        b = CN["u"] % NUB
                        CN["u"] += 1
                        P.op("sp", "dma_start", [], ["ub%d" % b], out=ub[b][:], in_=SC["pub"][c * 128:(c + 1) * 128, :])
                        z = 4 + (c // 2) % 2
                        reg = slice((c % 2) * 256, (c % 2) * 256 + 256)
                        for dk in range(16):
                            P.op("pe", "matmul", ["ub%d" % b, "hnTG%d" % gp], ["B%d" % z], out=B[z][:, reg], lhsT=ub[b][:, dk * 128:(dk + 1) * 128], rhs=hnTG[:, dk, :], start=(dk == 0), stop=(dk == 15))
                        k = CN["a"] % 2
                        CN["a"] += 1
                        P.op("act", "activation", ["B%d" % z], ["ga%d" % k], out=ga[k][:], in_=B[z][:, reg], func=AF.Gelu)
                        P.op("dve", "tensor_tensor", ["ga%d" % k, "GT"], ["GT"], out=GT[:, :, c], in0=ga[k][:], in1=GT[:, :, c], op=ALU.mult)
                        step(3)

                def stage_V(G, step):
                    for p in range(2):
                        T = 2 * G + p
                        P.op("sp", "dma_start", [], ["x2t%d" % p], out=x2t[p][:], in_=SC["x2"][T * 128:(T + 1) * 128, :])
                    for hv in range(2):
                        for c in range(128):
                            b = CN["v"] % 3
                            CN["v"] += 1
                            P.op("sp", "dma_start", [], ["vb%d" % b], out=vb[b][:], in_=SC["pvb"][c * 128:(c + 1) * 128, hv * 1024:(hv + 1) * 1024])
                            for p in range(2):
                                for cg in range(2):
                                    z = p * 2 + cg
                                    P.op("pe", "matmul", ["GT", "vb%d" % b], ["B%d" % z], out=B[z][:], lhsT=GT[:, p * 128:(p + 1) * 128, c], rhs=vb[b][:, cg * 512:(cg + 1) * 512],
                                         start=(c == 0), stop=(c == 127))
                            step(3)
                        for p in range(2):
                            for cg in range(2):
                                z = p * 2 + cg
                                csl = slice(hv * 1024 + cg * 512, hv * 1024 + (cg + 1) * 512)
                                P.op("dve", "tensor_tensor", ["B%d" % z, "x2t%d" % p], ["x2t%d" % p], out=x2t[p][:, csl], in0=B[z][:], in1=x2t[p][:, csl], op=ALU.add)
                    for p in range(2):
                        T = 2 * G + p
                        tsl = slice(T * 128, (T + 1) * 128)
                        X = "x2t%d" % p
                        P.op("act", "activation", [X], ["OH2", "stC0"], out=OH2[:].rearrange("p a b -> p (a b)"), in_=x2t[p][:], func=AF.Square, accum_out=stC[:, 0:1])
                        P.op("act", "activation", ["stC0"], ["stC1"], out=stC[:, 1:2], in_=stC[:, 0:1], func=AF.Sqrt, scale=1.0 / D, bias=EPS)
                        P.op("dve", "reciprocal", ["stC1"], ["stC2"], out=stC[:, 2:3], in_=stC[:, 1:2])
                        P.op("dve", "scalar_tensor_tensor", [X, "stC2", "gfin"], [X], out=x2t[p][:], in0=x2t[p][:], scalar=stC[:, 2:3], in1=gfin[:], op0=ALU.mult, op1=ALU.mult)
                        P.op("sp", "dma_start", [X], [], out=y[tsl, :], in_=x2t[p][:])

                def run_all(gen):
                    for _ in gen:
                        pass

                def chain(*gens):
                    for g_ in gens:
                        for v_ in g_:
                            yield v_

                run_all(stage_A(0))
                run_all(stage_A(1))
                for G in range(NT // 2):
                    stage_GT(2 * G)
                    stage_GT(2 * G + 1)
                    nxt = chain(stage_A(2 * G + 2), stage_A(2 * G + 3)) if G + 1 < NT // 2 else iter(())

                    def step(n, nxt=nxt):
                        for _ in range(n):
                            next(nxt, None)

                    stage_U(G, step)
                    stage_V(G, step)
                    run_all(nxt)
                P.emit(st)
    return nc


def kernel(**inputs):
    H = host_layout(inputs)
    x = np.asarray(inputs["x"], np.float32)
    pos = np.asarray(inputs["positions"]).astype(np.int32)
    nc = build_nc()
    in_maps = []
    for b in range(8):
        m = {k: H[k] for k in SHAPES if k not in ("x", "pos")}
        m["x"] = np.ascontiguousarray(x[b])
        m["pos"] = np.ascontiguousarray(pos[b])
        in_maps.append(m)
    res = run_bass_kernel_spmd(nc, in_maps, core_ids=list(range(8)))
    return np.stack([np.asarray(r["y"], dtype=np.float32) for r in res.results], axis=0)
```

```python
import numpy as np
from contextlib import ExitStack
import concourse.bass as bass
import concourse.mybir as mybir
from concourse.bass_utils import run_bass_kernel_spmd

F32 = mybir.dt.float32
BF16 = mybir.dt.bfloat16
I32 = mybir.dt.int32
U32 = mybir.dt.uint32
AF = mybir.ActivationFunctionType
ALU = mybir.AluOpType
AX = mybir.AxisListType

D = 2048
S = 2048
NT = 16
EPS = 1e-6
PI = float(np.pi)
MAGIC = 12582912.0
NEG = -1e30
ROPE_THETA = 500000.0

ENGS = ["pe", "act", "dve", "pool", "sp"]
N_DMA_SEMS = {"sp": 40, "pool": 24, "act": 8}


class Prog:
    def __init__(self, nc, semstate):
        self.nc = nc
        self.semstate = semstate
        self.ins = {e: [] for e in ENGS}
        self.last_w = {}
        self.readers = {}
        self.dma_rr = {e: 0 for e in N_DMA_SEMS}
        self.dma_cnt = {}

    def add(self, eng, fn, reads=(), writes=(), dma=False):
        idx = len(self.ins[eng])
        deps = set()
        for r in reads:
            w = self.last_w.get(r)
            if w is not None:
                deps.add(w)
        for r in writes:
            w = self.last_w.get(r)
            if w is not None:
                deps.add(w)
            for rd in self.readers.get(r, ()):
                deps.add(rd)
        deps.discard((eng, idx))
        rec = dict(fn=fn, deps=deps, dma=dma)
        if dma:
            j = self.dma_rr[eng]
            self.dma_rr[eng] = (j + 1) % N_DMA_SEMS[eng]
            n = self.dma_cnt.get((eng, j), 0) + 1
            self.dma_cnt[(eng, j)] = n
            rec["dsem"] = (eng, j, n)
        self.ins[eng].append(rec)
        for r in reads:
            self.readers.setdefault(r, []).append((eng, idx))
        for r in writes:
            self.last_w[r] = (eng, idx)
            self.readers[r] = []
        return (eng, idx)

    def op(self, eng, method, reads, writes, **kw):
        dma = method in ("dma_start", "indirect_dma_start")
        return self.add(eng, lambda e: getattr(e, method)(**kw), reads, writes, dma=dma)

    def emit(self, stack):
        nc = self.nc
        need = {e: set() for e in ENGS}
        for e in ENGS:
            for ins in self.ins[e]:
                for (de, di) in ins["deps"]:
                    if self.ins[de][di]["dma"]:
                        continue
                    if de == e and e == "pe":
                        continue
                    need[de].add(di)
        sigcount = {}
        for e in ENGS:
            c = 0
            for i, ins in enumerate(self.ins[e]):
                if ins["dma"]:
                    continue
                if i in need[e]:
                    c += 1
                    sigcount[(e, i)] = c
        ss = self.semstate
        gstack = ss["stack"]
        for e in ["pe", "act", "dve", "pool"]:
            if e not in ss["esem"]:
                ss["esem"][e] = gstack.enter_context(nc.semaphore("se_%s" % e))
                ss["ebase"][e] = 0
        for e, n in N_DMA_SEMS.items():
            for j in range(n):
                if self.dma_cnt.get((e, j), 0) > 0 and (e, j) not in ss["dsem"]:
                    ss["dsem"][(e, j)] = gstack.enter_context(nc.semaphore("sd_%s_%d" % (e, j)))
                    ss["dbase"][(e, j)] = 0
        esem = ss["esem"]
        dsem = ss["dsem"]
        ebase = dict(ss["ebase"])
        dbase = dict(ss["dbase"])
        for (e, i) in list(sigcount.keys()):
            sigcount[(e, i)] += ebase[e]
        for e in ["pe", "act", "dve", "pool"]:
            ss["ebase"][e] += sum(1 for (ee, i) in sigcount if ee == e)
        for (e, j), n in self.dma_cnt.items():
            ss["dbase"][(e, j)] += n
        block = stack.enter_context(nc.Block())
        prog = self

        def run(ename, eng):
            waited = {}

            def w(key, sem, val):
                if waited.get(key, 0) >= val:
                    return
                eng.wait_ge(sem, val)
                waited[key] = val

            for i, ins in enumerate(prog.ins[ename]):
                for (de, di) in sorted(ins["deps"]):
                    dins = prog.ins[de][di]
                    if dins["dma"]:
                        (qe, j, n) = dins["dsem"]
                        w(("d", qe, j), dsem[(qe, j)], 16 * (n + dbase[(qe, j)]))
                    else:
                        if de == ename and ename == "pe":
                            continue
                        w(("e", de), esem[de], sigcount[(de, di)])
                if ins["dma"]:
                    (qe, j, n) = ins["dsem"]
                    if n > 1:
                        w(("d", qe, j), dsem[(qe, j)], 16 * (n - 1 + dbase[(qe, j)]))
                    inst = ins["fn"](eng)
                    inst.then_inc(dsem[(qe, j)], 16)
                else:
                    inst = ins["fn"](eng)
                    if (ename, i) in sigcount:
                        inst.then_inc(esem[ename], 1)
            for (qe, j), n in prog.dma_cnt.items():
                if qe == ename:
                    w(("d", qe, j), dsem[(qe, j)], 16 * (n + dbase[(qe, j)]))

        block.tensor(lambda eng: run("pe", eng))
        block.scalar(lambda eng: run("act", eng))
        block.vector(lambda eng: run("dve", eng))
        block.gpsimd(lambda eng: run("pool", eng))
        block.sync(lambda eng: run("sp", eng))


O_CQ, O_CKV, O_KR, O_DQ, O_DK, O_DV, O_IQ, O_IK, O_IW, O_G = 0, 512, 768, 832, 1856, 2112, 2368, 3392, 3456, 3472


def _rot_perm(n, half, rot, blocks=1, bw=None):
    bw = bw or n
    idx = np.arange(n)
    out = idx.copy()
    for b in range(n // bw):
        o = b * bw
        out[o:o + half] = idx[o + half:o + rot]
        out[o + half:o + rot] = idx[o:o + half]
    return out


def _pk(w, ncol):
    K = w.shape[0]
    return np.ascontiguousarray(w.reshape(K // 128, 128, ncol).transpose(1, 0, 2))


def host_layout(inp):
    f = np.float32
    w_in = np.asarray(inp["w_in"], f)[0]
    out = {}
    tiles = []
    for h in range(8):
        tiles.append(np.arange(O_DQ + h * 128, O_DQ + (h + 1) * 128))
    for g in range(2):
        tiles.append(np.arange(O_DK + g * 128, O_DK + (g + 1) * 128))
    for hp in range(8):
        tiles.append(np.arange(O_IQ + hp * 128, O_IQ + (hp + 1) * 128))
    tiles.append(np.concatenate([np.arange(O_IK, O_IK + 64)] * 2))
    tiles.append(np.concatenate([np.arange(O_KR, O_KR + 64)] * 2))
    perms = [_rot_perm(128, 16, 32)] * 10 + [_rot_perm(128, 8, 16, bw=64)] * 9 + [_rot_perm(128, 32, 64, bw=64)]
    w1f = np.empty((20, 128, 16, 256), f)
    for i, (cols, pm) in enumerate(zip(tiles, perms)):
        w1f[i, :, :, 0:128] = _pk(w_in[:, cols], 128)
        w1f[i, :, :, 128:256] = _pk(w_in[:, cols[pm]], 128)
    out["w1f"] = w1f
    tcols = [np.arange(O_CQ, O_CQ + 512), np.concatenate([np.arange(O_CKV, O_CKV + 256), np.arange(O_DV, O_DV + 256)])]
    for j in range(8):
        tcols.append(np.arange(O_G + j * 512, O_G + (j + 1) * 512))
    out["w1t"] = np.stack([_pk(w_in[:, c], 512) for c in tcols])
    out["w1iw"] = _pk(w_in[:, O_IW:O_IW + 16], 16)
    out["gmixT"] = np.ascontiguousarray(np.asarray(inp["norm_mix_g"], f)[0].reshape(16, 128).T)
    rc = np.zeros((128, 9), f)
    for ty, (half, rot, bw) in enumerate([(16, 32, 128), (8, 16, 64), (32, 64, 64)]):
        invf = (ROPE_THETA ** (-(np.arange(half, dtype=np.float32) * 2.0) / rot)).astype(f)
        for r in range(128):
            j = r % bw
            rc[r, ty * 3 + 1] = PI / 2
            if j < rot:
                rc[r, ty * 3 + 0] = invf[j % half]
                rc[r, ty * 3 + 2] = PI if j < half else 0.0
    out["ropec"] = rc
    out["ident"] = np.eye(128, dtype=f)
    wuq = np.asarray(inp["mla_w_uq"], f)[0]
    pm = _rot_perm(64, 32, 64)
    wq = np.empty((8, 128, 4, 256), f)
    for h in range(8):
        wq[h, :, :, 0:128] = _pk(wuq[:, h, 0:128], 128)
        wq[h, :, :, 128:192] = _pk(wuq[:, h, 128:192], 64)
        wq[h, :, :, 192:256] = _pk(wuq[:, h, 128:192][:, pm], 64)
    out["wq"] = wq
    out["gq"] = np.ascontiguousarray(np.asarray(inp["mla_q_norm_g"], f)[0].reshape(4, 128).T)
    out["gkv"] = np.ascontiguousarray(np.asarray(inp["mla_kv_norm_g"], f)[0].reshape(2, 128).T)
    out["wuk"] = _pk(np.asarray(inp["mla_w_uk"], f)[0].reshape(256, 1024), 1024)
    out["wuv"] = _pk(np.asarray(inp["mla_w_uv"], f)[0].reshape(256, 1024), 1024)
    out["wa"] = _pk(np.asarray(inp["w_branch_a"], f)[0], 2048)
    out["wb"] = _pk(np.asarray(inp["w_branch_b"], f)[0], 2048)
    out["wo"] = _pk(np.asarray(inp["w_out"], f)[0], 2048)
    out["wpq"] = _pk(np.asarray(inp["peer_w_q"], f)[0].reshape(2048, 1024), 1024)
    sk = np.asarray(inp["peer_sub_keys"], f)[0]
    kb = np.zeros((128, 8, 256), f)
    for h in range(8):
        for p in range(2):
            kb[p * 64:(p + 1) * 64, h, p * 128:(p + 1) * 128] = sk[h, p].T
    out["kb"] = kb
    out["gffn"] = np.asarray(inp["norm_ffn_g"], f)[0]
    out["gfin"] = np.asarray(inp["norm_final_g"], f)
    out["put"] = np.ascontiguousarray(np.asarray(inp["peer_u"], f)[0].reshape(128, 128, 16, 128).transpose(0, 3, 2, 1)).reshape(16384, 2048)
    out["gffnT"] = np.ascontiguousarray(np.asarray(inp["norm_ffn_g"], f)[0].reshape(16, 128).T)
    out["iota128"] = np.tile(np.arange(128, dtype=f)[None, :], (128, 1))
    out["pv"] = np.asarray(inp["peer_v"], f)[0]
    out["iota16"] = np.tile(np.arange(16, dtype=f)[None, :], (128, 1))
    return out


SHAPES = {
    "x": ([S, D], F32), "pos": ([S], I32),
    "w1f": ([20, 128, 16, 256], F32), "w1t": ([10, 128, 16, 512], F32), "w1iw": ([128, 16, 16], F32),
    "gmixT": ([128, 16], F32), "ropec": ([128, 9], F32), "ident": ([128, 128], F32),
    "wq": ([8, 128, 4, 256], F32), "gq": ([128, 4], F32), "gkv": ([128, 2], F32),
    "wuk": ([128, 2, 1024], F32), "wuv": ([128, 2, 1024], F32),
    "wa": ([128, 8, 2048], F32), "wb": ([128, 8, 2048], F32), "wo": ([128, 16, 2048], F32),
    "wpq": ([128, 16, 1024], F32), "kb": ([128, 8, 256], F32), "gffn": ([D], F32), "gfin": ([D], F32),
    "put": ([16384, D], F32), "pv": ([16384, D], F32), "iota16": ([128, 16], F32),
    "gffnT": ([128, 16], F32), "iota128": ([128, 128], F32),
}


def build_nc(phases=("p0", "p1", "p2", "p3", "p4", "p5"), debug=()):
    nc = bass.Bass("TRN2", target_bir_lowering=False)
    IN = {k: nc.dram_tensor(k, sh, dt, kind="ExternalInput").ap() for k, (sh, dt) in SHAPES.items()}
    y = nc.dram_tensor("y", [S, D], F32, kind="ExternalOutput").ap()

    def scratch(name, shape, dt):
        kind = "ExternalOutput" if name in debug else "Internal"
        return nc.dram_tensor(name, shape, dt, kind=kind).ap()

    SC = dict(
        dqT=scratch("dqT", [8, 128, S], BF16), dkT=scratch("dkT", [2, 128, S], BF16),
        iqT=scratch("iqT", [8, 128, S], BF16), ikT=scratch("ikT", [128, S], BF16),
        kpeT=scratch("kpeT", [128, S], BF16), cqnT=scratch("cqnT", [128, 4, S], BF16),
        ckvnT=scratch("ckvnT", [128, 2, S], BF16), dv=scratch("dv", [S, 256], BF16),
        iw=scratch("iw", [S, 16], F32), gs=scratch("gs", [S, 4096], BF16),
        oaT=scratch("oaT", [8, 128, S], BF16), obT=scratch("obT", [8, 128, S], BF16),
        x2=scratch("x2", [S, D], F32),
        pub=scratch("pub", [16384, D], BF16), pvb=scratch("pvb", [16384, D], BF16),
    )

    with ExitStack() as gst:
        def gsb(name, shape, dt):
            return gst.enter_context(nc.sbuf_tensor(name, shape, dt))

        SEMS = dict(stack=gst, esem={}, dsem={}, ebase={}, dbase={})
        identb = gsb("identb", [128, 128], BF16)
        identf = gsb("identf", [128, 128], F32)
        onesb = gsb("onesb", [128, 128], BF16)
        trig_cm = nc.sbuf_tensor("trig", [128, 6, S], BF16)
        trig = trig_cm.__enter__()

        if "p0" in phases:
            with ExitStack() as st:
                sb = lambda n, s, d, _p="q1_": st.enter_context(nc.sbuf_tensor(_p + n, s, d))
                P = Prog(nc, SEMS)
                posi = sb("posi", [128, S], I32)
                posf = sb("posf", [128, S], F32)
                ang = sb("ang", [128, S], F32)
                kk = sb("kk", [128, S], F32)
                rc = sb("rc", [128, 9], F32)
                P.op("sp", "dma_start", [], ["identf"], out=identf[:], in_=IN["ident"])
                P.op("sp", "dma_start", [], ["rc"], out=rc[:], in_=IN["ropec"])
                P.op("sp", "dma_start", [], ["posi"], out=posi[:], in_=IN["pos"].partition_broadcast(128))
                P.op("dve", "tensor_copy", ["identf"], ["identb"], out=identb[:], in_=identf[:])
                P.op("pool", "memset", [], ["onesb"], ap=onesb[:], constant=1.0)
                P.op("dve", "tensor_copy", ["posi"], ["posf"], out=posf[:], in_=posi[:])
                for ty in range(3):
                    for cs in range(2):
                        P.op("dve", "tensor_scalar", ["posf", "rc"], ["ang"], out=ang[:], in0=posf[:],
                             scalar1=rc[:, ty * 3:ty * 3 + 1], scalar2=rc[:, ty * 3 + 1 + cs:ty * 3 + 2 + cs],
                             op0=ALU.mult, op1=ALU.add)
                        P.op("dve", "tensor_scalar", ["ang"], ["kk"], out=kk[:], in0=ang[:], scalar1=1.0 / (2 * PI),
                             scalar2=MAGIC, op0=ALU.mult, op1=ALU.add)
                        P.op("dve", "tensor_scalar", ["kk"], ["kk"], out=kk[:], in0=kk[:], scalar1=-MAGIC, scalar2=None,
                             op0=ALU.add)
                        P.op("dve", "scalar_tensor_tensor", ["kk", "ang"], ["kk"], out=kk[:], in0=kk[:], scalar=-2 * PI,
                             in1=ang[:], op0=ALU.mult, op1=ALU.add)
                        P.op("dve", "tensor_scalar", ["kk"], ["kk"], out=kk[:], in0=kk[:], scalar1=-PI, scalar2=PI,
                             op0=ALU.max, op1=ALU.min)
                        P.op("act", "activation", ["kk"], ["trig%d" % (ty * 2 + cs)], out=trig[:, ty * 2 + cs, :],
                             in_=kk[:], func=AF.Sin)
                P.emit(st)

        if "p1" in phases:
            with ExitStack() as st:
                sb = lambda n, s, d, _p="q2_": st.enter_context(nc.sbuf_tensor(_p + n, s, d))
                psb = lambda n, s, d, _p="q4_": st.enter_context(nc.psum_tensor(_p + n, s, d))
                P = Prog(nc, SEMS)
                hT = sb("hT", [128, 16, S], BF16)
                xt = [sb("xt%d" % i, [128, D], F32) for i in range(2)]
                xs = [sb("xs%d" % i, [128, D], BF16) for i in range(2)]
                junk = sb("junk", [128, D], BF16)
                ss = sb("ss", [128, 16], F32)
                sq = sb("sq", [128, 16], F32)
                rstd = sb("rstd", [128, 16], F32)
                gmix = sb("gmix", [128, 16], F32)
                wbuf = [sb("wbuf%d" % i, [128, 16, 512], BF16) for i in range(2)]
                ost = [sb("ost%d" % i, [128, 16, 512], BF16) for i in range(2)]
                tmp = [sb("tmp%d" % i, [128, 512], F32) for i in range(4)]
                cqs = sb("cqs", [128, 16], F32)
                cqn = [sb("cqn%d" % i, [128, 512], BF16) for i in range(2)]
                iwst = sb("iwst", [128, 16, 16], F32)
                wiw = sb("wiw", [128, 16, 16], BF16)
                ptr = [psb("ptr%d" % i, [128, 8, 128], BF16) for i in range(2)]
                pA = [psb("pA%d" % i, [128, 512], F32) for i in range(3)]
                pB = [psb("pB%d" % i, [128, 512], F32) for i in range(3)]
                P.op("sp", "dma_start", [], ["gmix"], out=gmix[:], in_=IN["gmixT"])
                for T in range(NT):
                    s = T % 2
                    tsl = slice(T * 128, (T + 1) * 128)
                    P.op("sp", "dma_start", [], ["xt%d" % s], out=xt[s][:], in_=IN["x"][tsl, :])
                    P.op("act", "activation", ["xt%d" % s], ["junk", "ss%d" % T], out=junk[:], in_=xt[s][:], func=AF.Square,
                         accum_out=ss[:, T:T + 1])
                    P.op("act", "activation", ["ss%d" % T], ["sq%d" % T], out=sq[:, T:T + 1], in_=ss[:, T:T + 1], func=AF.Sqrt,
                         scale=1.0 / D, bias=EPS)
                    P.op("dve", "reciprocal", ["sq%d" % T], ["rstd%d" % T], out=rstd[:, T:T + 1], in_=sq[:, T:T + 1])
                    P.op("act", "activation", ["xt%d" % s, "rstd%d" % T], ["xs%d" % s], out=xs[s][:], in_=xt[s][:], func=AF.Copy,
                         scale=rstd[:, T:T + 1])
                    for dk in range(16):
                        b = dk // 8
                        P.op("pe", "transpose", ["xs%d" % s, "identb"], ["ptr%d" % b], out=ptr[b][:, dk % 8, :],
                             in_=xs[s][:, dk * 128:(dk + 1) * 128], identity=identb[:])
                    for dk in range(16):
                        b = dk // 8
                        if dk % 2 == 0:
                            P.op("act", "activation", ["ptr%d" % b, "gmix"], ["hT%d" % T], out=hT[:, dk, tsl], in_=ptr[b][:, dk % 8, :],
                                 func=AF.Copy, scale=gmix[:, dk:dk + 1])
                        else:
                            P.op("dve", "tensor_scalar", ["ptr%d" % b, "gmix"], ["hT%d" % T], out=hT[:, dk, tsl], in0=ptr[b][:, dk % 8, :],
                                 scalar1=gmix[:, dk:dk + 1], scalar2=None, op0=ALU.mult)
                hT_all = ["hT%d" % T for T in range(NT)]
                dests = [SC["dqT"][h] for h in range(8)] + [SC["dkT"][g] for g in range(2)] + [SC["iqT"][h] for h in range(8)] + [SC["ikT"], SC["kpeT"]]
                types = [0] * 10 + [1] * 9 + [2]
                nblk = 0
                for bi in range(20):
                    ws = nblk % 2
                    osl = nblk % 2
                    nblk += 1
                    ty = types[bi]
                    P.op("pool", "dma_start", [], ["wbuf%d" % ws], out=wbuf[ws][:, :, 0:256], in_=IN["w1f"][bi])
                    for tg in range(4):
                        csl = slice(tg * 512, (tg + 1) * 512)
                        pi = (bi * 4 + tg) % 3
                        for dk in range(16):
                            P.op("pe", "matmul", ["wbuf%d" % ws] + hT_all[tg * 4:tg * 4 + 4], ["pA%d" % pi], out=pA[pi][:],
                                 lhsT=wbuf[ws][:, dk, 0:128], rhs=hT[:, dk, csl], start=(dk == 0), stop=(dk == 15))
                        for dk in range(16):
                            P.op("pe", "matmul", ["wbuf%d" % ws] + hT_all[tg * 4:tg * 4 + 4], ["pB%d" % pi], out=pB[pi][:],
                                 lhsT=wbuf[ws][:, dk, 128:256], rhs=hT[:, dk, csl], start=(dk == 0), stop=(dk == 15))
                        ti = (bi * 4 + tg) % 2
                        P.op("dve", "tensor_tensor", ["pA%d" % pi, "trig"], ["tmp%d" % (2 * ti)], out=tmp[2 * ti][:], in0=pA[pi][:],
                             in1=trig[:, ty * 2, csl], op=ALU.mult)
                        P.op("dve", "tensor_tensor", ["pB%d" % pi, "trig"], ["tmp%d" % (2 * ti + 1)], out=tmp[2 * ti + 1][:], in0=pB[pi][:],
                             in1=trig[:, ty * 2 + 1, csl], op=ALU.mult)
                        P.op("pool", "tensor_tensor", ["tmp%d" % (2 * ti), "tmp%d" % (2 * ti + 1)], ["ost%d" % osl],
                             out=ost[osl][:].rearrange("p a b -> p (a b)")[:, csl], in0=tmp[2 * ti][:], in1=tmp[2 * ti + 1][:], op=ALU.add)
                    P.op("sp", "dma_start", ["ost%d" % osl], [], out=dests[bi], in_=ost[osl][:].rearrange("p a b -> p (a b)")[:, 0:S])
                for bi in range(10):
                    ws = nblk % 2
                    osl = nblk % 2
                    nblk += 1
                    P.op("pool", "dma_start", [], ["wbuf%d" % ws], out=wbuf[ws][:], in_=IN["w1t"][bi])
                    for T in range(NT):
                        tsl = slice(T * 128, (T + 1) * 128)
                        pi = T % 3
                        for dk in range(16):
                            P.op("pe", "matmul", ["wbuf%d" % ws, "hT%d" % T], ["pA%d" % pi], out=pA[pi][:], lhsT=hT[:, dk, tsl],
                                 rhs=wbuf[ws][:, dk, :], start=(dk == 0), stop=(dk == 15))
                        if bi >= 2:
                            P.op("act", "activation", ["pA%d" % pi], ["ost%d" % osl], out=ost[osl][:, T, :], in_=pA[pi][:], func=AF.Sigmoid)
                        else:
                            ncq = 512 if bi == 0 else 256
                            c = T % 2
                            P.op("act", "activation", ["pA%d" % pi], ["tmp0", "cqs"], out=tmp[0][:, 0:ncq], in_=pA[pi][:, 0:ncq], func=AF.Square,
                                 accum_out=cqs[:, 0:1])
                            P.op("act", "activation", ["cqs"], ["cqs1"], out=cqs[:, 1:2], in_=cqs[:, 0:1], func=AF.Sqrt, scale=1.0 / ncq, bias=EPS)
                            P.op("dve", "reciprocal", ["cqs1"], ["cqs2"], out=cqs[:, 2:3], in_=cqs[:, 1:2])
                            P.op("dve", "tensor_scalar", ["pA%d" % pi, "cqs2"], ["cqn%d" % c], out=cqn[c][:, 0:ncq], in0=pA[pi][:, 0:ncq],
                                 scalar1=cqs[:, 2:3], scalar2=None, op0=ALU.mult)
                            if bi == 1:
                                P.op("act", "activation", ["pA%d" % pi], ["ost%d" % osl], out=ost[osl][:, T, 0:256], in_=pA[pi][:, 256:512], func=AF.Copy)
                            nk = ncq // 128
                            for kc in range(nk):
                                P.op("pe", "transpose", ["cqn%d" % c, "identb"], ["ptr%d" % c], out=ptr[c][:, kc, :],
                                     in_=cqn[c][:, kc * 128:(kc + 1) * 128], identity=identb[:])
                            lo = 0 if bi == 0 else 256
                            P.op("act", "activation", ["ptr%d" % c], ["ost%d" % osl], out=ost[osl][:, T, lo:lo + ncq].rearrange("p (k q) -> p k q", q=128),
                                 in_=ptr[c][:, 0:nk, :], func=AF.Copy)
                    if bi >= 2:
                        P.op("sp", "dma_start", ["ost%d" % osl], [], out=SC["gs"][:, (bi - 2) * 512:(bi - 1) * 512].rearrange("(t p) c -> p t c", p=128),
                             in_=ost[osl][:])
                    else:
                        nk = 4 if bi == 0 else 2
                        lo = 0 if bi == 0 else 256
                        dst = SC["cqnT"] if bi == 0 else SC["ckvnT"]
                        for kc in range(nk):
                            P.op("sp", "dma_start", ["ost%d" % osl], [], out=dst[:, kc, :].rearrange("p (t q) -> p t q", q=128),
                                 in_=ost[osl][:, :, lo + kc * 128:lo + (kc + 1) * 128])
                        if bi == 1:
                            P.op("sp", "dma_start", ["ost%d" % osl], [], out=SC["dv"].rearrange("(t p) c -> p t c", p=128), in_=ost[osl][:, :, 0:256])
                P.op("pool", "dma_start", [], ["wiw"], out=wiw[:], in_=IN["w1iw"])
                for T in range(NT):
                    tsl = slice(T * 128, (T + 1) * 128)
                    pi = T % 3
                    for dk in range(16):
                        P.op("pe", "matmul", ["wiw", "hT%d" % T], ["pB%d" % pi], out=pB[pi][:, 0:16], lhsT=hT[:, dk, tsl], rhs=wiw[:, dk, :],
                             start=(dk == 0), stop=(dk == 15))
                    P.op("act", "activation", ["pB%d" % pi], ["iwst"], out=iwst[:, T, :], in_=pB[pi][:, 0:16], func=AF.Copy)
                P.op("sp", "dma_start", ["iwst"], [], out=SC["iw"].rearrange("(t p) c -> p t c", p=128), in_=iwst[:])
                P.emit(st)

        if "p2" in phases:
            with ExitStack() as st:
                sb = lambda n, s, d, _p="q3_": st.enter_context(nc.sbuf_tensor(_p + n, s, d))
                psb = lambda n, s, d, _p="q5_": st.enter_context(nc.psum_tensor(_p + n, s, d))
                P = Prog(nc, SEMS)
                cq = sb("cq_s", [128, 4, S], BF16)
                ckv = sb("ckv_s", [128, 2, S], BF16)
                kpe = sb("kpe_s", [128, S], BF16)
                wq = [sb("wq%d" % i, [128, 4, 256], BF16) for i in range(2)]
                gq = sb("gq", [128, 4], F32)
                gkv = sb("gkv", [128, 2], F32)
                wuk = sb("wuk", [128, 2, 1024], BF16)
                wuv = sb("wuv", [128, 2, 1024], BF16)
                vall = sb("vall", [128, 16, 1024], BF16)
                qn = [sb("qn%d" % i, [128, S], BF16) for i in range(2)]
                qr = [sb("qr%d" % i, [128, S], BF16) for i in range(2)]
                kn = [sb("kn%d" % i, [128, S], BF16) for i in range(2)]
                pT = [sb("pT%d" % i, [128, 512], BF16) for i in range(3)]
                rden = sb("rden", [128, 512], F32)
                ost = [sb("oast%d" % i, [128, S], BF16) for i in range(2)]
                t1 = sb("t1", [128, 512], F32)
                t2 = sb("t2", [128, 512], F32)
                pp = [psb("pp%d" % i, [128, 512], F32) for i in range(4)]
                pS = [psb("pS%d" % i, [128, 512], F32) for i in range(2)]
                pO = psb("pO", [128, 512], F32)
                pD = psb("pD", [128, 512], F32)
                P.op("sp", "dma_start", [], ["cq"], out=cq[:], in_=SC["cqnT"])
                P.op("sp", "dma_start", [], ["ckv"], out=ckv[:], in_=SC["ckvnT"])
                P.op("sp", "dma_start", [], ["kpe"], out=kpe[:], in_=SC["kpeT"])
                P.op("sp", "dma_start", [], ["gq"], out=gq[:], in_=IN["gq"])
                P.op("sp", "dma_start", [], ["gkv"], out=gkv[:], in_=IN["gkv"])
                for kc in range(2):
                    P.op("pool", "dma_start", [], ["wuk"], out=wuk[:, kc, :], in_=IN["wuk"][:, kc, :])
                    P.op("pool", "dma_start", [], ["wuv"], out=wuv[:, kc, :], in_=IN["wuv"][:, kc, :])
                for kc in range(2):
                    P.op("dve", "tensor_scalar", ["wuk", "gkv"], ["wuk"], out=wuk[:, kc, :], in0=wuk[:, kc, :], scalar1=gkv[:, kc:kc + 1], scalar2=None, op0=ALU.mult)
                    P.op("dve", "tensor_scalar", ["wuv", "gkv"], ["wuv"], out=wuv[:, kc, :], in0=wuv[:, kc, :], scalar1=gkv[:, kc:kc + 1], scalar2=None, op0=ALU.mult)
                n = 0
                for kt in range(16):
                    ksl = slice(kt * 128, (kt + 1) * 128)
                    for hf in range(2):
                        pi = n % 4
                        n += 1
                        for kc in range(2):
                            P.op("pe", "matmul", ["ckv", "wuv"], ["pp%d" % pi], out=pp[pi][:], lhsT=ckv[:, kc, ksl], rhs=wuv[:, kc, hf * 512:(hf + 1) * 512],
                                 start=(kc == 0), stop=(kc == 1))
                        P.op("act", "activation", ["pp%d" % pi], ["vall"], out=vall[:, kt, hf * 512:(hf + 1) * 512], in_=pp[pi][:], func=AF.Copy)
                sc_mla = float(192 ** -0.5)
                def prep(h):
                    s = h % 2
                    yield P.op("pool", "dma_start", [], ["wq%d" % s], out=wq[s][:], in_=IN["wq"][h])
                    for kc in range(4):
                        yield P.op("dve", "tensor_scalar", ["wq%d" % s, "gq"], ["wq%d" % s], out=wq[s][:, kc, :], in0=wq[s][:, kc, :], scalar1=gq[:, kc:kc + 1], scalar2=None, op0=ALU.mult)
                    for tg in range(4):
                        csl = slice(tg * 512, (tg + 1) * 512)
                        for kc in range(4):
                            yield P.op("pe", "matmul", ["wq%d" % s, "cq"], ["pp0"], out=pp[0][:], lhsT=wq[s][:, kc, 0:128], rhs=cq[:, kc, csl], start=(kc == 0), stop=(kc == 3))
                        yield P.op("act", "activation", ["pp0"], ["qn%d" % s], out=qn[s][:, csl], in_=pp[0][:], func=AF.Copy)
                        for kc in range(4):
                            yield P.op("pe", "matmul", ["wq%d" % s, "cq"], ["pp1"], out=pp[1][0:64, :], lhsT=wq[s][:, kc, 128:192], rhs=cq[:, kc, csl], start=(kc == 0), stop=(kc == 3))
                        for kc in range(4):
                            yield P.op("pe", "matmul", ["wq%d" % s, "cq"], ["pp2"], out=pp[2][0:64, :], lhsT=wq[s][:, kc, 192:256], rhs=cq[:, kc, csl], start=(kc == 0), stop=(kc == 3))
                        yield P.op("dve", "tensor_tensor", ["pp1", "trig"], ["t1"], out=t1[0:64, :], in0=pp[1][0:64, :], in1=trig[0:64, 4, csl], op=ALU.mult)
                        yield P.op("dve", "tensor_tensor", ["pp2", "trig"], ["t2"], out=t2[0:64, :], in0=pp[2][0:64, :], in1=trig[0:64, 5, csl], op=ALU.mult)
                        yield P.op("pool", "tensor_tensor", ["t1", "t2"], ["qr%d" % s], out=qr[s][0:64, csl], in0=t1[0:64, :], in1=t2[0:64, :], op=ALU.add)
                        for kc in range(2):
                            yield P.op("pe", "matmul", ["wuk", "ckv"], ["pp3"], out=pp[3][:], lhsT=wuk[:, kc, h * 128:(h + 1) * 128], rhs=ckv[:, kc, csl], start=(kc == 0), stop=(kc == 1))
                        yield P.op("act", "activation", ["pp3"], ["kn%d" % s], out=kn[s][:, csl], in_=pp[3][:], func=AF.Copy)

                def att(h, step):
                    s = h % 2
                    cnt = 0
                    for qg in range(4):
                        nkt = 4 * (qg + 1)
                        for kt in range(nkt):
                            j = kt - 4 * qg
                            c0 = 128 * j if j > 0 else 0
                            cols = slice(qg * 512 + c0, (qg + 1) * 512)
                            ksl = slice(kt * 128, (kt + 1) * 128)
                            a = cnt % 2
                            k = cnt % 3
                            cnt += 1
                            P.op("pe", "matmul", ["kn%d" % s, "qn%d" % s], ["pS%d" % a], out=pS[a][:, c0:512], lhsT=kn[s][:, ksl], rhs=qn[s][:, cols], start=True, stop=False)
                            P.op("pe", "matmul", ["kpe", "qr%d" % s], ["pS%d" % a], out=pS[a][:, c0:512], lhsT=kpe[0:64, ksl], rhs=qr[s][0:64, cols], start=False, stop=True)
                            P.op("act", "activation", ["pS%d" % a], ["pT%d" % k], out=pT[k][:, c0:512], in_=pS[a][:, c0:512], func=AF.Exp, scale=sc_mla)
                            if j >= 0:
                                P.op("pool", "memset", [], ["pT%d" % k], ap=pT[k][64:128, c0:c0 + 64], constant=0.0)
                            P.op("pe", "matmul", ["vall", "pT%d" % k], ["pO"], out=pO[:, c0:512], lhsT=vall[:, kt, h * 128:(h + 1) * 128], rhs=pT[k][:, c0:512],
                                 start=(kt == 0), stop=(kt == nkt - 1))
                            P.op("pe", "matmul", ["onesb", "pT%d" % k], ["pD"], out=pD[:, c0:512], lhsT=onesb[:], rhs=pT[k][:, c0:512],
                                 start=(kt == 0), stop=(kt == nkt - 1))
                            step(3)
                        P.op("dve", "reciprocal", ["pD"], ["rden"], out=rden[:], in_=pD[:])
                        P.op("dve", "tensor_tensor", ["pO", "rden"], ["oast%d" % s], out=ost[s][:, qg * 512:(qg + 1) * 512], in0=pO[:], in1=rden[:], op=ALU.mult)
                    P.op("sp", "dma_start", ["oast%d" % s], [], out=SC["oaT"][h], in_=ost[s][:])

                for _ in prep(0):
                    pass
                for h in range(8):
                    nxt = prep(h + 1) if h + 1 < 8 else iter(())

                    def step(n, nxt=nxt):
                        for _ in range(n):
                            next(nxt, None)

                    att(h, step)
                    for _ in nxt:
                        pass
                P.emit(st)

        trig_cm.__exit__(None, None, None)

        if "p3" in phases:
            with ExitStack() as st:
                sb = lambda n, s, d, _p="p3_": st.enter_context(nc.sbuf_tensor(_p + n, s, d))
                psb = lambda n, s, d, _p="p3_": st.enter_context(nc.psum_tensor(_p + n, s, d))
                P = Prog(nc, SEMS)
                iqT = sb("iqT", [128, 8, S], BF16)
                ikT = sb("ikT", [128, S], BF16)
                iw = sb("iw", [128, 16, 16], F32)
                dqT = sb("dqT", [128, 8, S], BF16)
                dkT = sb("dkT", [128, 2, S], BF16)
                dvs = sb("dvs", [128, 16, 256], BF16)
                origs = [sb("orig%d" % i, [128, S], F32) for i in range(4)]
                works = [sb("work%d" % i, [128, S], F32) for i in range(2)]
                masks = [sb("mask%d" % i, [128, S], BF16) for i in range(2)]
                mxs = [sb("mx%d" % i, [128, 8], F32) for i in range(2)]
                MTs = [sb("MT%d" % i, [128, 16, 512], BF16) for i in range(2)]
                diag = [sb("diag%d" % i, [128, 16, 128], BF16) for i in range(2)]
                Rb = [sb("R%d" % i, [128, 512], BF16) for i in range(4)]
                pT = [sb("pT%d" % i, [128, 512], BF16) for i in range(3)]
                rden = sb("rden", [128, 512], F32)
                obst = [sb("obst%d" % i, [128, 8, 512], BF16) for i in range(2)]
                pDs = [psb("pDs%d" % i, [128, 512], F32) for i in range(2)]
                pIS = psb("pIS", [128, 512], F32)
                ptr = psb("ptr", [128, 8, 128], BF16)
                pS = [psb("pS%d" % i, [128, 512], F32) for i in range(2)]
                pO = psb("pO", [128, 512], F32)
                pD = psb("pD", [128, 512], F32)
                for h in range(8):
                    P.op("sp", "dma_start", [], ["iqT"], out=iqT[:, h, :], in_=SC["iqT"][h])
                    P.op("sp", "dma_start", [], ["dqT"], out=dqT[:, h, :], in_=SC["dqT"][h])
                for g in range(2):
                    P.op("sp", "dma_start", [], ["dkT"], out=dkT[:, g, :], in_=SC["dkT"][g])
                P.op("sp", "dma_start", [], ["ikT"], out=ikT[:], in_=SC["ikT"])
                P.op("sp", "dma_start", [], ["iw"], out=iw[:], in_=SC["iw"].rearrange("(t p) c -> p t c", p=128))
                P.op("sp", "dma_start", [], ["dvs"], out=dvs[:], in_=SC["dv"].rearrange("(t p) c -> p t c", p=128))
                for (src_, dst_) in ((IN["put"], SC["pub"]), (IN["pv"], SC["pvb"])):
                    for r0 in range(0, 16384, 1024):
                        P.op("pool", "dma_start", ["iqT", "dqT", "dkT", "ikT", "iw", "dvs"], [], out=dst_[r0:r0 + 1024, :].rearrange("r (a b) -> (r a) b", b=1024),
                             in_=src_[r0:r0 + 1024, :].rearrange("r (a b) -> (r a) b", b=1024))
                sc_idx = float(64 ** -0.5 * 16 ** -0.5)
                sc_dsa = float(128 ** -0.5)
                nR = 0
                nD = 0
                cnt = 0
                def stage_IS(g):
                    qg = g // 2
                    st_ = dict(nD=0)
                    for T in (2 * g, 2 * g + 1):
                        i = T % 4
                        tsl = slice(T * 128, (T + 1) * 128)
                        nk = 128 * (T + 1)
                        dd = T % 2
                        orig = origs[i]
                        P.op("dve", "tensor_tensor", ["identf", "iw"], ["diag%d" % dd], out=diag[dd][:],
                             in0=identf[:].unsqueeze(1).to_broadcast([128, 16, 128]), in1=iw[:, T, :].unsqueeze(2).to_broadcast([128, 16, 128]), op=ALU.mult)
                        for kg in range((nk + 511) // 512):
                            ncol = min(512, nk - kg * 512)
                            ksl = slice(kg * 512, kg * 512 + ncol)
                            for h in range(16):
                                rows = slice((h % 2) * 64, (h % 2) * 64 + 64)
                                a = CN["nD"] % 2
                                CN["nD"] += 1
                                r = CN["nR"] % 4
                                CN["nR"] += 1
                                P.op("pe", "matmul", ["iqT", "ikT"], ["pDs%d" % a], out=pDs[a][:, 0:ncol], lhsT=iqT[rows, h // 2, tsl], rhs=ikT[rows, ksl], start=True, stop=True)
                                P.op("act", "activation", ["pDs%d" % a], ["R%d" % r], out=Rb[r][:, 0:ncol], in_=pDs[a][:, 0:ncol], func=AF.Relu, scale=sc_idx)
                                P.op("pe", "matmul", ["diag%d" % dd, "R%d" % r], ["pIS"], out=pIS[:, 0:ncol], lhsT=diag[dd][:, h, :], rhs=Rb[r][:, 0:ncol],
                                     start=(h == 0), stop=(h == 15))
                            P.op("act", "activation", ["pIS"], ["orig%d" % i], out=orig[:, ksl], in_=pIS[:, 0:ncol], func=AF.Copy)
                        P.op("pool", "memset", [], ["orig%d" % i], ap=orig[0:64, nk - 64:nk], constant=NEG)

                def stage_topk(g, att_qg=None):
                    tiles = (2 * g, 2 * g + 1)
                    heads_done = 0
                    if tiles[0] >= 2:
                        for rnd in range(32):
                            for T in tiles:
                                i, dd, nk = T % 4, T % 2, 128 * (T + 1)
                                src = origs[i] if rnd == 0 else works[dd]
                                sname = ("orig%d" % i) if rnd == 0 else ("work%d" % dd)
                                P.op("dve", "max", [sname], ["mx%d" % dd], out=mxs[dd][:], in_=src[:, 0:nk])
                            for T in tiles:
                                i, dd, nk = T % 4, T % 2, 128 * (T + 1)
                                src = origs[i] if rnd == 0 else works[dd]
                                sname = ("orig%d" % i) if rnd == 0 else ("work%d" % dd)
                                P.op("dve", "match_replace", ["mx%d" % dd, sname], ["work%d" % dd], out=works[dd][:, 0:nk], in_to_replace=mxs[dd][:],
                                     in_values=src[:, 0:nk], imm_value=NEG)
                            if att_qg is not None and rnd % 4 == 3:
                                stage_att_head(att_qg, heads_done)
                                heads_done += 1
                    if att_qg is not None:
                        while heads_done < 8:
                            stage_att_head(att_qg, heads_done)
                            heads_done += 1
                    for T in tiles:
                        i, dd, nk = T % 4, T % 2, 128 * (T + 1)
                        mask = masks[dd]
                        if T >= 2:
                            P.op("dve", "tensor_tensor", ["work%d" % dd, "orig%d" % i], ["mask%d" % dd], out=mask[:, 0:nk], in0=works[dd][:, 0:nk], in1=origs[i][:, 0:nk], op=ALU.not_equal)
                        else:
                            P.op("dve", "tensor_scalar", ["orig%d" % i], ["mask%d" % dd], out=mask[:, 0:nk], in0=origs[i][:, 0:nk], scalar1=NEG / 2, scalar2=None, op0=ALU.is_gt)

                def stage_maskT(g):
                    mp = (g // 2) % 2
                    MT = MTs[mp]
                    if g % 2 == 0:
                        P.op("pool", "memset", [], ["MT%d" % mp], ap=MT[:], constant=0.0)
                    for T in (2 * g, 2 * g + 1):
                        i, dd = T % 4, T % 2
                        mask = masks[dd]
                        for k0 in range(0, T + 1, 8):
                            n8 = min(8, T + 1 - k0)
                            for kt in range(k0, k0 + n8):
                                P.op("pe", "transpose", ["mask%d" % dd, "identb"], ["ptr"], out=ptr[:, kt - k0, :], in_=mask[:, kt * 128:(kt + 1) * 128], identity=identb[:])
                            P.op("act", "activation", ["ptr"], ["MT%d" % mp], out=MT[:, k0:k0 + n8, i * 128:(i + 1) * 128], in_=ptr[:, 0:n8, :], func=AF.Copy)

                def stage_att_head(qg, h):
                    os_ = qg % 2
                    g = h // 4
                    nkt = 4 * (qg + 1)
                    for kt in range(nkt):
                        ksl = slice(kt * 128, (kt + 1) * 128)
                        a = CN["cnt"] % 2
                        k = CN["cnt"] % 3
                        CN["cnt"] += 1
                        P.op("pe", "matmul", ["dkT", "dqT"], ["pS%d" % a], out=pS[a][:], lhsT=dkT[:, g, ksl], rhs=dqT[:, h, qg * 512:(qg + 1) * 512], start=True, stop=True)
                        P.op("act", "activation", ["pS%d" % a], ["pT%d" % k], out=pT[k][:], in_=pS[a][:], func=AF.Exp, scale=sc_dsa)
                        P.op("pool", "tensor_tensor", ["pT%d" % k, "MT%d" % os_], ["pT%d" % k], out=pT[k][:], in0=pT[k][:], in1=MTs[os_][:, kt, :], op=ALU.mult)
                        P.op("pe", "matmul", ["dvs", "pT%d" % k], ["pO"], out=pO[:], lhsT=dvs[:, kt, g * 128:(g + 1) * 128], rhs=pT[k][:], start=(kt == 0), stop=(kt == nkt - 1))
                        P.op("pe", "matmul", ["onesb", "pT%d" % k], ["pD"], out=pD[:], lhsT=onesb[:], rhs=pT[k][:], start=(kt == 0), stop=(kt == nkt - 1))
                    P.op("dve", "reciprocal", ["pD"], ["rden"], out=rden[:], in_=pD[:])
                    P.op("dve", "tensor_tensor", ["pO", "rden"], ["obst%d" % os_], out=obst[os_][:, h, :], in0=pO[:], in1=rden[:], op=ALU.mult)
                    if h == 7:
                        P.op("sp", "dma_start", ["obst%d" % os_], [], out=SC["obT"][:, :, qg * 512:(qg + 1) * 512].rearrange("h p s -> p h s"), in_=obst[os_][:])

                CN = dict(nD=0, nR=0, cnt=0)
                stage_IS(0)
                pending = None
                for g in range(8):
                    if g + 1 < 8:
                        stage_IS(g + 1)
                    stage_topk(g, att_qg=pending)
                    pending = None
                    stage_maskT(g)
                    if g % 2 == 1:
                        pending = g // 2
                for h in range(8):
                    stage_att_head(pending, h)
                P.emit(st)

        if "p4" in phases:
            with ExitStack() as st:
                sb = lambda n, s, d, _p="p4_": st.enter_context(nc.sbuf_tensor(_p + n, s, d))
                psb = lambda n, s, d, _p="p4_": st.enter_context(nc.psum_tensor(_p + n, s, d))
                P = Prog(nc, SEMS)
                wa = sb("wa", [128, 8, D], BF16)
                wb = sb("wb", [128, 8, D], BF16)
                wo = sb("wo", [128, 16, D], BF16)
                oat = [sb("oat%d" % i, [128, 8, 128], BF16) for i in range(2)]
                obt = [sb("obt%d" % i, [128, 8, 128], BF16) for i in range(2)]
                gst_ = [sb("gs%d" % i, [128, 4096], BF16) for i in range(1)] * 2
                xt = [sb("xt%d" % i, [128, D], F32) for i in range(2)]
                mg = sb("mg", [128, D], BF16)
                mT = sb("mT", [128, 16, 128], BF16)
                t1 = [sb("t1_%d" % i, [128, 512], F32) for i in range(2)]
                t2 = [sb("t2_%d" % i, [128, 512], F32) for i in range(2)]
                pY = [psb("pY%d" % i, [128, 512], F32) for i in range(4)]
                ptr = [psb("ptr%d" % i, [128, 8, 128], BF16) for i in range(2)]
                pZ = [psb("pZ%d" % i, [128, 512], F32) for i in range(2)]
                for h in range(8):
                    for hf in range(2):
                        P.op("pool", "dma_start", [], ["wa"], out=wa[:, h, hf * 1024:(hf + 1) * 1024], in_=IN["wa"][:, h, hf * 1024:(hf + 1) * 1024])
                        P.op("pool", "dma_start", [], ["wb"], out=wb[:, h, hf * 1024:(hf + 1) * 1024], in_=IN["wb"][:, h, hf * 1024:(hf + 1) * 1024])
                for dk in range(16):
                    for hf in range(2):
                        P.op("pool", "dma_start", [], ["wo"], out=wo[:, dk, hf * 1024:(hf + 1) * 1024], in_=IN["wo"][:, dk, hf * 1024:(hf + 1) * 1024])
                ny = 0
                for T in range(NT):
                    s = T % 2
                    tsl = slice(T * 128, (T + 1) * 128)
                    P.op("sp", "dma_start", [], ["oat%d" % s], out=oat[s][:], in_=SC["oaT"][:, :, tsl].rearrange("h p s -> p h s"))
                    P.op("sp", "dma_start", [], ["obt%d" % s], out=obt[s][:], in_=SC["obT"][:, :, tsl].rearrange("h p s -> p h s"))
                    P.op("sp", "dma_start", [], ["gs0"], out=gst_[s][:], in_=SC["gs"][tsl, :])
                    P.op("sp", "dma_start", [], ["xt%d" % s], out=xt[s][:], in_=IN["x"][tsl, :])
                    for cg in range(4):
                        csl = slice(cg * 512, (cg + 1) * 512)
                        ya = ny % 4
                        yb = (ny + 1) % 4
                        ny += 2
                        u = cg % 2
                        for h in range(8):
                            P.op("pe", "matmul", ["oat%d" % s, "wa"], ["pY%d" % ya], out=pY[ya][:], lhsT=oat[s][:, h, :], rhs=wa[:, h, csl], start=(h == 0), stop=(h == 7))
                        for h in range(8):
                            P.op("pe", "matmul", ["obt%d" % s, "wb"], ["pY%d" % yb], out=pY[yb][:], lhsT=obt[s][:, h, :], rhs=wb[:, h, csl], start=(h == 0), stop=(h == 7))
                        P.op("dve", "tensor_tensor", ["pY%d" % ya, "gs0"], ["t1_%d" % u], out=t1[u][:], in0=pY[ya][:], in1=gst_[s][:, csl], op=ALU.mult)
                        P.op("dve", "tensor_tensor", ["pY%d" % yb, "gs0"], ["t2_%d" % u], out=t2[u][:], in0=pY[yb][:], in1=gst_[s][:, 2048 + cg * 512:2048 + (cg + 1) * 512], op=ALU.mult)
                        P.op("pool", "tensor_tensor", ["t1_%d" % u, "t2_%d" % u], ["mg"], out=mg[:, csl], in0=t1[u][:], in1=t2[u][:], op=ALU.add)
                    for dk in range(16):
                        b = dk // 8
                        P.op("pe", "transpose", ["mg", "identb"], ["ptr%d" % b], out=ptr[b][:, dk % 8, :], in_=mg[:, dk * 128:(dk + 1) * 128], identity=identb[:])
                    for b in range(2):
                        P.op("act", "activation", ["ptr%d" % b], ["mT"], out=mT[:, b * 8:(b + 1) * 8, :], in_=ptr[b][:], func=AF.Copy)
                    for og in range(4):
                        csl = slice(og * 512, (og + 1) * 512)
                        z = og % 2
                        for dk in range(16):
                            P.op("pe", "matmul", ["mT", "wo"], ["pZ%d" % z], out=pZ[z][:], lhsT=mT[:, dk, :], rhs=wo[:, dk, csl], start=(dk == 0), stop=(dk == 15))
                        P.op("dve", "tensor_tensor", ["pZ%d" % z, "xt%d" % s], ["xt%d" % s], out=xt[s][:, csl], in0=pZ[z][:], in1=xt[s][:, csl], op=ALU.add)
                    P.op("sp", "dma_start", ["xt%d" % s], [], out=SC["x2"][tsl, :], in_=xt[s][:])
                P.emit(st)

        if "p5" in phases:
            with ExitStack() as st:
                sb = lambda n, s, d, _p="p5_": st.enter_context(nc.sbuf_tensor(_p + n, s, d))
                psb = lambda n, s, d, _p="p5_": st.enter_context(nc.psum_tensor(_p + n, s, d))
                P = Prog(nc, SEMS)
                wpq = sb("wpq", [128, 16, 1024], BF16)
                kb = sb("kb", [128, 8, 256], BF16)
                gffnT = sb("gffnT", [128, 16], F32)
                gfin = sb("gfin", [128, D], F32)
                iota = sb("iota", [128, 16], F32)
                iota128 = sb("iota128", [128, 128], F32)
                GT = sb("GT", [128, 256, 128], BF16)
                NSB = 8
                OH2s = [sb("OH2_%d" % i, [128, NSB, 128], BF16) for i in range(2)]
                OH1s = [sb("OH1_%d" % i, [128, NSB, 128], BF16) for i in range(2)]
                hnTGs = [sb("hnTG%d" % i, [128, 16, 256], BF16) for i in range(2)]
                NUB = 4
                ub = [sb("ub%d" % i, [128, D], BF16) for i in range(NUB)]
                vb = [ub[i // 2][:, (i % 2) * 1024:(i % 2 + 1) * 1024] for i in range(2 * NUB)]
                x2t = [sb("x2t%d" % i, [128, D], F32) for i in range(2)]
                hnf = sb("hnf", [128, D], F32)
                qTs = sb("qTs", [128, 8, 128], BF16)
                ssb = sb("ssb", [128, 8, 256], F32)
                wk = sb("wk", [128, 256], F32)
                vals = sb("vals", [128, 16, 16], F32)
                idxs = sb("idxs", [128, 16, 16], U32)
                idxf = sb("idxf", [128, 16, 16], F32)
                tv = sb("tv", [128, 8, 16], F32)
                tp = sb("tp", [128, 8, 16], U32)
                ti = sb("ti", [128, 8, 16], U32)
                tj = sb("tj", [128, 8, 16], U32)
                tif = sb("tif", [128, 8, 16], F32)
                tjf = sb("tjf", [128, 8, 16], F32)
                e1 = [sb("e1_%d" % i, [128, 8, 16], F32) for i in range(4)]
                e2 = [sb("e2_%d" % i, [128, 8, 16], F32) for i in range(4)]
                gw = [sb("gw%d" % i, [128, 128], F32) for i in range(4)]
                selT = sb("selT", [128, 3, 128], F32)
                ex = sb("ex", [128, 8, 16], F32)
                zs = sb("zs", [128, 8], F32)
                ga = [sb("ga%d" % i, [128, 256], BF16) for i in range(2)]
                cst = [sb("cst%d" % i, [128, 256], BF16) for i in range(4)]
                stA = sb("stA", [128, 4], F32)
                stC = sb("stC", [128, 4], F32)
                B = [psb("B%d" % i, [128, 512], F32) for i in range(8)]
                for dk in range(16):
                    P.op("pool", "dma_start", [], ["wpq"], out=wpq[:, dk, :], in_=IN["wpq"][:, dk, :])
                for h in range(8):
                    P.op("pool", "dma_start", [], ["kb"], out=kb[:, h, :], in_=IN["kb"][:, h, :])
                P.op("sp", "dma_start", [], ["gffnT"], out=gffnT[:], in_=IN["gffnT"])
                P.op("sp", "dma_start", [], ["gfin"], out=gfin[:], in_=IN["gfin"].partition_broadcast(128))
                P.op("sp", "dma_start", [], ["iota"], out=iota[:], in_=IN["iota16"])
                P.op("sp", "dma_start", [], ["iota128"], out=iota128[:], in_=IN["iota128"])
                CN = dict(u=0, v=0, g=0, a=0, o=0)

                def stage_A(T):
                    p = T % 4
                    gp = (T // 2) % 2
                    hnTG = hnTGs[gp]
                    HT = "hnTG%d" % gp
                    tcol = slice((T % 2) * 128, (T % 2) * 128 + 128)
                    tsl = slice(T * 128, (T + 1) * 128)
                    yield P.op("sp", "dma_start", [], ["hnf"], out=hnf[:], in_=SC["x2"][tsl, :])
                    yield P.op("act", "activation", ["hnf"], ["ssb", "stA0"], out=ssb[:].rearrange("p a b -> p (a b)"), in_=hnf[:], func=AF.Square, accum_out=stA[:, 0:1])
                    yield P.op("act", "activation", ["stA0"], ["stA1"], out=stA[:, 1:2], in_=stA[:, 0:1], func=AF.Sqrt, scale=1.0 / D, bias=EPS)
                    yield P.op("dve", "reciprocal", ["stA1"], ["stA2"], out=stA[:, 2:3], in_=stA[:, 1:2])
                    yield P.op("act", "activation", ["hnf", "stA2"], ["hnf"], out=hnf[:], in_=hnf[:], func=AF.Copy, scale=stA[:, 2:3])
                    for r in range(4):
                        z = 6 + r % 2
                        for q4 in range(4):
                            dk = r * 4 + q4
                            yield P.op("pe", "transpose", ["hnf", "identf"], ["B%d" % z], out=B[z][:, q4 * 128:(q4 + 1) * 128], in_=hnf[:, dk * 128:(dk + 1) * 128], identity=identf[:])
                        for q4 in range(4):
                            dk = r * 4 + q4
                            if dk % 2 == 0:
                                yield P.op("act", "activation", ["B%d" % z, "gffnT"], [HT], out=hnTG[:, dk, tcol], in_=B[z][:, q4 * 128:(q4 + 1) * 128],
                                           func=AF.Copy, scale=gffnT[:, dk:dk + 1])
                            else:
                                yield P.op("dve", "tensor_scalar", ["B%d" % z, "gffnT"], [HT], out=hnTG[:, dk, tcol], in0=B[z][:, q4 * 128:(q4 + 1) * 128],
                                           scalar1=gffnT[:, dk:dk + 1], scalar2=None, op0=ALU.mult)
                    for hh in range(2):
                        z = 6 + hh
                        for h4 in range(4):
                            h = hh * 4 + h4
                            for dk in range(16):
                                yield P.op("pe", "matmul", ["wpq", HT], ["B%d" % z], out=B[z][:, h4 * 128:(h4 + 1) * 128], lhsT=wpq[:, dk, h * 128:(h + 1) * 128],
                                           rhs=hnTG[:, dk, tcol], start=(dk == 0), stop=(dk == 15))
                        yield P.op("act", "activation", ["B%d" % z], ["qTs"], out=qTs[:, hh * 4:(hh + 1) * 4, :].rearrange("p a b -> p (a b)"), in_=B[z][:], func=AF.Copy)
                    for h2 in range(4):
                        z = 6 + h2 % 2
                        for hi in range(2):
                            h = h2 * 2 + hi
                            yield P.op("pe", "matmul", ["qTs", "kb"], ["B%d" % z], out=B[z][:, hi * 256:(hi + 1) * 256], lhsT=qTs[:, h, :], rhs=kb[:, h, :], start=True, stop=True)
                        yield P.op("act", "activation", ["B%d" % z], ["ssb"], out=ssb[:, h2 * 2:h2 * 2 + 2, :].rearrange("p a b -> p (a b)"), in_=B[z][:], func=AF.Copy)
                    for hp in range(16):
                        src = ssb[:, hp // 2, (hp % 2) * 128:(hp % 2) * 128 + 128]
                        yield P.op("dve", "max", ["ssb"], ["vals"], out=vals[:, hp, 0:8], in_=src)
                        yield P.op("dve", "max_index", ["ssb", "vals"], ["idxs"], out=idxs[:, hp, 0:8], in_max=vals[:, hp, 0:8], in_values=src)
                        yield P.op("dve", "match_replace", ["ssb", "vals"], ["wk"], out=wk[:, 0:128], in_to_replace=vals[:, hp, 0:8], in_values=src, imm_value=NEG)
                        yield P.op("dve", "max", ["wk"], ["vals"], out=vals[:, hp, 8:16], in_=wk[:, 0:128])
                        yield P.op("dve", "max_index", ["wk", "vals"], ["idxs"], out=idxs[:, hp, 8:16], in_max=vals[:, hp, 8:16], in_values=wk[:, 0:128])
                    yield P.op("dve", "tensor_copy", ["idxs"], ["idxf"], out=idxf[:], in_=idxs[:])
                    cand = ssb[:].rearrange("p h (a b) -> p h a b", b=16)
                    v4 = vals[:].rearrange("p (h two) k -> p h two k", two=2)
                    i4 = idxf[:].rearrange("p (h two) k -> p h two k", two=2)
                    yield P.op("dve", "tensor_tensor", ["vals"], ["ssb"], out=cand, in0=v4[:, :, 0, :].unsqueeze(3).to_broadcast([128, 8, 16, 16]),
                         in1=v4[:, :, 1, :].unsqueeze(2).to_broadcast([128, 8, 16, 16]), op=ALU.add)
                    for h in range(8):
                        src = ssb[:, h, :]
                        yield P.op("dve", "max", ["ssb"], ["tv"], out=tv[:, h, 0:8], in_=src)
                        yield P.op("dve", "max_index", ["ssb", "tv"], ["tp"], out=tp[:, h, 0:8], in_max=tv[:, h, 0:8], in_values=src)
                        yield P.op("dve", "match_replace", ["ssb", "tv"], ["wk"], out=wk[:], in_to_replace=tv[:, h, 0:8], in_values=src, imm_value=NEG)
                        yield P.op("dve", "max", ["wk"], ["tv"], out=tv[:, h, 8:16], in_=wk[:])
                        yield P.op("dve", "max_index", ["wk", "tv"], ["tp"], out=tp[:, h, 8:16], in_max=tv[:, h, 8:16], in_values=wk[:])
                    yield P.op("dve", "tensor_single_scalar", ["tp"], ["ti"], out=ti[:], in_=tp[:], scalar=4, op=ALU.logical_shift_right)
                    yield P.op("dve", "tensor_single_scalar", ["tp"], ["tj"], out=tj[:], in_=tp[:], scalar=15, op=ALU.bitwise_and)
                    yield P.op("dve", "tensor_copy", ["ti"], ["tif"], out=tif[:], in_=ti[:])
                    yield P.op("dve", "tensor_copy", ["tj"], ["tjf"], out=tjf[:], in_=tj[:])
                    oh = ssb[:].rearrange("p h (a b) -> p h a b", b=16)
                    iob = iota[:].unsqueeze(1).unsqueeze(1).to_broadcast([128, 8, 16, 16])
                    for (sel, side, dst, dn) in ((tif, 0, e1[p], "e1_%d" % p), (tjf, 1, e2[p], "e2_%d" % p)):
                        yield P.op("dve", "tensor_tensor", ["tif", "tjf", "iota"], ["ssb"], out=oh, in0=sel[:].unsqueeze(3).to_broadcast([128, 8, 16, 16]), in1=iob, op=ALU.is_equal)
                        yield P.op("dve", "tensor_tensor", ["ssb", "idxf"], ["ssb"], out=oh, in0=oh, in1=i4[:, :, side, :].unsqueeze(2).to_broadcast([128, 8, 16, 16]), op=ALU.mult)
                        yield P.op("dve", "tensor_reduce", ["ssb"], [dn], out=dst[:], in_=oh, axis=AX.X, op=ALU.add)
                    yield P.op("dve", "tensor_tensor", ["tv"], ["ex"], out=ex[:], in0=tv[:], in1=tv[:, :, 0:1].to_broadcast([128, 8, 16]), op=ALU.subtract)
                    yield P.op("act", "activation", ["ex"], ["ex"], out=ex[:], in_=ex[:], func=AF.Exp)
                    yield P.op("dve", "tensor_reduce", ["ex"], ["zs"], out=zs[:], in_=ex[:], axis=AX.X, op=ALU.add)
                    yield P.op("dve", "reciprocal", ["zs"], ["zs"], out=zs[:], in_=zs[:])
                    yield P.op("dve", "tensor_tensor", ["ex", "zs"], ["gw%d" % p], out=gw[p][:].rearrange("p (a b) -> p a b", b=16), in0=ex[:], in1=zs[:].unsqueeze(2).to_broadcast([128, 8, 16]), op=ALU.mult)

                def stage_GT(T):
                    p = T % 4
                    tp_ = T % 2
                    srcs = [(e1[p][:].rearrange("p a b -> p (a b)"), "e1_%d" % p), (e2[p][:].rearrange("p a b -> p (a b)"), "e2_%d" % p), (gw[p][:], "gw%d" % p)]
                    for j, (ap_, nm) in enumerate(srcs):
                        P.op("pe", "transpose", [nm, "identf"], ["B7"], out=B[7][:, j * 128:(j + 1) * 128], in_=ap_, identity=identf[:])
                    P.op("act", "activation", ["B7"], ["selT"], out=selT[:].rearrange("p a b -> p (a b)"), in_=B[7][:, 0:384], func=AF.Copy)
                    for sub in range(128 // NSB):
                        tsub = slice(sub * NSB, (sub + 1) * NSB)
                        ob = CN["o"] % 2
                        CN["o"] += 1
                        OH2, OH1 = OH2s[ob], OH1s[ob]
                        iob = iota128[:].unsqueeze(1).to_broadcast([128, NSB, 128])
                        P.op("dve", "tensor_tensor", ["iota128", "selT"], ["OH2_%d" % ob], out=OH2[:], in0=iob, in1=selT[:, 1, tsub].unsqueeze(2).to_broadcast([128, NSB, 128]), op=ALU.is_equal)
                        P.op("dve", "tensor_tensor", ["iota128", "selT"], ["OH1_%d" % ob], out=OH1[:], in0=iob, in1=selT[:, 0, tsub].unsqueeze(2).to_broadcast([128, NSB, 128]), op=ALU.is_equal)
                        P.op("pool", "tensor_tensor", ["OH1_%d" % ob, "selT"], ["OH1_%d" % ob], out=OH1[:], in0=OH1[:], in1=selT[:, 2, tsub].unsqueeze(2).to_broadcast([128, NSB, 128]), op=ALU.mult)
                        for t4 in range(NSB // 4):
                            z = 6 + CN["g"] % 2
                            CN["g"] += 1
                            for tt in range(4):
                                t = t4 * 4 + tt
                                P.op("pe", "matmul", ["OH2_%d" % ob, "OH1_%d" % ob], ["B%d" % z], out=B[z][:, tt * 128:(tt + 1) * 128], lhsT=OH2[:, t, :], rhs=OH1[:, t, :], start=True, stop=True)
                            tg0 = tp_ * 128 + sub * NSB + t4 * 4
                            P.op("act", "activation", ["B%d" % z], ["GT"], out=GT[:, tg0:tg0 + 4, :].rearrange("p t c -> p (t c)"), in_=B[z][:], func=AF.Copy)

                def stage_U(G, step):
                    gp = G % 2
                    hnTG = hnTGs[gp]
                    for c in range(128):
                        b = CN["u"] % NUB
                        CN["u"] += 1
                        P.op("sp", "dma_start", [], ["ubh%d" % (2 * b), "ubh%d" % (2 * b + 1)], out=ub[b][:], in_=SC["pub"][c * 128:(c + 1) * 128, :])
                        z = 4 + (c // 2) % 2
                        reg = slice((c % 2) * 256, (c % 2) * 256 + 256)
                        for dk in range(16):
                            P.op("pe", "matmul", ["ubh%d" % (2 * b + dk // 8), "hnTG%d" % gp], ["B%d" % z], out=B[z][:, reg], lhsT=ub[b][:, dk * 128:(dk + 1) * 128], rhs=hnTG[:, dk, :], start=(dk == 0), stop=(dk == 15))
                        k = CN["a"] % 2
                        CN["a"] += 1
                        P.op("act", "activation", ["B%d" % z], ["ga%d" % k], out=ga[k][:], in_=B[z][:, reg], func=AF.Gelu)
                        P.op("dve", "tensor_tensor", ["ga%d" % k, "GT"], ["GT"], out=GT[:, :, c], in0=ga[k][:], in1=GT[:, :, c], op=ALU.mult)
                        step(3)

                def stage_V(G, step):
                    for p in range(2):
                        T = 2 * G + p
                        P.op("sp", "dma_start", [], ["x2t%d" % p], out=x2t[p][:], in_=SC["x2"][T * 128:(T + 1) * 128, :])
                    for hv in range(2):
                        for c in range(128):
                            b = CN["v"] % (2 * NUB)
                            CN["v"] += 1
                            P.op("sp", "dma_start", [], ["ubh%d" % b], out=vb[b], in_=SC["pvb"][c * 128:(c + 1) * 128, hv * 1024:(hv + 1) * 1024])
                            k = (CN["v"] - 1) % 4
                            if k % 2 == 0:
                                P.op("act", "activation", ["GT"], ["cst%d" % k], out=cst[k][:], in_=GT[:, :, c], func=AF.Copy)
                            else:
                                P.op("pool", "tensor_copy", ["GT"], ["cst%d" % k], out=cst[k][:], in_=GT[:, :, c])
                            for p in range(2):
                                for cg in range(2):
                                    z = p * 2 + cg
                                    P.op("pe", "matmul", ["cst%d" % k, "ubh%d" % b], ["B%d" % z], out=B[z][:], lhsT=cst[k][:, p * 128:(p + 1) * 128], rhs=vb[b][:, cg * 512:(cg + 1) * 512],
                                         start=(c == 0), stop=(c == 127))
                            step(3)
                        for p in range(2):
                            for cg in range(2):
                                z = p * 2 + cg
                                csl = slice(hv * 1024 + cg * 512, hv * 1024 + (cg + 1) * 512)
                                P.op("dve", "tensor_tensor", ["B%d" % z, "x2t%d" % p], ["x2t%d" % p], out=x2t[p][:, csl], in0=B[z][:], in1=x2t[p][:, csl], op=ALU.add)
                    for p in range(2):
                        T = 2 * G + p
                        tsl = slice(T * 128, (T + 1) * 128)
                        X = "x2t%d" % p
                        P.op("act", "activation", [X], ["ubh0", "ubh1", "stC0"], out=ub[0][:], in_=x2t[p][:], func=AF.Square, accum_out=stC[:, 0:1])
                        P.op("act", "activation", ["stC0"], ["stC1"], out=stC[:, 1:2], in_=stC[:, 0:1], func=AF.Sqrt, scale=1.0 / D, bias=EPS)
                        P.op("dve", "reciprocal", ["stC1"], ["stC2"], out=stC[:, 2:3], in_=stC[:, 1:2])
                        P.op("dve", "scalar_tensor_tensor", [X, "stC2", "gfin"], [X], out=x2t[p][:], in0=x2t[p][:], scalar=stC[:, 2:3], in1=gfin[:], op0=ALU.mult, op1=ALU.mult)
                        P.op("sp", "dma_start", [X], [], out=y[tsl, :], in_=x2t[p][:])

                def run_all(gen):
                    for _ in gen:
                        pass

                def chain(*gens):
                    for g_ in gens:
                        for v_ in g_:
                            yield v_

                run_all(stage_A(0))
                run_all(stage_A(1))
                for G in range(NT // 2):
                    stage_GT(2 * G)
                    stage_GT(2 * G + 1)
                    nxt = chain(stage_A(2 * G + 2), stage_A(2 * G + 3)) if G + 1 < NT // 2 else iter(())

                    def step(n, nxt=nxt):
                        for _ in range(n):
                            next(nxt, None)

                    stage_U(G, step)
                    stage_V(G, step)
                    run_all(nxt)
                P.emit(st)
    return nc


def kernel(**inputs):
    H = host_layout(inputs)
    x = np.asarray(inputs["x"], np.float32)
    pos = np.asarray(inputs["positions"]).astype(np.int32)
    nc = build_nc()
    in_maps = []
    for b in range(8):
        m = {k: H[k] for k in SHAPES if k not in ("x", "pos")}
        m["x"] = np.ascontiguousarray(x[b])
        m["pos"] = np.ascontiguousarray(pos[b])
        in_maps.append(m)
    res = run_bass_kernel_spmd(nc, in_maps, core_ids=list(range(8)))
    return np.stack([np.asarray(r["y"], dtype=np.float32) for r in res.results], axis=0)
```

```python
import numpy as np
from contextlib import ExitStack
import concourse.bass as bass
import concourse.mybir as mybir
from concourse.bass_utils import run_bass_kernel_spmd

F32 = mybir.dt.float32
BF16 = mybir.dt.bfloat16
I32 = mybir.dt.int32
U32 = mybir.dt.uint32
AF = mybir.ActivationFunctionType
ALU = mybir.AluOpType
AX = mybir.AxisListType

D = 2048
S = 2048
NT = 16
EPS = 1e-6
PI = float(np.pi)
MAGIC = 12582912.0
NEG = -1e30
ROPE_THETA = 500000.0

ENGS = ["pe", "act", "dve", "pool", "sp"]
N_DMA_SEMS = {"sp": 40, "pool": 24, "act": 8}


class Prog:
    def __init__(self, nc, semstate):
        self.nc = nc
        self.semstate = semstate
        self.ins = {e: [] for e in ENGS}
        self.last_w = {}
        self.readers = {}
        self.dma_rr = {e: 0 for e in N_DMA_SEMS}
        self.dma_cnt = {}

    def add(self, eng, fn, reads=(), writes=(), dma=False):
        idx = len(self.ins[eng])
        deps = set()
        for r in reads:
            w = self.last_w.get(r)
            if w is not None:
                deps.add(w)
        for r in writes:
            w = self.last_w.get(r)
            if w is not None:
                deps.add(w)
            for rd in self.readers.get(r, ()):
                deps.add(rd)
        deps.discard((eng, idx))
        rec = dict(fn=fn, deps=deps, dma=dma)
        if dma:
            j = self.dma_rr[eng]
            self.dma_rr[eng] = (j + 1) % N_DMA_SEMS[eng]
            n = self.dma_cnt.get((eng, j), 0) + 1
            self.dma_cnt[(eng, j)] = n
            rec["dsem"] = (eng, j, n)
        self.ins[eng].append(rec)
        for r in reads:
            self.readers.setdefault(r, []).append((eng, idx))
        for r in writes:
            self.last_w[r] = (eng, idx)
            self.readers[r] = []
        return (eng, idx)

    def op(self, eng, method, reads, writes, **kw):
        dma = method in ("dma_start", "indirect_dma_start")
        return self.add(eng, lambda e: getattr(e, method)(**kw), reads, writes, dma=dma)

    def emit(self, stack):
        nc = self.nc
        need = {e: set() for e in ENGS}
        for e in ENGS:
            for ins in self.ins[e]:
                for (de, di) in ins["deps"]:
                    if self.ins[de][di]["dma"]:
                        continue
                    if de == e and e == "pe":
                        continue
                    need[de].add(di)
        sigcount = {}
        for e in ENGS:
            c = 0
            for i, ins in enumerate(self.ins[e]):
                if ins["dma"]:
                    continue
                if i in need[e]:
                    c += 1
                    sigcount[(e, i)] = c
        ss = self.semstate
        gstack = ss["stack"]
        for e in ["pe", "act", "dve", "pool"]:
            if e not in ss["esem"]:
                ss["esem"][e] = gstack.enter_context(nc.semaphore("se_%s" % e))
                ss["ebase"][e] = 0
        for e, n in N_DMA_SEMS.items():
            for j in range(n):
                if self.dma_cnt.get((e, j), 0) > 0 and (e, j) not in ss["dsem"]:
                    ss["dsem"][(e, j)] = gstack.enter_context(nc.semaphore("sd_%s_%d" % (e, j)))
                    ss["dbase"][(e, j)] = 0
        esem = ss["esem"]
        dsem = ss["dsem"]
        ebase = dict(ss["ebase"])
        dbase = dict(ss["dbase"])
        for (e, i) in list(sigcount.keys()):
            sigcount[(e, i)] += ebase[e]
        for e in ["pe", "act", "dve", "pool"]:
            ss["ebase"][e] += sum(1 for (ee, i) in sigcount if ee == e)
        for (e, j), n in self.dma_cnt.items():
            ss["dbase"][(e, j)] += n
        block = stack.enter_context(nc.Block())
        prog = self

        def run(ename, eng):
            waited = {}

            def w(key, sem, val):
                if waited.get(key, 0) >= val:
                    return
                eng.wait_ge(sem, val)
                waited[key] = val

            for i, ins in enumerate(prog.ins[ename]):
                for (de, di) in sorted(ins["deps"]):
                    dins = prog.ins[de][di]
                    if dins["dma"]:
                        (qe, j, n) = dins["dsem"]
                        w(("d", qe, j), dsem[(qe, j)], 16 * (n + dbase[(qe, j)]))
                    else:
                        if de == ename and ename == "pe":
                            continue
                        w(("e", de), esem[de], sigcount[(de, di)])
                if ins["dma"]:
                    (qe, j, n) = ins["dsem"]
                    if n > 1:
                        w(("d", qe, j), dsem[(qe, j)], 16 * (n - 1 + dbase[(qe, j)]))
                    inst = ins["fn"](eng)
                    inst.then_inc(dsem[(qe, j)], 16)
                else:
                    inst = ins["fn"](eng)
                    if (ename, i) in sigcount:
                        inst.then_inc(esem[ename], 1)
            for (qe, j), n in prog.dma_cnt.items():
                if qe == ename:
                    w(("d", qe, j), dsem[(qe, j)], 16 * (n + dbase[(qe, j)]))

        block.tensor(lambda eng: run("pe", eng))
        block.scalar(lambda eng: run("act", eng))
        block.vector(lambda eng: run("dve", eng))
        block.gpsimd(lambda eng: run("pool", eng))
        block.sync(lambda eng: run("sp", eng))


O_CQ, O_CKV, O_KR, O_DQ, O_DK, O_DV, O_IQ, O_IK, O_IW, O_G = 0, 512, 768, 832, 1856, 2112, 2368, 3392, 3456, 3472


def _rot_perm(n, half, rot, blocks=1, bw=None):
    bw = bw or n
    idx = np.arange(n)
    out = idx.copy()
    for b in range(n // bw):
        o = b * bw
        out[o:o + half] = idx[o + half:o + rot]
        out[o + half:o + rot] = idx[o:o + half]
    return out


def _pk(w, ncol):
    K = w.shape[0]
    return np.ascontiguousarray(w.reshape(K // 128, 128, ncol).transpose(1, 0, 2))


def host_layout(inp):
    f = np.float32
    w_in = np.asarray(inp["w_in"], f)[0]
    out = {}
    tiles = []
    for h in range(8):
        tiles.append(np.arange(O_DQ + h * 128, O_DQ + (h + 1) * 128))
    for g in range(2):
        tiles.append(np.arange(O_DK + g * 128, O_DK + (g + 1) * 128))
    for hp in range(8):
        tiles.append(np.arange(O_IQ + hp * 128, O_IQ + (hp + 1) * 128))
    tiles.append(np.concatenate([np.arange(O_IK, O_IK + 64)] * 2))
    tiles.append(np.concatenate([np.arange(O_KR, O_KR + 64)] * 2))
    perms = [_rot_perm(128, 16, 32)] * 10 + [_rot_perm(128, 8, 16, bw=64)] * 9 + [_rot_perm(128, 32, 64, bw=64)]
    w1f = np.empty((20, 128, 16, 256), f)
    for i, (cols, pm) in enumerate(zip(tiles, perms)):
        w1f[i, :, :, 0:128] = _pk(w_in[:, cols], 128)
        w1f[i, :, :, 128:256] = _pk(w_in[:, cols[pm]], 128)
    out["w1f"] = w1f
    tcols = [np.arange(O_CQ, O_CQ + 512), np.concatenate([np.arange(O_CKV, O_CKV + 256), np.arange(O_DV, O_DV + 256)])]
    for j in range(8):
        tcols.append(np.arange(O_G + j * 512, O_G + (j + 1) * 512))
    out["w1t"] = np.stack([_pk(w_in[:, c], 512) for c in tcols])
    out["w1iw"] = _pk(w_in[:, O_IW:O_IW + 16], 16)
    out["gmixT"] = np.ascontiguousarray(np.asarray(inp["norm_mix_g"], f)[0].reshape(16, 128).T)
    rc = np.zeros((128, 9), f)
    for ty, (half, rot, bw) in enumerate([(16, 32, 128), (8, 16, 64), (32, 64, 64)]):
        invf = (ROPE_THETA ** (-(np.arange(half, dtype=np.float32) * 2.0) / rot)).astype(f)
        for r in range(128):
            j = r % bw
            rc[r, ty * 3 + 1] = PI / 2
            if j < rot:
                rc[r, ty * 3 + 0] = invf[j % half]
                rc[r, ty * 3 + 2] = PI if j < half else 0.0
    out["ropec"] = rc
    out["ident"] = np.eye(128, dtype=f)
    wuq = np.asarray(inp["mla_w_uq"], f)[0]
    pm = _rot_perm(64, 32, 64)
    wq = np.empty((8, 128, 4, 256), f)
    for h in range(8):
        wq[h, :, :, 0:128] = _pk(wuq[:, h, 0:128], 128)
        wq[h, :, :, 128:192] = _pk(wuq[:, h, 128:192], 64)
        wq[h, :, :, 192:256] = _pk(wuq[:, h, 128:192][:, pm], 64)
    out["wq"] = wq
    out["gq"] = np.ascontiguousarray(np.asarray(inp["mla_q_norm_g"], f)[0].reshape(4, 128).T)
    out["gkv"] = np.ascontiguousarray(np.asarray(inp["mla_kv_norm_g"], f)[0].reshape(2, 128).T)
    out["wuk"] = _pk(np.asarray(inp["mla_w_uk"], f)[0].reshape(256, 1024), 1024)
    out["wuv"] = _pk(np.asarray(inp["mla_w_uv"], f)[0].reshape(256, 1024), 1024)
    out["wa"] = _pk(np.asarray(inp["w_branch_a"], f)[0], 2048)
    out["wb"] = _pk(np.asarray(inp["w_branch_b"], f)[0], 2048)
    out["wo"] = _pk(np.asarray(inp["w_out"], f)[0], 2048)
    out["wpq"] = _pk(np.asarray(inp["peer_w_q"], f)[0].reshape(2048, 1024), 1024)
    sk = np.asarray(inp["peer_sub_keys"], f)[0]
    kb = np.zeros((128, 8, 256), f)
    for h in range(8):
        for p in range(2):
            kb[p * 64:(p + 1) * 64, h, p * 128:(p + 1) * 128] = sk[h, p].T
    out["kb"] = kb
    out["gffn"] = np.asarray(inp["norm_ffn_g"], f)[0]
    out["gfin"] = np.asarray(inp["norm_final_g"], f)
    out["put"] = np.ascontiguousarray(np.asarray(inp["peer_u"], f)[0].reshape(128, 128, 16, 128).transpose(0, 3, 2, 1)).reshape(16384, 2048)
    out["gffnT"] = np.ascontiguousarray(np.asarray(inp["norm_ffn_g"], f)[0].reshape(16, 128).T)
    out["iota128"] = np.tile(np.arange(128, dtype=f)[None, :], (128, 1))
    out["pv"] = np.asarray(inp["peer_v"], f)[0]
    out["iota16"] = np.tile(np.arange(16, dtype=f)[None, :], (128, 1))
    return out


SHAPES = {
    "x": ([S, D], F32), "pos": ([S], I32),
    "w1f": ([20, 128, 16, 256], F32), "w1t": ([10, 128, 16, 512], F32), "w1iw": ([128, 16, 16], F32),
    "gmixT": ([128, 16], F32), "ropec": ([128, 9], F32), "ident": ([128, 128], F32),
    "wq": ([8, 128, 4, 256], F32), "gq": ([128, 4], F32), "gkv": ([128, 2], F32),
    "wuk": ([128, 2, 1024], F32), "wuv": ([128, 2, 1024], F32),
    "wa": ([128, 8, 2048], F32), "wb": ([128, 8, 2048], F32), "wo": ([128, 16, 2048], F32),
    "wpq": ([128, 16, 1024], F32), "kb": ([128, 8, 256], F32), "gffn": ([D], F32), "gfin": ([D], F32),
    "put": ([16384, D], F32), "pv": ([16384, D], F32), "iota16": ([128, 16], F32),
    "gffnT": ([128, 16], F32), "iota128": ([128, 128], F32),
}


def build_nc(phases=("p0", "p1", "p2", "p3", "p4", "p5"), debug=()):
    nc = bass.Bass("TRN2", target_bir_lowering=False)
    IN = {k: nc.dram_tensor(k, sh, dt, kind="ExternalInput").ap() for k, (sh, dt) in SHAPES.items()}
    y = nc.dram_tensor("y", [S, D], F32, kind="ExternalOutput").ap()

    def scratch(name, shape, dt):
        kind = "ExternalOutput" if name in debug else "Internal"
        return nc.dram_tensor(name, shape, dt, kind=kind).ap()

    SC = dict(
        dqT=scratch("dqT", [8, 128, S], BF16), dkT=scratch("dkT", [2, 128, S], BF16),
        iqT=scratch("iqT", [8, 128, S], BF16), ikT=scratch("ikT", [128, S], BF16),
        kpeT=scratch("kpeT", [128, S], BF16), cqnT=scratch("cqnT", [128, 4, S], BF16),
        ckvnT=scratch("ckvnT", [128, 2, S], BF16), dv=scratch("dv", [S, 256], BF16),
        iw=scratch("iw", [S, 16], F32), gs=scratch("gs", [S, 4096], BF16),
        oaT=scratch("oaT", [8, 128, S], BF16), obT=scratch("obT", [8, 128, S], BF16),
        x2=scratch("x2", [S, D], F32),
        pub=scratch("pub", [16384, D], BF16), pvb=scratch("pvb", [16384, D], BF16),
    )

    with ExitStack() as gst:
        def gsb(name, shape, dt):
            return gst.enter_context(nc.sbuf_tensor(name, shape, dt))

        SEMS = dict(stack=gst, esem={}, dsem={}, ebase={}, dbase={})
        identb = gsb("identb", [128, 128], BF16)
        identf = gsb("identf", [128, 128], F32)
        onesb = gsb("onesb", [128, 128], BF16)
        trig_cm = nc.sbuf_tensor("trig", [128, 6, S], BF16)
        trig = trig_cm.__enter__()

        if "p0" in phases:
            with ExitStack() as st:
                sb = lambda n, s, d, _p="q1_": st.enter_context(nc.sbuf_tensor(_p + n, s, d))
                P = Prog(nc, SEMS)
                posi = sb("posi", [128, S], I32)
                posf = sb("posf", [128, S], F32)
                ang = sb("ang", [128, S], F32)
                kk = sb("kk", [128, S], F32)
                rc = sb("rc", [128, 9], F32)
                P.op("sp", "dma_start", [], ["identf"], out=identf[:], in_=IN["ident"])
                P.op("sp", "dma_start", [], ["rc"], out=rc[:], in_=IN["ropec"])
                P.op("sp", "dma_start", [], ["posi"], out=posi[:], in_=IN["pos"].partition_broadcast(128))
                P.op("dve", "tensor_copy", ["identf"], ["identb"], out=identb[:], in_=identf[:])
                P.op("pool", "memset", [], ["onesb"], ap=onesb[:], constant=1.0)
                P.op("dve", "tensor_copy", ["posi"], ["posf"], out=posf[:], in_=posi[:])
                for ty in range(3):
                    for cs in range(2):
                        P.op("dve", "tensor_scalar", ["posf", "rc"], ["ang"], out=ang[:], in0=posf[:],
                             scalar1=rc[:, ty * 3:ty * 3 + 1], scalar2=rc[:, ty * 3 + 1 + cs:ty * 3 + 2 + cs],
                             op0=ALU.mult, op1=ALU.add)
                        P.op("dve", "tensor_scalar", ["ang"], ["kk"], out=kk[:], in0=ang[:], scalar1=1.0 / (2 * PI),
                             scalar2=MAGIC, op0=ALU.mult, op1=ALU.add)
                        P.op("dve", "tensor_scalar", ["kk"], ["kk"], out=kk[:], in0=kk[:], scalar1=-MAGIC, scalar2=None,
                             op0=ALU.add)
                        P.op("dve", "scalar_tensor_tensor", ["kk", "ang"], ["kk"], out=kk[:], in0=kk[:], scalar=-2 * PI,
                             in1=ang[:], op0=ALU.mult, op1=ALU.add)
                        P.op("dve", "tensor_scalar", ["kk"], ["kk"], out=kk[:], in0=kk[:], scalar1=-PI, scalar2=PI,
                             op0=ALU.max, op1=ALU.min)
                        P.op("act", "activation", ["kk"], ["trig%d" % (ty * 2 + cs)], out=trig[:, ty * 2 + cs, :],
                             in_=kk[:], func=AF.Sin)
                P.emit(st)

        if "p1" in phases:
            with ExitStack() as st:
                sb = lambda n, s, d, _p="q2_": st.enter_context(nc.sbuf_tensor(_p + n, s, d))
                psb = lambda n, s, d, _p="q4_": st.enter_context(nc.psum_tensor(_p + n, s, d))
                P = Prog(nc, SEMS)
                hT = sb("hT", [128, 16, S], BF16)
                xt = [sb("xt%d" % i, [128, D], F32) for i in range(2)]
                xs = [sb("xs%d" % i, [128, D], BF16) for i in range(2)]
                junk = sb("junk", [128, D], BF16)
                ss = sb("ss", [128, 16], F32)
                sq = sb("sq", [128, 16], F32)
                rstd = sb("rstd", [128, 16], F32)
                gmix = sb("gmix", [128, 16], F32)
                wbuf = [sb("wbuf%d" % i, [128, 16, 512], BF16) for i in range(2)]
                ost = [sb("ost%d" % i, [128, 16, 512], BF16) for i in range(2)]
                tmp = [sb("tmp%d" % i, [128, 512], F32) for i in range(4)]
                cqs = sb("cqs", [128, 16], F32)
                cqn = [sb("cqn%d" % i, [128, 512], BF16) for i in range(2)]
                iwst = sb("iwst", [128, 16, 16], F32)
                wiw = sb("wiw", [128, 16, 16], BF16)
                ptr = [psb("ptr%d" % i, [128, 8, 128], BF16) for i in range(2)]
                pA = [psb("pA%d" % i, [128, 512], F32) for i in range(3)]
                pB = [psb("pB%d" % i, [128, 512], F32) for i in range(3)]
                P.op("sp", "dma_start", [], ["gmix"], out=gmix[:], in_=IN["gmixT"])
                for T in range(NT):
                    s = T % 2
                    tsl = slice(T * 128, (T + 1) * 128)
                    P.op("sp", "dma_start", [], ["xt%d" % s], out=xt[s][:], in_=IN["x"][tsl, :])
                    P.op("act", "activation", ["xt%d" % s], ["junk", "ss%d" % T], out=junk[:], in_=xt[s][:], func=AF.Square,
                         accum_out=ss[:, T:T + 1])
                    P.op("act", "activation", ["ss%d" % T], ["sq%d" % T], out=sq[:, T:T + 1], in_=ss[:, T:T + 1], func=AF.Sqrt,
                         scale=1.0 / D, bias=EPS)
                    P.op("dve", "reciprocal", ["sq%d" % T], ["rstd%d" % T], out=rstd[:, T:T + 1], in_=sq[:, T:T + 1])
                    P.op("act", "activation", ["xt%d" % s, "rstd%d" % T], ["xs%d" % s], out=xs[s][:], in_=xt[s][:], func=AF.Copy,
                         scale=rstd[:, T:T + 1])
                    for dk in range(16):
                        b = dk // 8
                        P.op("pe", "transpose", ["xs%d" % s, "identb"], ["ptr%d" % b], out=ptr[b][:, dk % 8, :],
                             in_=xs[s][:, dk * 128:(dk + 1) * 128], identity=identb[:])
                    for dk in range(16):
                        b = dk // 8
                        if dk % 2 == 0:
                            P.op("act", "activation", ["ptr%d" % b, "gmix"], ["hT%d" % T], out=hT[:, dk, tsl], in_=ptr[b][:, dk % 8, :],
                                 func=AF.Copy, scale=gmix[:, dk:dk + 1])
                        else:
                            P.op("dve", "tensor_scalar", ["ptr%d" % b, "gmix"], ["hT%d" % T], out=hT[:, dk, tsl], in0=ptr[b][:, dk % 8, :],
                                 scalar1=gmix[:, dk:dk + 1], scalar2=None, op0=ALU.mult)
                hT_all = ["hT%d" % T for T in range(NT)]
                dests = [SC["dqT"][h] for h in range(8)] + [SC["dkT"][g] for g in range(2)] + [SC["iqT"][h] for h in range(8)] + [SC["ikT"], SC["kpeT"]]
                types = [0] * 10 + [1] * 9 + [2]
                nblk = 0
                for bi in range(20):
                    ws = nblk % 2
                    osl = nblk % 2
                    nblk += 1
                    ty = types[bi]
                    if bi == 0:
                        P.op("pool", "dma_start", [], ["wbuf%d" % ws], out=wbuf[ws][:, :, 0:256], in_=IN["w1f"][bi])
                    if bi + 1 < 20:
                        P.op("pool", "dma_start", [], ["wbuf%d" % (1 - ws)], out=wbuf[1 - ws][:, :, 0:256], in_=IN["w1f"][bi + 1])
                    else:
                        P.op("pool", "dma_start", [], ["wbuf%d" % (1 - ws)], out=wbuf[1 - ws][:], in_=IN["w1t"][0])
                    for tg in range(4):
                        csl = slice(tg * 512, (tg + 1) * 512)
                        pi = (bi * 4 + tg) % 3
                        for dk in range(16):
                            P.op("pe", "matmul", ["wbuf%d" % ws] + hT_all[tg * 4:tg * 4 + 4], ["pA%d" % pi], out=pA[pi][:],
                                 lhsT=wbuf[ws][:, dk, 0:128], rhs=hT[:, dk, csl], start=(dk == 0), stop=(dk == 15))
                        for dk in range(16):
                            P.op("pe", "matmul", ["wbuf%d" % ws] + hT_all[tg * 4:tg * 4 + 4], ["pB%d" % pi], out=pB[pi][:],
                                 lhsT=wbuf[ws][:, dk, 128:256], rhs=hT[:, dk, csl], start=(dk == 0), stop=(dk == 15))
                        ti = (bi * 4 + tg) % 2
                        P.op("dve", "tensor_tensor", ["pA%d" % pi, "trig"], ["tmp%d" % (2 * ti)], out=tmp[2 * ti][:], in0=pA[pi][:],
                             in1=trig[:, ty * 2, csl], op=ALU.mult)
                        P.op("dve", "tensor_tensor", ["pB%d" % pi, "trig"], ["tmp%d" % (2 * ti + 1)], out=tmp[2 * ti + 1][:], in0=pB[pi][:],
                             in1=trig[:, ty * 2 + 1, csl], op=ALU.mult)
                        P.op("pool", "tensor_tensor", ["tmp%d" % (2 * ti), "tmp%d" % (2 * ti + 1)], ["ost%d" % osl],
                             out=ost[osl][:].rearrange("p a b -> p (a b)")[:, csl], in0=tmp[2 * ti][:], in1=tmp[2 * ti + 1][:], op=ALU.add)
                    P.op("sp", "dma_start", ["ost%d" % osl], [], out=dests[bi], in_=ost[osl][:].rearrange("p a b -> p (a b)")[:, 0:S])
                for bi in range(10):
                    ws = nblk % 2
                    osl = nblk % 2
                    nblk += 1
                    if bi + 1 < 10:
                        P.op("pool", "dma_start", [], ["wbuf%d" % (1 - ws)], out=wbuf[1 - ws][:], in_=IN["w1t"][bi + 1])
                    for T in range(NT):
                        tsl = slice(T * 128, (T + 1) * 128)
                        pi = T % 3
                        for dk in range(16):
                            P.op("pe", "matmul", ["wbuf%d" % ws, "hT%d" % T], ["pA%d" % pi], out=pA[pi][:], lhsT=hT[:, dk, tsl],
                                 rhs=wbuf[ws][:, dk, :], start=(dk == 0), stop=(dk == 15))
                        if bi >= 2:
                            P.op("act", "activation", ["pA%d" % pi], ["ost%d" % osl], out=ost[osl][:, T, :], in_=pA[pi][:], func=AF.Sigmoid)
                        else:
                            ncq = 512 if bi == 0 else 256
                            c = T % 2
                            P.op("act", "activation", ["pA%d" % pi], ["tmp0", "cqs"], out=tmp[0][:, 0:ncq], in_=pA[pi][:, 0:ncq], func=AF.Square,
                                 accum_out=cqs[:, 0:1])
                            P.op("act", "activation", ["cqs"], ["cqs1"], out=cqs[:, 1:2], in_=cqs[:, 0:1], func=AF.Sqrt, scale=1.0 / ncq, bias=EPS)
                            P.op("dve", "reciprocal", ["cqs1"], ["cqs2"], out=cqs[:, 2:3], in_=cqs[:, 1:2])
                            P.op("dve", "tensor_scalar", ["pA%d" % pi, "cqs2"], ["cqn%d" % c], out=cqn[c][:, 0:ncq], in0=pA[pi][:, 0:ncq],
                                 scalar1=cqs[:, 2:3], scalar2=None, op0=ALU.mult)
                            if bi == 1:
                                P.op("act", "activation", ["pA%d" % pi], ["ost%d" % osl], out=ost[osl][:, T, 0:256], in_=pA[pi][:, 256:512], func=AF.Copy)
                            nk = ncq // 128
                            for kc in range(nk):
                                P.op("pe", "transpose", ["cqn%d" % c, "identb"], ["ptr%d" % c], out=ptr[c][:, kc, :],
                                     in_=cqn[c][:, kc * 128:(kc + 1) * 128], identity=identb[:])
                            lo = 0 if bi == 0 else 256
                            P.op("act", "activation", ["ptr%d" % c], ["ost%d" % osl], out=ost[osl][:, T, lo:lo + ncq].rearrange("p (k q) -> p k q", q=128),
                                 in_=ptr[c][:, 0:nk, :], func=AF.Copy)
                    if bi >= 2:
                        P.op("sp", "dma_start", ["ost%d" % osl], [], out=SC["gs"][:, (bi - 2) * 512:(bi - 1) * 512].rearrange("(t p) c -> p t c", p=128),
                             in_=ost[osl][:])
                    else:
                        nk = 4 if bi == 0 else 2
                        lo = 0 if bi == 0 else 256
                        dst = SC["cqnT"] if bi == 0 else SC["ckvnT"]
                        for kc in range(nk):
                            P.op("sp", "dma_start", ["ost%d" % osl], [], out=dst[:, kc, :].rearrange("p (t q) -> p t q", q=128),
                                 in_=ost[osl][:, :, lo + kc * 128:lo + (kc + 1) * 128])
                        if bi == 1:
                            P.op("sp", "dma_start", ["ost%d" % osl], [], out=SC["dv"].rearrange("(t p) c -> p t c", p=128), in_=ost[osl][:, :, 0:256])
                P.op("pool", "dma_start", [], ["wiw"], out=wiw[:], in_=IN["w1iw"])
                for T in range(NT):
                    tsl = slice(T * 128, (T + 1) * 128)
                    pi = T % 3
                    for dk in range(16):
                        P.op("pe", "matmul", ["wiw", "hT%d" % T], ["pB%d" % pi], out=pB[pi][:, 0:16], lhsT=hT[:, dk, tsl], rhs=wiw[:, dk, :],
                             start=(dk == 0), stop=(dk == 15))
                    P.op("act", "activation", ["pB%d" % pi], ["iwst"], out=iwst[:, T, :], in_=pB[pi][:, 0:16], func=AF.Copy)
                P.op("sp", "dma_start", ["iwst"], [], out=SC["iw"].rearrange("(t p) c -> p t c", p=128), in_=iwst[:])
                P.emit(st)

        if "p2" in phases:
            with ExitStack() as st:
                sb = lambda n, s, d, _p="q3_": st.enter_context(nc.sbuf_tensor(_p + n, s, d))
                psb = lambda n, s, d, _p="q5_": st.enter_context(nc.psum_tensor(_p + n, s, d))
                P = Prog(nc, SEMS)
                cq = sb("cq_s", [128, 4, S], BF16)
                ckv = sb("ckv_s", [128, 2, S], BF16)
                kpe = sb("kpe_s", [128, S], BF16)
                wq = [sb("wq%d" % i, [128, 4, 256], BF16) for i in range(2)]
                gq = sb("gq", [128, 4], F32)
                gkv = sb("gkv", [128, 2], F32)
                wuk = sb("wuk", [128, 2, 1024], BF16)
                wuv = sb("wuv", [128, 2, 1024], BF16)
                vall = sb("vall", [128, 16, 1024], BF16)
                qn = [sb("qn%d" % i, [128, S], BF16) for i in range(2)]
                qr = [sb("qr%d" % i, [128, S], BF16) for i in range(2)]
                kn = [sb("kn%d" % i, [128, S], BF16) for i in range(2)]
                pT = [sb("pT%d" % i, [128, 512], BF16) for i in range(3)]
                rden = sb("rden", [128, 512], F32)
                ost = [sb("oast%d" % i, [128, S], BF16) for i in range(2)]
                t1 = sb("t1", [128, 512], F32)
                t2 = sb("t2", [128, 512], F32)
                pp = [psb("pp%d" % i, [128, 512], F32) for i in range(4)]
                pS = [psb("pS%d" % i, [128, 512], F32) for i in range(2)]
                pO = psb("pO", [128, 512], F32)
                pD = psb("pD", [128, 512], F32)
                P.op("sp", "dma_start", [], ["cq"], out=cq[:], in_=SC["cqnT"])
                P.op("sp", "dma_start", [], ["ckv"], out=ckv[:], in_=SC["ckvnT"])
                P.op("sp", "dma_start", [], ["kpe"], out=kpe[:], in_=SC["kpeT"])
                P.op("sp", "dma_start", [], ["gq"], out=gq[:], in_=IN["gq"])
                P.op("sp", "dma_start", [], ["gkv"], out=gkv[:], in_=IN["gkv"])
                for kc in range(2):
                    P.op("pool", "dma_start", [], ["wuk"], out=wuk[:, kc, :], in_=IN["wuk"][:, kc, :])
                    P.op("pool", "dma_start", [], ["wuv"], out=wuv[:, kc, :], in_=IN["wuv"][:, kc, :])
                for kc in range(2):
                    P.op("dve", "tensor_scalar", ["wuk", "gkv"], ["wuk"], out=wuk[:, kc, :], in0=wuk[:, kc, :], scalar1=gkv[:, kc:kc + 1], scalar2=None, op0=ALU.mult)
                    P.op("dve", "tensor_scalar", ["wuv", "gkv"], ["wuv"], out=wuv[:, kc, :], in0=wuv[:, kc, :], scalar1=gkv[:, kc:kc + 1], scalar2=None, op0=ALU.mult)
                n = 0
                for kt in range(16):
                    ksl = slice(kt * 128, (kt + 1) * 128)
                    for hf in range(2):
                        pi = n % 4
                        n += 1
                        for kc in range(2):
                            P.op("pe", "matmul", ["ckv", "wuv"], ["pp%d" % pi], out=pp[pi][:], lhsT=ckv[:, kc, ksl], rhs=wuv[:, kc, hf * 512:(hf + 1) * 512],
                                 start=(kc == 0), stop=(kc == 1))
                        P.op("act", "activation", ["pp%d" % pi], ["vall"], out=vall[:, kt, hf * 512:(hf + 1) * 512], in_=pp[pi][:], func=AF.Copy)
                sc_mla = float(192 ** -0.5)
                def prep(h):
                    s = h % 2
                    yield P.op("pool", "dma_start", [], ["wq%d" % s], out=wq[s][:], in_=IN["wq"][h])
                    for kc in range(4):
                        yield P.op("dve", "tensor_scalar", ["wq%d" % s, "gq"], ["wq%d" % s], out=wq[s][:, kc, :], in0=wq[s][:, kc, :], scalar1=gq[:, kc:kc + 1], scalar2=None, op0=ALU.mult)
                    for tg in range(4):
                        csl = slice(tg * 512, (tg + 1) * 512)
                        for kc in range(4):
                            yield P.op("pe", "matmul", ["wq%d" % s, "cq"], ["pp0"], out=pp[0][:], lhsT=wq[s][:, kc, 0:128], rhs=cq[:, kc, csl], start=(kc == 0), stop=(kc == 3))
                        yield P.op("act", "activation", ["pp0"], ["qn%d" % s], out=qn[s][:, csl], in_=pp[0][:], func=AF.Copy)
                        for kc in range(4):
                            yield P.op("pe", "matmul", ["wq%d" % s, "cq"], ["pp1"], out=pp[1][0:64, :], lhsT=wq[s][:, kc, 128:192], rhs=cq[:, kc, csl], start=(kc == 0), stop=(kc == 3))
                        for kc in range(4):
                            yield P.op("pe", "matmul", ["wq%d" % s, "cq"], ["pp2"], out=pp[2][0:64, :], lhsT=wq[s][:, kc, 192:256], rhs=cq[:, kc, csl], start=(kc == 0), stop=(kc == 3))
                        yield P.op("dve", "tensor_tensor", ["pp1", "trig"], ["t1"], out=t1[0:64, :], in0=pp[1][0:64, :], in1=trig[0:64, 4, csl], op=ALU.mult)
                        yield P.op("dve", "tensor_tensor", ["pp2", "trig"], ["t2"], out=t2[0:64, :], in0=pp[2][0:64, :], in1=trig[0:64, 5, csl], op=ALU.mult)
                        yield P.op("pool", "tensor_tensor", ["t1", "t2"], ["qr%d" % s], out=qr[s][0:64, csl], in0=t1[0:64, :], in1=t2[0:64, :], op=ALU.add)
                        for kc in range(2):
                            yield P.op("pe", "matmul", ["wuk", "ckv"], ["pp3"], out=pp[3][:], lhsT=wuk[:, kc, h * 128:(h + 1) * 128], rhs=ckv[:, kc, csl], start=(kc == 0), stop=(kc == 1))
                        yield P.op("act", "activation", ["pp3"], ["kn%d" % s], out=kn[s][:, csl], in_=pp[3][:], func=AF.Copy)

                def att(h, step):
                    s = h % 2
                    cnt = 0
                    for qg in range(4):
                        nkt = 4 * (qg + 1)
                        for kt in range(nkt):
                            j = kt - 4 * qg
                            c0 = 128 * j if j > 0 else 0
                            cols = slice(qg * 512 + c0, (qg + 1) * 512)
                            ksl = slice(kt * 128, (kt + 1) * 128)
                            a = cnt % 2
                            k = cnt % 3
                            cnt += 1
                            P.op("pe", "matmul", ["kn%d" % s, "qn%d" % s], ["pS%d" % a], out=pS[a][:, c0:512], lhsT=kn[s][:, ksl], rhs=qn[s][:, cols], start=True, stop=False)
                            P.op("pe", "matmul", ["kpe", "qr%d" % s], ["pS%d" % a], out=pS[a][:, c0:512], lhsT=kpe[0:64, ksl], rhs=qr[s][0:64, cols], start=False, stop=True)
                            P.op("act", "activation", ["pS%d" % a], ["pT%d" % k], out=pT[k][:, c0:512], in_=pS[a][:, c0:512], func=AF.Exp, scale=sc_mla)
                            if j >= 0:
                                P.op("pool", "memset", [], ["pT%d" % k], ap=pT[k][64:128, c0:c0 + 64], constant=0.0)
                            P.op("pe", "matmul", ["vall", "pT%d" % k], ["pO"], out=pO[:, c0:512], lhsT=vall[:, kt, h * 128:(h + 1) * 128], rhs=pT[k][:, c0:512],
                                 start=(kt == 0), stop=(kt == nkt - 1))
                            P.op("pe", "matmul", ["onesb", "pT%d" % k], ["pD"], out=pD[:, c0:512], lhsT=onesb[:], rhs=pT[k][:, c0:512],
                                 start=(kt == 0), stop=(kt == nkt - 1))
                            step(3)
                        P.op("dve", "reciprocal", ["pD"], ["rden"], out=rden[:], in_=pD[:])
                        P.op("dve", "tensor_tensor", ["pO", "rden"], ["oast%d" % s], out=ost[s][:, qg * 512:(qg + 1) * 512], in0=pO[:], in1=rden[:], op=ALU.mult)
                    P.op("sp", "dma_start", ["oast%d" % s], [], out=SC["oaT"][h], in_=ost[s][:])

                for _ in prep(0):
                    pass
                for h in range(8):
                    nxt = prep(h + 1) if h + 1 < 8 else iter(())

                    def step(n, nxt=nxt):
                        for _ in range(n):
                            next(nxt, None)

                    att(h, step)
                    for _ in nxt:
                        pass
                P.emit(st)

        trig_cm.__exit__(None, None, None)

        if "p3" in phases:
            with ExitStack() as st:
                sb = lambda n, s, d, _p="p3_": st.enter_context(nc.sbuf_tensor(_p + n, s, d))
                psb = lambda n, s, d, _p="p3_": st.enter_context(nc.psum_tensor(_p + n, s, d))
                P = Prog(nc, SEMS)
                iqT = sb("iqT", [128, 8, S], BF16)
                ikT = sb("ikT", [128, S], BF16)
                iw = sb("iw", [128, 16, 16], F32)
                dqT = sb("dqT", [128, 8, S], BF16)
                dkT = sb("dkT", [128, 2, S], BF16)
                dvs = sb("dvs", [128, 16, 256], BF16)
                origs = [sb("orig%d" % i, [128, S], F32) for i in range(4)]
                works = [sb("work%d" % i, [128, S], F32) for i in range(2)]
                masks = [sb("mask%d" % i, [128, S], BF16) for i in range(2)]
                mxs = [sb("mx%d" % i, [128, 8], F32) for i in range(2)]
                MTs = [sb("MT%d" % i, [128, 16, 512], BF16) for i in range(2)]
                diag = [sb("diag%d" % i, [128, 16, 128], BF16) for i in range(2)]
                Rb = [sb("R%d" % i, [128, 512], BF16) for i in range(4)]
                pT = [sb("pT%d" % i, [128, 512], BF16) for i in range(3)]
                rden = sb("rden", [128, 512], F32)
                obst = [sb("obst%d" % i, [128, 8, 512], BF16) for i in range(2)]
                pDs = [psb("pDs%d" % i, [128, 512], F32) for i in range(2)]
                pIS = psb("pIS", [128, 512], F32)
                ptr = psb("ptr", [128, 8, 128], BF16)
                pS = [psb("pS%d" % i, [128, 512], F32) for i in range(2)]
                pO = psb("pO", [128, 512], F32)
                pD = psb("pD", [128, 512], F32)
                for h in range(8):
                    P.op("sp", "dma_start", [], ["iqT"], out=iqT[:, h, :], in_=SC["iqT"][h])
                    P.op("sp", "dma_start", [], ["dqT"], out=dqT[:, h, :], in_=SC["dqT"][h])
                for g in range(2):
                    P.op("sp", "dma_start", [], ["dkT"], out=dkT[:, g, :], in_=SC["dkT"][g])
                P.op("sp", "dma_start", [], ["ikT"], out=ikT[:], in_=SC["ikT"])
                P.op("sp", "dma_start", [], ["iw"], out=iw[:], in_=SC["iw"].rearrange("(t p) c -> p t c", p=128))
                P.op("sp", "dma_start", [], ["dvs"], out=dvs[:], in_=SC["dv"].rearrange("(t p) c -> p t c", p=128))
                for (src_, dst_) in ((IN["put"], SC["pub"]), (IN["pv"], SC["pvb"])):
                    for r0 in range(0, 16384, 1024):
                        P.op("pool", "dma_start", ["iqT", "dqT", "dkT", "ikT", "iw", "dvs"], [], out=dst_[r0:r0 + 1024, :].rearrange("r (a b) -> (r a) b", b=1024),
                             in_=src_[r0:r0 + 1024, :].rearrange("r (a b) -> (r a) b", b=1024))
                sc_idx = float(64 ** -0.5 * 16 ** -0.5)
                sc_dsa = float(128 ** -0.5)
                nR = 0
                nD = 0
                cnt = 0
                def stage_IS(g):
                    qg = g // 2
                    st_ = dict(nD=0)
                    for T in (2 * g, 2 * g + 1):
                        i = T % 4
                        tsl = slice(T * 128, (T + 1) * 128)
                        nk = 128 * (T + 1)
                        dd = T % 2
                        orig = origs[i]
                        P.op("dve", "tensor_tensor", ["identf", "iw"], ["diag%d" % dd], out=diag[dd][:],
                             in0=identf[:].unsqueeze(1).to_broadcast([128, 16, 128]), in1=iw[:, T, :].unsqueeze(2).to_broadcast([128, 16, 128]), op=ALU.mult)
                        for kg in range((nk + 511) // 512):
                            ncol = min(512, nk - kg * 512)
                            ksl = slice(kg * 512, kg * 512 + ncol)
                            for h in range(16):
                                rows = slice((h % 2) * 64, (h % 2) * 64 + 64)
                                a = CN["nD"] % 2
                                CN["nD"] += 1
                                r = CN["nR"] % 4
                                CN["nR"] += 1
                                P.op("pe", "matmul", ["iqT", "ikT"], ["pDs%d" % a], out=pDs[a][:, 0:ncol], lhsT=iqT[rows, h // 2, tsl], rhs=ikT[rows, ksl], start=True, stop=True)
                                P.op("act", "activation", ["pDs%d" % a], ["R%d" % r], out=Rb[r][:, 0:ncol], in_=pDs[a][:, 0:ncol], func=AF.Relu, scale=sc_idx)
                                P.op("pe", "matmul", ["diag%d" % dd, "R%d" % r], ["pIS"], out=pIS[:, 0:ncol], lhsT=diag[dd][:, h, :], rhs=Rb[r][:, 0:ncol],
                                     start=(h == 0), stop=(h == 15))
                            P.op("act", "activation", ["pIS"], ["orig%d" % i], out=orig[:, ksl], in_=pIS[:, 0:ncol], func=AF.Copy)
                        P.op("pool", "memset", [], ["orig%d" % i], ap=orig[0:64, nk - 64:nk], constant=NEG)

                def stage_topk(g, att_qg=None):
                    tiles = (2 * g, 2 * g + 1)
                    heads_done = 0
                    if tiles[0] >= 2:
                        for rnd in range(32):
                            for T in tiles:
                                i, dd, nk = T % 4, T % 2, 128 * (T + 1)
                                src = origs[i] if rnd == 0 else works[dd]
                                sname = ("orig%d" % i) if rnd == 0 else ("work%d" % dd)
                                P.op("dve", "max", [sname], ["mx%d" % dd], out=mxs[dd][:], in_=src[:, 0:nk])
                            for T in tiles:
                                i, dd, nk = T % 4, T % 2, 128 * (T + 1)
                                src = origs[i] if rnd == 0 else works[dd]
                                sname = ("orig%d" % i) if rnd == 0 else ("work%d" % dd)
                                P.op("dve", "match_replace", ["mx%d" % dd, sname], ["work%d" % dd], out=works[dd][:, 0:nk], in_to_replace=mxs[dd][:],
                                     in_values=src[:, 0:nk], imm_value=NEG)
                            if att_qg is not None and rnd % 4 == 3:
                                stage_att_head(att_qg, heads_done)
                                heads_done += 1
                    if att_qg is not None:
                        while heads_done < 8:
                            stage_att_head(att_qg, heads_done)
                            heads_done += 1
                    for T in tiles:
                        i, dd, nk = T % 4, T % 2, 128 * (T + 1)
                        mask = masks[dd]
                        if T >= 2:
                            P.op("dve", "tensor_tensor", ["work%d" % dd, "orig%d" % i], ["mask%d" % dd], out=mask[:, 0:nk], in0=works[dd][:, 0:nk], in1=origs[i][:, 0:nk], op=ALU.not_equal)
                        else:
                            P.op("dve", "tensor_scalar", ["orig%d" % i], ["mask%d" % dd], out=mask[:, 0:nk], in0=origs[i][:, 0:nk], scalar1=NEG / 2, scalar2=None, op0=ALU.is_gt)

                def stage_maskT(g):
                    mp = (g // 2) % 2
                    MT = MTs[mp]
                    if g % 2 == 0:
                        P.op("pool", "memset", [], ["MT%d" % mp], ap=MT[:], constant=0.0)
                    for T in (2 * g, 2 * g + 1):
                        i, dd = T % 4, T % 2
                        mask = masks[dd]
                        for k0 in range(0, T + 1, 8):
                            n8 = min(8, T + 1 - k0)
                            for kt in range(k0, k0 + n8):
                                P.op("pe", "transpose", ["mask%d" % dd, "identb"], ["ptr"], out=ptr[:, kt - k0, :], in_=mask[:, kt * 128:(kt + 1) * 128], identity=identb[:])
                            P.op("act", "activation", ["ptr"], ["MT%d" % mp], out=MT[:, k0:k0 + n8, i * 128:(i + 1) * 128], in_=ptr[:, 0:n8, :], func=AF.Copy)

                def stage_att_head(qg, h):
                    os_ = qg % 2
                    g = h // 4
                    nkt = 4 * (qg + 1)
                    for kt in range(nkt):
                        ksl = slice(kt * 128, (kt + 1) * 128)
                        a = CN["cnt"] % 2
                        k = CN["cnt"] % 3
                        CN["cnt"] += 1
                        P.op("pe", "matmul", ["dkT", "dqT"], ["pS%d" % a], out=pS[a][:], lhsT=dkT[:, g, ksl], rhs=dqT[:, h, qg * 512:(qg + 1) * 512], start=True, stop=True)
                        P.op("act", "activation", ["pS%d" % a], ["pT%d" % k], out=pT[k][:], in_=pS[a][:], func=AF.Exp, scale=sc_dsa)
                        P.op("pool", "tensor_tensor", ["pT%d" % k, "MT%d" % os_], ["pT%d" % k], out=pT[k][:], in0=pT[k][:], in1=MTs[os_][:, kt, :], op=ALU.mult)
                        P.op("pe", "matmul", ["dvs", "pT%d" % k], ["pO"], out=pO[:], lhsT=dvs[:, kt, g * 128:(g + 1) * 128], rhs=pT[k][:], start=(kt == 0), stop=(kt == nkt - 1))
                        P.op("pe", "matmul", ["onesb", "pT%d" % k], ["pD"], out=pD[:], lhsT=onesb[:], rhs=pT[k][:], start=(kt == 0), stop=(kt == nkt - 1))
                    P.op("dve", "reciprocal", ["pD"], ["rden"], out=rden[:], in_=pD[:])
                    P.op("dve", "tensor_tensor", ["pO", "rden"], ["obst%d" % os_], out=obst[os_][:, h, :], in0=pO[:], in1=rden[:], op=ALU.mult)
                    if h == 7:
                        P.op("sp", "dma_start", ["obst%d" % os_], [], out=SC["obT"][:, :, qg * 512:(qg + 1) * 512].rearrange("h p s -> p h s"), in_=obst[os_][:])

                CN = dict(nD=0, nR=0, cnt=0)
                stage_IS(0)
                pending = None
                for g in range(8):
                    if g + 1 < 8:
                        stage_IS(g + 1)
                    stage_topk(g, att_qg=pending)
                    pending = None
                    stage_maskT(g)
                    if g % 2 == 1:
                        pending = g // 2
                for h in range(8):
                    stage_att_head(pending, h)
                P.emit(st)

        if "p4" in phases:
            with ExitStack() as st:
                sb = lambda n, s, d, _p="p4_": st.enter_context(nc.sbuf_tensor(_p + n, s, d))
                psb = lambda n, s, d, _p="p4_": st.enter_context(nc.psum_tensor(_p + n, s, d))
                P = Prog(nc, SEMS)
                wa = sb("wa", [128, 8, D], BF16)
                wb = sb("wb", [128, 8, D], BF16)
                wo = sb("wo", [128, 16, D], BF16)
                oat = [sb("oat%d" % i, [128, 8, 128], BF16) for i in range(2)]
                obt = [sb("obt%d" % i, [128, 8, 128], BF16) for i in range(2)]
                gst_ = [sb("gs%d" % i, [128, 4096], BF16) for i in range(1)] * 2
                xt = [sb("xt%d" % i, [128, D], F32) for i in range(2)]
                mgs = [sb("mg%d" % i, [128, D], BF16) for i in range(2)]
                mTs = [sb("mT%d" % i, [128, 16, 128], BF16) for i in range(2)]
                gsb2 = [sb("gsb%d" % i, [128, 4096], BF16) for i in range(2)]
                t1 = [sb("t1_%d" % i, [128, 512], F32) for i in range(2)]
                t2 = [sb("t2_%d" % i, [128, 512], F32) for i in range(2)]
                pY = [psb("pY%d" % i, [128, 512], F32) for i in range(4)]
                ptr = [psb("ptr%d" % i, [128, 8, 128], BF16) for i in range(2)]
                pZ = [psb("pZ%d" % i, [128, 512], F32) for i in range(2)]
                for h in range(8):
                    for hf in range(2):
                        P.op("pool", "dma_start", [], ["wa"], out=wa[:, h, hf * 1024:(hf + 1) * 1024], in_=IN["wa"][:, h, hf * 1024:(hf + 1) * 1024])
                        P.op("pool", "dma_start", [], ["wb"], out=wb[:, h, hf * 1024:(hf + 1) * 1024], in_=IN["wb"][:, h, hf * 1024:(hf + 1) * 1024])
                for dk in range(16):
                    for hf in range(2):
                        P.op("pool", "dma_start", [], ["wo"], out=wo[:, dk, hf * 1024:(hf + 1) * 1024], in_=IN["wo"][:, dk, hf * 1024:(hf + 1) * 1024])
                CNY = dict(ny=0)

                def stage_Y(T):
                    s = T % 2
                    tsl = slice(T * 128, (T + 1) * 128)
                    P.op("sp", "dma_start", [], ["oat%d" % s], out=oat[s][:], in_=SC["oaT"][:, :, tsl].rearrange("h p s -> p h s"))
                    P.op("sp", "dma_start", [], ["obt%d" % s], out=obt[s][:], in_=SC["obT"][:, :, tsl].rearrange("h p s -> p h s"))
                    P.op("sp", "dma_start", [], ["gs%d" % s], out=gsb2[s][:], in_=SC["gs"][tsl, :])
                    P.op("sp", "dma_start", [], ["xt%d" % s], out=xt[s][:], in_=IN["x"][tsl, :])
                    for cg in range(4):
                        csl = slice(cg * 512, (cg + 1) * 512)
                        ya = CNY["ny"] % 4
                        yb = (CNY["ny"] + 1) % 4
                        CNY["ny"] += 2
                        u = cg % 2
                        for h in range(8):
                            P.op("pe", "matmul", ["oat%d" % s, "wa"], ["pY%d" % ya], out=pY[ya][:], lhsT=oat[s][:, h, :], rhs=wa[:, h, csl], start=(h == 0), stop=(h == 7))
                        for h in range(8):
                            P.op("pe", "matmul", ["obt%d" % s, "wb"], ["pY%d" % yb], out=pY[yb][:], lhsT=obt[s][:, h, :], rhs=wb[:, h, csl], start=(h == 0), stop=(h == 7))
                        P.op("dve", "tensor_tensor", ["pY%d" % ya, "gs%d" % s], ["t1_%d" % u], out=t1[u][:], in0=pY[ya][:], in1=gsb2[s][:, csl], op=ALU.mult)
                        P.op("dve", "tensor_tensor", ["pY%d" % yb, "gs%d" % s], ["t2_%d" % u], out=t2[u][:], in0=pY[yb][:], in1=gsb2[s][:, 2048 + cg * 512:2048 + (cg + 1) * 512], op=ALU.mult)
                        P.op("pool", "tensor_tensor", ["t1_%d" % u, "t2_%d" % u], ["mg%d" % s], out=mgs[s][:, csl], in0=t1[u][:], in1=t2[u][:], op=ALU.add)

                def stage_TZ(T):
                    s = T % 2
                    tsl = slice(T * 128, (T + 1) * 128)
                    for dk in range(16):
                        b_ = dk // 8
                        P.op("pe", "transpose", ["mg%d" % s, "identb"], ["ptr%d" % b_], out=ptr[b_][:, dk % 8, :], in_=mgs[s][:, dk * 128:(dk + 1) * 128], identity=identb[:])
                    for b_ in range(2):
                        P.op("act", "activation", ["ptr%d" % b_], ["mT%d" % s], out=mTs[s][:, b_ * 8:(b_ + 1) * 8, :], in_=ptr[b_][:], func=AF.Copy)
                    for og in range(4):
                        csl = slice(og * 512, (og + 1) * 512)
                        z = og % 2
                        for dk in range(16):
                            P.op("pe", "matmul", ["mT%d" % s, "wo"], ["pZ%d" % z], out=pZ[z][:], lhsT=mTs[s][:, dk, :], rhs=wo[:, dk, csl], start=(dk == 0), stop=(dk == 15))
                        P.op("dve", "tensor_tensor", ["pZ%d" % z, "xt%d" % s], ["xt%d" % s], out=xt[s][:, csl], in0=pZ[z][:], in1=xt[s][:, csl], op=ALU.add)
                    P.op("sp", "dma_start", ["xt%d" % s], [], out=SC["x2"][tsl, :], in_=xt[s][:])

                stage_Y(0)
                for T in range(NT):
                    if T + 1 < NT:
                        stage_Y(T + 1)
                    stage_TZ(T)
                P.emit(st)

        if "p5" in phases:
            with ExitStack() as st:
                sb = lambda n, s, d, _p="p5_": st.enter_context(nc.sbuf_tensor(_p + n, s, d))
                psb = lambda n, s, d, _p="p5_": st.enter_context(nc.psum_tensor(_p + n, s, d))
                P = Prog(nc, SEMS)
                wpq = sb("wpq", [128, 16, 1024], BF16)
                kb = sb("kb", [128, 8, 256], BF16)
                gffnT = sb("gffnT", [128, 16], F32)
                gfin = sb("gfin", [128, D], F32)
                iota = sb("iota", [128, 16], F32)
                iota128 = sb("iota128", [128, 128], F32)
                GT = sb("GT", [128, 256, 128], BF16)
                NSB = 8
                OH2s = [sb("OH2_%d" % i, [128, NSB, 128], BF16) for i in range(2)]
                OH1s = [sb("OH1_%d" % i, [128, NSB, 128], BF16) for i in range(2)]
                hnTGs = [sb("hnTG%d" % i, [128, 16, 256], BF16) for i in range(2)]
                NUB = 4
                ub = [sb("ub%d" % i, [128, D], BF16) for i in range(NUB)]
                vb = [ub[i // 2][:, (i % 2) * 1024:(i % 2 + 1) * 1024] for i in range(2 * NUB)]
                x2t = [sb("x2t%d" % i, [128, D], F32) for i in range(2)]
                hnf = sb("hnf", [128, D], F32)
                qTs = sb("qTs", [128, 8, 128], BF16)
                ssb = sb("ssb", [128, 8, 256], F32)
                wk = sb("wk", [128, 256], F32)
                vals = sb("vals", [128, 16, 16], F32)
                idxs = sb("idxs", [128, 16, 16], U32)
                idxf = sb("idxf", [128, 16, 16], F32)
                tv = sb("tv", [128, 8, 16], F32)
                tp = sb("tp", [128, 8, 16], U32)
                ti = sb("ti", [128, 8, 16], U32)
                tj = sb("tj", [128, 8, 16], U32)
                tif = sb("tif", [128, 8, 16], F32)
                tjf = sb("tjf", [128, 8, 16], F32)
                e1 = [sb("e1_%d" % i, [128, 8, 16], F32) for i in range(4)]
                e2 = [sb("e2_%d" % i, [128, 8, 16], F32) for i in range(4)]
                gw = [sb("gw%d" % i, [128, 128], F32) for i in range(4)]
                selT = sb("selT", [128, 3, 128], F32)
                ex = sb("ex", [128, 8, 16], F32)
                zs = sb("zs", [128, 8], F32)
                ga = [sb("ga%d" % i, [128, 256], BF16) for i in range(2)]
                cst = [sb("cst%d" % i, [128, 256], BF16) for i in range(4)]
                stA = sb("stA", [128, 4], F32)
                stC = sb("stC", [128, 4], F32)
                B = [psb("B%d" % i, [128, 512], F32) for i in range(8)]
                for dk in range(16):
                    P.op("pool", "dma_start", [], ["wpq"], out=wpq[:, dk, :], in_=IN["wpq"][:, dk, :])
                for h in range(8):
                    P.op("pool", "dma_start", [], ["kb"], out=kb[:, h, :], in_=IN["kb"][:, h, :])
                P.op("sp", "dma_start", [], ["gffnT"], out=gffnT[:], in_=IN["gffnT"])
                P.op("sp", "dma_start", [], ["gfin"], out=gfin[:], in_=IN["gfin"].partition_broadcast(128))
                P.op("sp", "dma_start", [], ["iota"], out=iota[:], in_=IN["iota16"])
                P.op("sp", "dma_start", [], ["iota128"], out=iota128[:], in_=IN["iota128"])
                CN = dict(u=0, v=0, g=0, a=0, o=0)

                def stage_A(T):
                    p = T % 4
                    gp = (T // 2) % 2
                    hnTG = hnTGs[gp]
                    HT = "hnTG%d" % gp
                    tcol = slice((T % 2) * 128, (T % 2) * 128 + 128)
                    tsl = slice(T * 128, (T + 1) * 128)
                    yield P.op("sp", "dma_start", [], ["hnf"], out=hnf[:], in_=SC["x2"][tsl, :])
                    yield P.op("act", "activation", ["hnf"], ["ssb", "stA0"], out=ssb[:].rearrange("p a b -> p (a b)"), in_=hnf[:], func=AF.Square, accum_out=stA[:, 0:1])
                    yield P.op("act", "activation", ["stA0"], ["stA1"], out=stA[:, 1:2], in_=stA[:, 0:1], func=AF.Sqrt, scale=1.0 / D, bias=EPS)
                    yield P.op("dve", "reciprocal", ["stA1"], ["stA2"], out=stA[:, 2:3], in_=stA[:, 1:2])
                    yield P.op("act", "activation", ["hnf", "stA2"], ["hnf"], out=hnf[:], in_=hnf[:], func=AF.Copy, scale=stA[:, 2:3])
                    for r in range(4):
                        z = 6 + r % 2
                        for q4 in range(4):
                            dk = r * 4 + q4
                            yield P.op("pe", "transpose", ["hnf", "identf"], ["B%d" % z], out=B[z][:, q4 * 128:(q4 + 1) * 128], in_=hnf[:, dk * 128:(dk + 1) * 128], identity=identf[:])
                        for q4 in range(4):
                            dk = r * 4 + q4
                            if dk % 2 == 0:
                                yield P.op("act", "activation", ["B%d" % z, "gffnT"], [HT], out=hnTG[:, dk, tcol], in_=B[z][:, q4 * 128:(q4 + 1) * 128],
                                           func=AF.Copy, scale=gffnT[:, dk:dk + 1])
                            else:
                                yield P.op("dve", "tensor_scalar", ["B%d" % z, "gffnT"], [HT], out=hnTG[:, dk, tcol], in0=B[z][:, q4 * 128:(q4 + 1) * 128],
                                           scalar1=gffnT[:, dk:dk + 1], scalar2=None, op0=ALU.mult)
                    for hh in range(2):
                        z = 6 + hh
                        for h4 in range(4):
                            h = hh * 4 + h4
                            for dk in range(16):
                                yield P.op("pe", "matmul", ["wpq", HT], ["B%d" % z], out=B[z][:, h4 * 128:(h4 + 1) * 128], lhsT=wpq[:, dk, h * 128:(h + 1) * 128],
                                           rhs=hnTG[:, dk, tcol], start=(dk == 0), stop=(dk == 15))
                        yield P.op("act", "activation", ["B%d" % z], ["qTs"], out=qTs[:, hh * 4:(hh + 1) * 4, :].rearrange("p a b -> p (a b)"), in_=B[z][:], func=AF.Copy)
                    for h2 in range(4):
                        z = 6 + h2 % 2
                        for hi in range(2):
                            h = h2 * 2 + hi
                            yield P.op("pe", "matmul", ["qTs", "kb"], ["B%d" % z], out=B[z][:, hi * 256:(hi + 1) * 256], lhsT=qTs[:, h, :], rhs=kb[:, h, :], start=True, stop=True)
                        yield P.op("act", "activation", ["B%d" % z], ["ssb"], out=ssb[:, h2 * 2:h2 * 2 + 2, :].rearrange("p a b -> p (a b)"), in_=B[z][:], func=AF.Copy)
                    for hp in range(16):
                        src = ssb[:, hp // 2, (hp % 2) * 128:(hp % 2) * 128 + 128]
                        yield P.op("dve", "max", ["ssb"], ["vals"], out=vals[:, hp, 0:8], in_=src)
                        yield P.op("dve", "max_index", ["ssb", "vals"], ["idxs"], out=idxs[:, hp, 0:8], in_max=vals[:, hp, 0:8], in_values=src)
                        yield P.op("dve", "match_replace", ["ssb", "vals"], ["wk"], out=wk[:, 0:128], in_to_replace=vals[:, hp, 0:8], in_values=src, imm_value=NEG)
                        yield P.op("dve", "max", ["wk"], ["vals"], out=vals[:, hp, 8:16], in_=wk[:, 0:128])
                        yield P.op("dve", "max_index", ["wk", "vals"], ["idxs"], out=idxs[:, hp, 8:16], in_max=vals[:, hp, 8:16], in_values=wk[:, 0:128])
                    yield P.op("dve", "tensor_copy", ["idxs"], ["idxf"], out=idxf[:], in_=idxs[:])
                    cand = ssb[:].rearrange("p h (a b) -> p h a b", b=16)
                    v4 = vals[:].rearrange("p (h two) k -> p h two k", two=2)
                    i4 = idxf[:].rearrange("p (h two) k -> p h two k", two=2)
                    yield P.op("dve", "tensor_tensor", ["vals"], ["ssb"], out=cand, in0=v4[:, :, 0, :].unsqueeze(3).to_broadcast([128, 8, 16, 16]),
                         in1=v4[:, :, 1, :].unsqueeze(2).to_broadcast([128, 8, 16, 16]), op=ALU.add)
                    for h in range(8):
                        src = ssb[:, h, :]
                        yield P.op("dve", "max", ["ssb"], ["tv"], out=tv[:, h, 0:8], in_=src)
                        yield P.op("dve", "max_index", ["ssb", "tv"], ["tp"], out=tp[:, h, 0:8], in_max=tv[:, h, 0:8], in_values=src)
                        yield P.op("dve", "match_replace", ["ssb", "tv"], ["wk"], out=wk[:], in_to_replace=tv[:, h, 0:8], in_values=src, imm_value=NEG)
                        yield P.op("dve", "max", ["wk"], ["tv"], out=tv[:, h, 8:16], in_=wk[:])
                        yield P.op("dve", "max_index", ["wk", "tv"], ["tp"], out=tp[:, h, 8:16], in_max=tv[:, h, 8:16], in_values=wk[:])
                    yield P.op("dve", "tensor_single_scalar", ["tp"], ["ti"], out=ti[:], in_=tp[:], scalar=4, op=ALU.logical_shift_right)
                    yield P.op("dve", "tensor_single_scalar", ["tp"], ["tj"], out=tj[:], in_=tp[:], scalar=15, op=ALU.bitwise_and)
                    yield P.op("dve", "tensor_copy", ["ti"], ["tif"], out=tif[:], in_=ti[:])
                    yield P.op("dve", "tensor_copy", ["tj"], ["tjf"], out=tjf[:], in_=tj[:])
                    oh = ssb[:].rearrange("p h (a b) -> p h a b", b=16)
                    iob = iota[:].unsqueeze(1).unsqueeze(1).to_broadcast([128, 8, 16, 16])
                    for (sel, side, dst, dn) in ((tif, 0, e1[p], "e1_%d" % p), (tjf, 1, e2[p], "e2_%d" % p)):
                        yield P.op("dve", "tensor_tensor", ["tif", "tjf", "iota"], ["ssb"], out=oh, in0=sel[:].unsqueeze(3).to_broadcast([128, 8, 16, 16]), in1=iob, op=ALU.is_equal)
                        yield P.op("dve", "tensor_tensor", ["ssb", "idxf"], ["ssb"], out=oh, in0=oh, in1=i4[:, :, side, :].unsqueeze(2).to_broadcast([128, 8, 16, 16]), op=ALU.mult)
                        yield P.op("dve", "tensor_reduce", ["ssb"], [dn], out=dst[:], in_=oh, axis=AX.X, op=ALU.add)
                    yield P.op("dve", "tensor_tensor", ["tv"], ["ex"], out=ex[:], in0=tv[:], in1=tv[:, :, 0:1].to_broadcast([128, 8, 16]), op=ALU.subtract)
                    yield P.op("act", "activation", ["ex"], ["ex"], out=ex[:], in_=ex[:], func=AF.Exp)
                    yield P.op("dve", "tensor_reduce", ["ex"], ["zs"], out=zs[:], in_=ex[:], axis=AX.X, op=ALU.add)
                    yield P.op("dve", "reciprocal", ["zs"], ["zs"], out=zs[:], in_=zs[:])
                    yield P.op("dve", "tensor_tensor", ["ex", "zs"], ["gw%d" % p], out=gw[p][:].rearrange("p (a b) -> p a b", b=16), in0=ex[:], in1=zs[:].unsqueeze(2).to_broadcast([128, 8, 16]), op=ALU.mult)

                def stage_GT(T):
                    p = T % 4
                    tp_ = T % 2
                    srcs = [(e1[p][:].rearrange("p a b -> p (a b)"), "e1_%d" % p), (e2[p][:].rearrange("p a b -> p (a b)"), "e2_%d" % p), (gw[p][:], "gw%d" % p)]
                    for j, (ap_, nm) in enumerate(srcs):
                        P.op("pe", "transpose", [nm, "identf"], ["B7"], out=B[7][:, j * 128:(j + 1) * 128], in_=ap_, identity=identf[:])
                    P.op("act", "activation", ["B7"], ["selT"], out=selT[:].rearrange("p a b -> p (a b)"), in_=B[7][:, 0:384], func=AF.Copy)
                    for sub in range(128 // NSB):
                        tsub = slice(sub * NSB, (sub + 1) * NSB)
                        ob = CN["o"] % 2
                        CN["o"] += 1
                        OH2, OH1 = OH2s[ob], OH1s[ob]
                        iob = iota128[:].unsqueeze(1).to_broadcast([128, NSB, 128])
                        P.op("dve", "tensor_tensor", ["iota128", "selT"], ["OH2_%d" % ob], out=OH2[:], in0=iob, in1=selT[:, 1, tsub].unsqueeze(2).to_broadcast([128, NSB, 128]), op=ALU.is_equal)
                        P.op("dve", "tensor_tensor", ["iota128", "selT"], ["OH1_%d" % ob], out=OH1[:], in0=iob, in1=selT[:, 0, tsub].unsqueeze(2).to_broadcast([128, NSB, 128]), op=ALU.is_equal)
                        P.op("pool", "tensor_tensor", ["OH1_%d" % ob, "selT"], ["OH1_%d" % ob], out=OH1[:], in0=OH1[:], in1=selT[:, 2, tsub].unsqueeze(2).to_broadcast([128, NSB, 128]), op=ALU.mult)
                        for t4 in range(NSB // 4):
                            z = 6 + CN["g"] % 2
                            CN["g"] += 1
                            for tt in range(4):
                                t = t4 * 4 + tt
                                P.op("pe", "matmul", ["OH2_%d" % ob, "OH1_%d" % ob], ["B%d" % z], out=B[z][:, tt * 128:(tt + 1) * 128], lhsT=OH2[:, t, :], rhs=OH1[:, t, :], start=True, stop=True)
                            tg0 = tp_ * 128 + sub * NSB + t4 * 4
                            P.op("act", "activation", ["B%d" % z], ["GT"], out=GT[:, tg0:tg0 + 4, :].rearrange("p t c -> p (t c)"), in_=B[z][:], func=AF.Copy)

                def stage_U(G, step):
                    gp = G % 2
                    hnTG = hnTGs[gp]
                    for c in range(128):
                        b = CN["u"] % NUB
                        CN["u"] += 1
                        P.op("sp", "dma_start", [], ["ubh%d" % (2 * b), "ubh%d" % (2 * b + 1)], out=ub[b][:], in_=SC["pub"][c * 128:(c + 1) * 128, :])
                        z = 4 + (c // 2) % 2
                        reg = slice((c % 2) * 256, (c % 2) * 256 + 256)
                        for dk in range(16):
                            P.op("pe", "matmul", ["ubh%d" % (2 * b + dk // 8), "hnTG%d" % gp], ["B%d" % z], out=B[z][:, reg], lhsT=ub[b][:, dk * 128:(dk + 1) * 128], rhs=hnTG[:, dk, :], start=(dk == 0), stop=(dk == 15))
                        k = CN["a"] % 2
                        CN["a"] += 1
                        P.op("act", "activation", ["B%d" % z], ["ga%d" % k], out=ga[k][:], in_=B[z][:, reg], func=AF.Gelu)
                        P.op("dve", "tensor_tensor", ["ga%d" % k, "GT"], ["GT"], out=GT[:, :, c], in0=ga[k][:], in1=GT[:, :, c], op=ALU.mult)
                        step(3)

                def stage_V(G, step):
                    for p in range(2):
                        T = 2 * G + p
                        P.op("sp", "dma_start", [], ["x2t%d" % p], out=x2t[p][:], in_=SC["x2"][T * 128:(T + 1) * 128, :])
                    for hv in range(2):
                        for c in range(128):
                            b = CN["v"] % (2 * NUB)
                            CN["v"] += 1
                            P.op("sp", "dma_start", [], ["ubh%d" % b], out=vb[b], in_=SC["pvb"][c * 128:(c + 1) * 128, hv * 1024:(hv + 1) * 1024])
                            k = (CN["v"] - 1) % 4
                            if k % 2 == 0:
                                P.op("act", "activation", ["GT"], ["cst%d" % k], out=cst[k][:], in_=GT[:, :, c], func=AF.Copy)
                            else:
                                P.op("pool", "tensor_copy", ["GT"], ["cst%d" % k], out=cst[k][:], in_=GT[:, :, c])
                            for p in range(2):
                                for cg in range(2):
                                    z = p * 2 + cg
                                    P.op("pe", "matmul", ["cst%d" % k, "ubh%d" % b], ["B%d" % z], out=B[z][:], lhsT=cst[k][:, p * 128:(p + 1) * 128], rhs=vb[b][:, cg * 512:(cg + 1) * 512],
                                         start=(c == 0), stop=(c == 127))
                            step(3)
                        for p in range(2):
                            for cg in range(2):
                                z = p * 2 + cg
                                csl = slice(hv * 1024 + cg * 512, hv * 1024 + (cg + 1) * 512)
                                P.op("dve", "tensor_tensor", ["B%d" % z, "x2t%d" % p], ["x2t%d" % p], out=x2t[p][:, csl], in0=B[z][:], in1=x2t[p][:, csl], op=ALU.add)
                    for p in range(2):
                        T = 2 * G + p
                        tsl = slice(T * 128, (T + 1) * 128)
                        X = "x2t%d" % p
                        P.op("act", "activation", [X], ["ubh0", "ubh1", "stC0"], out=ub[0][:], in_=x2t[p][:], func=AF.Square, accum_out=stC[:, 0:1])
                        P.op("act", "activation", ["stC0"], ["stC1"], out=stC[:, 1:2], in_=stC[:, 0:1], func=AF.Sqrt, scale=1.0 / D, bias=EPS)
                        P.op("dve", "reciprocal", ["stC1"], ["stC2"], out=stC[:, 2:3], in_=stC[:, 1:2])
                        P.op("dve", "scalar_tensor_tensor", [X, "stC2", "gfin"], [X], out=x2t[p][:], in0=x2t[p][:], scalar=stC[:, 2:3], in1=gfin[:], op0=ALU.mult, op1=ALU.mult)
                        P.op("sp", "dma_start", [X], [], out=y[tsl, :], in_=x2t[p][:])

                def run_all(gen):
                    for _ in gen:
                        pass

                def chain(*gens):
                    for g_ in gens:
                        for v_ in g_:
                            yield v_

                run_all(stage_A(0))
                run_all(stage_A(1))
                for G in range(NT // 2):
                    stage_GT(2 * G)
                    stage_GT(2 * G + 1)
                    nxt = chain(stage_A(2 * G + 2), stage_A(2 * G + 3)) if G + 1 < NT // 2 else iter(())

                    def step(n, nxt=nxt):
                        for _ in range(n):
                            next(nxt, None)

                    stage_U(G, step)
                    stage_V(G, step)
                    run_all(nxt)
                P.emit(st)
    return nc


def kernel(**inputs):
    H = host_layout(inputs)
    x = np.asarray(inputs["x"], np.float32)
    pos = np.asarray(inputs["positions"]).astype(np.int32)
    nc = build_nc()
    in_maps = []
    for b in range(8):
        m = {k: H[k] for k in SHAPES if k not in ("x", "pos")}
        m["x"] = np.ascontiguousarray(x[b])
        m["pos"] = np.ascontiguousarray(pos[b])
        in_maps.append(m)
    res = run_bass_kernel_spmd(nc, in_maps, core_ids=list(range(8)))
    return np.stack([np.asarray(r["y"], dtype=np.float32) for r in res.results], axis=0)
```

```python
import numpy as np
from contextlib import ExitStack
import concourse.bass as bass
import concourse.mybir as mybir
from concourse.bass_utils import run_bass_kernel_spmd

F32 = mybir.dt.float32
BF16 = mybir.dt.bfloat16
I32 = mybir.dt.int32
U32 = mybir.dt.uint32
AF = mybir.ActivationFunctionType
ALU = mybir.AluOpType
AX = mybir.AxisListType

D = 2048
S = 2048
NT = 16
EPS = 1e-6
PI = float(np.pi)
MAGIC = 12582912.0
NEG = -1e30
ROPE_THETA = 500000.0

ENGS = ["pe", "act", "dve", "pool", "sp"]
N_DMA_SEMS = {"sp": 40, "pool": 24, "act": 8}


class Prog:
    def __init__(self, nc, semstate):
        self.nc = nc
        self.semstate = semstate
        self.ins = {e: [] for e in ENGS}
        self.last_w = {}
        self.readers = {}
        self.dma_rr = {e: 0 for e in N_DMA_SEMS}
        self.dma_cnt = {}

    def add(self, eng, fn, reads=(), writes=(), dma=False):
        idx = len(self.ins[eng])
        deps = set()
        for r in reads:
            w = self.last_w.get(r)
            if w is not None:
                deps.add(w)
        for r in writes:
            w = self.last_w.get(r)
            if w is not None:
                deps.add(w)
            for rd in self.readers.get(r, ()):
                deps.add(rd)
        deps.discard((eng, idx))
        rec = dict(fn=fn, deps=deps, dma=dma)
        if dma:
            j = self.dma_rr[eng]
            self.dma_rr[eng] = (j + 1) % N_DMA_SEMS[eng]
            n = self.dma_cnt.get((eng, j), 0) + 1
            self.dma_cnt[(eng, j)] = n
            rec["dsem"] = (eng, j, n)
        self.ins[eng].append(rec)
        for r in reads:
            self.readers.setdefault(r, []).append((eng, idx))
        for r in writes:
            self.last_w[r] = (eng, idx)
            self.readers[r] = []
        return (eng, idx)

    def op(self, eng, method, reads, writes, **kw):
        dma = method in ("dma_start", "indirect_dma_start")
        return self.add(eng, lambda e: getattr(e, method)(**kw), reads, writes, dma=dma)

    def emit(self, stack):
        nc = self.nc
        need = {e: set() for e in ENGS}
        for e in ENGS:
            for ins in self.ins[e]:
                for (de, di) in ins["deps"]:
                    if self.ins[de][di]["dma"]:
                        continue
                    if de == e and e == "pe":
                        continue
                    need[de].add(di)
        sigcount = {}
        for e in ENGS:
            c = 0
            for i, ins in enumerate(self.ins[e]):
                if ins["dma"]:
                    continue
                if i in need[e]:
                    c += 1
                    sigcount[(e, i)] = c
        ss = self.semstate
        gstack = ss["stack"]
        for e in ["pe", "act", "dve", "pool"]:
            if e not in ss["esem"]:
                ss["esem"][e] = gstack.enter_context(nc.semaphore("se_%s" % e))
                ss["ebase"][e] = 0
        for e, n in N_DMA_SEMS.items():
            for j in range(n):
                if self.dma_cnt.get((e, j), 0) > 0 and (e, j) not in ss["dsem"]:
                    ss["dsem"][(e, j)] = gstack.enter_context(nc.semaphore("sd_%s_%d" % (e, j)))
                    ss["dbase"][(e, j)] = 0
        esem = ss["esem"]
        dsem = ss["dsem"]
        ebase = dict(ss["ebase"])
        dbase = dict(ss["dbase"])
        for (e, i) in list(sigcount.keys()):
            sigcount[(e, i)] += ebase[e]
        for e in ["pe", "act", "dve", "pool"]:
            ss["ebase"][e] += sum(1 for (ee, i) in sigcount if ee == e)
        for (e, j), n in self.dma_cnt.items():
            ss["dbase"][(e, j)] += n
        block = stack.enter_context(nc.Block())
        prog = self

        def run(ename, eng):
            waited = {}

            def w(key, sem, val):
                if waited.get(key, 0) >= val:
                    return
                eng.wait_ge(sem, val)
                waited[key] = val

            for i, ins in enumerate(prog.ins[ename]):
                for (de, di) in sorted(ins["deps"]):
                    dins = prog.ins[de][di]
                    if dins["dma"]:
                        (qe, j, n) = dins["dsem"]
                        w(("d", qe, j), dsem[(qe, j)], 16 * (n + dbase[(qe, j)]))
                    else:
                        if de == ename and ename == "pe":
                            continue
                        w(("e", de), esem[de], sigcount[(de, di)])
                if ins["dma"]:
                    (qe, j, n) = ins["dsem"]
                    if n > 1:
                        w(("d", qe, j), dsem[(qe, j)], 16 * (n - 1 + dbase[(qe, j)]))
                    inst = ins["fn"](eng)
                    inst.then_inc(dsem[(qe, j)], 16)
                else:
                    inst = ins["fn"](eng)
                    if (ename, i) in sigcount:
                        inst.then_inc(esem[ename], 1)
            for (qe, j), n in prog.dma_cnt.items():
                if qe == ename:
                    w(("d", qe, j), dsem[(qe, j)], 16 * (n + dbase[(qe, j)]))

        block.tensor(lambda eng: run("pe", eng))
        block.scalar(lambda eng: run("act", eng))
        block.vector(lambda eng: run("dve", eng))
        block.gpsimd(lambda eng: run("pool", eng))
        block.sync(lambda eng: run("sp", eng))


O_CQ, O_CKV, O_KR, O_DQ, O_DK, O_DV, O_IQ, O_IK, O_IW, O_G = 0, 512, 768, 832, 1856, 2112, 2368, 3392, 3456, 3472


def _rot_perm(n, half, rot, blocks=1, bw=None):
    bw = bw or n
    idx = np.arange(n)
    out = idx.copy()
    for b in range(n // bw):
        o = b * bw
        out[o:o + half] = idx[o + half:o + rot]
        out[o + half:o + rot] = idx[o:o + half]
    return out


def _pk(w, ncol):
    K = w.shape[0]
    return np.ascontiguousarray(w.reshape(K // 128, 128, ncol).transpose(1, 0, 2))


def host_layout(inp):
    f = np.float32
    w_in = np.asarray(inp["w_in"], f)[0]
    out = {}
    tiles = []
    for h in range(8):
        tiles.append(np.arange(O_DQ + h * 128, O_DQ + (h + 1) * 128))
    for g in range(2):
        tiles.append(np.arange(O_DK + g * 128, O_DK + (g + 1) * 128))
    for hp in range(8):
        tiles.append(np.arange(O_IQ + hp * 128, O_IQ + (hp + 1) * 128))
    tiles.append(np.concatenate([np.arange(O_IK, O_IK + 64)] * 2))
    tiles.append(np.concatenate([np.arange(O_KR, O_KR + 64)] * 2))
    perms = [_rot_perm(128, 16, 32)] * 10 + [_rot_perm(128, 8, 16, bw=64)] * 9 + [_rot_perm(128, 32, 64, bw=64)]
    w1f = np.empty((20, 128, 16, 256), f)
    for i, (cols, pm) in enumerate(zip(tiles, perms)):
        w1f[i, :, :, 0:128] = _pk(w_in[:, cols], 128)
        w1f[i, :, :, 128:256] = _pk(w_in[:, cols[pm]], 128)
    out["w1f"] = w1f
    tcols = [np.arange(O_CQ, O_CQ + 512), np.concatenate([np.arange(O_CKV, O_CKV + 256), np.arange(O_DV, O_DV + 256)])]
    for j in range(8):
        tcols.append(np.arange(O_G + j * 512, O_G + (j + 1) * 512))
    out["w1t"] = np.stack([_pk(w_in[:, c], 512) for c in tcols])
    out["w1iw"] = _pk(w_in[:, O_IW:O_IW + 16], 16)
    out["gmixT"] = np.ascontiguousarray(np.asarray(inp["norm_mix_g"], f)[0].reshape(16, 128).T)
    rc = np.zeros((128, 9), f)
    for ty, (half, rot, bw) in enumerate([(16, 32, 128), (8, 16, 64), (32, 64, 64)]):
        invf = (ROPE_THETA ** (-(np.arange(half, dtype=np.float32) * 2.0) / rot)).astype(f)
        for r in range(128):
            j = r % bw
            rc[r, ty * 3 + 1] = PI / 2
            if j < rot:
                rc[r, ty * 3 + 0] = invf[j % half]
                rc[r, ty * 3 + 2] = PI if j < half else 0.0
    out["ropec"] = rc
    out["ident"] = np.eye(128, dtype=f)
    wuq = np.asarray(inp["mla_w_uq"], f)[0]
    pm = _rot_perm(64, 32, 64)
    wq = np.empty((8, 128, 4, 256), f)
    for h in range(8):
        wq[h, :, :, 0:128] = _pk(wuq[:, h, 0:128], 128)
        wq[h, :, :, 128:192] = _pk(wuq[:, h, 128:192], 64)
        wq[h, :, :, 192:256] = _pk(wuq[:, h, 128:192][:, pm], 64)
    out["wq"] = wq
    out["gq"] = np.ascontiguousarray(np.asarray(inp["mla_q_norm_g"], f)[0].reshape(4, 128).T)
    out["gkv"] = np.ascontiguousarray(np.asarray(inp["mla_kv_norm_g"], f)[0].reshape(2, 128).T)
    out["wuk"] = _pk(np.asarray(inp["mla_w_uk"], f)[0].reshape(256, 1024), 1024)
    out["wuv"] = _pk(np.asarray(inp["mla_w_uv"], f)[0].reshape(256, 1024), 1024)
    out["wa"] = _pk(np.asarray(inp["w_branch_a"], f)[0], 2048)
    out["wb"] = _pk(np.asarray(inp["w_branch_b"], f)[0], 2048)
    out["wo"] = _pk(np.asarray(inp["w_out"], f)[0], 2048)
    out["wpq"] = _pk(np.asarray(inp["peer_w_q"], f)[0].reshape(2048, 1024), 1024)
    sk = np.asarray(inp["peer_sub_keys"], f)[0]
    kb = np.zeros((128, 8, 256), f)
    for h in range(8):
        for p in range(2):
            kb[p * 64:(p + 1) * 64, h, p * 128:(p + 1) * 128] = sk[h, p].T
    out["kb"] = kb
    out["gffn"] = np.asarray(inp["norm_ffn_g"], f)[0]
    out["gfin"] = np.asarray(inp["norm_final_g"], f)
    out["put"] = np.ascontiguousarray(np.asarray(inp["peer_u"], f)[0].reshape(128, 128, 16, 128).transpose(0, 3, 2, 1)).reshape(16384, 2048)
    out["gffnT"] = np.ascontiguousarray(np.asarray(inp["norm_ffn_g"], f)[0].reshape(16, 128).T)
    out["iota128"] = np.tile(np.arange(128, dtype=f)[None, :], (128, 1))
    out["pv"] = np.asarray(inp["peer_v"], f)[0]
    out["iota16"] = np.tile(np.arange(16, dtype=f)[None, :], (128, 1))
    return out


SHAPES = {
    "x": ([S, D], F32), "pos": ([S], I32),
    "w1f": ([20, 128, 16, 256], F32), "w1t": ([10, 128, 16, 512], F32), "w1iw": ([128, 16, 16], F32),
    "gmixT": ([128, 16], F32), "ropec": ([128, 9], F32), "ident": ([128, 128], F32),
    "wq": ([8, 128, 4, 256], F32), "gq": ([128, 4], F32), "gkv": ([128, 2], F32),
    "wuk": ([128, 2, 1024], F32), "wuv": ([128, 2, 1024], F32),
    "wa": ([128, 8, 2048], F32), "wb": ([128, 8, 2048], F32), "wo": ([128, 16, 2048], F32),
    "wpq": ([128, 16, 1024], F32), "kb": ([128, 8, 256], F32), "gffn": ([D], F32), "gfin": ([D], F32),
    "put": ([16384, D], F32), "pv": ([16384, D], F32), "iota16": ([128, 16], F32),
    "gffnT": ([128, 16], F32), "iota128": ([128, 128], F32),
}


def build_nc(phases=("p0", "p1", "p2", "p3", "p4", "p5"), debug=()):
    nc = bass.Bass("TRN2", target_bir_lowering=False)
    IN = {k: nc.dram_tensor(k, sh, dt, kind="ExternalInput").ap() for k, (sh, dt) in SHAPES.items()}
    y = nc.dram_tensor("y", [S, D], F32, kind="ExternalOutput").ap()

    def scratch(name, shape, dt):
        kind = "ExternalOutput" if name in debug else "Internal"
        return nc.dram_tensor(name, shape, dt, kind=kind).ap()

    SC = dict(
        dqT=scratch("dqT", [8, 128, S], BF16), dkT=scratch("dkT", [2, 128, S], BF16),
        iqT=scratch("iqT", [8, 128, S], BF16), ikT=scratch("ikT", [128, S], BF16),
        kpeT=scratch("kpeT", [128, S], BF16), cqnT=scratch("cqnT", [128, 4, S], BF16),
        ckvnT=scratch("ckvnT", [128, 2, S], BF16), dv=scratch("dv", [S, 256], BF16),
        iw=scratch("iw", [S, 16], F32), gs=scratch("gs", [S, 4096], BF16),
        oaT=scratch("oaT", [8, 128, S], BF16), obT=scratch("obT", [8, 128, S], BF16),
        x2=scratch("x2", [S, D], F32),
        pub=scratch("pub", [16384, D], BF16), pvb=scratch("pvb", [16384, D], BF16),
    )

    with ExitStack() as gst:
        def gsb(name, shape, dt):
            return gst.enter_context(nc.sbuf_tensor(name, shape, dt))

        SEMS = dict(stack=gst, esem={}, dsem={}, ebase={}, dbase={})
        identb = gsb("identb", [128, 128], BF16)
        identf = gsb("identf", [128, 128], F32)
        onesb = gsb("onesb", [128, 128], BF16)
        trig_cm = nc.sbuf_tensor("trig", [128, 6, S], BF16)
        trig = trig_cm.__enter__()

        if "p0" in phases:
            with ExitStack() as st:
                sb = lambda n, s, d, _p="q1_": st.enter_context(nc.sbuf_tensor(_p + n, s, d))
                P = Prog(nc, SEMS)
                posi = sb("posi", [128, S], I32)
                posf = sb("posf", [128, S], F32)
                ang = sb("ang", [128, S], F32)
                kk = sb("kk", [128, S], F32)
                rc = sb("rc", [128, 9], F32)
                P.op("sp", "dma_start", [], ["identf"], out=identf[:], in_=IN["ident"])
                P.op("sp", "dma_start", [], ["rc"], out=rc[:], in_=IN["ropec"])
                P.op("sp", "dma_start", [], ["posi"], out=posi[:], in_=IN["pos"].partition_broadcast(128))
                P.op("dve", "tensor_copy", ["identf"], ["identb"], out=identb[:], in_=identf[:])
                P.op("pool", "memset", [], ["onesb"], ap=onesb[:], constant=1.0)
                P.op("dve", "tensor_copy", ["posi"], ["posf"], out=posf[:], in_=posi[:])
                for ty in range(3):
                    for cs in range(2):
                        P.op("dve", "tensor_scalar", ["posf", "rc"], ["ang"], out=ang[:], in0=posf[:],
                             scalar1=rc[:, ty * 3:ty * 3 + 1], scalar2=rc[:, ty * 3 + 1 + cs:ty * 3 + 2 + cs],
                             op0=ALU.mult, op1=ALU.add)
                        P.op("dve", "tensor_scalar", ["ang"], ["kk"], out=kk[:], in0=ang[:], scalar1=1.0 / (2 * PI),
                             scalar2=MAGIC, op0=ALU.mult, op1=ALU.add)
                        P.op("dve", "tensor_scalar", ["kk"], ["kk"], out=kk[:], in0=kk[:], scalar1=-MAGIC, scalar2=None,
                             op0=ALU.add)
                        P.op("dve", "scalar_tensor_tensor", ["kk", "ang"], ["kk"], out=kk[:], in0=kk[:], scalar=-2 * PI,
                             in1=ang[:], op0=ALU.mult, op1=ALU.add)
                        P.op("dve", "tensor_scalar", ["kk"], ["kk"], out=kk[:], in0=kk[:], scalar1=-PI, scalar2=PI,
                             op0=ALU.max, op1=ALU.min)
                        P.op("act", "activation", ["kk"], ["trig%d" % (ty * 2 + cs)], out=trig[:, ty * 2 + cs, :],
                             in_=kk[:], func=AF.Sin)
                P.emit(st)

        if "p1" in phases:
            with ExitStack() as st:
                sb = lambda n, s, d, _p="q2_": st.enter_context(nc.sbuf_tensor(_p + n, s, d))
                psb = lambda n, s, d, _p="q4_": st.enter_context(nc.psum_tensor(_p + n, s, d))
                P = Prog(nc, SEMS)
                hT = sb("hT", [128, 16, S], BF16)
                xt = [sb("xt%d" % i, [128, D], F32) for i in range(2)]
                xs = [sb("xs%d" % i, [128, D], BF16) for i in range(2)]
                junk = sb("junk", [128, D], BF16)
                ss = sb("ss", [128, 16], F32)
                sq = sb("sq", [128, 16], F32)
                rstd = sb("rstd", [128, 16], F32)
                gmix = sb("gmix", [128, 16], F32)
                wbuf = [sb("wbuf%d" % i, [128, 16, 512], BF16) for i in range(2)]
                ost = [sb("ost%d" % i, [128, 16, 512], BF16) for i in range(2)]
                tmp = [sb("tmp%d" % i, [128, 512], F32) for i in range(4)]
                cqs = sb("cqs", [128, 16], F32)
                cqn = [sb("cqn%d" % i, [128, 512], BF16) for i in range(2)]
                iwst = sb("iwst", [128, 16, 16], F32)
                wiw = sb("wiw", [128, 16, 16], BF16)
                ptr = [psb("ptr%d" % i, [128, 8, 128], BF16) for i in range(2)]
                pA = [psb("pA%d" % i, [128, 512], F32) for i in range(3)]
                pB = [psb("pB%d" % i, [128, 512], F32) for i in range(3)]
                P.op("sp", "dma_start", [], ["gmix"], out=gmix[:], in_=IN["gmixT"])
                for T in range(NT):
                    s = T % 2
                    tsl = slice(T * 128, (T + 1) * 128)
                    P.op("sp", "dma_start", [], ["xt%d" % s], out=xt[s][:], in_=IN["x"][tsl, :])
                    P.op("act", "activation", ["xt%d" % s], ["junk", "ss%d" % T], out=junk[:], in_=xt[s][:], func=AF.Square,
                         accum_out=ss[:, T:T + 1])
                    P.op("act", "activation", ["ss%d" % T], ["sq%d" % T], out=sq[:, T:T + 1], in_=ss[:, T:T + 1], func=AF.Sqrt,
                         scale=1.0 / D, bias=EPS)
                    P.op("dve", "reciprocal", ["sq%d" % T], ["rstd%d" % T], out=rstd[:, T:T + 1], in_=sq[:, T:T + 1])
                    P.op("act", "activation", ["xt%d" % s, "rstd%d" % T], ["xs%d" % s], out=xs[s][:], in_=xt[s][:], func=AF.Copy,
                         scale=rstd[:, T:T + 1])
                    for dk in range(16):
                        b = dk // 8
                        P.op("pe", "transpose", ["xs%d" % s, "identb"], ["ptr%d" % b], out=ptr[b][:, dk % 8, :],
                             in_=xs[s][:, dk * 128:(dk + 1) * 128], identity=identb[:])
                    for dk in range(16):
                        b = dk // 8
                        if dk % 2 == 0:
                            P.op("act", "activation", ["ptr%d" % b, "gmix"], ["hT%d" % T], out=hT[:, dk, tsl], in_=ptr[b][:, dk % 8, :],
                                 func=AF.Copy, scale=gmix[:, dk:dk + 1])
                        else:
                            P.op("dve", "tensor_scalar", ["ptr%d" % b, "gmix"], ["hT%d" % T], out=hT[:, dk, tsl], in0=ptr[b][:, dk % 8, :],
                                 scalar1=gmix[:, dk:dk + 1], scalar2=None, op0=ALU.mult)
                hT_all = ["hT%d" % T for T in range(NT)]
                dests = [SC["dqT"][h] for h in range(8)] + [SC["dkT"][g] for g in range(2)] + [SC["iqT"][h] for h in range(8)] + [SC["ikT"], SC["kpeT"]]
                types = [0] * 10 + [1] * 9 + [2]
                nblk = 0
                for bi in range(20):
                    ws = nblk % 2
                    osl = nblk % 2
                    nblk += 1
                    ty = types[bi]
                    if bi == 0:
                        P.op("pool", "dma_start", [], ["wbuf%d" % ws], out=wbuf[ws][:, :, 0:256], in_=IN["w1f"][bi])
                    if bi + 1 < 20:
                        P.op("pool", "dma_start", [], ["wbuf%d" % (1 - ws)], out=wbuf[1 - ws][:, :, 0:256], in_=IN["w1f"][bi + 1])
                    else:
                        P.op("pool", "dma_start", [], ["wbuf%d" % (1 - ws)], out=wbuf[1 - ws][:], in_=IN["w1t"][0])
                    for tg in range(4):
                        csl = slice(tg * 512, (tg + 1) * 512)
                        pi = (bi * 4 + tg) % 3
                        for dk in range(16):
                            P.op("pe", "matmul", ["wbuf%d" % ws] + hT_all[tg * 4:tg * 4 + 4], ["pA%d" % pi], out=pA[pi][:],
                                 lhsT=wbuf[ws][:, dk, 0:128], rhs=hT[:, dk, csl], start=(dk == 0), stop=(dk == 15))
                        for dk in range(16):
                            P.op("pe", "matmul", ["wbuf%d" % ws] + hT_all[tg * 4:tg * 4 + 4], ["pB%d" % pi], out=pB[pi][:],
                                 lhsT=wbuf[ws][:, dk, 128:256], rhs=hT[:, dk, csl], start=(dk == 0), stop=(dk == 15))
                        ti = (bi * 4 + tg) % 2
                        P.op("dve", "tensor_tensor", ["pA%d" % pi, "trig"], ["tmp%d" % (2 * ti)], out=tmp[2 * ti][:], in0=pA[pi][:],
                             in1=trig[:, ty * 2, csl], op=ALU.mult)
                        P.op("dve", "tensor_tensor", ["pB%d" % pi, "trig"], ["tmp%d" % (2 * ti + 1)], out=tmp[2 * ti + 1][:], in0=pB[pi][:],
                             in1=trig[:, ty * 2 + 1, csl], op=ALU.mult)
                        P.op("pool", "tensor_tensor", ["tmp%d" % (2 * ti), "tmp%d" % (2 * ti + 1)], ["ost%d" % osl],
                             out=ost[osl][:].rearrange("p a b -> p (a b)")[:, csl], in0=tmp[2 * ti][:], in1=tmp[2 * ti + 1][:], op=ALU.add)
                    P.op("sp", "dma_start", ["ost%d" % osl], [], out=dests[bi], in_=ost[osl][:].rearrange("p a b -> p (a b)")[:, 0:S])
                for bi in range(10):
                    ws = nblk % 2
                    osl = nblk % 2
                    nblk += 1
                    if bi + 1 < 10:
                        P.op("pool", "dma_start", [], ["wbuf%d" % (1 - ws)], out=wbuf[1 - ws][:], in_=IN["w1t"][bi + 1])
                    for T in range(NT):
                        tsl = slice(T * 128, (T + 1) * 128)
                        pi = T % 3
                        for dk in range(16):
                            P.op("pe", "matmul", ["wbuf%d" % ws, "hT%d" % T], ["pA%d" % pi], out=pA[pi][:], lhsT=hT[:, dk, tsl],
                                 rhs=wbuf[ws][:, dk, :], start=(dk == 0), stop=(dk == 15))
                        if bi >= 2:
                            P.op("act", "activation", ["pA%d" % pi], ["ost%d" % osl], out=ost[osl][:, T, :], in_=pA[pi][:], func=AF.Sigmoid)
                        else:
                            ncq = 512 if bi == 0 else 256
                            c = T % 2
                            P.op("act", "activation", ["pA%d" % pi], ["tmp0", "cqs"], out=tmp[0][:, 0:ncq], in_=pA[pi][:, 0:ncq], func=AF.Square,
                                 accum_out=cqs[:, 0:1])
                            P.op("act", "activation", ["cqs"], ["cqs1"], out=cqs[:, 1:2], in_=cqs[:, 0:1], func=AF.Sqrt, scale=1.0 / ncq, bias=EPS)
                            P.op("dve", "reciprocal", ["cqs1"], ["cqs2"], out=cqs[:, 2:3], in_=cqs[:, 1:2])
                            P.op("dve", "tensor_scalar", ["pA%d" % pi, "cqs2"], ["cqn%d" % c], out=cqn[c][:, 0:ncq], in0=pA[pi][:, 0:ncq],
                                 scalar1=cqs[:, 2:3], scalar2=None, op0=ALU.mult)
                            if bi == 1:
                                P.op("act", "activation", ["pA%d" % pi], ["ost%d" % osl], out=ost[osl][:, T, 0:256], in_=pA[pi][:, 256:512], func=AF.Copy)
                            nk = ncq // 128
                            for kc in range(nk):
                                P.op("pe", "transpose", ["cqn%d" % c, "identb"], ["ptr%d" % c], out=ptr[c][:, kc, :],
                                     in_=cqn[c][:, kc * 128:(kc + 1) * 128], identity=identb[:])
                            lo = 0 if bi == 0 else 256
                            P.op("act", "activation", ["ptr%d" % c], ["ost%d" % osl], out=ost[osl][:, T, lo:lo + ncq].rearrange("p (k q) -> p k q", q=128),
                                 in_=ptr[c][:, 0:nk, :], func=AF.Copy)
                    if bi >= 2:
                        P.op("sp", "dma_start", ["ost%d" % osl], [], out=SC["gs"][:, (bi - 2) * 512:(bi - 1) * 512].rearrange("(t p) c -> p t c", p=128),
                             in_=ost[osl][:])
                    else:
                        nk = 4 if bi == 0 else 2
                        lo = 0 if bi == 0 else 256
                        dst = SC["cqnT"] if bi == 0 else SC["ckvnT"]
                        for kc in range(nk):
                            P.op("sp", "dma_start", ["ost%d" % osl], [], out=dst[:, kc, :].rearrange("p (t q) -> p t q", q=128),
                                 in_=ost[osl][:, :, lo + kc * 128:lo + (kc + 1) * 128])
                        if bi == 1:
                            P.op("sp", "dma_start", ["ost%d" % osl], [], out=SC["dv"].rearrange("(t p) c -> p t c", p=128), in_=ost[osl][:, :, 0:256])
                P.op("pool", "dma_start", [], ["wiw"], out=wiw[:], in_=IN["w1iw"])
                for T in range(NT):
                    tsl = slice(T * 128, (T + 1) * 128)
                    pi = T % 3
                    for dk in range(16):
                        P.op("pe", "matmul", ["wiw", "hT%d" % T], ["pB%d" % pi], out=pB[pi][:, 0:16], lhsT=hT[:, dk, tsl], rhs=wiw[:, dk, :],
                             start=(dk == 0), stop=(dk == 15))
                    P.op("act", "activation", ["pB%d" % pi], ["iwst"], out=iwst[:, T, :], in_=pB[pi][:, 0:16], func=AF.Copy)
                P.op("sp", "dma_start", ["iwst"], [], out=SC["iw"].rearrange("(t p) c -> p t c", p=128), in_=iwst[:])
                P.emit(st)

        if "p2" in phases:
            with ExitStack() as st:
                sb = lambda n, s, d, _p="q3_": st.enter_context(nc.sbuf_tensor(_p + n, s, d))
                psb = lambda n, s, d, _p="q5_": st.enter_context(nc.psum_tensor(_p + n, s, d))
                P = Prog(nc, SEMS)
                cq = sb("cq_s", [128, 4, S], BF16)
                ckv = sb("ckv_s", [128, 2, S], BF16)
                kpe = sb("kpe_s", [128, S], BF16)
                wq = [sb("wq%d" % i, [128, 4, 256], BF16) for i in range(2)]
                gq = sb("gq", [128, 4], F32)
                gkv = sb("gkv", [128, 2], F32)
                wuk = sb("wuk", [128, 2, 1024], BF16)
                wuv = sb("wuv", [128, 2, 1024], BF16)
                vall = sb("vall", [128, 16, 1024], BF16)
                qn = [sb("qn%d" % i, [128, S], BF16) for i in range(2)]
                qr = [sb("qr%d" % i, [128, S], BF16) for i in range(2)]
                kn = [sb("kn%d" % i, [128, S], BF16) for i in range(2)]
                pT = [sb("pT%d" % i, [128, 512], BF16) for i in range(3)]
                rden = sb("rden", [128, 512], F32)
                ost = [sb("oast%d" % i, [128, S], BF16) for i in range(2)]
                t1 = sb("t1", [128, 512], F32)
                t2 = sb("t2", [128, 512], F32)
                pp = [psb("pp%d" % i, [128, 512], F32) for i in range(4)]
                pS = [psb("pS%d" % i, [128, 512], F32) for i in range(2)]
                pO = psb("pO", [128, 512], F32)
                pD = psb("pD", [128, 512], F32)
                P.op("sp", "dma_start", [], ["cq"], out=cq[:], in_=SC["cqnT"])
                P.op("sp", "dma_start", [], ["ckv"], out=ckv[:], in_=SC["ckvnT"])
                P.op("sp", "dma_start", [], ["kpe"], out=kpe[:], in_=SC["kpeT"])
                P.op("sp", "dma_start", [], ["gq"], out=gq[:], in_=IN["gq"])
                P.op("sp", "dma_start", [], ["gkv"], out=gkv[:], in_=IN["gkv"])
                for kc in range(2):
                    P.op("pool", "dma_start", [], ["wuk"], out=wuk[:, kc, :], in_=IN["wuk"][:, kc, :])
                    P.op("pool", "dma_start", [], ["wuv"], out=wuv[:, kc, :], in_=IN["wuv"][:, kc, :])
                for kc in range(2):
                    P.op("dve", "tensor_scalar", ["wuk", "gkv"], ["wuk"], out=wuk[:, kc, :], in0=wuk[:, kc, :], scalar1=gkv[:, kc:kc + 1], scalar2=None, op0=ALU.mult)
                    P.op("dve", "tensor_scalar", ["wuv", "gkv"], ["wuv"], out=wuv[:, kc, :], in0=wuv[:, kc, :], scalar1=gkv[:, kc:kc + 1], scalar2=None, op0=ALU.mult)
                n = 0
                for kt in range(16):
                    ksl = slice(kt * 128, (kt + 1) * 128)
                    for hf in range(2):
                        pi = n % 4
                        n += 1
                        for kc in range(2):
                            P.op("pe", "matmul", ["ckv", "wuv"], ["pp%d" % pi], out=pp[pi][:], lhsT=ckv[:, kc, ksl], rhs=wuv[:, kc, hf * 512:(hf + 1) * 512],
                                 start=(kc == 0), stop=(kc == 1))
                        P.op("act", "activation", ["pp%d" % pi], ["vall"], out=vall[:, kt, hf * 512:(hf + 1) * 512], in_=pp[pi][:], func=AF.Copy)
                sc_mla = float(192 ** -0.5)
                def prep(h):
                    s = h % 2
                    yield P.op("pool", "dma_start", [], ["wq%d" % s], out=wq[s][:], in_=IN["wq"][h])
                    for kc in range(4):
                        yield P.op("dve", "tensor_scalar", ["wq%d" % s, "gq"], ["wq%d" % s], out=wq[s][:, kc, :], in0=wq[s][:, kc, :], scalar1=gq[:, kc:kc + 1], scalar2=None, op0=ALU.mult)
                    for tg in range(4):
                        csl = slice(tg * 512, (tg + 1) * 512)
                        for kc in range(4):
                            yield P.op("pe", "matmul", ["wq%d" % s, "cq"], ["pp0"], out=pp[0][:], lhsT=wq[s][:, kc, 0:128], rhs=cq[:, kc, csl], start=(kc == 0), stop=(kc == 3))
                        yield P.op("act", "activation", ["pp0"], ["qn%d" % s], out=qn[s][:, csl], in_=pp[0][:], func=AF.Copy)
                        for kc in range(4):
                            yield P.op("pe", "matmul", ["wq%d" % s, "cq"], ["pp1"], out=pp[1][0:64, :], lhsT=wq[s][:, kc, 128:192], rhs=cq[:, kc, csl], start=(kc == 0), stop=(kc == 3))
                        for kc in range(4):
                            yield P.op("pe", "matmul", ["wq%d" % s, "cq"], ["pp2"], out=pp[2][0:64, :], lhsT=wq[s][:, kc, 192:256], rhs=cq[:, kc, csl], start=(kc == 0), stop=(kc == 3))
                        yield P.op("dve", "tensor_tensor", ["pp1", "trig"], ["t1"], out=t1[0:64, :], in0=pp[1][0:64, :], in1=trig[0:64, 4, csl], op=ALU.mult)
                        yield P.op("dve", "tensor_tensor", ["pp2", "trig"], ["t2"], out=t2[0:64, :], in0=pp[2][0:64, :], in1=trig[0:64, 5, csl], op=ALU.mult)
                        yield P.op("pool", "tensor_tensor", ["t1", "t2"], ["qr%d" % s], out=qr[s][0:64, csl], in0=t1[0:64, :], in1=t2[0:64, :], op=ALU.add)
                        for kc in range(2):
                            yield P.op("pe", "matmul", ["wuk", "ckv"], ["pp3"], out=pp[3][:], lhsT=wuk[:, kc, h * 128:(h + 1) * 128], rhs=ckv[:, kc, csl], start=(kc == 0), stop=(kc == 1))
                        yield P.op("act", "activation", ["pp3"], ["kn%d" % s], out=kn[s][:, csl], in_=pp[3][:], func=AF.Copy)

                def att(h, step):
                    s = h % 2
                    cnt = 0
                    for qg in range(4):
                        nkt = 4 * (qg + 1)
                        for kt in range(nkt):
                            j = kt - 4 * qg
                            c0 = 128 * j if j > 0 else 0
                            cols = slice(qg * 512 + c0, (qg + 1) * 512)
                            ksl = slice(kt * 128, (kt + 1) * 128)
                            a = cnt % 2
                            k = cnt % 3
                            cnt += 1
                            P.op("pe", "matmul", ["kn%d" % s, "qn%d" % s], ["pS%d" % a], out=pS[a][:, c0:512], lhsT=kn[s][:, ksl], rhs=qn[s][:, cols], start=True, stop=False)
                            P.op("pe", "matmul", ["kpe", "qr%d" % s], ["pS%d" % a], out=pS[a][:, c0:512], lhsT=kpe[0:64, ksl], rhs=qr[s][0:64, cols], start=False, stop=True)
                            P.op("act", "activation", ["pS%d" % a], ["pT%d" % k], out=pT[k][:, c0:512], in_=pS[a][:, c0:512], func=AF.Exp, scale=sc_mla)
                            if j >= 0:
                                P.op("pool", "memset", [], ["pT%d" % k], ap=pT[k][64:128, c0:c0 + 64], constant=0.0)
                            P.op("pe", "matmul", ["vall", "pT%d" % k], ["pO"], out=pO[:, c0:512], lhsT=vall[:, kt, h * 128:(h + 1) * 128], rhs=pT[k][:, c0:512],
                                 start=(kt == 0), stop=(kt == nkt - 1))
                            P.op("pe", "matmul", ["onesb", "pT%d" % k], ["pD"], out=pD[:, c0:512], lhsT=onesb[:], rhs=pT[k][:, c0:512],
                                 start=(kt == 0), stop=(kt == nkt - 1))
                            step(3)
                        P.op("dve", "reciprocal", ["pD"], ["rden"], out=rden[:], in_=pD[:])
                        P.op("dve", "tensor_tensor", ["pO", "rden"], ["oast%d" % s], out=ost[s][:, qg * 512:(qg + 1) * 512], in0=pO[:], in1=rden[:], op=ALU.mult)
                    P.op("sp", "dma_start", ["oast%d" % s], [], out=SC["oaT"][h], in_=ost[s][:])

                for _ in prep(0):
                    pass
                for h in range(8):
                    nxt = prep(h + 1) if h + 1 < 8 else iter(())

                    def step(n, nxt=nxt):
                        for _ in range(n):
                            next(nxt, None)

                    att(h, step)
                    for _ in nxt:
                        pass
                P.emit(st)

        trig_cm.__exit__(None, None, None)

        if "p3" in phases:
            with ExitStack() as st:
                sb = lambda n, s, d, _p="p3_": st.enter_context(nc.sbuf_tensor(_p + n, s, d))
                psb = lambda n, s, d, _p="p3_": st.enter_context(nc.psum_tensor(_p + n, s, d))
                P = Prog(nc, SEMS)
                iqT = sb("iqT", [128, 8, S], BF16)
                ikT = sb("ikT", [128, S], BF16)
                iw = sb("iw", [128, 16, 16], F32)
                dqT = sb("dqT", [128, 8, S], BF16)
                dkT = sb("dkT", [128, 2, S], BF16)
                dvs = sb("dvs", [128, 16, 256], BF16)
                origs = [sb("orig%d" % i, [128, S], F32) for i in range(4)]
                works = [sb("work%d" % i, [128, S], F32) for i in range(2)]
                masks = [sb("mask%d" % i, [128, S], BF16) for i in range(2)]
                mxs = [sb("mx%d" % i, [128, 8], F32) for i in range(2)]
                MTs = [sb("MT%d" % i, [128, 16, 512], BF16) for i in range(2)]
                diag = [sb("diag%d" % i, [128, 16, 128], BF16) for i in range(2)]
                Rb = [sb("R%d" % i, [128, 512], BF16) for i in range(4)]
                pT = [sb("pT%d" % i, [128, 512], BF16) for i in range(3)]
                rden = sb("rden", [128, 512], F32)
                obst = [sb("obst%d" % i, [128, 8, 512], BF16) for i in range(2)]
                pDs = [psb("pDs%d" % i, [128, 512], F32) for i in range(2)]
                pIS = psb("pIS", [128, 512], F32)
                ptr = psb("ptr", [128, 8, 128], BF16)
                pS = [psb("pS%d" % i, [128, 512], F32) for i in range(2)]
                pO = psb("pO", [128, 512], F32)
                pD = psb("pD", [128, 512], F32)
                for h in range(8):
                    P.op("sp", "dma_start", [], ["iqT"], out=iqT[:, h, :], in_=SC["iqT"][h])
                    P.op("sp", "dma_start", [], ["dqT"], out=dqT[:, h, :], in_=SC["dqT"][h])
                for g in range(2):
                    P.op("sp", "dma_start", [], ["dkT"], out=dkT[:, g, :], in_=SC["dkT"][g])
                P.op("sp", "dma_start", [], ["ikT"], out=ikT[:], in_=SC["ikT"])
                P.op("sp", "dma_start", [], ["iw"], out=iw[:], in_=SC["iw"].rearrange("(t p) c -> p t c", p=128))
                P.op("sp", "dma_start", [], ["dvs"], out=dvs[:], in_=SC["dv"].rearrange("(t p) c -> p t c", p=128))
                for (src_, dst_) in ((IN["put"], SC["pub"]), (IN["pv"], SC["pvb"])):
                    for r0 in range(0, 16384, 1024):
                        P.op("pool", "dma_start", ["iqT", "dqT", "dkT", "ikT", "iw", "dvs"], [], out=dst_[r0:r0 + 1024, :].rearrange("r (a b) -> (r a) b", b=1024),
                             in_=src_[r0:r0 + 1024, :].rearrange("r (a b) -> (r a) b", b=1024))
                sc_idx = float(64 ** -0.5 * 16 ** -0.5)
                sc_dsa = float(128 ** -0.5)
                nR = 0
                nD = 0
                cnt = 0
                def stage_IS(g):
                    qg = g // 2
                    st_ = dict(nD=0)
                    for T in (2 * g, 2 * g + 1):
                        i = T % 4
                        tsl = slice(T * 128, (T + 1) * 128)
                        nk = 128 * (T + 1)
                        dd = T % 2
                        orig = origs[i]
                        P.op("dve", "tensor_tensor", ["identf", "iw"], ["diag%d" % dd], out=diag[dd][:],
                             in0=identf[:].unsqueeze(1).to_broadcast([128, 16, 128]), in1=iw[:, T, :].unsqueeze(2).to_broadcast([128, 16, 128]), op=ALU.mult)
                        for kg in range((nk + 511) // 512):
                            ncol = min(512, nk - kg * 512)
                            ksl = slice(kg * 512, kg * 512 + ncol)
                            for h in range(16):
                                rows = slice((h % 2) * 64, (h % 2) * 64 + 64)
                                a = CN["nD"] % 2
                                CN["nD"] += 1
                                r = CN["nR"] % 4
                                CN["nR"] += 1
                                P.op("pe", "matmul", ["iqT", "ikT"], ["pDs%d" % a], out=pDs[a][:, 0:ncol], lhsT=iqT[rows, h // 2, tsl], rhs=ikT[rows, ksl], start=True, stop=True)
                                P.op("act", "activation", ["pDs%d" % a], ["R%d" % r], out=Rb[r][:, 0:ncol], in_=pDs[a][:, 0:ncol], func=AF.Relu, scale=sc_idx)
                                P.op("pe", "matmul", ["diag%d" % dd, "R%d" % r], ["pIS"], out=pIS[:, 0:ncol], lhsT=diag[dd][:, h, :], rhs=Rb[r][:, 0:ncol],
                                     start=(h == 0), stop=(h == 15))
                            P.op("act", "activation", ["pIS"], ["orig%d" % i], out=orig[:, ksl], in_=pIS[:, 0:ncol], func=AF.Copy)
                        P.op("pool", "memset", [], ["orig%d" % i], ap=orig[0:64, nk - 64:nk], constant=NEG)

                def stage_topk(g, att_qg=None):
                    tiles = (2 * g, 2 * g + 1)
                    heads_done = 0
                    if tiles[0] >= 2:
                        for rnd in range(32):
                            for T in tiles:
                                i, dd, nk = T % 4, T % 2, 128 * (T + 1)
                                src = origs[i] if rnd == 0 else works[dd]
                                sname = ("orig%d" % i) if rnd == 0 else ("work%d" % dd)
                                P.op("dve", "max", [sname], ["mx%d" % dd], out=mxs[dd][:], in_=src[:, 0:nk])
                            for T in tiles:
                                i, dd, nk = T % 4, T % 2, 128 * (T + 1)
                                src = origs[i] if rnd == 0 else works[dd]
                                sname = ("orig%d" % i) if rnd == 0 else ("work%d" % dd)
                                P.op("dve", "match_replace", ["mx%d" % dd, sname], ["work%d" % dd], out=works[dd][:, 0:nk], in_to_replace=mxs[dd][:],
                                     in_values=src[:, 0:nk], imm_value=NEG)
                            if att_qg is not None and rnd % 4 == 3:
                                stage_att_head(att_qg, heads_done)
                                heads_done += 1
                    if att_qg is not None:
                        while heads_done < 8:
                            stage_att_head(att_qg, heads_done)
                            heads_done += 1
                    for T in tiles:
                        i, dd, nk = T % 4, T % 2, 128 * (T + 1)
                        mask = masks[dd]
                        if T >= 2:
                            P.op("dve", "tensor_tensor", ["work%d" % dd, "orig%d" % i], ["mask%d" % dd], out=mask[:, 0:nk], in0=works[dd][:, 0:nk], in1=origs[i][:, 0:nk], op=ALU.not_equal)
                        else:
                            P.op("dve", "tensor_scalar", ["orig%d" % i], ["mask%d" % dd], out=mask[:, 0:nk], in0=origs[i][:, 0:nk], scalar1=NEG / 2, scalar2=None, op0=ALU.is_gt)

                def stage_maskT(g, first):
                    mp = (g // 2) % 2
                    MT = MTs[mp]
                    if first:
                        P.op("pool", "memset", [], ["MT%d" % mp], ap=MT[:], constant=0.0)
                    for T in (2 * g, 2 * g + 1):
                        i, dd = T % 4, T % 2
                        mask = masks[dd]
                        for k0 in range(0, T + 1, 8):
                            n8 = min(8, T + 1 - k0)
                            for kt in range(k0, k0 + n8):
                                P.op("pe", "transpose", ["mask%d" % dd, "identb"], ["ptr"], out=ptr[:, kt - k0, :], in_=mask[:, kt * 128:(kt + 1) * 128], identity=identb[:])
                            P.op("act", "activation", ["ptr"], ["MT%d" % mp], out=MT[:, k0:k0 + n8, i * 128:(i + 1) * 128], in_=ptr[:, 0:n8, :], func=AF.Copy)

                def stage_att_head(qg, h):
                    os_ = qg % 2
                    g = h // 4
                    nkt = 4 * (qg + 1)
                    for kt in range(nkt):
                        ksl = slice(kt * 128, (kt + 1) * 128)
                        a = CN["cnt"] % 2
                        k = CN["cnt"] % 3
                        CN["cnt"] += 1
                        P.op("pe", "matmul", ["dkT", "dqT"], ["pS%d" % a], out=pS[a][:], lhsT=dkT[:, g, ksl], rhs=dqT[:, h, qg * 512:(qg + 1) * 512], start=True, stop=True)
                        P.op("act", "activation", ["pS%d" % a], ["pT%d" % k], out=pT[k][:], in_=pS[a][:], func=AF.Exp, scale=sc_dsa)
                        P.op("pool", "tensor_tensor", ["pT%d" % k, "MT%d" % os_], ["pT%d" % k], out=pT[k][:], in0=pT[k][:], in1=MTs[os_][:, kt, :], op=ALU.mult)
                        P.op("pe", "matmul", ["dvs", "pT%d" % k], ["pO"], out=pO[:], lhsT=dvs[:, kt, g * 128:(g + 1) * 128], rhs=pT[k][:], start=(kt == 0), stop=(kt == nkt - 1))
                        P.op("pe", "matmul", ["onesb", "pT%d" % k], ["pD"], out=pD[:], lhsT=onesb[:], rhs=pT[k][:], start=(kt == 0), stop=(kt == nkt - 1))
                    P.op("dve", "reciprocal", ["pD"], ["rden"], out=rden[:], in_=pD[:])
                    P.op("dve", "tensor_tensor", ["pO", "rden"], ["obst%d" % os_], out=obst[os_][:, h, :], in0=pO[:], in1=rden[:], op=ALU.mult)
                    if h == 7:
                        P.op("sp", "dma_start", ["obst%d" % os_], [], out=SC["obT"][:, :, qg * 512:(qg + 1) * 512].rearrange("h p s -> p h s"), in_=obst[os_][:])

                CN = dict(nD=0, nR=0, cnt=0)
                seq = [7, 6, 5, 4, 3, 2, 1, 0]
                stage_IS(seq[0])
                pending = None
                for idx, g in enumerate(seq):
                    if idx + 1 < len(seq):
                        stage_IS(seq[idx + 1])
                    stage_topk(g, att_qg=pending)
                    pending = None
                    stage_maskT(g, first=(g % 2 == 1))
                    if g % 2 == 0:
                        pending = g // 2
                for h in range(8):
                    stage_att_head(pending, h)
                P.emit(st)

        if "p4" in phases:
            with ExitStack() as st:
                sb = lambda n, s, d, _p="p4_": st.enter_context(nc.sbuf_tensor(_p + n, s, d))
                psb = lambda n, s, d, _p="p4_": st.enter_context(nc.psum_tensor(_p + n, s, d))
                P = Prog(nc, SEMS)
                wa = sb("wa", [128, 8, D], BF16)
                wb = sb("wb", [128, 8, D], BF16)
                wo = sb("wo", [128, 16, D], BF16)
                oat = [sb("oat%d" % i, [128, 8, 128], BF16) for i in range(2)]
                obt = [sb("obt%d" % i, [128, 8, 128], BF16) for i in range(2)]
                gst_ = [sb("gs%d" % i, [128, 4096], BF16) for i in range(1)] * 2
                xt = [sb("xt%d" % i, [128, D], F32) for i in range(2)]
                mgs = [sb("mg%d" % i, [128, D], BF16) for i in range(2)]
                mTs = [sb("mT%d" % i, [128, 16, 128], BF16) for i in range(2)]
                gsb2 = [sb("gsb%d" % i, [128, 4096], BF16) for i in range(2)]
                t1 = [sb("t1_%d" % i, [128, 512], F32) for i in range(2)]
                t2 = [sb("t2_%d" % i, [128, 512], F32) for i in range(2)]
                pY = [psb("pY%d" % i, [128, 512], F32) for i in range(4)]
                ptr = [psb("ptr%d" % i, [128, 8, 128], BF16) for i in range(2)]
                pZ = [psb("pZ%d" % i, [128, 512], F32) for i in range(2)]
                for h in range(8):
                    for hf in range(2):
                        P.op("pool", "dma_start", [], ["wa"], out=wa[:, h, hf * 1024:(hf + 1) * 1024], in_=IN["wa"][:, h, hf * 1024:(hf + 1) * 1024])
                        P.op("pool", "dma_start", [], ["wb"], out=wb[:, h, hf * 1024:(hf + 1) * 1024], in_=IN["wb"][:, h, hf * 1024:(hf + 1) * 1024])
                for dk in range(16):
                    for hf in range(2):
                        P.op("pool", "dma_start", [], ["wo"], out=wo[:, dk, hf * 1024:(hf + 1) * 1024], in_=IN["wo"][:, dk, hf * 1024:(hf + 1) * 1024])
                CNY = dict(ny=0)

                def stage_Y(T):
                    s = T % 2
                    tsl = slice(T * 128, (T + 1) * 128)
                    P.op("sp", "dma_start", [], ["oat%d" % s], out=oat[s][:], in_=SC["oaT"][:, :, tsl].rearrange("h p s -> p h s"))
                    P.op("sp", "dma_start", [], ["obt%d" % s], out=obt[s][:], in_=SC["obT"][:, :, tsl].rearrange("h p s -> p h s"))
                    P.op("sp", "dma_start", [], ["gs%d" % s], out=gsb2[s][:], in_=SC["gs"][tsl, :])
                    P.op("sp", "dma_start", [], ["xt%d" % s], out=xt[s][:], in_=IN["x"][tsl, :])
                    for cg in range(4):
                        csl = slice(cg * 512, (cg + 1) * 512)
                        ya = CNY["ny"] % 4
                        yb = (CNY["ny"] + 1) % 4
                        CNY["ny"] += 2
                        u = cg % 2
                        for h in range(8):
                            P.op("pe", "matmul", ["oat%d" % s, "wa"], ["pY%d" % ya], out=pY[ya][:], lhsT=oat[s][:, h, :], rhs=wa[:, h, csl], start=(h == 0), stop=(h == 7))
                        for h in range(8):
                            P.op("pe", "matmul", ["obt%d" % s, "wb"], ["pY%d" % yb], out=pY[yb][:], lhsT=obt[s][:, h, :], rhs=wb[:, h, csl], start=(h == 0), stop=(h == 7))
                        P.op("dve", "tensor_tensor", ["pY%d" % ya, "gs%d" % s], ["t1_%d" % u], out=t1[u][:], in0=pY[ya][:], in1=gsb2[s][:, csl], op=ALU.mult)
                        P.op("dve", "tensor_tensor", ["pY%d" % yb, "gs%d" % s], ["t2_%d" % u], out=t2[u][:], in0=pY[yb][:], in1=gsb2[s][:, 2048 + cg * 512:2048 + (cg + 1) * 512], op=ALU.mult)
                        P.op("pool", "tensor_tensor", ["t1_%d" % u, "t2_%d" % u], ["mg%d" % s], out=mgs[s][:, csl], in0=t1[u][:], in1=t2[u][:], op=ALU.add)

                def stage_TZ(T):
                    s = T % 2
                    tsl = slice(T * 128, (T + 1) * 128)
                    for dk in range(16):
                        b_ = dk // 8
                        P.op("pe", "transpose", ["mg%d" % s, "identb"], ["ptr%d" % b_], out=ptr[b_][:, dk % 8, :], in_=mgs[s][:, dk * 128:(dk + 1) * 128], identity=identb[:])
                    for b_ in range(2):
                        P.op("act", "activation", ["ptr%d" % b_], ["mT%d" % s], out=mTs[s][:, b_ * 8:(b_ + 1) * 8, :], in_=ptr[b_][:], func=AF.Copy)
                    for og in range(4):
                        csl = slice(og * 512, (og + 1) * 512)
                        z = og % 2
                        for dk in range(16):
                            P.op("pe", "matmul", ["mT%d" % s, "wo"], ["pZ%d" % z], out=pZ[z][:], lhsT=mTs[s][:, dk, :], rhs=wo[:, dk, csl], start=(dk == 0), stop=(dk == 15))
                        P.op("dve", "tensor_tensor", ["pZ%d" % z, "xt%d" % s], ["xt%d" % s], out=xt[s][:, csl], in0=pZ[z][:], in1=xt[s][:, csl], op=ALU.add)
                    P.op("sp", "dma_start", ["xt%d" % s], [], out=SC["x2"][tsl, :], in_=xt[s][:])

                stage_Y(0)
                for T in range(NT):
                    if T + 1 < NT:
                        stage_Y(T + 1)
                    stage_TZ(T)
                P.emit(st)

        if "p5" in phases:
            with ExitStack() as st:
                sb = lambda n, s, d, _p="p5_": st.enter_context(nc.sbuf_tensor(_p + n, s, d))
                psb = lambda n, s, d, _p="p5_": st.enter_context(nc.psum_tensor(_p + n, s, d))
                P = Prog(nc, SEMS)
                wpq = sb("wpq", [128, 16, 1024], BF16)
                kb = sb("kb", [128, 8, 256], BF16)
                gffnT = sb("gffnT", [128, 16], F32)
                gfin = sb("gfin", [128, D], F32)
                iota = sb("iota", [128, 16], F32)
                iota128 = sb("iota128", [128, 128], F32)
                GT = sb("GT", [128, 256, 128], BF16)
                NSB = 8
                OH2s = [sb("OH2_%d" % i, [128, NSB, 128], BF16) for i in range(2)]
                OH1s = [sb("OH1_%d" % i, [128, NSB, 128], BF16) for i in range(2)]
                hnTGs = [sb("hnTG%d" % i, [128, 16, 256], BF16) for i in range(2)]
                NUB = 4
                ub = [sb("ub%d" % i, [128, D], BF16) for i in range(NUB)]
                vb = [ub[i // 2][:, (i % 2) * 1024:(i % 2 + 1) * 1024] for i in range(2 * NUB)]
                x2t = [sb("x2t%d" % i, [128, D], F32) for i in range(2)]
                hnf = sb("hnf", [128, D], F32)
                qTs = sb("qTs", [128, 8, 128], BF16)
                ssb = sb("ssb", [128, 8, 256], F32)
                wk = sb("wk", [128, 256], F32)
                vals = sb("vals", [128, 16, 16], F32)
                idxs = sb("idxs", [128, 16, 16], U32)
                idxf = sb("idxf", [128, 16, 16], F32)
                tv = sb("tv", [128, 8, 16], F32)
                tp = sb("tp", [128, 8, 16], U32)
                ti = sb("ti", [128, 8, 16], U32)
                tj = sb("tj", [128, 8, 16], U32)
                tif = sb("tif", [128, 8, 16], F32)
                tjf = sb("tjf", [128, 8, 16], F32)
                e1 = [sb("e1_%d" % i, [128, 8, 16], F32) for i in range(4)]
                e2 = [sb("e2_%d" % i, [128, 8, 16], F32) for i in range(4)]
                gw = [sb("gw%d" % i, [128, 128], F32) for i in range(4)]
                selT = sb("selT", [128, 3, 128], F32)
                ex = sb("ex", [128, 8, 16], F32)
                zs = sb("zs", [128, 8], F32)
                ga = [sb("ga%d" % i, [128, 256], BF16) for i in range(2)]
                cst = [sb("cst%d" % i, [128, 256], BF16) for i in range(4)]
                stA = sb("stA", [128, 4], F32)
                stC = sb("stC", [128, 4], F32)
                B = [psb("B%d" % i, [128, 512], F32) for i in range(8)]
                for dk in range(16):
                    P.op("pool", "dma_start", [], ["wpq"], out=wpq[:, dk, :], in_=IN["wpq"][:, dk, :])
                for h in range(8):
                    P.op("pool", "dma_start", [], ["kb"], out=kb[:, h, :], in_=IN["kb"][:, h, :])
                P.op("sp", "dma_start", [], ["gffnT"], out=gffnT[:], in_=IN["gffnT"])
                P.op("sp", "dma_start", [], ["gfin"], out=gfin[:], in_=IN["gfin"].partition_broadcast(128))
                P.op("sp", "dma_start", [], ["iota"], out=iota[:], in_=IN["iota16"])
                P.op("sp", "dma_start", [], ["iota128"], out=iota128[:], in_=IN["iota128"])
                CN = dict(u=0, v=0, g=0, a=0, o=0)

                def stage_A(T):
                    p = T % 4
                    gp = (T // 2) % 2
                    hnTG = hnTGs[gp]
                    HT = "hnTG%d" % gp
                    tcol = slice((T % 2) * 128, (T % 2) * 128 + 128)
                    tsl = slice(T * 128, (T + 1) * 128)
                    yield P.op("sp", "dma_start", [], ["hnf"], out=hnf[:], in_=SC["x2"][tsl, :])
                    yield P.op("act", "activation", ["hnf"], ["ssb", "stA0"], out=ssb[:].rearrange("p a b -> p (a b)"), in_=hnf[:], func=AF.Square, accum_out=stA[:, 0:1])
                    yield P.op("act", "activation", ["stA0"], ["stA1"], out=stA[:, 1:2], in_=stA[:, 0:1], func=AF.Sqrt, scale=1.0 / D, bias=EPS)
                    yield P.op("dve", "reciprocal", ["stA1"], ["stA2"], out=stA[:, 2:3], in_=stA[:, 1:2])
                    yield P.op("act", "activation", ["hnf", "stA2"], ["hnf"], out=hnf[:], in_=hnf[:], func=AF.Copy, scale=stA[:, 2:3])
                    for r in range(4):
                        z = 6 + r % 2
                        for q4 in range(4):
                            dk = r * 4 + q4
                            yield P.op("pe", "transpose", ["hnf", "identf"], ["B%d" % z], out=B[z][:, q4 * 128:(q4 + 1) * 128], in_=hnf[:, dk * 128:(dk + 1) * 128], identity=identf[:])
                        for q4 in range(4):
                            dk = r * 4 + q4
                            if dk % 2 == 0:
                                yield P.op("act", "activation", ["B%d" % z, "gffnT"], [HT], out=hnTG[:, dk, tcol], in_=B[z][:, q4 * 128:(q4 + 1) * 128],
                                           func=AF.Copy, scale=gffnT[:, dk:dk + 1])
                            else:
                                yield P.op("dve", "tensor_scalar", ["B%d" % z, "gffnT"], [HT], out=hnTG[:, dk, tcol], in0=B[z][:, q4 * 128:(q4 + 1) * 128],
                                           scalar1=gffnT[:, dk:dk + 1], scalar2=None, op0=ALU.mult)
                    for hh in range(2):
                        z = 6 + hh
                        for h4 in range(4):
                            h = hh * 4 + h4
                            for dk in range(16):
                                yield P.op("pe", "matmul", ["wpq", HT], ["B%d" % z], out=B[z][:, h4 * 128:(h4 + 1) * 128], lhsT=wpq[:, dk, h * 128:(h + 1) * 128],
                                           rhs=hnTG[:, dk, tcol], start=(dk == 0), stop=(dk == 15))
                        yield P.op("act", "activation", ["B%d" % z], ["qTs"], out=qTs[:, hh * 4:(hh + 1) * 4, :].rearrange("p a b -> p (a b)"), in_=B[z][:], func=AF.Copy)
                    for h2 in range(4):
                        z = 6 + h2 % 2
                        for hi in range(2):
                            h = h2 * 2 + hi
                            yield P.op("pe", "matmul", ["qTs", "kb"], ["B%d" % z], out=B[z][:, hi * 256:(hi + 1) * 256], lhsT=qTs[:, h, :], rhs=kb[:, h, :], start=True, stop=True)
                        yield P.op("act", "activation", ["B%d" % z], ["ssb"], out=ssb[:, h2 * 2:h2 * 2 + 2, :].rearrange("p a b -> p (a b)"), in_=B[z][:], func=AF.Copy)
                    for hp in range(16):
                        src = ssb[:, hp // 2, (hp % 2) * 128:(hp % 2) * 128 + 128]
                        yield P.op("dve", "max", ["ssb"], ["vals"], out=vals[:, hp, 0:8], in_=src)
                        yield P.op("dve", "max_index", ["ssb", "vals"], ["idxs"], out=idxs[:, hp, 0:8], in_max=vals[:, hp, 0:8], in_values=src)
                        yield P.op("dve", "match_replace", ["ssb", "vals"], ["wk"], out=wk[:, 0:128], in_to_replace=vals[:, hp, 0:8], in_values=src, imm_value=NEG)
                        yield P.op("dve", "max", ["wk"], ["vals"], out=vals[:, hp, 8:16], in_=wk[:, 0:128])
                        yield P.op("dve", "max_index", ["wk", "vals"], ["idxs"], out=idxs[:, hp, 8:16], in_max=vals[:, hp, 8:16], in_values=wk[:, 0:128])
                    yield P.op("dve", "tensor_copy", ["idxs"], ["idxf"], out=idxf[:], in_=idxs[:])
                    cand = ssb[:].rearrange("p h (a b) -> p h a b", b=16)
                    v4 = vals[:].rearrange("p (h two) k -> p h two k", two=2)
                    i4 = idxf[:].rearrange("p (h two) k -> p h two k", two=2)
                    yield P.op("dve", "tensor_tensor", ["vals"], ["ssb"], out=cand, in0=v4[:, :, 0, :].unsqueeze(3).to_broadcast([128, 8, 16, 16]),
                         in1=v4[:, :, 1, :].unsqueeze(2).to_broadcast([128, 8, 16, 16]), op=ALU.add)
                    for h in range(8):
                        src = ssb[:, h, :]
                        yield P.op("dve", "max", ["ssb"], ["tv"], out=tv[:, h, 0:8], in_=src)
                        yield P.op("dve", "max_index", ["ssb", "tv"], ["tp"], out=tp[:, h, 0:8], in_max=tv[:, h, 0:8], in_values=src)
                        yield P.op("dve", "match_replace", ["ssb", "tv"], ["wk"], out=wk[:], in_to_replace=tv[:, h, 0:8], in_values=src, imm_value=NEG)
                        yield P.op("dve", "max", ["wk"], ["tv"], out=tv[:, h, 8:16], in_=wk[:])
                        yield P.op("dve", "max_index", ["wk", "tv"], ["tp"], out=tp[:, h, 8:16], in_max=tv[:, h, 8:16], in_values=wk[:])
                    yield P.op("dve", "tensor_single_scalar", ["tp"], ["ti"], out=ti[:], in_=tp[:], scalar=4, op=ALU.logical_shift_right)
                    yield P.op("dve", "tensor_single_scalar", ["tp"], ["tj"], out=tj[:], in_=tp[:], scalar=15, op=ALU.bitwise_and)
                    yield P.op("dve", "tensor_copy", ["ti"], ["tif"], out=tif[:], in_=ti[:])
                    yield P.op("dve", "tensor_copy", ["tj"], ["tjf"], out=tjf[:], in_=tj[:])
                    oh = ssb[:].rearrange("p h (a b) -> p h a b", b=16)
                    iob = iota[:].unsqueeze(1).unsqueeze(1).to_broadcast([128, 8, 16, 16])
                    for (sel, side, dst, dn) in ((tif, 0, e1[p], "e1_%d" % p), (tjf, 1, e2[p], "e2_%d" % p)):
                        yield P.op("dve", "tensor_tensor", ["tif", "tjf", "iota"], ["ssb"], out=oh, in0=sel[:].unsqueeze(3).to_broadcast([128, 8, 16, 16]), in1=iob, op=ALU.is_equal)
                        yield P.op("dve", "tensor_tensor", ["ssb", "idxf"], ["ssb"], out=oh, in0=oh, in1=i4[:, :, side, :].unsqueeze(2).to_broadcast([128, 8, 16, 16]), op=ALU.mult)
                        yield P.op("dve", "tensor_reduce", ["ssb"], [dn], out=dst[:], in_=oh, axis=AX.X, op=ALU.add)
                    yield P.op("dve", "tensor_tensor", ["tv"], ["ex"], out=ex[:], in0=tv[:], in1=tv[:, :, 0:1].to_broadcast([128, 8, 16]), op=ALU.subtract)
                    yield P.op("act", "activation", ["ex"], ["ex"], out=ex[:], in_=ex[:], func=AF.Exp)
                    yield P.op("dve", "tensor_reduce", ["ex"], ["zs"], out=zs[:], in_=ex[:], axis=AX.X, op=ALU.add)
                    yield P.op("dve", "reciprocal", ["zs"], ["zs"], out=zs[:], in_=zs[:])
                    yield P.op("dve", "tensor_tensor", ["ex", "zs"], ["gw%d" % p], out=gw[p][:].rearrange("p (a b) -> p a b", b=16), in0=ex[:], in1=zs[:].unsqueeze(2).to_broadcast([128, 8, 16]), op=ALU.mult)

                def stage_GT(T):
                    p = T % 4
                    tp_ = T % 2
                    srcs = [(e1[p][:].rearrange("p a b -> p (a b)"), "e1_%d" % p), (e2[p][:].rearrange("p a b -> p (a b)"), "e2_%d" % p), (gw[p][:], "gw%d" % p)]
                    for j, (ap_, nm) in enumerate(srcs):
                        P.op("pe", "transpose", [nm, "identf"], ["B7"], out=B[7][:, j * 128:(j + 1) * 128], in_=ap_, identity=identf[:])
                    P.op("act", "activation", ["B7"], ["selT"], out=selT[:].rearrange("p a b -> p (a b)"), in_=B[7][:, 0:384], func=AF.Copy)
                    for sub in range(128 // NSB):
                        tsub = slice(sub * NSB, (sub + 1) * NSB)
                        ob = CN["o"] % 2
                        CN["o"] += 1
                        OH2, OH1 = OH2s[ob], OH1s[ob]
                        iob = iota128[:].unsqueeze(1).to_broadcast([128, NSB, 128])
                        P.op("dve", "tensor_tensor", ["iota128", "selT"], ["OH2_%d" % ob], out=OH2[:], in0=iob, in1=selT[:, 1, tsub].unsqueeze(2).to_broadcast([128, NSB, 128]), op=ALU.is_equal)
                        P.op("dve", "tensor_tensor", ["iota128", "selT"], ["OH1_%d" % ob], out=OH1[:], in0=iob, in1=selT[:, 0, tsub].unsqueeze(2).to_broadcast([128, NSB, 128]), op=ALU.is_equal)
                        P.op("pool", "tensor_tensor", ["OH1_%d" % ob, "selT"], ["OH1_%d" % ob], out=OH1[:], in0=OH1[:], in1=selT[:, 2, tsub].unsqueeze(2).to_broadcast([128, NSB, 128]), op=ALU.mult)
                        for t4 in range(NSB // 4):
                            z = 6 + CN["g"] % 2
                            CN["g"] += 1
                            for tt in range(4):
                                t = t4 * 4 + tt
                                P.op("pe", "matmul", ["OH2_%d" % ob, "OH1_%d" % ob], ["B%d" % z], out=B[z][:, tt * 128:(tt + 1) * 128], lhsT=OH2[:, t, :], rhs=OH1[:, t, :], start=True, stop=True)
                            tg0 = tp_ * 128 + sub * NSB + t4 * 4
                            P.op("act", "activation", ["B%d" % z], ["GT"], out=GT[:, tg0:tg0 + 4, :].rearrange("p t c -> p (t c)"), in_=B[z][:], func=AF.Copy)

                def stage_U(G, step):
                    gp = G % 2
                    hnTG = hnTGs[gp]
                    for c in range(128):
                        b = CN["u"] % NUB
                        CN["u"] += 1
                        P.op("sp", "dma_start", [], ["ubh%d" % (2 * b), "ubh%d" % (2 * b + 1)], out=ub[b][:], in_=SC["pub"][c * 128:(c + 1) * 128, :])
                        z = 4 + (c // 2) % 2
                        reg = slice((c % 2) * 256, (c % 2) * 256 + 256)
                        for dk in range(16):
                            P.op("pe", "matmul", ["ubh%d" % (2 * b + dk // 8), "hnTG%d" % gp], ["B%d" % z], out=B[z][:, reg], lhsT=ub[b][:, dk * 128:(dk + 1) * 128], rhs=hnTG[:, dk, :], start=(dk == 0), stop=(dk == 15))
                        k = CN["a"] % 2
                        CN["a"] += 1
                        P.op("act", "activation", ["B%d" % z], ["ga%d" % k], out=ga[k][:], in_=B[z][:, reg], func=AF.Gelu)
                        P.op("dve", "tensor_tensor", ["ga%d" % k, "GT"], ["GT"], out=GT[:, :, c], in0=ga[k][:], in1=GT[:, :, c], op=ALU.mult)
                        step(3)

                def stage_V(G, step):
                    for p in range(2):
                        T = 2 * G + p
                        P.op("sp", "dma_start", [], ["x2t%d" % p], out=x2t[p][:], in_=SC["x2"][T * 128:(T + 1) * 128, :])
                    for hv in range(2):
                        for c in range(128):
                            b = CN["v"] % (2 * NUB)
                            CN["v"] += 1
                            P.op("sp", "dma_start", [], ["ubh%d" % b], out=vb[b], in_=SC["pvb"][c * 128:(c + 1) * 128, hv * 1024:(hv + 1) * 1024])
                            k = (CN["v"] - 1) % 4
                            if k % 2 == 0:
                                P.op("act", "activation", ["GT"], ["cst%d" % k], out=cst[k][:], in_=GT[:, :, c], func=AF.Copy)
                            else:
                                P.op("pool", "tensor_copy", ["GT"], ["cst%d" % k], out=cst[k][:], in_=GT[:, :, c])
                            for p in range(2):
                                for cg in range(2):
                                    z = p * 2 + cg
                                    P.op("pe", "matmul", ["cst%d" % k, "ubh%d" % b], ["B%d" % z], out=B[z][:], lhsT=cst[k][:, p * 128:(p + 1) * 128], rhs=vb[b][:, cg * 512:(cg + 1) * 512],
                                         start=(c == 0), stop=(c == 127))
                            step(3)
                        for p in range(2):
                            for cg in range(2):
                                z = p * 2 + cg
                                csl = slice(hv * 1024 + cg * 512, hv * 1024 + (cg + 1) * 512)
                                P.op("dve", "tensor_tensor", ["B%d" % z, "x2t%d" % p], ["x2t%d" % p], out=x2t[p][:, csl], in0=B[z][:], in1=x2t[p][:, csl], op=ALU.add)
                    for p in range(2):
                        T = 2 * G + p
                        tsl = slice(T * 128, (T + 1) * 128)
                        X = "x2t%d" % p
                        P.op("act", "activation", [X], ["ubh0", "ubh1", "stC0"], out=ub[0][:], in_=x2t[p][:], func=AF.Square, accum_out=stC[:, 0:1])
                        P.op("act", "activation", ["stC0"], ["stC1"], out=stC[:, 1:2], in_=stC[:, 0:1], func=AF.Sqrt, scale=1.0 / D, bias=EPS)
                        P.op("dve", "reciprocal", ["stC1"], ["stC2"], out=stC[:, 2:3], in_=stC[:, 1:2])
                        P.op("dve", "scalar_tensor_tensor", [X, "stC2", "gfin"], [X], out=x2t[p][:], in0=x2t[p][:], scalar=stC[:, 2:3], in1=gfin[:], op0=ALU.mult, op1=ALU.mult)
                        P.op("sp", "dma_start", [X], [], out=y[tsl, :], in_=x2t[p][:])

                def run_all(gen):
                    for _ in gen:
                        pass

                def chain(*gens):
                    for g_ in gens:
                        for v_ in g_:
                            yield v_

                run_all(stage_A(0))
                run_all(stage_A(1))
                for G in range(NT // 2):
                    stage_GT(2 * G)
                    stage_GT(2 * G + 1)
                    nxt = chain(stage_A(2 * G + 2), stage_A(2 * G + 3)) if G + 1 < NT // 2 else iter(())

                    def step(n, nxt=nxt):
                        for _ in range(n):
                            next(nxt, None)

                    stage_U(G, step)
                    stage_V(G, step)
                    run_all(nxt)
                P.emit(st)
    return nc


def kernel(**inputs):
    H = host_layout(inputs)
    x = np.asarray(inputs["x"], np.float32)
    pos = np.asarray(inputs["positions"]).astype(np.int32)
    nc = build_nc()
    in_maps = []
    for b in range(8):
        m = {k: H[k] for k in SHAPES if k not in ("x", "pos")}
        m["x"] = np.ascontiguousarray(x[b])
        m["pos"] = np.ascontiguousarray(pos[b])
        in_maps.append(m)
    res = run_bass_kernel_spmd(nc, in_maps, core_ids=list(range(8)))
    return np.stack([np.asarray(r["y"], dtype=np.float32) for r in res.results], axis=0)
```

```python
import numpy as np
from contextlib import ExitStack
import concourse.bass as bass
import concourse.mybir as mybir
from concourse.bass_utils import run_bass_kernel_spmd

F32 = mybir.dt.float32
BF16 = mybir.dt.bfloat16
I32 = mybir.dt.int32
U32 = mybir.dt.uint32
AF = mybir.ActivationFunctionType
ALU = mybir.AluOpType
AX = mybir.AxisListType

D = 2048
S = 2048
NT = 16
EPS = 1e-6
PI = float(np.pi)
MAGIC = 12582912.0
NEG = -1e30
ROPE_THETA = 500000.0

ENGS = ["pe", "act", "dve", "pool", "sp"]
N_DMA_SEMS = {"sp": 40, "pool": 24, "act": 8}


class Prog:
    def __init__(self, nc, semstate):
        self.nc = nc
        self.semstate = semstate
        self.ins = {e: [] for e in ENGS}
        self.last_w = {}
        self.readers = {}
        self.dma_rr = {e: 0 for e in N_DMA_SEMS}
        self.dma_cnt = {}

    def add(self, eng, fn, reads=(), writes=(), dma=False):
        idx = len(self.ins[eng])
        deps = set()
        for r in reads:
            w = self.last_w.get(r)
            if w is not None:
                deps.add(w)
        for r in writes:
            w = self.last_w.get(r)
            if w is not None:
                deps.add(w)
            for rd in self.readers.get(r, ()):
                deps.add(rd)
        deps.discard((eng, idx))
        rec = dict(fn=fn, deps=deps, dma=dma)
        if dma:
            j = self.dma_rr[eng]
            self.dma_rr[eng] = (j + 1) % N_DMA_SEMS[eng]
            n = self.dma_cnt.get((eng, j), 0) + 1
            self.dma_cnt[(eng, j)] = n
            rec["dsem"] = (eng, j, n)
        self.ins[eng].append(rec)
        for r in reads:
            self.readers.setdefault(r, []).append((eng, idx))
        for r in writes:
            self.last_w[r] = (eng, idx)
            self.readers[r] = []
        return (eng, idx)

    def op(self, eng, method, reads, writes, **kw):
        dma = method in ("dma_start", "indirect_dma_start")
        return self.add(eng, lambda e: getattr(e, method)(**kw), reads, writes, dma=dma)

    def emit(self, stack):
        nc = self.nc
        need = {e: set() for e in ENGS}
        for e in ENGS:
            for ins in self.ins[e]:
                pmax = {}
                ed = set()
                for (de, di) in ins["deps"]:
                    if not self.ins[de][di]["dma"]:
                        if not (de == "pe" and e == "pe"):
                            pmax[de] = max(pmax.get(de, -1), di)
                        continue
                    ed.add((de, di))
                for de, di in pmax.items():
                    ed.add((de, di))
                ins["deps"] = ed
                for (de, di) in ed:
                    if self.ins[de][di]["dma"]:
                        continue
                    need[de].add(di)
        sigcount = {}
        for e in ENGS:
            c = 0
            for i, ins in enumerate(self.ins[e]):
                if ins["dma"]:
                    continue
                if i in need[e]:
                    c += 1
                    sigcount[(e, i)] = c
        ss = self.semstate
        gstack = ss["stack"]
        for e in ["pe", "act", "dve", "pool"]:
            if e not in ss["esem"]:
                ss["esem"][e] = gstack.enter_context(nc.semaphore("se_%s" % e))
                ss["ebase"][e] = 0
        for e, n in N_DMA_SEMS.items():
            for j in range(n):
                if self.dma_cnt.get((e, j), 0) > 0 and (e, j) not in ss["dsem"]:
                    ss["dsem"][(e, j)] = gstack.enter_context(nc.semaphore("sd_%s_%d" % (e, j)))
                    ss["dbase"][(e, j)] = 0
        esem = ss["esem"]
        dsem = ss["dsem"]
        ebase = dict(ss["ebase"])
        dbase = dict(ss["dbase"])
        for (e, i) in list(sigcount.keys()):
            sigcount[(e, i)] += ebase[e]
        for e in ["pe", "act", "dve", "pool"]:
            ss["ebase"][e] += sum(1 for (ee, i) in sigcount if ee == e)
        for (e, j), n in self.dma_cnt.items():
            ss["dbase"][(e, j)] += n
        block = stack.enter_context(nc.Block())
        prog = self

        def run(ename, eng):
            waited = {}

            def w(key, sem, val):
                if waited.get(key, 0) >= val:
                    return
                eng.wait_ge(sem, val)
                waited[key] = val

            for i, ins in enumerate(prog.ins[ename]):
                for (de, di) in sorted(ins["deps"]):
                    dins = prog.ins[de][di]
                    if dins["dma"]:
                        (qe, j, n) = dins["dsem"]
                        w(("d", qe, j), dsem[(qe, j)], 16 * (n + dbase[(qe, j)]))
                    else:
                        if de == ename and ename == "pe":
                            continue
                        w(("e", de), esem[de], sigcount[(de, di)])
                if ins["dma"]:
                    (qe, j, n) = ins["dsem"]
                    if n > 1:
                        w(("d", qe, j), dsem[(qe, j)], 16 * (n - 1 + dbase[(qe, j)]))
                    inst = ins["fn"](eng)
                    inst.then_inc(dsem[(qe, j)], 16)
                else:
                    inst = ins["fn"](eng)
                    if (ename, i) in sigcount:
                        inst.then_inc(esem[ename], 1)
            for (qe, j), n in prog.dma_cnt.items():
                if qe == ename:
                    w(("d", qe, j), dsem[(qe, j)], 16 * (n + dbase[(qe, j)]))

        block.tensor(lambda eng: run("pe", eng))
        block.scalar(lambda eng: run("act", eng))
        block.vector(lambda eng: run("dve", eng))
        block.gpsimd(lambda eng: run("pool", eng))
        block.sync(lambda eng: run("sp", eng))


O_CQ, O_CKV, O_KR, O_DQ, O_DK, O_DV, O_IQ, O_IK, O_IW, O_G = 0, 512, 768, 832, 1856, 2112, 2368, 3392, 3456, 3472


def _rot_perm(n, half, rot, blocks=1, bw=None):
    bw = bw or n
    idx = np.arange(n)
    out = idx.copy()
    for b in range(n // bw):
        o = b * bw
        out[o:o + half] = idx[o + half:o + rot]
        out[o + half:o + rot] = idx[o:o + half]
    return out


def _pk(w, ncol):
    K = w.shape[0]
    return np.ascontiguousarray(w.reshape(K // 128, 128, ncol).transpose(1, 0, 2))


def host_layout(inp):
    f = np.float32
    w_in = np.asarray(inp["w_in"], f)[0]
    out = {}
    tiles = []
    for h in range(8):
        tiles.append(np.arange(O_DQ + h * 128, O_DQ + (h + 1) * 128))
    for g in range(2):
        tiles.append(np.arange(O_DK + g * 128, O_DK + (g + 1) * 128))
    for hp in range(8):
        tiles.append(np.arange(O_IQ + hp * 128, O_IQ + (hp + 1) * 128))
    tiles.append(np.concatenate([np.arange(O_IK, O_IK + 64)] * 2))
    tiles.append(np.concatenate([np.arange(O_KR, O_KR + 64)] * 2))
    perms = [_rot_perm(128, 16, 32)] * 10 + [_rot_perm(128, 8, 16, bw=64)] * 9 + [_rot_perm(128, 32, 64, bw=64)]
    w1f = np.empty((20, 128, 16, 256), f)
    for i, (cols, pm) in enumerate(zip(tiles, perms)):
        w1f[i, :, :, 0:128] = _pk(w_in[:, cols], 128)
        w1f[i, :, :, 128:256] = _pk(w_in[:, cols[pm]], 128)
    out["w1f"] = w1f
    tcols = [np.arange(O_CQ, O_CQ + 512), np.concatenate([np.arange(O_CKV, O_CKV + 256), np.arange(O_DV, O_DV + 256)])]
    for j in range(8):
        tcols.append(np.arange(O_G + j * 512, O_G + (j + 1) * 512))
    out["w1t"] = np.stack([_pk(w_in[:, c], 512) for c in tcols])
    out["w1iw"] = _pk(w_in[:, O_IW:O_IW + 16], 16)
    out["gmixT"] = np.ascontiguousarray(np.asarray(inp["norm_mix_g"], f)[0].reshape(16, 128).T)
    rc = np.zeros((128, 9), f)
    for ty, (half, rot, bw) in enumerate([(16, 32, 128), (8, 16, 64), (32, 64, 64)]):
        invf = (ROPE_THETA ** (-(np.arange(half, dtype=np.float32) * 2.0) / rot)).astype(f)
        for r in range(128):
            j = r % bw
            rc[r, ty * 3 + 1] = PI / 2
            if j < rot:
                rc[r, ty * 3 + 0] = invf[j % half]
                rc[r, ty * 3 + 2] = PI if j < half else 0.0
    out["ropec"] = rc
    out["ident"] = np.eye(128, dtype=f)
    wuq = np.asarray(inp["mla_w_uq"], f)[0]
    pm = _rot_perm(64, 32, 64)
    wq = np.empty((8, 128, 4, 256), f)
    for h in range(8):
        wq[h, :, :, 0:128] = _pk(wuq[:, h, 0:128], 128)
        wq[h, :, :, 128:192] = _pk(wuq[:, h, 128:192], 64)
        wq[h, :, :, 192:256] = _pk(wuq[:, h, 128:192][:, pm], 64)
    out["wq"] = wq
    out["gq"] = np.ascontiguousarray(np.asarray(inp["mla_q_norm_g"], f)[0].reshape(4, 128).T)
    out["gkv"] = np.ascontiguousarray(np.asarray(inp["mla_kv_norm_g"], f)[0].reshape(2, 128).T)
    out["wuk"] = _pk(np.asarray(inp["mla_w_uk"], f)[0].reshape(256, 1024), 1024)
    out["wuv"] = _pk(np.asarray(inp["mla_w_uv"], f)[0].reshape(256, 1024), 1024)
    out["wa"] = _pk(np.asarray(inp["w_branch_a"], f)[0], 2048)
    out["wb"] = _pk(np.asarray(inp["w_branch_b"], f)[0], 2048)
    out["wo"] = _pk(np.asarray(inp["w_out"], f)[0], 2048)
    out["wpq"] = _pk(np.asarray(inp["peer_w_q"], f)[0].reshape(2048, 1024), 1024)
    sk = np.asarray(inp["peer_sub_keys"], f)[0]
    kb = np.zeros((128, 8, 256), f)
    for h in range(8):
        for p in range(2):
            kb[p * 64:(p + 1) * 64, h, p * 128:(p + 1) * 128] = sk[h, p].T
    out["kb"] = kb
    out["gffn"] = np.asarray(inp["norm_ffn_g"], f)[0]
    out["gfin"] = np.asarray(inp["norm_final_g"], f)
    out["put"] = np.ascontiguousarray(np.asarray(inp["peer_u"], f)[0].reshape(128, 128, 16, 128).transpose(0, 3, 2, 1)).reshape(16384, 2048)
    out["gffnT"] = np.ascontiguousarray(np.asarray(inp["norm_ffn_g"], f)[0].reshape(16, 128).T)
    out["iota128"] = np.tile(np.arange(128, dtype=f)[None, :], (128, 1))
    out["pv"] = np.asarray(inp["peer_v"], f)[0]
    out["iota16"] = np.tile(np.arange(16, dtype=f)[None, :], (128, 1))
    return out


SHAPES = {
    "x": ([S, D], F32), "pos": ([S], I32),
    "w1f": ([20, 128, 16, 256], F32), "w1t": ([10, 128, 16, 512], F32), "w1iw": ([128, 16, 16], F32),
    "gmixT": ([128, 16], F32), "ropec": ([128, 9], F32), "ident": ([128, 128], F32),
    "wq": ([8, 128, 4, 256], F32), "gq": ([128, 4], F32), "gkv": ([128, 2], F32),
    "wuk": ([128, 2, 1024], F32), "wuv": ([128, 2, 1024], F32),
    "wa": ([128, 8, 2048], F32), "wb": ([128, 8, 2048], F32), "wo": ([128, 16, 2048], F32),
    "wpq": ([128, 16, 1024], F32), "kb": ([128, 8, 256], F32), "gffn": ([D], F32), "gfin": ([D], F32),
    "put": ([16384, D], F32), "pv": ([16384, D], F32), "iota16": ([128, 16], F32),
    "gffnT": ([128, 16], F32), "iota128": ([128, 128], F32),
}


def build_nc(phases=("p0", "p1", "p2", "p3", "p4", "p5"), debug=()):
    nc = bass.Bass("TRN2", target_bir_lowering=False)
    IN = {k: nc.dram_tensor(k, sh, dt, kind="ExternalInput").ap() for k, (sh, dt) in SHAPES.items()}
    y = nc.dram_tensor("y", [S, D], F32, kind="ExternalOutput").ap()

    def scratch(name, shape, dt):
        kind = "ExternalOutput" if name in debug else "Internal"
        return nc.dram_tensor(name, shape, dt, kind=kind).ap()

    SC = dict(
        dqT=scratch("dqT", [8, 128, S], BF16), dkT=scratch("dkT", [2, 128, S], BF16),
        iqT=scratch("iqT", [8, 128, S], BF16), ikT=scratch("ikT", [128, S], BF16),
        kpeT=scratch("kpeT", [128, S], BF16), cqnT=scratch("cqnT", [128, 4, S], BF16),
        ckvnT=scratch("ckvnT", [128, 2, S], BF16), dv=scratch("dv", [S, 256], BF16),
        iw=scratch("iw", [S, 16], F32), gs=scratch("gs", [S, 4096], BF16),
        oaT=scratch("oaT", [8, 128, S], BF16), obT=scratch("obT", [8, 128, S], BF16),
        x2=scratch("x2", [S, D], F32),
        pub=scratch("pub", [16384, D], BF16), pvb=scratch("pvb", [16384, D], BF16),
    )

    with ExitStack() as gst:
        def gsb(name, shape, dt):
            return gst.enter_context(nc.sbuf_tensor(name, shape, dt))

        SEMS = dict(stack=gst, esem={}, dsem={}, ebase={}, dbase={})
        identb = gsb("identb", [128, 128], BF16)
        identf = gsb("identf", [128, 128], F32)
        onesb = gsb("onesb", [128, 128], BF16)
        trig_cm = nc.sbuf_tensor("trig", [128, 6, S], BF16)
        trig = trig_cm.__enter__()

        if "p0" in phases:
            with ExitStack() as st:
                sb = lambda n, s, d, _p="q1_": st.enter_context(nc.sbuf_tensor(_p + n, s, d))
                P = Prog(nc, SEMS)
                posi = sb("posi", [128, S], I32)
                posf = sb("posf", [128, S], F32)
                ang = sb("ang", [128, S], F32)
                kk = sb("kk", [128, S], F32)
                rc = sb("rc", [128, 9], F32)
                P.op("sp", "dma_start", [], ["identf"], out=identf[:], in_=IN["ident"])
                P.op("sp", "dma_start", [], ["rc"], out=rc[:], in_=IN["ropec"])
                P.op("sp", "dma_start", [], ["posi"], out=posi[:], in_=IN["pos"].partition_broadcast(128))
                P.op("dve", "tensor_copy", ["identf"], ["identb"], out=identb[:], in_=identf[:])
                P.op("pool", "memset", [], ["onesb"], ap=onesb[:], constant=1.0)
                P.op("dve", "tensor_copy", ["posi"], ["posf"], out=posf[:], in_=posi[:])
                for ty in range(3):
                    for cs in range(2):
                        P.op("dve", "tensor_scalar", ["posf", "rc"], ["ang"], out=ang[:], in0=posf[:],
                             scalar1=rc[:, ty * 3:ty * 3 + 1], scalar2=rc[:, ty * 3 + 1 + cs:ty * 3 + 2 + cs],
                             op0=ALU.mult, op1=ALU.add)
                        P.op("dve", "tensor_scalar", ["ang"], ["kk"], out=kk[:], in0=ang[:], scalar1=1.0 / (2 * PI),
                             scalar2=MAGIC, op0=ALU.mult, op1=ALU.add)
                        P.op("dve", "tensor_scalar", ["kk"], ["kk"], out=kk[:], in0=kk[:], scalar1=-MAGIC, scalar2=None,
                             op0=ALU.add)
                        P.op("dve", "scalar_tensor_tensor", ["kk", "ang"], ["kk"], out=kk[:], in0=kk[:], scalar=-2 * PI,
                             in1=ang[:], op0=ALU.mult, op1=ALU.add)
                        P.op("dve", "tensor_scalar", ["kk"], ["kk"], out=kk[:], in0=kk[:], scalar1=-PI, scalar2=PI,
                             op0=ALU.max, op1=ALU.min)
                        P.op("act", "activation", ["kk"], ["trig%d" % (ty * 2 + cs)], out=trig[:, ty * 2 + cs, :],
                             in_=kk[:], func=AF.Sin)
                P.emit(st)

        if "p1" in phases:
            with ExitStack() as st:
                sb = lambda n, s, d, _p="q2_": st.enter_context(nc.sbuf_tensor(_p + n, s, d))
                psb = lambda n, s, d, _p="q4_": st.enter_context(nc.psum_tensor(_p + n, s, d))
                P = Prog(nc, SEMS)
                hT = sb("hT", [128, 16, S], BF16)
                xt = [sb("xt%d" % i, [128, D], F32) for i in range(2)]
                xs = [sb("xs%d" % i, [128, D], BF16) for i in range(2)]
                junk = sb("junk", [128, D], BF16)
                ss = sb("ss", [128, 16], F32)
                sq = sb("sq", [128, 16], F32)
                rstd = sb("rstd", [128, 16], F32)
                gmix = sb("gmix", [128, 16], F32)
                wbuf = [sb("wbuf%d" % i, [128, 16, 512], BF16) for i in range(2)]
                ost = [sb("ost%d" % i, [128, 16, 512], BF16) for i in range(2)]
                tmp = [sb("tmp%d" % i, [128, 512], F32) for i in range(4)]
                cqs = sb("cqs", [128, 16], F32)
                cqn = [sb("cqn%d" % i, [128, 512], BF16) for i in range(2)]
                iwst = sb("iwst", [128, 16, 16], F32)
                wiw = sb("wiw", [128, 16, 16], BF16)
                ptr = [psb("ptr%d" % i, [128, 8, 128], BF16) for i in range(2)]
                pA = [psb("pA%d" % i, [128, 512], F32) for i in range(3)]
                pB = [psb("pB%d" % i, [128, 512], F32) for i in range(3)]
                P.op("sp", "dma_start", [], ["gmix"], out=gmix[:], in_=IN["gmixT"])
                for T in range(NT):
                    s = T % 2
                    tsl = slice(T * 128, (T + 1) * 128)
                    P.op("sp", "dma_start", [], ["xt%d" % s], out=xt[s][:], in_=IN["x"][tsl, :])
                    P.op("act", "activation", ["xt%d" % s], ["junk", "ss%d" % T], out=junk[:], in_=xt[s][:], func=AF.Square,
                         accum_out=ss[:, T:T + 1])
                    P.op("act", "activation", ["ss%d" % T], ["sq%d" % T], out=sq[:, T:T + 1], in_=ss[:, T:T + 1], func=AF.Sqrt,
                         scale=1.0 / D, bias=EPS)
                    P.op("dve", "reciprocal", ["sq%d" % T], ["rstd%d" % T], out=rstd[:, T:T + 1], in_=sq[:, T:T + 1])
                    P.op("act", "activation", ["xt%d" % s, "rstd%d" % T], ["xs%d" % s], out=xs[s][:], in_=xt[s][:], func=AF.Copy,
                         scale=rstd[:, T:T + 1])
                    for dk in range(16):
                        b = dk // 8
                        P.op("pe", "transpose", ["xs%d" % s, "identb"], ["ptr%d" % b], out=ptr[b][:, dk % 8, :],
                             in_=xs[s][:, dk * 128:(dk + 1) * 128], identity=identb[:])
                    for dk in range(16):
                        b = dk // 8
                        if dk % 2 == 0:
                            P.op("act", "activation", ["ptr%d" % b, "gmix"], ["hT%d" % T], out=hT[:, dk, tsl], in_=ptr[b][:, dk % 8, :],
                                 func=AF.Copy, scale=gmix[:, dk:dk + 1])
                        else:
                            P.op("dve", "tensor_scalar", ["ptr%d" % b, "gmix"], ["hT%d" % T], out=hT[:, dk, tsl], in0=ptr[b][:, dk % 8, :],
                                 scalar1=gmix[:, dk:dk + 1], scalar2=None, op0=ALU.mult)
                hT_all = ["hT%d" % T for T in range(NT)]
                dests = [SC["dqT"][h] for h in range(8)] + [SC["dkT"][g] for g in range(2)] + [SC["iqT"][h] for h in range(8)] + [SC["ikT"], SC["kpeT"]]
                types = [0] * 10 + [1] * 9 + [2]
                nblk = 0
                for bi in range(20):
                    ws = nblk % 2
                    osl = nblk % 2
                    nblk += 1
                    ty = types[bi]
                    if bi == 0:
                        P.op("pool", "dma_start", [], ["wbuf%d" % ws], out=wbuf[ws][:, :, 0:256], in_=IN["w1f"][bi])
                    if bi + 1 < 20:
                        P.op("pool", "dma_start", [], ["wbuf%d" % (1 - ws)], out=wbuf[1 - ws][:, :, 0:256], in_=IN["w1f"][bi + 1])
                    else:
                        P.op("pool", "dma_start", [], ["wbuf%d" % (1 - ws)], out=wbuf[1 - ws][:], in_=IN["w1t"][0])
                    for tg in range(4):
                        csl = slice(tg * 512, (tg + 1) * 512)
                        pi = (bi * 4 + tg) % 3
                        for dk in range(16):
                            P.op("pe", "matmul", ["wbuf%d" % ws] + hT_all[tg * 4:tg * 4 + 4], ["pA%d" % pi], out=pA[pi][:],
                                 lhsT=wbuf[ws][:, dk, 0:128], rhs=hT[:, dk, csl], start=(dk == 0), stop=(dk == 15))
                        for dk in range(16):
                            P.op("pe", "matmul", ["wbuf%d" % ws] + hT_all[tg * 4:tg * 4 + 4], ["pB%d" % pi], out=pB[pi][:],
                                 lhsT=wbuf[ws][:, dk, 128:256], rhs=hT[:, dk, csl], start=(dk == 0), stop=(dk == 15))
                        ti = (bi * 4 + tg) % 2
                        P.op("dve", "tensor_tensor", ["pA%d" % pi, "trig"], ["tmp%d" % (2 * ti)], out=tmp[2 * ti][:], in0=pA[pi][:],
                             in1=trig[:, ty * 2, csl], op=ALU.mult)
                        P.op("dve", "tensor_tensor", ["pB%d" % pi, "trig"], ["tmp%d" % (2 * ti + 1)], out=tmp[2 * ti + 1][:], in0=pB[pi][:],
                             in1=trig[:, ty * 2 + 1, csl], op=ALU.mult)
                        P.op("pool", "tensor_tensor", ["tmp%d" % (2 * ti), "tmp%d" % (2 * ti + 1)], ["ost%d" % osl],
                             out=ost[osl][:].rearrange("p a b -> p (a b)")[:, csl], in0=tmp[2 * ti][:], in1=tmp[2 * ti + 1][:], op=ALU.add)
                    P.op("sp", "dma_start", ["ost%d" % osl], [], out=dests[bi], in_=ost[osl][:].rearrange("p a b -> p (a b)")[:, 0:S])
                for bi in range(10):
                    ws = nblk % 2
                    osl = nblk % 2
                    nblk += 1
                    if bi + 1 < 10:
                        P.op("pool", "dma_start", [], ["wbuf%d" % (1 - ws)], out=wbuf[1 - ws][:], in_=IN["w1t"][bi + 1])
                    for T in range(NT):
                        tsl = slice(T * 128, (T + 1) * 128)
                        pi = T % 3
                        for dk in range(16):
                            P.op("pe", "matmul", ["wbuf%d" % ws, "hT%d" % T], ["pA%d" % pi], out=pA[pi][:], lhsT=hT[:, dk, tsl],
                                 rhs=wbuf[ws][:, dk, :], start=(dk == 0), stop=(dk == 15))
                        if bi >= 2:
                            P.op("act", "activation", ["pA%d" % pi], ["ost%d" % osl], out=ost[osl][:, T, :], in_=pA[pi][:], func=AF.Sigmoid)
                        else:
                            ncq = 512 if bi == 0 else 256
                            c = T % 2
                            P.op("act", "activation", ["pA%d" % pi], ["tmp0", "cqs"], out=tmp[0][:, 0:ncq], in_=pA[pi][:, 0:ncq], func=AF.Square,
                                 accum_out=cqs[:, 0:1])
                            P.op("act", "activation", ["cqs"], ["cqs1"], out=cqs[:, 1:2], in_=cqs[:, 0:1], func=AF.Sqrt, scale=1.0 / ncq, bias=EPS)
                            P.op("dve", "reciprocal", ["cqs1"], ["cqs2"], out=cqs[:, 2:3], in_=cqs[:, 1:2])
                            P.op("dve", "tensor_scalar", ["pA%d" % pi, "cqs2"], ["cqn%d" % c], out=cqn[c][:, 0:ncq], in0=pA[pi][:, 0:ncq],
                                 scalar1=cqs[:, 2:3], scalar2=None, op0=ALU.mult)
                            if bi == 1:
                                P.op("act", "activation", ["pA%d" % pi], ["ost%d" % osl], out=ost[osl][:, T, 0:256], in_=pA[pi][:, 256:512], func=AF.Copy)
                            nk = ncq // 128
                            for kc in range(nk):
                                P.op("pe", "transpose", ["cqn%d" % c, "identb"], ["ptr%d" % c], out=ptr[c][:, kc, :],
                                     in_=cqn[c][:, kc * 128:(kc + 1) * 128], identity=identb[:])
                            lo = 0 if bi == 0 else 256
                            P.op("act", "activation", ["ptr%d" % c], ["ost%d" % osl], out=ost[osl][:, T, lo:lo + ncq].rearrange("p (k q) -> p k q", q=128),
                                 in_=ptr[c][:, 0:nk, :], func=AF.Copy)
                    if bi >= 2:
                        P.op("sp", "dma_start", ["ost%d" % osl], [], out=SC["gs"][:, (bi - 2) * 512:(bi - 1) * 512].rearrange("(t p) c -> p t c", p=128),
                             in_=ost[osl][:])
                    else:
                        nk = 4 if bi == 0 else 2
                        lo = 0 if bi == 0 else 256
                        dst = SC["cqnT"] if bi == 0 else SC["ckvnT"]
                        for kc in range(nk):
                            P.op("sp", "dma_start", ["ost%d" % osl], [], out=dst[:, kc, :].rearrange("p (t q) -> p t q", q=128),
                                 in_=ost[osl][:, :, lo + kc * 128:lo + (kc + 1) * 128])
                        if bi == 1:
                            P.op("sp", "dma_start", ["ost%d" % osl], [], out=SC["dv"].rearrange("(t p) c -> p t c", p=128), in_=ost[osl][:, :, 0:256])
                P.op("pool", "dma_start", [], ["wiw"], out=wiw[:], in_=IN["w1iw"])
                for T in range(NT):
                    tsl = slice(T * 128, (T + 1) * 128)
                    pi = T % 3
                    for dk in range(16):
                        P.op("pe", "matmul", ["wiw", "hT%d" % T], ["pB%d" % pi], out=pB[pi][:, 0:16], lhsT=hT[:, dk, tsl], rhs=wiw[:, dk, :],
                             start=(dk == 0), stop=(dk == 15))
                    P.op("act", "activation", ["pB%d" % pi], ["iwst"], out=iwst[:, T, :], in_=pB[pi][:, 0:16], func=AF.Copy)
                P.op("sp", "dma_start", ["iwst"], [], out=SC["iw"].rearrange("(t p) c -> p t c", p=128), in_=iwst[:])
                P.emit(st)

        if "p2" in phases:
            with ExitStack() as st:
                sb = lambda n, s, d, _p="q3_": st.enter_context(nc.sbuf_tensor(_p + n, s, d))
                psb = lambda n, s, d, _p="q5_": st.enter_context(nc.psum_tensor(_p + n, s, d))
                P = Prog(nc, SEMS)
                cq = sb("cq_s", [128, 4, S], BF16)
                ckv = sb("ckv_s", [128, 2, S], BF16)
                kpe = sb("kpe_s", [128, S], BF16)
                wq = [sb("wq%d" % i, [128, 4, 256], BF16) for i in range(2)]
                gq = sb("gq", [128, 4], F32)
                gkv = sb("gkv", [128, 2], F32)
                wuk = sb("wuk", [128, 2, 1024], BF16)
                wuv = sb("wuv", [128, 2, 1024], BF16)
                vall = sb("vall", [128, 16, 1024], BF16)
                qn = [sb("qn%d" % i, [128, S], BF16) for i in range(2)]
                qr = [sb("qr%d" % i, [128, S], BF16) for i in range(2)]
                kn = [sb("kn%d" % i, [128, S], BF16) for i in range(2)]
                pT = [sb("pT%d" % i, [128, 512], BF16) for i in range(3)]
                rden = sb("rden", [128, 512], F32)
                ost = [sb("oast%d" % i, [128, S], BF16) for i in range(2)]
                t1 = sb("t1", [128, 512], F32)
                t2 = sb("t2", [128, 512], F32)
                pp = [psb("pp%d" % i, [128, 512], F32) for i in range(4)]
                pS = [psb("pS%d" % i, [128, 512], F32) for i in range(2)]
                pO = psb("pO", [128, 512], F32)
                pD = psb("pD", [128, 512], F32)
                P.op("sp", "dma_start", [], ["cq"], out=cq[:], in_=SC["cqnT"])
                P.op("sp", "dma_start", [], ["ckv"], out=ckv[:], in_=SC["ckvnT"])
                P.op("sp", "dma_start", [], ["kpe"], out=kpe[:], in_=SC["kpeT"])
                P.op("sp", "dma_start", [], ["gq"], out=gq[:], in_=IN["gq"])
                P.op("sp", "dma_start", [], ["gkv"], out=gkv[:], in_=IN["gkv"])
                for kc in range(2):
                    P.op("pool", "dma_start", [], ["wuk"], out=wuk[:, kc, :], in_=IN["wuk"][:, kc, :])
                    P.op("pool", "dma_start", [], ["wuv"], out=wuv[:, kc, :], in_=IN["wuv"][:, kc, :])
                for kc in range(2):
                    P.op("dve", "tensor_scalar", ["wuk", "gkv"], ["wuk"], out=wuk[:, kc, :], in0=wuk[:, kc, :], scalar1=gkv[:, kc:kc + 1], scalar2=None, op0=ALU.mult)
                    P.op("dve", "tensor_scalar", ["wuv", "gkv"], ["wuv"], out=wuv[:, kc, :], in0=wuv[:, kc, :], scalar1=gkv[:, kc:kc + 1], scalar2=None, op0=ALU.mult)
                n = 0
                for kt in range(16):
                    ksl = slice(kt * 128, (kt + 1) * 128)
                    for hf in range(2):
                        pi = n % 4
                        n += 1
                        for kc in range(2):
                            P.op("pe", "matmul", ["ckv", "wuv"], ["pp%d" % pi], out=pp[pi][:], lhsT=ckv[:, kc, ksl], rhs=wuv[:, kc, hf * 512:(hf + 1) * 512],
                                 start=(kc == 0), stop=(kc == 1))
                        P.op("act", "activation", ["pp%d" % pi], ["vall"], out=vall[:, kt, hf * 512:(hf + 1) * 512], in_=pp[pi][:], func=AF.Copy)
                sc_mla = float(192 ** -0.5)
                def prep(h):
                    s = h % 2
                    yield P.op("pool", "dma_start", [], ["wq%d" % s], out=wq[s][:], in_=IN["wq"][h])
                    for kc in range(4):
                        yield P.op("dve", "tensor_scalar", ["wq%d" % s, "gq"], ["wq%d" % s], out=wq[s][:, kc, :], in0=wq[s][:, kc, :], scalar1=gq[:, kc:kc + 1], scalar2=None, op0=ALU.mult)
                    for tg in range(4):
                        csl = slice(tg * 512, (tg + 1) * 512)
                        for kc in range(4):
                            yield P.op("pe", "matmul", ["wq%d" % s, "cq"], ["pp0"], out=pp[0][:], lhsT=wq[s][:, kc, 0:128], rhs=cq[:, kc, csl], start=(kc == 0), stop=(kc == 3))
                        yield P.op("act", "activation", ["pp0"], ["qn%d" % s], out=qn[s][:, csl], in_=pp[0][:], func=AF.Copy)
                        for kc in range(4):
                            yield P.op("pe", "matmul", ["wq%d" % s, "cq"], ["pp1"], out=pp[1][0:64, :], lhsT=wq[s][:, kc, 128:192], rhs=cq[:, kc, csl], start=(kc == 0), stop=(kc == 3))
                        for kc in range(4):
                            yield P.op("pe", "matmul", ["wq%d" % s, "cq"], ["pp2"], out=pp[2][0:64, :], lhsT=wq[s][:, kc, 192:256], rhs=cq[:, kc, csl], start=(kc == 0), stop=(kc == 3))
                        yield P.op("dve", "tensor_tensor", ["pp1", "trig"], ["t1"], out=t1[0:64, :], in0=pp[1][0:64, :], in1=trig[0:64, 4, csl], op=ALU.mult)
                        yield P.op("dve", "tensor_tensor", ["pp2", "trig"], ["t2"], out=t2[0:64, :], in0=pp[2][0:64, :], in1=trig[0:64, 5, csl], op=ALU.mult)
                        yield P.op("pool", "tensor_tensor", ["t1", "t2"], ["qr%d" % s], out=qr[s][0:64, csl], in0=t1[0:64, :], in1=t2[0:64, :], op=ALU.add)
                        for kc in range(2):
                            yield P.op("pe", "matmul", ["wuk", "ckv"], ["pp3"], out=pp[3][:], lhsT=wuk[:, kc, h * 128:(h + 1) * 128], rhs=ckv[:, kc, csl], start=(kc == 0), stop=(kc == 1))
                        yield P.op("act", "activation", ["pp3"], ["kn%d" % s], out=kn[s][:, csl], in_=pp[3][:], func=AF.Copy)

                def att(h, step):
                    s = h % 2
                    cnt = 0
                    for qg in range(4):
                        nkt = 4 * (qg + 1)
                        for kt in range(nkt):
                            j = kt - 4 * qg
                            c0 = 128 * j if j > 0 else 0
                            cols = slice(qg * 512 + c0, (qg + 1) * 512)
                            ksl = slice(kt * 128, (kt + 1) * 128)
                            a = cnt % 2
                            k = cnt % 3
                            cnt += 1
                            P.op("pe", "matmul", ["kn%d" % s, "qn%d" % s], ["pS%d" % a], out=pS[a][:, c0:512], lhsT=kn[s][:, ksl], rhs=qn[s][:, cols], start=True, stop=False)
                            P.op("pe", "matmul", ["kpe", "qr%d" % s], ["pS%d" % a], out=pS[a][:, c0:512], lhsT=kpe[0:64, ksl], rhs=qr[s][0:64, cols], start=False, stop=True)
                            P.op("act", "activation", ["pS%d" % a], ["pT%d" % k], out=pT[k][:, c0:512], in_=pS[a][:, c0:512], func=AF.Exp, scale=sc_mla)
                            if j >= 0:
                                P.op("pool", "memset", [], ["pT%d" % k], ap=pT[k][64:128, c0:c0 + 64], constant=0.0)
                            P.op("pe", "matmul", ["vall", "pT%d" % k], ["pO"], out=pO[:, c0:512], lhsT=vall[:, kt, h * 128:(h + 1) * 128], rhs=pT[k][:, c0:512],
                                 start=(kt == 0), stop=(kt == nkt - 1))
                            P.op("pe", "matmul", ["onesb", "pT%d" % k], ["pD"], out=pD[:, c0:512], lhsT=onesb[:], rhs=pT[k][:, c0:512],
                                 start=(kt == 0), stop=(kt == nkt - 1))
                            step(3)
                        P.op("dve", "reciprocal", ["pD"], ["rden"], out=rden[:], in_=pD[:])
                        P.op("dve", "tensor_tensor", ["pO", "rden"], ["oast%d" % s], out=ost[s][:, qg * 512:(qg + 1) * 512], in0=pO[:], in1=rden[:], op=ALU.mult)
                    P.op("sp", "dma_start", ["oast%d" % s], [], out=SC["oaT"][h], in_=ost[s][:])

                for _ in prep(0):
                    pass
                for h in range(8):
                    nxt = prep(h + 1) if h + 1 < 8 else iter(())

                    def step(n, nxt=nxt):
                        for _ in range(n):
                            next(nxt, None)

                    att(h, step)
                    for _ in nxt:
                        pass
                P.emit(st)

        trig_cm.__exit__(None, None, None)

        if "p3" in phases:
            with ExitStack() as st:
                sb = lambda n, s, d, _p="p3_": st.enter_context(nc.sbuf_tensor(_p + n, s, d))
                psb = lambda n, s, d, _p="p3_": st.enter_context(nc.psum_tensor(_p + n, s, d))
                P = Prog(nc, SEMS)
                iqT = sb("iqT", [128, 8, S], BF16)
                ikT = sb("ikT", [128, S], BF16)
                iw = sb("iw", [128, 16, 16], F32)
                dqT = sb("dqT", [128, 8, S], BF16)
                dkT = sb("dkT", [128, 2, S], BF16)
                dvs = sb("dvs", [128, 16, 256], BF16)
                origs = [sb("orig%d" % i, [128, S], F32) for i in range(4)]
                works = [sb("work%d" % i, [128, S], F32) for i in range(2)]
                masks = [sb("mask%d" % i, [128, S], BF16) for i in range(2)]
                mxs = [sb("mx%d" % i, [128, 8], F32) for i in range(2)]
                MTs = [sb("MT%d" % i, [128, 16, 512], BF16) for i in range(2)]
                diag = [sb("diag%d" % i, [128, 16, 128], BF16) for i in range(2)]
                Rb = [sb("R%d" % i, [128, 512], BF16) for i in range(4)]
                pT = [sb("pT%d" % i, [128, 512], BF16) for i in range(3)]
                rden = sb("rden", [128, 512], F32)
                obst = [sb("obst%d" % i, [128, 8, 512], BF16) for i in range(2)]
                pDs = [psb("pDs%d" % i, [128, 512], F32) for i in range(2)]
                pIS = psb("pIS", [128, 512], F32)
                ptr = psb("ptr", [128, 8, 128], BF16)
                pS = [psb("pS%d" % i, [128, 512], F32) for i in range(2)]
                pO = psb("pO", [128, 512], F32)
                pD = psb("pD", [128, 512], F32)
                for h in range(8):
                    P.op("sp", "dma_start", [], ["iqT"], out=iqT[:, h, :], in_=SC["iqT"][h])
                    P.op("sp", "dma_start", [], ["dqT"], out=dqT[:, h, :], in_=SC["dqT"][h])
                for g in range(2):
                    P.op("sp", "dma_start", [], ["dkT"], out=dkT[:, g, :], in_=SC["dkT"][g])
                P.op("sp", "dma_start", [], ["ikT"], out=ikT[:], in_=SC["ikT"])
                P.op("sp", "dma_start", [], ["iw"], out=iw[:], in_=SC["iw"].rearrange("(t p) c -> p t c", p=128))
                P.op("sp", "dma_start", [], ["dvs"], out=dvs[:], in_=SC["dv"].rearrange("(t p) c -> p t c", p=128))
                for (src_, dst_) in ((IN["put"], SC["pub"]), (IN["pv"], SC["pvb"])):
                    for r0 in range(0, 16384, 1024):
                        P.op("pool", "dma_start", ["iqT", "dqT", "dkT", "ikT", "iw", "dvs"], [], out=dst_[r0:r0 + 1024, :].rearrange("r (a b) -> (r a) b", b=1024),
                             in_=src_[r0:r0 + 1024, :].rearrange("r (a b) -> (r a) b", b=1024))
                sc_idx = float(64 ** -0.5 * 16 ** -0.5)
                sc_dsa = float(128 ** -0.5)
                nR = 0
                nD = 0
                cnt = 0
                def stage_IS(g):
                    qg = g // 2
                    st_ = dict(nD=0)
                    for T in (2 * g, 2 * g + 1):
                        i = T % 4
                        tsl = slice(T * 128, (T + 1) * 128)
                        nk = 128 * (T + 1)
                        dd = T % 2
                        orig = origs[i]
                        P.op("dve", "tensor_tensor", ["identf", "iw"], ["diag%d" % dd], out=diag[dd][:],
                             in0=identf[:].unsqueeze(1).to_broadcast([128, 16, 128]), in1=iw[:, T, :].unsqueeze(2).to_broadcast([128, 16, 128]), op=ALU.mult)
                        for kg in range((nk + 511) // 512):
                            ncol = min(512, nk - kg * 512)
                            ksl = slice(kg * 512, kg * 512 + ncol)
                            for h in range(16):
                                rows = slice((h % 2) * 64, (h % 2) * 64 + 64)
                                a = CN["nD"] % 2
                                CN["nD"] += 1
                                r = CN["nR"] % 4
                                CN["nR"] += 1
                                P.op("pe", "matmul", ["iqT", "ikT"], ["pDs%d" % a], out=pDs[a][:, 0:ncol], lhsT=iqT[rows, h // 2, tsl], rhs=ikT[rows, ksl], start=True, stop=True)
                                P.op("act", "activation", ["pDs%d" % a], ["R%d" % r], out=Rb[r][:, 0:ncol], in_=pDs[a][:, 0:ncol], func=AF.Relu, scale=sc_idx)
                                P.op("pe", "matmul", ["diag%d" % dd, "R%d" % r], ["pIS"], out=pIS[:, 0:ncol], lhsT=diag[dd][:, h, :], rhs=Rb[r][:, 0:ncol],
                                     start=(h == 0), stop=(h == 15))
                            P.op("act", "activation", ["pIS"], ["orig%d" % i], out=orig[:, ksl], in_=pIS[:, 0:ncol], func=AF.Copy)
                        P.op("pool", "memset", [], ["orig%d" % i], ap=orig[0:64, nk - 64:nk], constant=NEG)

                def stage_topk(g, att_qg=None):
                    tiles = (2 * g, 2 * g + 1)
                    heads_done = 0
                    if tiles[0] >= 2:
                        for rnd in range(32):
                            for T in tiles:
                                i, dd, nk = T % 4, T % 2, 128 * (T + 1)
                                src = origs[i] if rnd == 0 else works[dd]
                                sname = ("orig%d" % i) if rnd == 0 else ("work%d" % dd)
                                P.op("dve", "max", [sname], ["mx%d" % dd], out=mxs[dd][:], in_=src[:, 0:nk])
                            for T in tiles:
                                i, dd, nk = T % 4, T % 2, 128 * (T + 1)
                                src = origs[i] if rnd == 0 else works[dd]
                                sname = ("orig%d" % i) if rnd == 0 else ("work%d" % dd)
                                P.op("dve", "match_replace", ["mx%d" % dd, sname], ["work%d" % dd], out=works[dd][:, 0:nk], in_to_replace=mxs[dd][:],
                                     in_values=src[:, 0:nk], imm_value=NEG)
                            if att_qg is not None and rnd % 4 == 3:
                                stage_att_head(att_qg, heads_done)
                                heads_done += 1
                    if att_qg is not None:
                        while heads_done < 8:
                            stage_att_head(att_qg, heads_done)
                            heads_done += 1
                    for T in tiles:
                        i, dd, nk = T % 4, T % 2, 128 * (T + 1)
                        mask = masks[dd]
                        if T >= 2:
                            P.op("dve", "tensor_tensor", ["work%d" % dd, "orig%d" % i], ["mask%d" % dd], out=mask[:, 0:nk], in0=works[dd][:, 0:nk], in1=origs[i][:, 0:nk], op=ALU.not_equal)
                        else:
                            P.op("dve", "tensor_scalar", ["orig%d" % i], ["mask%d" % dd], out=mask[:, 0:nk], in0=origs[i][:, 0:nk], scalar1=NEG / 2, scalar2=None, op0=ALU.is_gt)

                def stage_maskT(g, first):
                    mp = (g // 2) % 2
                    MT = MTs[mp]
                    if first:
                        P.op("pool", "memset", [], ["MT%d" % mp], ap=MT[:], constant=0.0)
                    for T in (2 * g, 2 * g + 1):
                        i, dd = T % 4, T % 2
                        mask = masks[dd]
                        for k0 in range(0, T + 1, 8):
                            n8 = min(8, T + 1 - k0)
                            for kt in range(k0, k0 + n8):
                                P.op("pe", "transpose", ["mask%d" % dd, "identb"], ["ptr"], out=ptr[:, kt - k0, :], in_=mask[:, kt * 128:(kt + 1) * 128], identity=identb[:])
                            P.op("act", "activation", ["ptr"], ["MT%d" % mp], out=MT[:, k0:k0 + n8, i * 128:(i + 1) * 128], in_=ptr[:, 0:n8, :], func=AF.Copy)

                def stage_att_head(qg, h):
                    os_ = qg % 2
                    g = h // 4
                    nkt = 4 * (qg + 1)
                    for kt in range(nkt):
                        ksl = slice(kt * 128, (kt + 1) * 128)
                        a = CN["cnt"] % 2
                        k = CN["cnt"] % 3
                        CN["cnt"] += 1
                        P.op("pe", "matmul", ["dkT", "dqT"], ["pS%d" % a], out=pS[a][:], lhsT=dkT[:, g, ksl], rhs=dqT[:, h, qg * 512:(qg + 1) * 512], start=True, stop=True)
                        P.op("act", "activation", ["pS%d" % a], ["pT%d" % k], out=pT[k][:], in_=pS[a][:], func=AF.Exp, scale=sc_dsa)
                        P.op("pool", "tensor_tensor", ["pT%d" % k, "MT%d" % os_], ["pT%d" % k], out=pT[k][:], in0=pT[k][:], in1=MTs[os_][:, kt, :], op=ALU.mult)
                        P.op("pe", "matmul", ["dvs", "pT%d" % k], ["pO"], out=pO[:], lhsT=dvs[:, kt, g * 128:(g + 1) * 128], rhs=pT[k][:], start=(kt == 0), stop=(kt == nkt - 1))
                        P.op("pe", "matmul", ["onesb", "pT%d" % k], ["pD"], out=pD[:], lhsT=onesb[:], rhs=pT[k][:], start=(kt == 0), stop=(kt == nkt - 1))
                    P.op("dve", "reciprocal", ["pD"], ["rden"], out=rden[:], in_=pD[:])
                    P.op("dve", "tensor_tensor", ["pO", "rden"], ["obst%d" % os_], out=obst[os_][:, h, :], in0=pO[:], in1=rden[:], op=ALU.mult)
                    if h == 7:
                        P.op("sp", "dma_start", ["obst%d" % os_], [], out=SC["obT"][:, :, qg * 512:(qg + 1) * 512].rearrange("h p s -> p h s"), in_=obst[os_][:])

                CN = dict(nD=0, nR=0, cnt=0)
                seq = [7, 6, 5, 4, 3, 2, 1, 0]
                stage_IS(seq[0])
                pending = None
                for idx, g in enumerate(seq):
                    if idx + 1 < len(seq):
                        stage_IS(seq[idx + 1])
                    stage_topk(g, att_qg=pending)
                    pending = None
                    stage_maskT(g, first=(g % 2 == 1))
                    if g % 2 == 0:
                        pending = g // 2
                for h in range(8):
                    stage_att_head(pending, h)
                P.emit(st)

        if "p4" in phases:
            with ExitStack() as st:
                sb = lambda n, s, d, _p="p4_": st.enter_context(nc.sbuf_tensor(_p + n, s, d))
                psb = lambda n, s, d, _p="p4_": st.enter_context(nc.psum_tensor(_p + n, s, d))
                P = Prog(nc, SEMS)
                wa = sb("wa", [128, 8, D], BF16)
                wb = sb("wb", [128, 8, D], BF16)
                wo = sb("wo", [128, 16, D], BF16)
                oat = [sb("oat%d" % i, [128, 8, 128], BF16) for i in range(2)]
                obt = [sb("obt%d" % i, [128, 8, 128], BF16) for i in range(2)]
                gst_ = [sb("gs%d" % i, [128, 4096], BF16) for i in range(1)] * 2
                xt = [sb("xt%d" % i, [128, D], F32) for i in range(2)]
                mgs = [sb("mg%d" % i, [128, D], BF16) for i in range(2)]
                mTs = [sb("mT%d" % i, [128, 16, 128], BF16) for i in range(2)]
                gsb2 = [sb("gsb%d" % i, [128, 4096], BF16) for i in range(2)]
                t1 = [sb("t1_%d" % i, [128, 512], F32) for i in range(2)]
                t2 = [sb("t2_%d" % i, [128, 512], F32) for i in range(2)]
                pY = [psb("pY%d" % i, [128, 512], F32) for i in range(4)]
                ptr = [psb("ptr%d" % i, [128, 8, 128], BF16) for i in range(2)]
                pZ = [psb("pZ%d" % i, [128, 512], F32) for i in range(2)]
                for h in range(8):
                    for hf in range(2):
                        P.op("pool", "dma_start", [], ["wa"], out=wa[:, h, hf * 1024:(hf + 1) * 1024], in_=IN["wa"][:, h, hf * 1024:(hf + 1) * 1024])
                        P.op("pool", "dma_start", [], ["wb"], out=wb[:, h, hf * 1024:(hf + 1) * 1024], in_=IN["wb"][:, h, hf * 1024:(hf + 1) * 1024])
                for dk in range(16):
                    for hf in range(2):
                        P.op("pool", "dma_start", [], ["wo"], out=wo[:, dk, hf * 1024:(hf + 1) * 1024], in_=IN["wo"][:, dk, hf * 1024:(hf + 1) * 1024])
                CNY = dict(ny=0)

                def stage_Y(T):
                    s = T % 2
                    tsl = slice(T * 128, (T + 1) * 128)
                    P.op("sp", "dma_start", [], ["oat%d" % s], out=oat[s][:], in_=SC["oaT"][:, :, tsl].rearrange("h p s -> p h s"))
                    P.op("sp", "dma_start", [], ["obt%d" % s], out=obt[s][:], in_=SC["obT"][:, :, tsl].rearrange("h p s -> p h s"))
                    P.op("sp", "dma_start", [], ["gs%d" % s], out=gsb2[s][:], in_=SC["gs"][tsl, :])
                    P.op("sp", "dma_start", [], ["xt%d" % s], out=xt[s][:], in_=IN["x"][tsl, :])
                    for cg in range(4):
                        csl = slice(cg * 512, (cg + 1) * 512)
                        ya = CNY["ny"] % 4
                        yb = (CNY["ny"] + 1) % 4
                        CNY["ny"] += 2
                        u = cg % 2
                        for h in range(8):
                            P.op("pe", "matmul", ["oat%d" % s, "wa"], ["pY%d" % ya], out=pY[ya][:], lhsT=oat[s][:, h, :], rhs=wa[:, h, csl], start=(h == 0), stop=(h == 7))
                        for h in range(8):
                            P.op("pe", "matmul", ["obt%d" % s, "wb"], ["pY%d" % yb], out=pY[yb][:], lhsT=obt[s][:, h, :], rhs=wb[:, h, csl], start=(h == 0), stop=(h == 7))
                        P.op("dve", "tensor_tensor", ["pY%d" % ya, "gs%d" % s], ["t1_%d" % u], out=t1[u][:], in0=pY[ya][:], in1=gsb2[s][:, csl], op=ALU.mult)
                        P.op("dve", "tensor_tensor", ["pY%d" % yb, "gs%d" % s], ["t2_%d" % u], out=t2[u][:], in0=pY[yb][:], in1=gsb2[s][:, 2048 + cg * 512:2048 + (cg + 1) * 512], op=ALU.mult)
                        P.op("pool", "tensor_tensor", ["t1_%d" % u, "t2_%d" % u], ["mg%d" % s], out=mgs[s][:, csl], in0=t1[u][:], in1=t2[u][:], op=ALU.add)

                def stage_TZ(T):
                    s = T % 2
                    tsl = slice(T * 128, (T + 1) * 128)
                    for dk in range(16):
                        b_ = dk // 8
                        P.op("pe", "transpose", ["mg%d" % s, "identb"], ["ptr%d" % b_], out=ptr[b_][:, dk % 8, :], in_=mgs[s][:, dk * 128:(dk + 1) * 128], identity=identb[:])
                    for b_ in range(2):
                        P.op("act", "activation", ["ptr%d" % b_], ["mT%d" % s], out=mTs[s][:, b_ * 8:(b_ + 1) * 8, :], in_=ptr[b_][:], func=AF.Copy)
                    for og in range(4):
                        csl = slice(og * 512, (og + 1) * 512)
                        z = og % 2
                        for dk in range(16):
                            P.op("pe", "matmul", ["mT%d" % s, "wo"], ["pZ%d" % z], out=pZ[z][:], lhsT=mTs[s][:, dk, :], rhs=wo[:, dk, csl], start=(dk == 0), stop=(dk == 15))
                        P.op("dve", "tensor_tensor", ["pZ%d" % z, "xt%d" % s], ["xt%d" % s], out=xt[s][:, csl], in0=pZ[z][:], in1=xt[s][:, csl], op=ALU.add)
                    P.op("sp", "dma_start", ["xt%d" % s], [], out=SC["x2"][tsl, :], in_=xt[s][:])

                stage_Y(0)
                for T in range(NT):
                    if T + 1 < NT:
                        stage_Y(T + 1)
                    stage_TZ(T)
                P.emit(st)

        if "p5" in phases:
            with ExitStack() as st:
                sb = lambda n, s, d, _p="p5_": st.enter_context(nc.sbuf_tensor(_p + n, s, d))
                psb = lambda n, s, d, _p="p5_": st.enter_context(nc.psum_tensor(_p + n, s, d))
                P = Prog(nc, SEMS)
                wpq = sb("wpq", [128, 16, 1024], BF16)
                kb = sb("kb", [128, 8, 256], BF16)
                gffnT = sb("gffnT", [128, 16], F32)
                gfin = sb("gfin", [128, D], F32)
                iota = sb("iota", [128, 16], F32)
                iota128 = sb("iota128", [128, 128], F32)
                GT = sb("GT", [128, 256, 128], BF16)
                NSB = 8
                OH2s = [sb("OH2_%d" % i, [128, NSB, 128], BF16) for i in range(2)]
                OH1s = [sb("OH1_%d" % i, [128, NSB, 128], BF16) for i in range(2)]
                hnTGs = [sb("hnTG%d" % i, [128, 16, 256], BF16) for i in range(2)]
                NUB = 4
                ub = [sb("ub%d" % i, [128, D], BF16) for i in range(NUB)]
                vb = [ub[i // 2][:, (i % 2) * 1024:(i % 2 + 1) * 1024] for i in range(2 * NUB)]
                x2t = [sb("x2t%d" % i, [128, D], F32) for i in range(2)]
                hnf = sb("hnf", [128, D], F32)
                qTs = sb("qTs", [128, 8, 128], BF16)
                ssb = sb("ssb", [128, 8, 256], F32)
                wk = sb("wk", [128, 256], F32)
                vals = sb("vals", [128, 16, 16], F32)
                idxs = sb("idxs", [128, 16, 16], U32)
                idxf = sb("idxf", [128, 16, 16], F32)
                tv = sb("tv", [128, 8, 16], F32)
                tp = sb("tp", [128, 8, 16], U32)
                ti = sb("ti", [128, 8, 16], U32)
                tj = sb("tj", [128, 8, 16], U32)
                tif = sb("tif", [128, 8, 16], F32)
                tjf = sb("tjf", [128, 8, 16], F32)
                e1 = [sb("e1_%d" % i, [128, 8, 16], F32) for i in range(4)]
                e2 = [sb("e2_%d" % i, [128, 8, 16], F32) for i in range(4)]
                gw = [sb("gw%d" % i, [128, 128], F32) for i in range(4)]
                selT = sb("selT", [128, 3, 128], F32)
                ex = sb("ex", [128, 8, 16], F32)
                zs = sb("zs", [128, 8], F32)
                ga = [sb("ga%d" % i, [128, 256], BF16) for i in range(2)]
                cst = [sb("cst%d" % i, [128, 256], BF16) for i in range(4)]
                stA = sb("stA", [128, 4], F32)
                stC = sb("stC", [128, 4], F32)
                B = [psb("B%d" % i, [128, 512], F32) for i in range(8)]
                for dk in range(16):
                    P.op("pool", "dma_start", [], ["wpq"], out=wpq[:, dk, :], in_=IN["wpq"][:, dk, :])
                for h in range(8):
                    P.op("pool", "dma_start", [], ["kb"], out=kb[:, h, :], in_=IN["kb"][:, h, :])
                P.op("sp", "dma_start", [], ["gffnT"], out=gffnT[:], in_=IN["gffnT"])
                P.op("sp", "dma_start", [], ["gfin"], out=gfin[:], in_=IN["gfin"].partition_broadcast(128))
                P.op("sp", "dma_start", [], ["iota"], out=iota[:], in_=IN["iota16"])
                P.op("sp", "dma_start", [], ["iota128"], out=iota128[:], in_=IN["iota128"])
                CN = dict(u=0, v=0, g=0, a=0, o=0)

                def stage_A(T):
                    p = T % 4
                    gp = (T // 2) % 2
                    hnTG = hnTGs[gp]
                    HT = "hnTG%d" % gp
                    tcol = slice((T % 2) * 128, (T % 2) * 128 + 128)
                    tsl = slice(T * 128, (T + 1) * 128)
                    yield P.op("sp", "dma_start", [], ["hnf"], out=hnf[:], in_=SC["x2"][tsl, :])
                    yield P.op("act", "activation", ["hnf"], ["ssb", "stA0"], out=ssb[:].rearrange("p a b -> p (a b)"), in_=hnf[:], func=AF.Square, accum_out=stA[:, 0:1])
                    yield P.op("act", "activation", ["stA0"], ["stA1"], out=stA[:, 1:2], in_=stA[:, 0:1], func=AF.Sqrt, scale=1.0 / D, bias=EPS)
                    yield P.op("dve", "reciprocal", ["stA1"], ["stA2"], out=stA[:, 2:3], in_=stA[:, 1:2])
                    yield P.op("act", "activation", ["hnf", "stA2"], ["hnf"], out=hnf[:], in_=hnf[:], func=AF.Copy, scale=stA[:, 2:3])
                    for r in range(4):
                        z = 6 + r % 2
                        for q4 in range(4):
                            dk = r * 4 + q4
                            yield P.op("pe", "transpose", ["hnf", "identf"], ["B%d" % z], out=B[z][:, q4 * 128:(q4 + 1) * 128], in_=hnf[:, dk * 128:(dk + 1) * 128], identity=identf[:])
                        for q4 in range(4):
                            dk = r * 4 + q4
                            if dk % 2 == 0:
                                yield P.op("act", "activation", ["B%d" % z, "gffnT"], [HT], out=hnTG[:, dk, tcol], in_=B[z][:, q4 * 128:(q4 + 1) * 128],
                                           func=AF.Copy, scale=gffnT[:, dk:dk + 1])
                            else:
                                yield P.op("dve", "tensor_scalar", ["B%d" % z, "gffnT"], [HT], out=hnTG[:, dk, tcol], in0=B[z][:, q4 * 128:(q4 + 1) * 128],
                                           scalar1=gffnT[:, dk:dk + 1], scalar2=None, op0=ALU.mult)
                    for hh in range(2):
                        z = 6 + hh
                        for h4 in range(4):
                            h = hh * 4 + h4
                            for dk in range(16):
                                yield P.op("pe", "matmul", ["wpq", HT], ["B%d" % z], out=B[z][:, h4 * 128:(h4 + 1) * 128], lhsT=wpq[:, dk, h * 128:(h + 1) * 128],
                                           rhs=hnTG[:, dk, tcol], start=(dk == 0), stop=(dk == 15))
                        yield P.op("act", "activation", ["B%d" % z], ["qTs"], out=qTs[:, hh * 4:(hh + 1) * 4, :].rearrange("p a b -> p (a b)"), in_=B[z][:], func=AF.Copy)
                    for h2 in range(4):
                        z = 6 + h2 % 2
                        for hi in range(2):
                            h = h2 * 2 + hi
                            yield P.op("pe", "matmul", ["qTs", "kb"], ["B%d" % z], out=B[z][:, hi * 256:(hi + 1) * 256], lhsT=qTs[:, h, :], rhs=kb[:, h, :], start=True, stop=True)
                        yield P.op("act", "activation", ["B%d" % z], ["ssb"], out=ssb[:, h2 * 2:h2 * 2 + 2, :].rearrange("p a b -> p (a b)"), in_=B[z][:], func=AF.Copy)
                    for hp in range(16):
                        src = ssb[:, hp // 2, (hp % 2) * 128:(hp % 2) * 128 + 128]
                        yield P.op("dve", "max", ["ssb"], ["vals"], out=vals[:, hp, 0:8], in_=src)
                        yield P.op("dve", "max_index", ["ssb", "vals"], ["idxs"], out=idxs[:, hp, 0:8], in_max=vals[:, hp, 0:8], in_values=src)
                        yield P.op("dve", "match_replace", ["ssb", "vals"], ["wk"], out=wk[:, 0:128], in_to_replace=vals[:, hp, 0:8], in_values=src, imm_value=NEG)
                        yield P.op("dve", "max", ["wk"], ["vals"], out=vals[:, hp, 8:16], in_=wk[:, 0:128])
                        yield P.op("dve", "max_index", ["wk", "vals"], ["idxs"], out=idxs[:, hp, 8:16], in_max=vals[:, hp, 8:16], in_values=wk[:, 0:128])
                    yield P.op("dve", "tensor_copy", ["idxs"], ["idxf"], out=idxf[:], in_=idxs[:])
                    cand = ssb[:].rearrange("p h (a b) -> p h a b", b=16)
                    v4 = vals[:].rearrange("p (h two) k -> p h two k", two=2)
                    i4 = idxf[:].rearrange("p (h two) k -> p h two k", two=2)
                    yield P.op("dve", "tensor_tensor", ["vals"], ["ssb"], out=cand, in0=v4[:, :, 0, :].unsqueeze(3).to_broadcast([128, 8, 16, 16]),
                         in1=v4[:, :, 1, :].unsqueeze(2).to_broadcast([128, 8, 16, 16]), op=ALU.add)
                    for h in range(8):
                        src = ssb[:, h, :]
                        yield P.op("dve", "max", ["ssb"], ["tv"], out=tv[:, h, 0:8], in_=src)
                        yield P.op("dve", "max_index", ["ssb", "tv"], ["tp"], out=tp[:, h, 0:8], in_max=tv[:, h, 0:8], in_values=src)
                        yield P.op("dve", "match_replace", ["ssb", "tv"], ["wk"], out=wk[:], in_to_replace=tv[:, h, 0:8], in_values=src, imm_value=NEG)
                        yield P.op("dve", "max", ["wk"], ["tv"], out=tv[:, h, 8:16], in_=wk[:])
                        yield P.op("dve", "max_index", ["wk", "tv"], ["tp"], out=tp[:, h, 8:16], in_max=tv[:, h, 8:16], in_values=wk[:])
                    yield P.op("dve", "tensor_single_scalar", ["tp"], ["ti"], out=ti[:], in_=tp[:], scalar=4, op=ALU.logical_shift_right)
                    yield P.op("dve", "tensor_single_scalar", ["tp"], ["tj"], out=tj[:], in_=tp[:], scalar=15, op=ALU.bitwise_and)
                    yield P.op("dve", "tensor_copy", ["ti"], ["tif"], out=tif[:], in_=ti[:])
                    yield P.op("dve", "tensor_copy", ["tj"], ["tjf"], out=tjf[:], in_=tj[:])
                    oh = ssb[:].rearrange("p h (a b) -> p h a b", b=16)
                    iob = iota[:].unsqueeze(1).unsqueeze(1).to_broadcast([128, 8, 16, 16])
                    for (sel, side, dst, dn) in ((tif, 0, e1[p], "e1_%d" % p), (tjf, 1, e2[p], "e2_%d" % p)):
                        yield P.op("dve", "tensor_tensor", ["tif", "tjf", "iota"], ["ssb"], out=oh, in0=sel[:].unsqueeze(3).to_broadcast([128, 8, 16, 16]), in1=iob, op=ALU.is_equal)
                        yield P.op("dve", "tensor_tensor", ["ssb", "idxf"], ["ssb"], out=oh, in0=oh, in1=i4[:, :, side, :].unsqueeze(2).to_broadcast([128, 8, 16, 16]), op=ALU.mult)
                        yield P.op("dve", "tensor_reduce", ["ssb"], [dn], out=dst[:], in_=oh, axis=AX.X, op=ALU.add)
                    yield P.op("dve", "tensor_tensor", ["tv"], ["ex"], out=ex[:], in0=tv[:], in1=tv[:, :, 0:1].to_broadcast([128, 8, 16]), op=ALU.subtract)
                    yield P.op("act", "activation", ["ex"], ["ex"], out=ex[:], in_=ex[:], func=AF.Exp)
                    yield P.op("dve", "tensor_reduce", ["ex"], ["zs"], out=zs[:], in_=ex[:], axis=AX.X, op=ALU.add)
                    yield P.op("dve", "reciprocal", ["zs"], ["zs"], out=zs[:], in_=zs[:])
                    yield P.op("dve", "tensor_tensor", ["ex", "zs"], ["gw%d" % p], out=gw[p][:].rearrange("p (a b) -> p a b", b=16), in0=ex[:], in1=zs[:].unsqueeze(2).to_broadcast([128, 8, 16]), op=ALU.mult)

                def stage_GT(T):
                    p = T % 4
                    tp_ = T % 2
                    srcs = [(e1[p][:].rearrange("p a b -> p (a b)"), "e1_%d" % p), (e2[p][:].rearrange("p a b -> p (a b)"), "e2_%d" % p), (gw[p][:], "gw%d" % p)]
                    for j, (ap_, nm) in enumerate(srcs):
                        P.op("pe", "transpose", [nm, "identf"], ["B7"], out=B[7][:, j * 128:(j + 1) * 128], in_=ap_, identity=identf[:])
                    P.op("act", "activation", ["B7"], ["selT"], out=selT[:].rearrange("p a b -> p (a b)"), in_=B[7][:, 0:384], func=AF.Copy)
                    for sub in range(128 // NSB):
                        tsub = slice(sub * NSB, (sub + 1) * NSB)
                        ob = CN["o"] % 2
                        CN["o"] += 1
                        OH2, OH1 = OH2s[ob], OH1s[ob]
                        iob = iota128[:].unsqueeze(1).to_broadcast([128, NSB, 128])
                        P.op("dve", "tensor_tensor", ["iota128", "selT"], ["OH2_%d" % ob], out=OH2[:], in0=iob, in1=selT[:, 1, tsub].unsqueeze(2).to_broadcast([128, NSB, 128]), op=ALU.is_equal)
                        P.op("dve", "tensor_tensor", ["iota128", "selT"], ["OH1_%d" % ob], out=OH1[:], in0=iob, in1=selT[:, 0, tsub].unsqueeze(2).to_broadcast([128, NSB, 128]), op=ALU.is_equal)
                        P.op("pool", "tensor_tensor", ["OH1_%d" % ob, "selT"], ["OH1_%d" % ob], out=OH1[:], in0=OH1[:], in1=selT[:, 2, tsub].unsqueeze(2).to_broadcast([128, NSB, 128]), op=ALU.mult)
                        for t4 in range(NSB // 4):
                            z = 6 + CN["g"] % 2
                            CN["g"] += 1
                            for tt in range(4):
                                t = t4 * 4 + tt
                                P.op("pe", "matmul", ["OH2_%d" % ob, "OH1_%d" % ob], ["B%d" % z], out=B[z][:, tt * 128:(tt + 1) * 128], lhsT=OH2[:, t, :], rhs=OH1[:, t, :], start=True, stop=True)
                            tg0 = tp_ * 128 + sub * NSB + t4 * 4
                            P.op("act", "activation", ["B%d" % z], ["GT"], out=GT[:, tg0:tg0 + 4, :].rearrange("p t c -> p (t c)"), in_=B[z][:], func=AF.Copy)

                def stage_U(G, step):
                    gp = G % 2
                    hnTG = hnTGs[gp]
                    for c in range(128):
                        b = CN["u"] % NUB
                        CN["u"] += 1
                        P.op("sp", "dma_start", [], ["ubh%d" % (2 * b), "ubh%d" % (2 * b + 1)], out=ub[b][:], in_=SC["pub"][c * 128:(c + 1) * 128, :])
                        z = 4 + (c // 2) % 2
                        reg = slice((c % 2) * 256, (c % 2) * 256 + 256)
                        for dk in range(16):
                            P.op("pe", "matmul", ["ubh%d" % (2 * b + dk // 8), "hnTG%d" % gp], ["B%d" % z], out=B[z][:, reg], lhsT=ub[b][:, dk * 128:(dk + 1) * 128], rhs=hnTG[:, dk, :], start=(dk == 0), stop=(dk == 15))
                        k = CN["a"] % 2
                        CN["a"] += 1
                        P.op("act", "activation", ["B%d" % z], ["ga%d" % k], out=ga[k][:], in_=B[z][:, reg], func=AF.Gelu)
                        P.op("dve", "tensor_tensor", ["ga%d" % k, "GT"], ["GT"], out=GT[:, :, c], in0=ga[k][:], in1=GT[:, :, c], op=ALU.mult)
                        step(3)

                def stage_V(G, step):
                    for p in range(2):
                        T = 2 * G + p
                        P.op("sp", "dma_start", [], ["x2t%d" % p], out=x2t[p][:], in_=SC["x2"][T * 128:(T + 1) * 128, :])
                    for hv in range(2):
                        for c in range(128):
                            b = CN["v"] % (2 * NUB)
                            CN["v"] += 1
                            P.op("sp", "dma_start", [], ["ubh%d" % b], out=vb[b], in_=SC["pvb"][c * 128:(c + 1) * 128, hv * 1024:(hv + 1) * 1024])
                            for p in range(2):
                                for cg in range(2):
                                    z = p * 2 + cg
                                    P.op("pe", "matmul", ["GT", "ubh%d" % b], ["B%d" % z], out=B[z][:], lhsT=GT[:, p * 128:(p + 1) * 128, c], rhs=vb[b][:, cg * 512:(cg + 1) * 512],
                                         start=(c == 0), stop=(c == 127))
                            step(3)
                        for p in range(2):
                            for cg in range(2):
                                z = p * 2 + cg
                                csl = slice(hv * 1024 + cg * 512, hv * 1024 + (cg + 1) * 512)
                                P.op("dve", "tensor_tensor", ["B%d" % z, "x2t%d" % p], ["x2t%d" % p], out=x2t[p][:, csl], in0=B[z][:], in1=x2t[p][:, csl], op=ALU.add)
                    for p in range(2):
                        T = 2 * G + p
                        tsl = slice(T * 128, (T + 1) * 128)
                        X = "x2t%d" % p
                        P.op("act", "activation", [X], ["ubh0", "ubh1", "stC0"], out=ub[0][:], in_=x2t[p][:], func=AF.Square, accum_out=stC[:, 0:1])
                        P.op("act", "activation", ["stC0"], ["stC1"], out=stC[:, 1:2], in_=stC[:, 0:1], func=AF.Sqrt, scale=1.0 / D, bias=EPS)
                        P.op("dve", "reciprocal", ["stC1"], ["stC2"], out=stC[:, 2:3], in_=stC[:, 1:2])
                        P.op("dve", "scalar_tensor_tensor", [X, "stC2", "gfin"], [X], out=x2t[p][:], in0=x2t[p][:], scalar=stC[:, 2:3], in1=gfin[:], op0=ALU.mult, op1=ALU.mult)
                        P.op("sp", "dma_start", [X], [], out=y[tsl, :], in_=x2t[p][:])

                def run_all(gen):
                    for _ in gen:
                        pass

                def chain(*gens):
                    for g_ in gens:
                        for v_ in g_:
                            yield v_

                run_all(stage_A(0))
                run_all(stage_A(1))
                for G in range(NT // 2):
                    stage_GT(2 * G)
                    stage_GT(2 * G + 1)
                    nxt = chain(stage_A(2 * G + 2), stage_A(2 * G + 3)) if G + 1 < NT // 2 else iter(())

                    def step(n, nxt=nxt):
                        for _ in range(n):
                            next(nxt, None)

                    stage_U(G, step)
                    stage_V(G, step)
                    run_all(nxt)
                P.emit(st)
    return nc


def kernel(**inputs):
    H = host_layout(inputs)
    x = np.asarray(inputs["x"], np.float32)
    pos = np.asarray(inputs["positions"]).astype(np.int32)
    nc = build_nc()
    in_maps = []
    for b in range(8):
        m = {k: H[k] for k in SHAPES if k not in ("x", "pos")}
        m["x"] = np.ascontiguousarray(x[b])
        m["pos"] = np.ascontiguousarray(pos[b])
        in_maps.append(m)
    res = run_bass_kernel_spmd(nc, in_maps, core_ids=list(range(8)))
    return np.stack([np.asarray(r["y"], dtype=np.float32) for r in res.results], axis=0)
```

```python
import numpy as np
from contextlib import ExitStack
import concourse.bass as bass
import concourse.mybir as mybir
from concourse.bass_utils import run_bass_kernel_spmd

F32 = mybir.dt.float32
BF16 = mybir.dt.bfloat16
I32 = mybir.dt.int32
U32 = mybir.dt.uint32
AF = mybir.ActivationFunctionType
ALU = mybir.AluOpType
AX = mybir.AxisListType

D = 2048
S = 2048
NT = 16
EPS = 1e-6
PI = float(np.pi)
MAGIC = 12582912.0
NEG = -1e30
ROPE_THETA = 500000.0

ENGS = ["pe", "act", "dve", "pool", "sp"]
N_DMA_SEMS = {"sp": 40, "pool": 24, "act": 8}


class Prog:
    def __init__(self, nc, semstate):
        self.nc = nc
        self.semstate = semstate
        self.ins = {e: [] for e in ENGS}
        self.last_w = {}
        self.readers = {}
        self.dma_rr = {e: 0 for e in N_DMA_SEMS}
        self.dma_cnt = {}

    def add(self, eng, fn, reads=(), writes=(), dma=False):
        idx = len(self.ins[eng])
        deps = set()
        for r in reads:
            w = self.last_w.get(r)
            if w is not None:
                deps.add(w)
        for r in writes:
            w = self.last_w.get(r)
            if w is not None:
                deps.add(w)
            for rd in self.readers.get(r, ()):
                deps.add(rd)
        deps.discard((eng, idx))
        rec = dict(fn=fn, deps=deps, dma=dma)
        if dma:
            j = self.dma_rr[eng]
            self.dma_rr[eng] = (j + 1) % N_DMA_SEMS[eng]
            n = self.dma_cnt.get((eng, j), 0) + 1
            self.dma_cnt[(eng, j)] = n
            rec["dsem"] = (eng, j, n)
        self.ins[eng].append(rec)
        for r in reads:
            self.readers.setdefault(r, []).append((eng, idx))
        for r in writes:
            self.last_w[r] = (eng, idx)
            self.readers[r] = []
        return (eng, idx)

    def op(self, eng, method, reads, writes, **kw):
        dma = method in ("dma_start", "indirect_dma_start")
        return self.add(eng, lambda e: getattr(e, method)(**kw), reads, writes, dma=dma)

    def emit(self, stack):
        nc = self.nc
        need = {e: set() for e in ENGS}
        for e in ENGS:
            for ins in self.ins[e]:
                pmax = {}
                ed = set()
                for (de, di) in ins["deps"]:
                    if not self.ins[de][di]["dma"]:
                        if not (de == "pe" and e == "pe"):
                            pmax[de] = max(pmax.get(de, -1), di)
                        continue
                    ed.add((de, di))
                for de, di in pmax.items():
                    ed.add((de, di))
                ins["deps"] = ed
                for (de, di) in ed:
                    if self.ins[de][di]["dma"]:
                        continue
                    need[de].add(di)
        sigcount = {}
        for e in ENGS:
            c = 0
            for i, ins in enumerate(self.ins[e]):
                if ins["dma"]:
                    continue
                if i in need[e]:
                    c += 1
                    sigcount[(e, i)] = c
        ss = self.semstate
        gstack = ss["stack"]
        for e in ["pe", "act", "dve", "pool"]:
            if e not in ss["esem"]:
                ss["esem"][e] = gstack.enter_context(nc.semaphore("se_%s" % e))
                ss["ebase"][e] = 0
        for e, n in N_DMA_SEMS.items():
            for j in range(n):
                if self.dma_cnt.get((e, j), 0) > 0 and (e, j) not in ss["dsem"]:
                    ss["dsem"][(e, j)] = gstack.enter_context(nc.semaphore("sd_%s_%d" % (e, j)))
                    ss["dbase"][(e, j)] = 0
        esem = ss["esem"]
        dsem = ss["dsem"]
        ebase = dict(ss["ebase"])
        dbase = dict(ss["dbase"])
        for (e, i) in list(sigcount.keys()):
            sigcount[(e, i)] += ebase[e]
        for e in ["pe", "act", "dve", "pool"]:
            ss["ebase"][e] += sum(1 for (ee, i) in sigcount if ee == e)
        for (e, j), n in self.dma_cnt.items():
            ss["dbase"][(e, j)] += n
        block = stack.enter_context(nc.Block())
        prog = self

        def run(ename, eng):
            waited = {}

            def w(key, sem, val):
                if waited.get(key, 0) >= val:
                    return
                eng.wait_ge(sem, val)
                waited[key] = val

            for i, ins in enumerate(prog.ins[ename]):
                for (de, di) in sorted(ins["deps"]):
                    dins = prog.ins[de][di]
                    if dins["dma"]:
                        (qe, j, n) = dins["dsem"]
                        w(("d", qe, j), dsem[(qe, j)], 16 * (n + dbase[(qe, j)]))
                    else:
                        if de == ename and ename == "pe":
                            continue
                        w(("e", de), esem[de], sigcount[(de, di)])
                if ins["dma"]:
                    (qe, j, n) = ins["dsem"]
                    if n > 1:
                        w(("d", qe, j), dsem[(qe, j)], 16 * (n - 1 + dbase[(qe, j)]))
                    inst = ins["fn"](eng)
                    inst.then_inc(dsem[(qe, j)], 16)
                else:
                    inst = ins["fn"](eng)
                    if (ename, i) in sigcount:
                        inst.then_inc(esem[ename], 1)
            for (qe, j), n in prog.dma_cnt.items():
                if qe == ename:
                    w(("d", qe, j), dsem[(qe, j)], 16 * (n + dbase[(qe, j)]))

        block.tensor(lambda eng: run("pe", eng))
        block.scalar(lambda eng: run("act", eng))
        block.vector(lambda eng: run("dve", eng))
        block.gpsimd(lambda eng: run("pool", eng))
        block.sync(lambda eng: run("sp", eng))


O_CQ, O_CKV, O_KR, O_DQ, O_DK, O_DV, O_IQ, O_IK, O_IW, O_G = 0, 512, 768, 832, 1856, 2112, 2368, 3392, 3456, 3472


def _rot_perm(n, half, rot, blocks=1, bw=None):
    bw = bw or n
    idx = np.arange(n)
    out = idx.copy()
    for b in range(n // bw):
        o = b * bw
        out[o:o + half] = idx[o + half:o + rot]
        out[o + half:o + rot] = idx[o:o + half]
    return out


def _pk(w, ncol):
    K = w.shape[0]
    return np.ascontiguousarray(w.reshape(K // 128, 128, ncol).transpose(1, 0, 2))


def host_layout(inp):
    f = np.float32
    w_in = np.asarray(inp["w_in"], f)[0]
    out = {}
    tiles = []
    for h in range(8):
        tiles.append(np.arange(O_DQ + h * 128, O_DQ + (h + 1) * 128))
    for g in range(2):
        tiles.append(np.arange(O_DK + g * 128, O_DK + (g + 1) * 128))
    for hp in range(8):
        tiles.append(np.arange(O_IQ + hp * 128, O_IQ + (hp + 1) * 128))
    tiles.append(np.concatenate([np.arange(O_IK, O_IK + 64)] * 2))
    tiles.append(np.concatenate([np.arange(O_KR, O_KR + 64)] * 2))
    perms = [_rot_perm(128, 16, 32)] * 10 + [_rot_perm(128, 8, 16, bw=64)] * 9 + [_rot_perm(128, 32, 64, bw=64)]
    w1f = np.empty((20, 128, 16, 256), f)
    for i, (cols, pm) in enumerate(zip(tiles, perms)):
        w1f[i, :, :, 0:128] = _pk(w_in[:, cols], 128)
        w1f[i, :, :, 128:256] = _pk(w_in[:, cols[pm]], 128)
    out["w1f"] = w1f
    tcols = [np.arange(O_CQ, O_CQ + 512), np.concatenate([np.arange(O_CKV, O_CKV + 256), np.arange(O_DV, O_DV + 256)])]
    for j in range(8):
        tcols.append(np.arange(O_G + j * 512, O_G + (j + 1) * 512))
    out["w1t"] = np.stack([_pk(w_in[:, c], 512) for c in tcols])
    out["w1iw"] = _pk(w_in[:, O_IW:O_IW + 16], 16)
    out["gmixT"] = np.ascontiguousarray(np.asarray(inp["norm_mix_g"], f)[0].reshape(16, 128).T)
    rc = np.zeros((128, 9), f)
    for ty, (half, rot, bw) in enumerate([(16, 32, 128), (8, 16, 64), (32, 64, 64)]):
        invf = (ROPE_THETA ** (-(np.arange(half, dtype=np.float32) * 2.0) / rot)).astype(f)
        for r in range(128):
            j = r % bw
            rc[r, ty * 3 + 1] = PI / 2
            if j < rot:
                rc[r, ty * 3 + 0] = invf[j % half]
                rc[r, ty * 3 + 2] = PI if j < half else 0.0
    out["ropec"] = rc
    out["ident"] = np.eye(128, dtype=f)
    wuq = np.asarray(inp["mla_w_uq"], f)[0]
    pm = _rot_perm(64, 32, 64)
    wq = np.empty((8, 128, 4, 256), f)
    for h in range(8):
        wq[h, :, :, 0:128] = _pk(wuq[:, h, 0:128], 128)
        wq[h, :, :, 128:192] = _pk(wuq[:, h, 128:192], 64)
        wq[h, :, :, 192:256] = _pk(wuq[:, h, 128:192][:, pm], 64)
    out["wq"] = wq
    out["gq"] = np.ascontiguousarray(np.asarray(inp["mla_q_norm_g"], f)[0].reshape(4, 128).T)
    out["gkv"] = np.ascontiguousarray(np.asarray(inp["mla_kv_norm_g"], f)[0].reshape(2, 128).T)
    out["wuk"] = _pk(np.asarray(inp["mla_w_uk"], f)[0].reshape(256, 1024), 1024)
    out["wuv"] = _pk(np.asarray(inp["mla_w_uv"], f)[0].reshape(256, 1024), 1024)
    out["wa"] = _pk(np.asarray(inp["w_branch_a"], f)[0], 2048)
    out["wb"] = _pk(np.asarray(inp["w_branch_b"], f)[0], 2048)
    out["wo"] = _pk(np.asarray(inp["w_out"], f)[0], 2048)
    out["wpq"] = _pk(np.asarray(inp["peer_w_q"], f)[0].reshape(2048, 1024), 1024)
    sk = np.asarray(inp["peer_sub_keys"], f)[0]
    kb = np.zeros((128, 8, 256), f)
    for h in range(8):
        for p in range(2):
            kb[p * 64:(p + 1) * 64, h, p * 128:(p + 1) * 128] = sk[h, p].T
    out["kb"] = kb
    out["gffn"] = np.asarray(inp["norm_ffn_g"], f)[0]
    out["gfin"] = np.asarray(inp["norm_final_g"], f)
    out["put"] = np.ascontiguousarray(np.asarray(inp["peer_u"], f)[0].reshape(128, 128, 16, 128).transpose(0, 3, 2, 1)).reshape(16384, 2048)
    out["gffnT"] = np.ascontiguousarray(np.asarray(inp["norm_ffn_g"], f)[0].reshape(16, 128).T)
    out["iota128"] = np.tile(np.arange(128, dtype=f)[None, :], (128, 1))
    out["pv"] = np.asarray(inp["peer_v"], f)[0]
    out["iota16"] = np.tile(np.arange(16, dtype=f)[None, :], (128, 1))
    return out


SHAPES = {
    "x": ([S, D], F32), "pos": ([S], I32),
    "w1f": ([20, 128, 16, 256], F32), "w1t": ([10, 128, 16, 512], F32), "w1iw": ([128, 16, 16], F32),
    "gmixT": ([128, 16], F32), "ropec": ([128, 9], F32), "ident": ([128, 128], F32),
    "wq": ([8, 128, 4, 256], F32), "gq": ([128, 4], F32), "gkv": ([128, 2], F32),
    "wuk": ([128, 2, 1024], F32), "wuv": ([128, 2, 1024], F32),
    "wa": ([128, 8, 2048], F32), "wb": ([128, 8, 2048], F32), "wo": ([128, 16, 2048], F32),
    "wpq": ([128, 16, 1024], F32), "kb": ([128, 8, 256], F32), "gffn": ([D], F32), "gfin": ([D], F32),
    "put": ([16384, D], F32), "pv": ([16384, D], F32), "iota16": ([128, 16], F32),
    "gffnT": ([128, 16], F32), "iota128": ([128, 128], F32),
}


def build_nc(phases=("p0", "p1", "p2", "p3", "p4", "p5"), debug=()):
    nc = bass.Bass("TRN2", target_bir_lowering=False)
    IN = {k: nc.dram_tensor(k, sh, dt, kind="ExternalInput").ap() for k, (sh, dt) in SHAPES.items()}
    y = nc.dram_tensor("y", [S, D], F32, kind="ExternalOutput").ap()

    def scratch(name, shape, dt):
        kind = "ExternalOutput" if name in debug else "Internal"
        return nc.dram_tensor(name, shape, dt, kind=kind).ap()

    SC = dict(
        dqT=scratch("dqT", [8, 128, S], BF16), dkT=scratch("dkT", [2, 128, S], BF16),
        iqT=scratch("iqT", [8, 128, S], BF16), ikT=scratch("ikT", [128, S], BF16),
        kpeT=scratch("kpeT", [128, S], BF16), cqnT=scratch("cqnT", [128, 4, S], BF16),
        ckvnT=scratch("ckvnT", [128, 2, S], BF16), dv=scratch("dv", [S, 256], BF16),
        iw=scratch("iw", [S, 16], F32), gs=scratch("gs", [S, 4096], BF16),
        oaT=scratch("oaT", [8, 128, S], BF16), obT=scratch("obT", [8, 128, S], BF16),
        x2=scratch("x2", [S, D], F32),
        pub=scratch("pub", [16384, D], BF16), pvb=scratch("pvb", [16384, D], BF16),
    )

    with ExitStack() as gst:
        def gsb(name, shape, dt):
            return gst.enter_context(nc.sbuf_tensor(name, shape, dt))

        SEMS = dict(stack=gst, esem={}, dsem={}, ebase={}, dbase={})
        identb = gsb("identb", [128, 128], BF16)
        identf = gsb("identf", [128, 128], F32)
        onesb = gsb("onesb", [128, 128], BF16)
        trig_cm = nc.sbuf_tensor("trig", [128, 6, S], BF16)
        trig = trig_cm.__enter__()

        if "p0" in phases:
            with ExitStack() as st:
                sb = lambda n, s, d, _p="q1_": st.enter_context(nc.sbuf_tensor(_p + n, s, d))
                P = Prog(nc, SEMS)
                posi = sb("posi", [128, S], I32)
                posf = sb("posf", [128, S], F32)
                ang = sb("ang", [128, S], F32)
                kk = sb("kk", [128, S], F32)
                rc = sb("rc", [128, 9], F32)
                P.op("sp", "dma_start", [], ["identf"], out=identf[:], in_=IN["ident"])
                P.op("sp", "dma_start", [], ["rc"], out=rc[:], in_=IN["ropec"])
                P.op("sp", "dma_start", [], ["posi"], out=posi[:], in_=IN["pos"].partition_broadcast(128))
                P.op("dve", "tensor_copy", ["identf"], ["identb"], out=identb[:], in_=identf[:])
                P.op("pool", "memset", [], ["onesb"], ap=onesb[:], constant=1.0)
                P.op("dve", "tensor_copy", ["posi"], ["posf"], out=posf[:], in_=posi[:])
                for ty in range(3):
                    for cs in range(2):
                        P.op("dve", "tensor_scalar", ["posf", "rc"], ["ang"], out=ang[:], in0=posf[:],
                             scalar1=rc[:, ty * 3:ty * 3 + 1], scalar2=rc[:, ty * 3 + 1 + cs:ty * 3 + 2 + cs],
                             op0=ALU.mult, op1=ALU.add)
                        P.op("dve", "tensor_scalar", ["ang"], ["kk"], out=kk[:], in0=ang[:], scalar1=1.0 / (2 * PI),
                             scalar2=MAGIC, op0=ALU.mult, op1=ALU.add)
                        P.op("dve", "tensor_scalar", ["kk"], ["kk"], out=kk[:], in0=kk[:], scalar1=-MAGIC, scalar2=None,
                             op0=ALU.add)
                        P.op("dve", "scalar_tensor_tensor", ["kk", "ang"], ["kk"], out=kk[:], in0=kk[:], scalar=-2 * PI,
                             in1=ang[:], op0=ALU.mult, op1=ALU.add)
                        P.op("dve", "tensor_scalar", ["kk"], ["kk"], out=kk[:], in0=kk[:], scalar1=-PI, scalar2=PI,
                             op0=ALU.max, op1=ALU.min)
                        P.op("act", "activation", ["kk"], ["trig%d" % (ty * 2 + cs)], out=trig[:, ty * 2 + cs, :],
                             in_=kk[:], func=AF.Sin)
                P.emit(st)

        if "p1" in phases:
            with ExitStack() as st:
                sb = lambda n, s, d, _p="q2_": st.enter_context(nc.sbuf_tensor(_p + n, s, d))
                psb = lambda n, s, d, _p="q4_": st.enter_context(nc.psum_tensor(_p + n, s, d))
                P = Prog(nc, SEMS)
                hT = sb("hT", [128, 16, S], BF16)
                xt = [sb("xt%d" % i, [128, D], F32) for i in range(2)]
                xs = [sb("xs%d" % i, [128, D], BF16) for i in range(2)]
                junk = sb("junk", [128, D], BF16)
                ss = sb("ss", [128, 16], F32)
                sq = sb("sq", [128, 16], F32)
                rstd = sb("rstd", [128, 16], F32)
                gmix = sb("gmix", [128, 16], F32)
                wbuf = [sb("wbuf%d" % i, [128, 16, 512], BF16) for i in range(2)]
                ost = [sb("ost%d" % i, [128, 16, 512], BF16) for i in range(2)]
                tmp = [sb("tmp%d" % i, [128, 512], F32) for i in range(4)]
                cqs = sb("cqs", [128, 16], F32)
                cqn = [sb("cqn%d" % i, [128, 512], BF16) for i in range(2)]
                iwst = sb("iwst", [128, 16, 16], F32)
                wiw = sb("wiw", [128, 16, 16], BF16)
                ptr = [psb("ptr%d" % i, [128, 8, 128], BF16) for i in range(2)]
                pA = [psb("pA%d" % i, [128, 512], F32) for i in range(3)]
                pB = [psb("pB%d" % i, [128, 512], F32) for i in range(3)]
                P.op("sp", "dma_start", [], ["gmix"], out=gmix[:], in_=IN["gmixT"])
                for T in range(NT):
                    s = T % 2
                    tsl = slice(T * 128, (T + 1) * 128)
                    P.op("sp", "dma_start", [], ["xt%d" % s], out=xt[s][:], in_=IN["x"][tsl, :])
                    P.op("act", "activation", ["xt%d" % s], ["junk", "ss%d" % T], out=junk[:], in_=xt[s][:], func=AF.Square,
                         accum_out=ss[:, T:T + 1])
                    P.op("act", "activation", ["ss%d" % T], ["sq%d" % T], out=sq[:, T:T + 1], in_=ss[:, T:T + 1], func=AF.Sqrt,
                         scale=1.0 / D, bias=EPS)
                    P.op("dve", "reciprocal", ["sq%d" % T], ["rstd%d" % T], out=rstd[:, T:T + 1], in_=sq[:, T:T + 1])
                    P.op("act", "activation", ["xt%d" % s, "rstd%d" % T], ["xs%d" % s], out=xs[s][:], in_=xt[s][:], func=AF.Copy,
                         scale=rstd[:, T:T + 1])
                    for dk in range(16):
                        b = dk // 8
                        P.op("pe", "transpose", ["xs%d" % s, "identb"], ["ptr%d" % b], out=ptr[b][:, dk % 8, :],
                             in_=xs[s][:, dk * 128:(dk + 1) * 128], identity=identb[:])
                    for dk in range(16):
                        b = dk // 8
                        if dk % 2 == 0:
                            P.op("act", "activation", ["ptr%d" % b, "gmix"], ["hT%d" % T], out=hT[:, dk, tsl], in_=ptr[b][:, dk % 8, :],
                                 func=AF.Copy, scale=gmix[:, dk:dk + 1])
                        else:
                            P.op("dve", "tensor_scalar", ["ptr%d" % b, "gmix"], ["hT%d" % T], out=hT[:, dk, tsl], in0=ptr[b][:, dk % 8, :],
                                 scalar1=gmix[:, dk:dk + 1], scalar2=None, op0=ALU.mult)
                hT_all = ["hT%d" % T for T in range(NT)]
                dests = [SC["dqT"][h] for h in range(8)] + [SC["dkT"][g] for g in range(2)] + [SC["iqT"][h] for h in range(8)] + [SC["ikT"], SC["kpeT"]]
                types = [0] * 10 + [1] * 9 + [2]
                nblk = 0
                for bi in range(20):
                    ws = nblk % 2
                    osl = nblk % 2
                    nblk += 1
                    ty = types[bi]
                    if bi == 0:
                        P.op("pool", "dma_start", [], ["wbuf%d" % ws], out=wbuf[ws][:, :, 0:256], in_=IN["w1f"][bi])
                    if bi + 1 < 20:
                        P.op("pool", "dma_start", [], ["wbuf%d" % (1 - ws)], out=wbuf[1 - ws][:, :, 0:256], in_=IN["w1f"][bi + 1])
                    else:
                        P.op("pool", "dma_start", [], ["wbuf%d" % (1 - ws)], out=wbuf[1 - ws][:], in_=IN["w1t"][0])
                    for tg in range(4):
                        csl = slice(tg * 512, (tg + 1) * 512)
                        pi = (bi * 4 + tg) % 3
                        for dk in range(16):
                            P.op("pe", "matmul", ["wbuf%d" % ws] + hT_all[tg * 4:tg * 4 + 4], ["pA%d" % pi], out=pA[pi][:],
                                 lhsT=wbuf[ws][:, dk, 0:128], rhs=hT[:, dk, csl], start=(dk == 0), stop=(dk == 15))
                        for dk in range(16):
                            P.op("pe", "matmul", ["wbuf%d" % ws] + hT_all[tg * 4:tg * 4 + 4], ["pB%d" % pi], out=pB[pi][:],
                                 lhsT=wbuf[ws][:, dk, 128:256], rhs=hT[:, dk, csl], start=(dk == 0), stop=(dk == 15))
                        ti = (bi * 4 + tg) % 2
                        P.op("dve", "tensor_tensor", ["pA%d" % pi, "trig"], ["tmp%d" % (2 * ti)], out=tmp[2 * ti][:], in0=pA[pi][:],
                             in1=trig[:, ty * 2, csl], op=ALU.mult)
                        P.op("dve", "tensor_tensor", ["pB%d" % pi, "trig"], ["tmp%d" % (2 * ti + 1)], out=tmp[2 * ti + 1][:], in0=pB[pi][:],
                             in1=trig[:, ty * 2 + 1, csl], op=ALU.mult)
                        P.op("pool", "tensor_tensor", ["tmp%d" % (2 * ti), "tmp%d" % (2 * ti + 1)], ["ost%d" % osl],
                             out=ost[osl][:].rearrange("p a b -> p (a b)")[:, csl], in0=tmp[2 * ti][:], in1=tmp[2 * ti + 1][:], op=ALU.add)
                    P.op("sp", "dma_start", ["ost%d" % osl], [], out=dests[bi], in_=ost[osl][:].rearrange("p a b -> p (a b)")[:, 0:S])
                for bi in range(10):
                    ws = nblk % 2
                    osl = nblk % 2
                    nblk += 1
                    if bi + 1 < 10:
                        P.op("pool", "dma_start", [], ["wbuf%d" % (1 - ws)], out=wbuf[1 - ws][:], in_=IN["w1t"][bi + 1])
                    for T in range(NT):
                        tsl = slice(T * 128, (T + 1) * 128)
                        pi = T % 3
                        for dk in range(16):
                            P.op("pe", "matmul", ["wbuf%d" % ws, "hT%d" % T], ["pA%d" % pi], out=pA[pi][:], lhsT=hT[:, dk, tsl],
                                 rhs=wbuf[ws][:, dk, :], start=(dk == 0), stop=(dk == 15))
                        if bi >= 2:
                            P.op("act", "activation", ["pA%d" % pi], ["ost%d" % osl], out=ost[osl][:, T, :], in_=pA[pi][:], func=AF.Sigmoid)
                        else:
                            ncq = 512 if bi == 0 else 256
                            c = T % 2
                            P.op("act", "activation", ["pA%d" % pi], ["tmp0", "cqs"], out=tmp[0][:, 0:ncq], in_=pA[pi][:, 0:ncq], func=AF.Square,
                                 accum_out=cqs[:, 0:1])
                            P.op("act", "activation", ["cqs"], ["cqs1"], out=cqs[:, 1:2], in_=cqs[:, 0:1], func=AF.Sqrt, scale=1.0 / ncq, bias=EPS)
                            P.op("dve", "reciprocal", ["cqs1"], ["cqs2"], out=cqs[:, 2:3], in_=cqs[:, 1:2])
                            P.op("dve", "tensor_scalar", ["pA%d" % pi, "cqs2"], ["cqn%d" % c], out=cqn[c][:, 0:ncq], in0=pA[pi][:, 0:ncq],
                                 scalar1=cqs[:, 2:3], scalar2=None, op0=ALU.mult)
                            if bi == 1:
                                P.op("act", "activation", ["pA%d" % pi], ["ost%d" % osl], out=ost[osl][:, T, 0:256], in_=pA[pi][:, 256:512], func=AF.Copy)
                            nk = ncq // 128
                            for kc in range(nk):
                                P.op("pe", "transpose", ["cqn%d" % c, "identb"], ["ptr%d" % c], out=ptr[c][:, kc, :],
                                     in_=cqn[c][:, kc * 128:(kc + 1) * 128], identity=identb[:])
                            lo = 0 if bi == 0 else 256
                            P.op("act", "activation", ["ptr%d" % c], ["ost%d" % osl], out=ost[osl][:, T, lo:lo + ncq].rearrange("p (k q) -> p k q", q=128),
                                 in_=ptr[c][:, 0:nk, :], func=AF.Copy)
                    if bi >= 2:
                        P.op("sp", "dma_start", ["ost%d" % osl], [], out=SC["gs"][:, (bi - 2) * 512:(bi - 1) * 512].rearrange("(t p) c -> p t c", p=128),
                             in_=ost[osl][:])
                    else:
                        nk = 4 if bi == 0 else 2
                        lo = 0 if bi == 0 else 256
                        dst = SC["cqnT"] if bi == 0 else SC["ckvnT"]
                        for kc in range(nk):
                            P.op("sp", "dma_start", ["ost%d" % osl], [], out=dst[:, kc, :].rearrange("p (t q) -> p t q", q=128),
                                 in_=ost[osl][:, :, lo + kc * 128:lo + (kc + 1) * 128])
                        if bi == 1:
                            P.op("sp", "dma_start", ["ost%d" % osl], [], out=SC["dv"].rearrange("(t p) c -> p t c", p=128), in_=ost[osl][:, :, 0:256])
                P.op("pool", "dma_start", [], ["wiw"], out=wiw[:], in_=IN["w1iw"])
                for T in range(NT):
                    tsl = slice(T * 128, (T + 1) * 128)
                    pi = T % 3
                    for dk in range(16):
                        P.op("pe", "matmul", ["wiw", "hT%d" % T], ["pB%d" % pi], out=pB[pi][:, 0:16], lhsT=hT[:, dk, tsl], rhs=wiw[:, dk, :],
                             start=(dk == 0), stop=(dk == 15))
                    P.op("act", "activation", ["pB%d" % pi], ["iwst"], out=iwst[:, T, :], in_=pB[pi][:, 0:16], func=AF.Copy)
                P.op("sp", "dma_start", ["iwst"], [], out=SC["iw"].rearrange("(t p) c -> p t c", p=128), in_=iwst[:])
                P.emit(st)

        if "p2" in phases:
            with ExitStack() as st:
                sb = lambda n, s, d, _p="q3_": st.enter_context(nc.sbuf_tensor(_p + n, s, d))
                psb = lambda n, s, d, _p="q5_": st.enter_context(nc.psum_tensor(_p + n, s, d))
                P = Prog(nc, SEMS)
                cq = sb("cq_s", [128, 4, S], BF16)
                ckv = sb("ckv_s", [128, 2, S], BF16)
                kpe = sb("kpe_s", [128, S], BF16)
                wq = [sb("wq%d" % i, [128, 4, 256], BF16) for i in range(2)]
                gq = sb("gq", [128, 4], F32)
                gkv = sb("gkv", [128, 2], F32)
                wuk = sb("wuk", [128, 2, 1024], BF16)
                wuv = sb("wuv", [128, 2, 1024], BF16)
                vall = sb("vall", [128, 16, 1024], BF16)
                qn = [sb("qn%d" % i, [128, S], BF16) for i in range(2)]
                qr = [sb("qr%d" % i, [128, S], BF16) for i in range(2)]
                kn = [sb("kn%d" % i, [128, S], BF16) for i in range(2)]
                pT = [sb("pT%d" % i, [128, 512], BF16) for i in range(3)]
                rden = sb("rden", [128, 512], F32)
                ost = [sb("oast%d" % i, [128, S], BF16) for i in range(2)]
                t1 = sb("t1", [128, 512], F32)
                t2 = sb("t2", [128, 512], F32)
                pp = [psb("pp%d" % i, [128, 512], F32) for i in range(4)]
                pS = [psb("pS%d" % i, [128, 512], F32) for i in range(2)]
                pO = psb("pO", [128, 512], F32)
                pD = psb("pD", [128, 512], F32)
                P.op("sp", "dma_start", [], ["cq"], out=cq[:], in_=SC["cqnT"])
                P.op("sp", "dma_start", [], ["ckv"], out=ckv[:], in_=SC["ckvnT"])
                P.op("sp", "dma_start", [], ["kpe"], out=kpe[:], in_=SC["kpeT"])
                P.op("sp", "dma_start", [], ["gq"], out=gq[:], in_=IN["gq"])
                P.op("sp", "dma_start", [], ["gkv"], out=gkv[:], in_=IN["gkv"])
                for kc in range(2):
                    P.op("pool", "dma_start", [], ["wuk"], out=wuk[:, kc, :], in_=IN["wuk"][:, kc, :])
                    P.op("pool", "dma_start", [], ["wuv"], out=wuv[:, kc, :], in_=IN["wuv"][:, kc, :])
                for kc in range(2):
                    P.op("dve", "tensor_scalar", ["wuk", "gkv"], ["wuk"], out=wuk[:, kc, :], in0=wuk[:, kc, :], scalar1=gkv[:, kc:kc + 1], scalar2=None, op0=ALU.mult)
                    P.op("dve", "tensor_scalar", ["wuv", "gkv"], ["wuv"], out=wuv[:, kc, :], in0=wuv[:, kc, :], scalar1=gkv[:, kc:kc + 1], scalar2=None, op0=ALU.mult)
                n = 0
                for kt in range(16):
                    ksl = slice(kt * 128, (kt + 1) * 128)
                    for hf in range(2):
                        pi = n % 4
                        n += 1
                        for kc in range(2):
                            P.op("pe", "matmul", ["ckv", "wuv"], ["pp%d" % pi], out=pp[pi][:], lhsT=ckv[:, kc, ksl], rhs=wuv[:, kc, hf * 512:(hf + 1) * 512],
                                 start=(kc == 0), stop=(kc == 1))
                        P.op("act", "activation", ["pp%d" % pi], ["vall"], out=vall[:, kt, hf * 512:(hf + 1) * 512], in_=pp[pi][:], func=AF.Copy)
                sc_mla = float(192 ** -0.5)
                def prep(h):
                    s = h % 2
                    yield P.op("pool", "dma_start", [], ["wq%d" % s], out=wq[s][:], in_=IN["wq"][h])
                    for kc in range(4):
                        yield P.op("dve", "tensor_scalar", ["wq%d" % s, "gq"], ["wq%d" % s], out=wq[s][:, kc, :], in0=wq[s][:, kc, :], scalar1=gq[:, kc:kc + 1], scalar2=None, op0=ALU.mult)
                    for tg in range(4):
                        csl = slice(tg * 512, (tg + 1) * 512)
                        for kc in range(4):
                            yield P.op("pe", "matmul", ["wq%d" % s, "cq"], ["pp0"], out=pp[0][:], lhsT=wq[s][:, kc, 0:128], rhs=cq[:, kc, csl], start=(kc == 0), stop=(kc == 3))
                        yield P.op("act", "activation", ["pp0"], ["qn%d" % s], out=qn[s][:, csl], in_=pp[0][:], func=AF.Copy)
                        for kc in range(4):
                            yield P.op("pe", "matmul", ["wq%d" % s, "cq"], ["pp1"], out=pp[1][0:64, :], lhsT=wq[s][:, kc, 128:192], rhs=cq[:, kc, csl], start=(kc == 0), stop=(kc == 3))
                        for kc in range(4):
                            yield P.op("pe", "matmul", ["wq%d" % s, "cq"], ["pp2"], out=pp[2][0:64, :], lhsT=wq[s][:, kc, 192:256], rhs=cq[:, kc, csl], start=(kc == 0), stop=(kc == 3))
                        yield P.op("dve", "tensor_tensor", ["pp1", "trig"], ["t1"], out=t1[0:64, :], in0=pp[1][0:64, :], in1=trig[0:64, 4, csl], op=ALU.mult)
                        yield P.op("dve", "tensor_tensor", ["pp2", "trig"], ["t2"], out=t2[0:64, :], in0=pp[2][0:64, :], in1=trig[0:64, 5, csl], op=ALU.mult)
                        yield P.op("pool", "tensor_tensor", ["t1", "t2"], ["qr%d" % s], out=qr[s][0:64, csl], in0=t1[0:64, :], in1=t2[0:64, :], op=ALU.add)
                        for kc in range(2):
                            yield P.op("pe", "matmul", ["wuk", "ckv"], ["pp3"], out=pp[3][:], lhsT=wuk[:, kc, h * 128:(h + 1) * 128], rhs=ckv[:, kc, csl], start=(kc == 0), stop=(kc == 1))
                        yield P.op("act", "activation", ["pp3"], ["kn%d" % s], out=kn[s][:, csl], in_=pp[3][:], func=AF.Copy)

                def att(h, step):
                    s = h % 2
                    cnt = 0
                    for qg in range(4):
                        nkt = 4 * (qg + 1)
                        for kt in range(nkt):
                            j = kt - 4 * qg
                            c0 = 128 * j if j > 0 else 0
                            cols = slice(qg * 512 + c0, (qg + 1) * 512)
                            ksl = slice(kt * 128, (kt + 1) * 128)
                            a = cnt % 2
                            k = cnt % 3
                            cnt += 1
                            P.op("pe", "matmul", ["kn%d" % s, "qn%d" % s], ["pS%d" % a], out=pS[a][:, c0:512], lhsT=kn[s][:, ksl], rhs=qn[s][:, cols], start=True, stop=False)
                            P.op("pe", "matmul", ["kpe", "qr%d" % s], ["pS%d" % a], out=pS[a][:, c0:512], lhsT=kpe[0:64, ksl], rhs=qr[s][0:64, cols], start=False, stop=True)
                            P.op("act", "activation", ["pS%d" % a], ["pT%d" % k], out=pT[k][:, c0:512], in_=pS[a][:, c0:512], func=AF.Exp, scale=sc_mla)
                            if j >= 0:
                                P.op("pool", "memset", [], ["pT%d" % k], ap=pT[k][64:128, c0:c0 + 64], constant=0.0)
                            P.op("pe", "matmul", ["vall", "pT%d" % k], ["pO"], out=pO[:, c0:512], lhsT=vall[:, kt, h * 128:(h + 1) * 128], rhs=pT[k][:, c0:512],
                                 start=(kt == 0), stop=(kt == nkt - 1))
                            P.op("pe", "matmul", ["onesb", "pT%d" % k], ["pD"], out=pD[:, c0:512], lhsT=onesb[:], rhs=pT[k][:, c0:512],
                                 start=(kt == 0), stop=(kt == nkt - 1))
                            step(3)
                        P.op("dve", "reciprocal", ["pD"], ["rden"], out=rden[:], in_=pD[:])
                        P.op("dve", "tensor_tensor", ["pO", "rden"], ["oast%d" % s], out=ost[s][:, qg * 512:(qg + 1) * 512], in0=pO[:], in1=rden[:], op=ALU.mult)
                    P.op("sp", "dma_start", ["oast%d" % s], [], out=SC["oaT"][h], in_=ost[s][:])

                for _ in prep(0):
                    pass
                for h in range(8):
                    nxt = prep(h + 1) if h + 1 < 8 else iter(())

                    def step(n, nxt=nxt):
                        for _ in range(n):
                            next(nxt, None)

                    att(h, step)
                    for _ in nxt:
                        pass
                P.emit(st)

        trig_cm.__exit__(None, None, None)

        if "p3" in phases:
            with ExitStack() as st:
                sb = lambda n, s, d, _p="p3_": st.enter_context(nc.sbuf_tensor(_p + n, s, d))
                psb = lambda n, s, d, _p="p3_": st.enter_context(nc.psum_tensor(_p + n, s, d))
                P = Prog(nc, SEMS)
                iqT = sb("iqT", [128, 8, S], BF16)
                ikT = sb("ikT", [128, S], BF16)
                iw = sb("iw", [128, 16, 16], F32)
                dqT = sb("dqT", [128, 8, S], BF16)
                dkT = sb("dkT", [128, 2, S], BF16)
                dvs = sb("dvs", [128, 16, 256], BF16)
                origs = [sb("orig%d" % i, [128, S], F32) for i in range(4)]
                works = [sb("work%d" % i, [128, S], F32) for i in range(2)]
                masks = [sb("mask%d" % i, [128, S], BF16) for i in range(2)]
                mxs = [sb("mx%d" % i, [128, 8], F32) for i in range(2)]
                MTs = [sb("MT%d" % i, [128, 16, 512], BF16) for i in range(2)]
                diag = [sb("diag%d" % i, [128, 16, 128], BF16) for i in range(2)]
                Rb = [sb("R%d" % i, [128, 512], BF16) for i in range(4)]
                pT = [sb("pT%d" % i, [128, 512], BF16) for i in range(3)]
                rden = sb("rden", [128, 512], F32)
                obst = [sb("obst%d" % i, [128, 8, 512], BF16) for i in range(2)]
                pDs = [psb("pDs%d" % i, [128, 512], F32) for i in range(2)]
                pIS = psb("pIS", [128, 512], F32)
                ptr = psb("ptr", [128, 8, 128], BF16)
                pS = [psb("pS%d" % i, [128, 512], F32) for i in range(2)]
                pO = psb("pO", [128, 512], F32)
                pD = psb("pD", [128, 512], F32)
                for h in range(8):
                    P.op("sp", "dma_start", [], ["iqT"], out=iqT[:, h, :], in_=SC["iqT"][h])
                    P.op("sp", "dma_start", [], ["dqT"], out=dqT[:, h, :], in_=SC["dqT"][h])
                for g in range(2):
                    P.op("sp", "dma_start", [], ["dkT"], out=dkT[:, g, :], in_=SC["dkT"][g])
                P.op("sp", "dma_start", [], ["ikT"], out=ikT[:], in_=SC["ikT"])
                P.op("sp", "dma_start", [], ["iw"], out=iw[:], in_=SC["iw"].rearrange("(t p) c -> p t c", p=128))
                P.op("sp", "dma_start", [], ["dvs"], out=dvs[:], in_=SC["dv"].rearrange("(t p) c -> p t c", p=128))
                for (src_, dst_) in ((IN["put"], SC["pub"]), (IN["pv"], SC["pvb"])):
                    for r0 in range(0, 16384, 1024):
                        P.op("pool", "dma_start", ["iqT", "dqT", "dkT", "ikT", "iw", "dvs"], [], out=dst_[r0:r0 + 1024, :].rearrange("r (a b) -> (r a) b", b=1024),
                             in_=src_[r0:r0 + 1024, :].rearrange("r (a b) -> (r a) b", b=1024))
                sc_idx = float(64 ** -0.5 * 16 ** -0.5)
                sc_dsa = float(128 ** -0.5)
                nR = 0
                nD = 0
                cnt = 0
                def stage_IS(g):
                    qg = g // 2
                    st_ = dict(nD=0)
                    for T in (2 * g, 2 * g + 1):
                        i = T % 4
                        tsl = slice(T * 128, (T + 1) * 128)
                        nk = 128 * (T + 1)
                        dd = T % 2
                        orig = origs[i]
                        P.op("dve", "tensor_tensor", ["identf", "iw"], ["diag%d" % dd], out=diag[dd][:],
                             in0=identf[:].unsqueeze(1).to_broadcast([128, 16, 128]), in1=iw[:, T, :].unsqueeze(2).to_broadcast([128, 16, 128]), op=ALU.mult)
                        for kg in range((nk + 511) // 512):
                            ncol = min(512, nk - kg * 512)
                            ksl = slice(kg * 512, kg * 512 + ncol)
                            for h in range(16):
                                rows = slice((h % 2) * 64, (h % 2) * 64 + 64)
                                a = CN["nD"] % 2
                                CN["nD"] += 1
                                r = CN["nR"] % 4
                                CN["nR"] += 1
                                P.op("pe", "matmul", ["iqT", "ikT"], ["pDs%d" % a], out=pDs[a][:, 0:ncol], lhsT=iqT[rows, h // 2, tsl], rhs=ikT[rows, ksl], start=True, stop=True)
                                P.op("act", "activation", ["pDs%d" % a], ["R%d" % r], out=Rb[r][:, 0:ncol], in_=pDs[a][:, 0:ncol], func=AF.Relu, scale=sc_idx)
                                P.op("pe", "matmul", ["diag%d" % dd, "R%d" % r], ["pIS"], out=pIS[:, 0:ncol], lhsT=diag[dd][:, h, :], rhs=Rb[r][:, 0:ncol],
                                     start=(h == 0), stop=(h == 15))
                            P.op("act", "activation", ["pIS"], ["orig%d" % i], out=orig[:, ksl], in_=pIS[:, 0:ncol], func=AF.Copy)
                        P.op("pool", "memset", [], ["orig%d" % i], ap=orig[0:64, nk - 64:nk], constant=NEG)

                def stage_topk(g, att_qg=None):
                    tiles = (2 * g, 2 * g + 1)
                    heads_done = 0
                    if tiles[0] >= 2:
                        for rnd in range(32):
                            for T in tiles:
                                i, dd, nk = T % 4, T % 2, 128 * (T + 1)
                                src = origs[i] if rnd == 0 else works[dd]
                                sname = ("orig%d" % i) if rnd == 0 else ("work%d" % dd)
                                P.op("dve", "max", [sname], ["mx%d" % dd], out=mxs[dd][:], in_=src[:, 0:nk])
                            for T in tiles:
                                i, dd, nk = T % 4, T % 2, 128 * (T + 1)
                                src = origs[i] if rnd == 0 else works[dd]
                                sname = ("orig%d" % i) if rnd == 0 else ("work%d" % dd)
                                P.op("dve", "match_replace", ["mx%d" % dd, sname], ["work%d" % dd], out=works[dd][:, 0:nk], in_to_replace=mxs[dd][:],
                                     in_values=src[:, 0:nk], imm_value=NEG)
                            if att_qg is not None and rnd % 4 == 3:
                                stage_att_head(att_qg, heads_done)
                                heads_done += 1
                    if att_qg is not None:
                        while heads_done < 8:
                            stage_att_head(att_qg, heads_done)
                            heads_done += 1
                    for T in tiles:
                        i, dd, nk = T % 4, T % 2, 128 * (T + 1)
                        mask = masks[dd]
                        if T >= 2:
                            P.op("dve", "tensor_tensor", ["work%d" % dd, "orig%d" % i], ["mask%d" % dd], out=mask[:, 0:nk], in0=works[dd][:, 0:nk], in1=origs[i][:, 0:nk], op=ALU.not_equal)
                        else:
                            P.op("dve", "tensor_scalar", ["orig%d" % i], ["mask%d" % dd], out=mask[:, 0:nk], in0=origs[i][:, 0:nk], scalar1=NEG / 2, scalar2=None, op0=ALU.is_gt)

                def stage_maskT(g, first):
                    mp = (g // 2) % 2
                    MT = MTs[mp]
                    if first:
                        P.op("pool", "memset", [], ["MT%d" % mp], ap=MT[:], constant=0.0)
                    for T in (2 * g, 2 * g + 1):
                        i, dd = T % 4, T % 2
                        mask = masks[dd]
                        for k0 in range(0, T + 1, 8):
                            n8 = min(8, T + 1 - k0)
                            for kt in range(k0, k0 + n8):
                                P.op("pe", "transpose", ["mask%d" % dd, "identb"], ["ptr"], out=ptr[:, kt - k0, :], in_=mask[:, kt * 128:(kt + 1) * 128], identity=identb[:])
                            P.op("act", "activation", ["ptr"], ["MT%d" % mp], out=MT[:, k0:k0 + n8, i * 128:(i + 1) * 128], in_=ptr[:, 0:n8, :], func=AF.Copy)

                def stage_att_head(qg, h):
                    os_ = qg % 2
                    g = h // 4
                    nkt = 4 * (qg + 1)
                    for kt in range(nkt):
                        ksl = slice(kt * 128, (kt + 1) * 128)
                        a = CN["cnt"] % 2
                        k = CN["cnt"] % 3
                        CN["cnt"] += 1
                        P.op("pe", "matmul", ["dkT", "dqT"], ["pS%d" % a], out=pS[a][:], lhsT=dkT[:, g, ksl], rhs=dqT[:, h, qg * 512:(qg + 1) * 512], start=True, stop=True)
                        P.op("act", "activation", ["pS%d" % a], ["pT%d" % k], out=pT[k][:], in_=pS[a][:], func=AF.Exp, scale=sc_dsa)
                        P.op("pool", "tensor_tensor", ["pT%d" % k, "MT%d" % os_], ["pT%d" % k], out=pT[k][:], in0=pT[k][:], in1=MTs[os_][:, kt, :], op=ALU.mult)
                        P.op("pe", "matmul", ["dvs", "pT%d" % k], ["pO"], out=pO[:], lhsT=dvs[:, kt, g * 128:(g + 1) * 128], rhs=pT[k][:], start=(kt == 0), stop=(kt == nkt - 1))
                        P.op("pe", "matmul", ["onesb", "pT%d" % k], ["pD"], out=pD[:], lhsT=onesb[:], rhs=pT[k][:], start=(kt == 0), stop=(kt == nkt - 1))
                    P.op("dve", "reciprocal", ["pD"], ["rden"], out=rden[:], in_=pD[:])
                    P.op("dve", "tensor_tensor", ["pO", "rden"], ["obst%d" % os_], out=obst[os_][:, h, :], in0=pO[:], in1=rden[:], op=ALU.mult)
                    if h == 7:
                        P.op("sp", "dma_start", ["obst%d" % os_], [], out=SC["obT"][:, :, qg * 512:(qg + 1) * 512].rearrange("h p s -> p h s"), in_=obst[os_][:])

                CN = dict(nD=0, nR=0, cnt=0)
                seq = [7, 6, 5, 4, 3, 2, 1, 0]
                stage_IS(seq[0])
                pending = None
                for idx, g in enumerate(seq):
                    if idx + 1 < len(seq):
                        stage_IS(seq[idx + 1])
                    stage_topk(g, att_qg=pending)
                    pending = None
                    stage_maskT(g, first=(g % 2 == 1))
                    if g % 2 == 0:
                        pending = g // 2
                for h in range(8):
                    stage_att_head(pending, h)
                P.emit(st)

        if "p4" in phases:
            with ExitStack() as st:
                sb = lambda n, s, d, _p="p4_": st.enter_context(nc.sbuf_tensor(_p + n, s, d))
                psb = lambda n, s, d, _p="p4_": st.enter_context(nc.psum_tensor(_p + n, s, d))
                P = Prog(nc, SEMS)
                wa = sb("wa", [128, 8, D], BF16)
                wb = sb("wb", [128, 8, D], BF16)
                wo = sb("wo", [128, 16, D], BF16)
                oat = [sb("oat%d" % i, [128, 8, 128], BF16) for i in range(2)]
                obt = [sb("obt%d" % i, [128, 8, 128], BF16) for i in range(2)]
                gst_ = [sb("gs%d" % i, [128, 4096], BF16) for i in range(1)] * 2
                xt = [sb("xt%d" % i, [128, D], F32) for i in range(2)]
                mgs = [sb("mg%d" % i, [128, D], BF16) for i in range(2)]
                mTs = [sb("mT%d" % i, [128, 16, 128], BF16) for i in range(2)]
                gsb2 = [sb("gsb%d" % i, [128, 4096], BF16) for i in range(2)]
                t1 = [sb("t1_%d" % i, [128, 512], F32) for i in range(2)]
                t2 = [sb("t2_%d" % i, [128, 512], F32) for i in range(2)]
                pY = [psb("pY%d" % i, [128, 512], F32) for i in range(4)]
                ptr = [psb("ptr%d" % i, [128, 8, 128], BF16) for i in range(2)]
                pZ = [psb("pZ%d" % i, [128, 512], F32) for i in range(2)]
                for h in range(8):
                    for hf in range(2):
                        P.op("pool", "dma_start", [], ["wa"], out=wa[:, h, hf * 1024:(hf + 1) * 1024], in_=IN["wa"][:, h, hf * 1024:(hf + 1) * 1024])
                        P.op("pool", "dma_start", [], ["wb"], out=wb[:, h, hf * 1024:(hf + 1) * 1024], in_=IN["wb"][:, h, hf * 1024:(hf + 1) * 1024])
                for dk in range(16):
                    for hf in range(2):
                        P.op("pool", "dma_start", [], ["wo"], out=wo[:, dk, hf * 1024:(hf + 1) * 1024], in_=IN["wo"][:, dk, hf * 1024:(hf + 1) * 1024])
                CNY = dict(ny=0)

                def stage_Y(T):
                    s = T % 2
                    tsl = slice(T * 128, (T + 1) * 128)
                    P.op("sp", "dma_start", [], ["oat%d" % s], out=oat[s][:], in_=SC["oaT"][:, :, tsl].rearrange("h p s -> p h s"))
                    P.op("sp", "dma_start", [], ["obt%d" % s], out=obt[s][:], in_=SC["obT"][:, :, tsl].rearrange("h p s -> p h s"))
                    P.op("sp", "dma_start", [], ["gs%d" % s], out=gsb2[s][:], in_=SC["gs"][tsl, :])
                    P.op("sp", "dma_start", [], ["xt%d" % s], out=xt[s][:], in_=IN["x"][tsl, :])
                    for cg in range(4):
                        csl = slice(cg * 512, (cg + 1) * 512)
                        ya = CNY["ny"] % 4
                        yb = (CNY["ny"] + 1) % 4
                        CNY["ny"] += 2
                        u = cg % 2
                        for h in range(8):
                            P.op("pe", "matmul", ["oat%d" % s, "wa"], ["pY%d" % ya], out=pY[ya][:], lhsT=oat[s][:, h, :], rhs=wa[:, h, csl], start=(h == 0), stop=(h == 7))
                        for h in range(8):
                            P.op("pe", "matmul", ["obt%d" % s, "wb"], ["pY%d" % yb], out=pY[yb][:], lhsT=obt[s][:, h, :], rhs=wb[:, h, csl], start=(h == 0), stop=(h == 7))
                        P.op("dve", "tensor_tensor", ["pY%d" % ya, "gs%d" % s], ["t1_%d" % u], out=t1[u][:], in0=pY[ya][:], in1=gsb2[s][:, csl], op=ALU.mult)
                        P.op("dve", "tensor_tensor", ["pY%d" % yb, "gs%d" % s], ["t2_%d" % u], out=t2[u][:], in0=pY[yb][:], in1=gsb2[s][:, 2048 + cg * 512:2048 + (cg + 1) * 512], op=ALU.mult)
                        P.op("pool", "tensor_tensor", ["t1_%d" % u, "t2_%d" % u], ["mg%d" % s], out=mgs[s][:, csl], in0=t1[u][:], in1=t2[u][:], op=ALU.add)

                def stage_TZ(T):
                    s = T % 2
                    tsl = slice(T * 128, (T + 1) * 128)
                    for dk in range(16):
                        b_ = dk // 8
                        P.op("pe", "transpose", ["mg%d" % s, "identb"], ["ptr%d" % b_], out=ptr[b_][:, dk % 8, :], in_=mgs[s][:, dk * 128:(dk + 1) * 128], identity=identb[:])
                    for b_ in range(2):
                        P.op("act", "activation", ["ptr%d" % b_], ["mT%d" % s], out=mTs[s][:, b_ * 8:(b_ + 1) * 8, :], in_=ptr[b_][:], func=AF.Copy)
                    for og in range(4):
                        csl = slice(og * 512, (og + 1) * 512)
                        z = og % 2
                        for dk in range(16):
                            P.op("pe", "matmul", ["mT%d" % s, "wo"], ["pZ%d" % z], out=pZ[z][:], lhsT=mTs[s][:, dk, :], rhs=wo[:, dk, csl], start=(dk == 0), stop=(dk == 15))
                        P.op("dve", "tensor_tensor", ["pZ%d" % z, "xt%d" % s], ["xt%d" % s], out=xt[s][:, csl], in0=pZ[z][:], in1=xt[s][:, csl], op=ALU.add)
                    P.op("sp", "dma_start", ["xt%d" % s], [], out=SC["x2"][tsl, :], in_=xt[s][:])

                stage_Y(0)
                for T in range(NT):
                    if T + 1 < NT:
                        stage_Y(T + 1)
                    stage_TZ(T)
                P.emit(st)

        if "p5" in phases:
            with ExitStack() as st:
                sb = lambda n, s, d, _p="p5_": st.enter_context(nc.sbuf_tensor(_p + n, s, d))
                psb = lambda n, s, d, _p="p5_": st.enter_context(nc.psum_tensor(_p + n, s, d))
                P = Prog(nc, SEMS)
                wpq = sb("wpq", [128, 16, 1024], BF16)
                kb = sb("kb", [128, 8, 256], BF16)
                gffnT = sb("gffnT", [128, 16], F32)
                gfin = sb("gfin", [128, D], F32)
                iota = sb("iota", [128, 16], F32)
                iota128 = sb("iota128", [128, 128], F32)
                GT = sb("GT", [128, 256, 128], BF16)
                NSB = 8
                OH2s = [sb("OH2_%d" % i, [128, NSB, 128], BF16) for i in range(2)]
                OH1s = [sb("OH1_%d" % i, [128, NSB, 128], BF16) for i in range(2)]
                hnTGs = [sb("hnTG%d" % i, [128, 16, 256], BF16) for i in range(2)]
                NUB = 5
                ub = [sb("ub%d" % i, [128, D], BF16) for i in range(NUB)]
                vb = [ub[i // 2][:, (i % 2) * 1024:(i % 2 + 1) * 1024] for i in range(2 * NUB)]
                x2t = [sb("x2t%d" % i, [128, D], F32) for i in range(2)]
                hnf = sb("hnf", [128, D], F32)
                qTs = sb("qTs", [128, 8, 128], BF16)
                ssb = sb("ssb", [128, 8, 256], F32)
                wk = sb("wk", [128, 256], F32)
                vals = sb("vals", [128, 16, 16], F32)
                idxs = sb("idxs", [128, 16, 16], U32)
                idxf = sb("idxf", [128, 16, 16], F32)
                tv = sb("tv", [128, 8, 16], F32)
                tp = sb("tp", [128, 8, 16], U32)
                ti = sb("ti", [128, 8, 16], U32)
                tj = sb("tj", [128, 8, 16], U32)
                tif = sb("tif", [128, 8, 16], F32)
                tjf = sb("tjf", [128, 8, 16], F32)
                e1 = [sb("e1_%d" % i, [128, 8, 16], F32) for i in range(4)]
                e2 = [sb("e2_%d" % i, [128, 8, 16], F32) for i in range(4)]
                gw = [sb("gw%d" % i, [128, 128], F32) for i in range(4)]
                selT = sb("selT", [128, 3, 128], F32)
                ex = sb("ex", [128, 8, 16], F32)
                zs = sb("zs", [128, 8], F32)
                ga = [sb("ga%d" % i, [128, 256], BF16) for i in range(2)]
                cst = [sb("cst%d" % i, [128, 256], BF16) for i in range(4)]
                stA = sb("stA", [128, 4], F32)
                stC = sb("stC", [128, 4], F32)
                B = [psb("B%d" % i, [128, 512], F32) for i in range(8)]
                for dk in range(16):
                    P.op("pool", "dma_start", [], ["wpq"], out=wpq[:, dk, :], in_=IN["wpq"][:, dk, :])
                for h in range(8):
                    P.op("pool", "dma_start", [], ["kb"], out=kb[:, h, :], in_=IN["kb"][:, h, :])
                P.op("sp", "dma_start", [], ["gffnT"], out=gffnT[:], in_=IN["gffnT"])
                P.op("sp", "dma_start", [], ["gfin"], out=gfin[:], in_=IN["gfin"].partition_broadcast(128))
                P.op("sp", "dma_start", [], ["iota"], out=iota[:], in_=IN["iota16"])
                P.op("sp", "dma_start", [], ["iota128"], out=iota128[:], in_=IN["iota128"])
                CN = dict(u=0, v=0, g=0, a=0, o=0)

                def stage_A(T):
                    p = T % 4
                    gp = (T // 2) % 2
                    hnTG = hnTGs[gp]
                    HT = "hnTG%d" % gp
                    tcol = slice((T % 2) * 128, (T % 2) * 128 + 128)
                    tsl = slice(T * 128, (T + 1) * 128)
                    yield P.op("sp", "dma_start", [], ["hnf"], out=hnf[:], in_=SC["x2"][tsl, :])
                    yield P.op("act", "activation", ["hnf"], ["ssb", "stA0"], out=ssb[:].rearrange("p a b -> p (a b)"), in_=hnf[:], func=AF.Square, accum_out=stA[:, 0:1])
                    yield P.op("act", "activation", ["stA0"], ["stA1"], out=stA[:, 1:2], in_=stA[:, 0:1], func=AF.Sqrt, scale=1.0 / D, bias=EPS)
                    yield P.op("dve", "reciprocal", ["stA1"], ["stA2"], out=stA[:, 2:3], in_=stA[:, 1:2])
                    yield P.op("act", "activation", ["hnf", "stA2"], ["hnf"], out=hnf[:], in_=hnf[:], func=AF.Copy, scale=stA[:, 2:3])
                    for r in range(4):
                        z = 6 + r % 2
                        for q4 in range(4):
                            dk = r * 4 + q4
                            yield P.op("pe", "transpose", ["hnf", "identf"], ["B%d" % z], out=B[z][:, q4 * 128:(q4 + 1) * 128], in_=hnf[:, dk * 128:(dk + 1) * 128], identity=identf[:])
                        for q4 in range(4):
                            dk = r * 4 + q4
                            if dk % 2 == 0:
                                yield P.op("act", "activation", ["B%d" % z, "gffnT"], [HT], out=hnTG[:, dk, tcol], in_=B[z][:, q4 * 128:(q4 + 1) * 128],
                                           func=AF.Copy, scale=gffnT[:, dk:dk + 1])
                            else:
                                yield P.op("dve", "tensor_scalar", ["B%d" % z, "gffnT"], [HT], out=hnTG[:, dk, tcol], in0=B[z][:, q4 * 128:(q4 + 1) * 128],
                                           scalar1=gffnT[:, dk:dk + 1], scalar2=None, op0=ALU.mult)
                    for hh in range(2):
                        z = 6 + hh
                        for h4 in range(4):
                            h = hh * 4 + h4
                            for dk in range(16):
                                yield P.op("pe", "matmul", ["wpq", HT], ["B%d" % z], out=B[z][:, h4 * 128:(h4 + 1) * 128], lhsT=wpq[:, dk, h * 128:(h + 1) * 128],
                                           rhs=hnTG[:, dk, tcol], start=(dk == 0), stop=(dk == 15))
                        yield P.op("act", "activation", ["B%d" % z], ["qTs"], out=qTs[:, hh * 4:(hh + 1) * 4, :].rearrange("p a b -> p (a b)"), in_=B[z][:], func=AF.Copy)
                    for h2 in range(4):
                        z = 6 + h2 % 2
                        for hi in range(2):
                            h = h2 * 2 + hi
                            yield P.op("pe", "matmul", ["qTs", "kb"], ["B%d" % z], out=B[z][:, hi * 256:(hi + 1) * 256], lhsT=qTs[:, h, :], rhs=kb[:, h, :], start=True, stop=True)
                        yield P.op("act", "activation", ["B%d" % z], ["ssb"], out=ssb[:, h2 * 2:h2 * 2 + 2, :].rearrange("p a b -> p (a b)"), in_=B[z][:], func=AF.Copy)
                    for hp in range(16):
                        src = ssb[:, hp // 2, (hp % 2) * 128:(hp % 2) * 128 + 128]
                        yield P.op("dve", "max", ["ssb"], ["vals"], out=vals[:, hp, 0:8], in_=src)
                        yield P.op("dve", "max_index", ["ssb", "vals"], ["idxs"], out=idxs[:, hp, 0:8], in_max=vals[:, hp, 0:8], in_values=src)
                        yield P.op("dve", "match_replace", ["ssb", "vals"], ["wk"], out=wk[:, 0:128], in_to_replace=vals[:, hp, 0:8], in_values=src, imm_value=NEG)
                        yield P.op("dve", "max", ["wk"], ["vals"], out=vals[:, hp, 8:16], in_=wk[:, 0:128])
                        yield P.op("dve", "max_index", ["wk", "vals"], ["idxs"], out=idxs[:, hp, 8:16], in_max=vals[:, hp, 8:16], in_values=wk[:, 0:128])
                    yield P.op("dve", "tensor_copy", ["idxs"], ["idxf"], out=idxf[:], in_=idxs[:])
                    cand = ssb[:].rearrange("p h (a b) -> p h a b", b=16)
                    v4 = vals[:].rearrange("p (h two) k -> p h two k", two=2)
                    i4 = idxf[:].rearrange("p (h two) k -> p h two k", two=2)
                    yield P.op("dve", "tensor_tensor", ["vals"], ["ssb"], out=cand, in0=v4[:, :, 0, :].unsqueeze(3).to_broadcast([128, 8, 16, 16]),
                         in1=v4[:, :, 1, :].unsqueeze(2).to_broadcast([128, 8, 16, 16]), op=ALU.add)
                    for h in range(8):
                        src = ssb[:, h, :]
                        yield P.op("dve", "max", ["ssb"], ["tv"], out=tv[:, h, 0:8], in_=src)
                        yield P.op("dve", "max_index", ["ssb", "tv"], ["tp"], out=tp[:, h, 0:8], in_max=tv[:, h, 0:8], in_values=src)
                        yield P.op("dve", "match_replace", ["ssb", "tv"], ["wk"], out=wk[:], in_to_replace=tv[:, h, 0:8], in_values=src, imm_value=NEG)
                        yield P.op("dve", "max", ["wk"], ["tv"], out=tv[:, h, 8:16], in_=wk[:])
                        yield P.op("dve", "max_index", ["wk", "tv"], ["tp"], out=tp[:, h, 8:16], in_max=tv[:, h, 8:16], in_values=wk[:])
                    yield P.op("dve", "tensor_single_scalar", ["tp"], ["ti"], out=ti[:], in_=tp[:], scalar=4, op=ALU.logical_shift_right)
                    yield P.op("dve", "tensor_single_scalar", ["tp"], ["tj"], out=tj[:], in_=tp[:], scalar=15, op=ALU.bitwise_and)
                    yield P.op("dve", "tensor_copy", ["ti"], ["tif"], out=tif[:], in_=ti[:])
                    yield P.op("dve", "tensor_copy", ["tj"], ["tjf"], out=tjf[:], in_=tj[:])
                    oh = ssb[:].rearrange("p h (a b) -> p h a b", b=16)
                    iob = iota[:].unsqueeze(1).unsqueeze(1).to_broadcast([128, 8, 16, 16])
                    for (sel, side, dst, dn) in ((tif, 0, e1[p], "e1_%d" % p), (tjf, 1, e2[p], "e2_%d" % p)):
                        yield P.op("dve", "tensor_tensor", ["tif", "tjf", "iota"], ["ssb"], out=oh, in0=sel[:].unsqueeze(3).to_broadcast([128, 8, 16, 16]), in1=iob, op=ALU.is_equal)
                        yield P.op("dve", "tensor_tensor", ["ssb", "idxf"], ["ssb"], out=oh, in0=oh, in1=i4[:, :, side, :].unsqueeze(2).to_broadcast([128, 8, 16, 16]), op=ALU.mult)
                        yield P.op("dve", "tensor_reduce", ["ssb"], [dn], out=dst[:], in_=oh, axis=AX.X, op=ALU.add)
                    yield P.op("dve", "tensor_tensor", ["tv"], ["ex"], out=ex[:], in0=tv[:], in1=tv[:, :, 0:1].to_broadcast([128, 8, 16]), op=ALU.subtract)
                    yield P.op("act", "activation", ["ex"], ["ex"], out=ex[:], in_=ex[:], func=AF.Exp)
                    yield P.op("dve", "tensor_reduce", ["ex"], ["zs"], out=zs[:], in_=ex[:], axis=AX.X, op=ALU.add)
                    yield P.op("dve", "reciprocal", ["zs"], ["zs"], out=zs[:], in_=zs[:])
                    yield P.op("dve", "tensor_tensor", ["ex", "zs"], ["gw%d" % p], out=gw[p][:].rearrange("p (a b) -> p a b", b=16), in0=ex[:], in1=zs[:].unsqueeze(2).to_broadcast([128, 8, 16]), op=ALU.mult)

                def stage_GT(T):
                    p = T % 4
                    tp_ = T % 2
                    srcs = [(e1[p][:].rearrange("p a b -> p (a b)"), "e1_%d" % p), (e2[p][:].rearrange("p a b -> p (a b)"), "e2_%d" % p), (gw[p][:], "gw%d" % p)]
                    for j, (ap_, nm) in enumerate(srcs):
                        P.op("pe", "transpose", [nm, "identf"], ["B7"], out=B[7][:, j * 128:(j + 1) * 128], in_=ap_, identity=identf[:])
                    P.op("act", "activation", ["B7"], ["selT"], out=selT[:].rearrange("p a b -> p (a b)"), in_=B[7][:, 0:384], func=AF.Copy)
                    for sub in range(128 // NSB):
                        tsub = slice(sub * NSB, (sub + 1) * NSB)
                        ob = CN["o"] % 2
                        CN["o"] += 1
                        OH2, OH1 = OH2s[ob], OH1s[ob]
                        iob = iota128[:].unsqueeze(1).to_broadcast([128, NSB, 128])
                        P.op("dve", "tensor_tensor", ["iota128", "selT"], ["OH2_%d" % ob], out=OH2[:], in0=iob, in1=selT[:, 1, tsub].unsqueeze(2).to_broadcast([128, NSB, 128]), op=ALU.is_equal)
                        P.op("dve", "tensor_tensor", ["iota128", "selT"], ["OH1_%d" % ob], out=OH1[:], in0=iob, in1=selT[:, 0, tsub].unsqueeze(2).to_broadcast([128, NSB, 128]), op=ALU.is_equal)
                        P.op("pool", "tensor_tensor", ["OH1_%d" % ob, "selT"], ["OH1_%d" % ob], out=OH1[:], in0=OH1[:], in1=selT[:, 2, tsub].unsqueeze(2).to_broadcast([128, NSB, 128]), op=ALU.mult)
                        for t4 in range(NSB // 4):
                            z = 6 + CN["g"] % 2
                            CN["g"] += 1
                            for tt in range(4):
                                t = t4 * 4 + tt
                                P.op("pe", "matmul", ["OH2_%d" % ob, "OH1_%d" % ob], ["B%d" % z], out=B[z][:, tt * 128:(tt + 1) * 128], lhsT=OH2[:, t, :], rhs=OH1[:, t, :], start=True, stop=True)
                            tg0 = tp_ * 128 + sub * NSB + t4 * 4
                            P.op("act", "activation", ["B%d" % z], ["GT"], out=GT[:, tg0:tg0 + 4, :].rearrange("p t c -> p (t c)"), in_=B[z][:], func=AF.Copy)

                def stage_U(G, step):
                    gp = G % 2
                    hnTG = hnTGs[gp]
                    for c in range(128):
                        b = CN["u"] % NUB
                        CN["u"] += 1
                        P.op("sp", "dma_start", [], ["ubh%d" % (2 * b), "ubh%d" % (2 * b + 1)], out=ub[b][:], in_=SC["pub"][c * 128:(c + 1) * 128, :])
                        z = 4 + (c // 2) % 2
                        reg = slice((c % 2) * 256, (c % 2) * 256 + 256)
                        for dk in range(16):
                            P.op("pe", "matmul", ["ubh%d" % (2 * b + dk // 8), "hnTG%d" % gp], ["B%d" % z], out=B[z][:, reg], lhsT=ub[b][:, dk * 128:(dk + 1) * 128], rhs=hnTG[:, dk, :], start=(dk == 0), stop=(dk == 15))
                        k = CN["a"] % 2
                        CN["a"] += 1
                        P.op("act", "activation", ["B%d" % z], ["ga%d" % k], out=ga[k][:], in_=B[z][:, reg], func=AF.Gelu)
                        P.op("dve", "tensor_tensor", ["ga%d" % k, "GT"], ["GT"], out=GT[:, :, c], in0=ga[k][:], in1=GT[:, :, c], op=ALU.mult)
                        step(3)

                def stage_V(G, step):
                    for p in range(2):
                        T = 2 * G + p
                        P.op("sp", "dma_start", [], ["x2t%d" % p], out=x2t[p][:], in_=SC["x2"][T * 128:(T + 1) * 128, :])
                    for hv in range(2):
                        for c in range(128):
                            b = CN["v"] % (2 * NUB)
                            CN["v"] += 1
                            P.op("sp", "dma_start", [], ["ubh%d" % b], out=vb[b], in_=SC["pvb"][c * 128:(c + 1) * 128, hv * 1024:(hv + 1) * 1024])
                            for p in range(2):
                                for cg in range(2):
                                    z = p * 2 + cg
                                    P.op("pe", "matmul", ["GT", "ubh%d" % b], ["B%d" % z], out=B[z][:], lhsT=GT[:, p * 128:(p + 1) * 128, c], rhs=vb[b][:, cg * 512:(cg + 1) * 512],
                                         start=(c == 0), stop=(c == 127))
                            step(3)
                        for p in range(2):
                            for cg in range(2):
                                z = p * 2 + cg
                                csl = slice(hv * 1024 + cg * 512, hv * 1024 + (cg + 1) * 512)
                                P.op("dve", "tensor_tensor", ["B%d" % z, "x2t%d" % p], ["x2t%d" % p], out=x2t[p][:, csl], in0=B[z][:], in1=x2t[p][:, csl], op=ALU.add)
                    for p in range(2):
                        T = 2 * G + p
                        tsl = slice(T * 128, (T + 1) * 128)
                        X = "x2t%d" % p
                        P.op("act", "activation", [X], ["ubh0", "ubh1", "stC0"], out=ub[0][:], in_=x2t[p][:], func=AF.Square, accum_out=stC[:, 0:1])
                        P.op("act", "activation", ["stC0"], ["stC1"], out=stC[:, 1:2], in_=stC[:, 0:1], func=AF.Sqrt, scale=1.0 / D, bias=EPS)
                        P.op("dve", "reciprocal", ["stC1"], ["stC2"], out=stC[:, 2:3], in_=stC[:, 1:2])
                        P.op("dve", "scalar_tensor_tensor", [X, "stC2", "gfin"], [X], out=x2t[p][:], in0=x2t[p][:], scalar=stC[:, 2:3], in1=gfin[:], op0=ALU.mult, op1=ALU.mult)
                        P.op("sp", "dma_start", [X], [], out=y[tsl, :], in_=x2t[p][:])

                def run_all(gen):
                    for _ in gen:
                        pass

                def chain(*gens):
                    for g_ in gens:
                        for v_ in g_:
                            yield v_

                run_all(stage_A(0))
                run_all(stage_A(1))
                for G in range(NT // 2):
                    stage_GT(2 * G)
                    stage_GT(2 * G + 1)
                    nxt = chain(stage_A(2 * G + 2), stage_A(2 * G + 3)) if G + 1 < NT // 2 else iter(())

                    def step(n, nxt=nxt):
                        for _ in range(n):
                            next(nxt, None)

                    stage_U(G, step)
                    stage_V(G, step)
                    run_all(nxt)
                P.emit(st)
    return nc


def kernel(**inputs):
    H = host_layout(inputs)
    x = np.asarray(inputs["x"], np.float32)
    pos = np.asarray(inputs["positions"]).astype(np.int32)
    nc = build_nc()
    in_maps = []
    for b in range(8):
        m = {k: H[k] for k in SHAPES if k not in ("x", "pos")}
        m["x"] = np.ascontiguousarray(x[b])
        m["pos"] = np.ascontiguousarray(pos[b])
        in_maps.append(m)
    res = run_bass_kernel_spmd(nc, in_maps, core_ids=list(range(8)))
    return np.stack([np.asarray(r["y"], dtype=np.float32) for r in res.results], axis=0)
```
